# Optimizing a Trainium2 kernel written in Bass

```python
import math
import jax, jax.numpy as jnp
from jax import lax
import numpy as np

D_MODEL = 1024
BATCH = 16
SEQ = 2048
DEPTH = 1

D_SSM = D_MODEL // 2
SSM_GROUP = 16
N_SSM_GROUPS = D_SSM // SSM_GROUP
SSM_STATE = 64
DT_MIN = 0.001
DT_MAX = 0.1
D_CONV = D_MODEL // 2
CONV_WIDTH = 31
N_BRANCH = 2
SPLITS = [D_SSM, D_SSM + D_CONV, D_SSM + 2 * D_CONV, D_SSM + 2 * D_CONV + D_MODEL]
D_IN = D_SSM + 2 * D_CONV + N_BRANCH * D_MODEL
N_GROUPS = 4
N_EXP_PER_GROUP = 8
N_EXPERTS = N_GROUPS * N_EXP_PER_GROUP
TOP_K_INNER = 2
D_FF_EXPERT = D_MODEL // 4
ROW_BLOCK = 128
LN_EPS = 1e-5
ALPHA = (2.0 * DEPTH) ** 0.25
BETA = (8.0 * DEPTH) ** -0.25

kernel_name = "hybrid_s5_conformer_hmoe_deepnorm"


def _layer_norm(x, g, b):
    xf = x.astype(jnp.float32)
    mu = jnp.mean(xf, axis=-1, keepdims=True)
    var = jnp.mean(jnp.square(xf - mu), axis=-1, keepdims=True)
    return ((xf - mu) * lax.rsqrt(var + LN_EPS)).astype(x.dtype) * g + b


def _s5_combine(e1, e2):
    a1r, a1i, b1r, b1i = e1
    a2r, a2i, b2r, b2i = e2
    return (a2r * a1r - a2i * a1i,
            a2r * a1i + a2i * a1r,
            a2r * b1r - a2i * b1i + b2r,
            a2r * b1i + a2i * b1r + b2i)


def _s5_mixer(u, lam_re, lam_im, log_dt, b_re, b_im, c_re, c_im, d_skip):
    bsz, seq, _ = u.shape
    f32 = jnp.float32
    lr, li = lam_re.astype(f32), lam_im.astype(f32)
    dt = jnp.exp(log_dt.astype(f32))[:, None]
    mag = jnp.exp(lr * dt)
    ar, ai = mag * jnp.cos(li * dt), mag * jnp.sin(li * dt)
    den = lr * lr + li * li
    fr = ((ar - 1.0) * lr + ai * li) / den
    fi = (ai * lr - (ar - 1.0) * li) / den
    br, bi = b_re.astype(f32), b_im.astype(f32)
    bbar_re = fr[..., None] * br - fi[..., None] * bi
    bbar_im = fr[..., None] * bi + fi[..., None] * br
    ug = u.astype(f32).reshape(bsz, seq, N_SSM_GROUPS, SSM_GROUP)
    bu_re = jnp.einsum('blgh,gph->blgp', ug, bbar_re)
    bu_im = jnp.einsum('blgh,gph->blgp', ug, bbar_im)
    a_shape = (1, seq, N_SSM_GROUPS, SSM_STATE)
    a_re = jnp.broadcast_to(ar[None, None], a_shape)
    a_im = jnp.broadcast_to(ai[None, None], a_shape)
    _, _, s_re, s_im = lax.associative_scan(_s5_combine, (a_re, a_im, bu_re, bu_im), axis=1)
    y = (jnp.einsum('blgp,ghp->blgh', s_re, c_re.astype(f32))
         - jnp.einsum('blgp,ghp->blgh', s_im, c_im.astype(f32)))
    y = y.reshape(bsz, seq, D_SSM).astype(u.dtype)
    return y + d_skip * u


def _conv_mixer(ca, cb, conv_w, conv_b, ln_g, ln_b, w_cout, b_cout):
    v = ca * jax.nn.sigmoid(cb)
    vp = jnp.pad(v, ((0, 0), (CONV_WIDTH - 1, 0), (0, 0)))
    c = lax.conv_general_dilated(vp, conv_w, window_strides=(1,), padding='VALID',
                                 dimension_numbers=('NWC', 'WIO', 'NWC'),
                                 feature_group_count=D_CONV) + conv_b
    c = jax.nn.silu(_layer_norm(c, ln_g, ln_b))
    return c @ w_cout + b_cout


def _hier_moe(h, w_rg, b_rg, w_re, b_re, w_gate, w_up, w_down):
    bsz, seq, d = h.shape
    xt = h.reshape(bsz * seq, d)
    n_tok = xt.shape[0]
    zg = (xt @ w_rg + b_rg).astype(jnp.float32)
    pg = jax.nn.softmax(zg, axis=-1)
    _, gsel = lax.top_k(zg, 1)
    p_group = jnp.take_along_axis(pg, gsel, axis=-1)
    ze = (xt @ w_re + b_re).astype(jnp.float32).reshape(n_tok, N_GROUPS, N_EXP_PER_GROUP)
    ze = jnp.take_along_axis(ze, gsel[:, :, None], axis=1)[:, 0]
    top_v, top_i = lax.top_k(ze, TOP_K_INNER)
    gate = (p_group * jax.nn.softmax(top_v, axis=-1)).astype(h.dtype)
    eid = (gsel * N_EXP_PER_GROUP + top_i).reshape(-1)
    wts = gate.reshape(-1)
    tok = jnp.repeat(jnp.arange(n_tok, dtype=jnp.int32), TOP_K_INNER)
    n_assign = n_tok * TOP_K_INNER
    order = jnp.argsort(eid)
    s_eid, s_tok, s_w = eid[order], tok[order], wts[order]
    counts = jnp.bincount(eid, length=N_EXPERTS)
    start = jnp.cumsum(counts) - counts
    pcounts = (counts + ROW_BLOCK - 1) // ROW_BLOCK * ROW_BLOCK
    pend = jnp.cumsum(pcounts)
    dest = (pend - pcounts)[s_eid] + jnp.arange(n_assign, dtype=jnp.int32) - start[s_eid]
    n_blocks = -(-n_assign // ROW_BLOCK) + N_EXPERTS
    n_rows = n_blocks * ROW_BLOCK
    tok_buf = jnp.zeros((n_rows,), jnp.int32).at[dest].set(s_tok)
    w_buf = jnp.zeros((n_rows,), h.dtype).at[dest].set(s_w)
    blk_start = jnp.arange(n_blocks, dtype=jnp.int32) * ROW_BLOCK
    blk_eid = jnp.minimum(jnp.sum(blk_start[:, None] >= pend[None, :], axis=1), N_EXPERTS - 1)

    def expert_block(args):
        idx, e = args
        xb = xt[idx]
        hid = jax.nn.silu(xb @ w_gate[e]) * (xb @ w_up[e])
        return hid @ w_down[e]

    y_buf = lax.map(expert_block, (tok_buf.reshape(n_blocks, ROW_BLOCK), blk_eid))
    y = jax.ops.segment_sum(y_buf.reshape(n_rows, d) * w_buf[:, None], tok_buf, num_segments=n_tok)
    return y.reshape(bsz, seq, d)


def _layer(x, w_in, b_in, lam_re, lam_im, log_dt, ssm_b_re, ssm_b_im, ssm_c_re, ssm_c_im,
           ssm_d, w_glu, b_glu, conv_w, conv_b, ln_c_g, ln_c_b, w_cout, b_cout, w_out, b_out,
           ln1_g, ln1_b, w_route_group, b_route_group, w_route_expert, b_route_expert,
           w_gate, w_up, w_down, ln2_g, ln2_b):
    proj = x @ w_in + b_in
    u, ca, cb, gs, gc = jnp.split(proj, SPLITS, axis=-1)
    y = jax.nn.gelu(_s5_mixer(u, lam_re, lam_im, log_dt, ssm_b_re, ssm_b_im,
                              ssm_c_re, ssm_c_im, ssm_d))
    glu = y @ w_glu + b_glu
    s_out = glu[..., :D_MODEL] * jax.nn.sigmoid(glu[..., D_MODEL:])
    c_out = _conv_mixer(ca, cb, conv_w, conv_b, ln_c_g, ln_c_b, w_cout, b_cout)
    merged = jax.nn.sigmoid(gs) * s_out + jax.nn.sigmoid(gc) * c_out
    mix = merged @ w_out + b_out
    h = _layer_norm(ALPHA * x + mix, ln1_g, ln1_b)
    moe = _hier_moe(h, w_route_group, b_route_group, w_route_expert, b_route_expert,
                    w_gate, w_up, w_down)
    return _layer_norm(ALPHA * h + moe, ln2_g, ln2_b)


def _nrm(k, shape, scale):
    return scale * jax.random.normal(k, shape, jnp.float32)


def setup_inputs(seed: int = 0) -> dict:
    key = jax.random.key(seed)
    ks = jax.random.split(key, 32)
    L = DEPTH
    G, P, H = N_SSM_GROUPS, SSM_STATE, SSM_GROUP
    lam_im = jnp.pi * jnp.arange(P, dtype=jnp.float32)
    return {
        'x': _nrm(ks[0], (BATCH, SEQ, D_MODEL), 1.0),
        'w_in': _nrm(ks[1], (L, D_MODEL, D_IN), D_MODEL ** -0.5),
        'b_in': _nrm(ks[2], (L, D_IN), 0.01),
        'lam_re': -0.5 + _nrm(ks[3], (L, G, P), 0.01),
        'lam_im': lam_im + _nrm(ks[4], (L, G, P), 0.01),
        'log_dt': jax.random.uniform(ks[5], (L, G), jnp.float32, math.log(DT_MIN), math.log(DT_MAX)),
        'ssm_b_re': _nrm(ks[6], (L, G, P, H), (2.0 * H) ** -0.5),
        'ssm_b_im': _nrm(ks[7], (L, G, P, H), (2.0 * H) ** -0.5),
        'ssm_c_re': _nrm(ks[8], (L, G, H, P), (2.0 * P) ** -0.5),
        'ssm_c_im': _nrm(ks[9], (L, G, H, P), (2.0 * P) ** -0.5),
        'ssm_d': _nrm(ks[10], (L, D_SSM), 1.0),
        'w_glu': _nrm(ks[11], (L, D_SSM, 2 * D_MODEL), D_SSM ** -0.5),
        'b_glu': _nrm(ks[12], (L, 2 * D_MODEL), 0.01),
        'conv_w': _nrm(ks[13], (L, CONV_WIDTH, 1, D_CONV), CONV_WIDTH ** -0.5),
        'conv_b': _nrm(ks[14], (L, D_CONV), 0.01),
        'ln_c_g': 1.0 + _nrm(ks[15], (L, D_CONV), 0.01),
        'ln_c_b': _nrm(ks[16], (L, D_CONV), 0.01),
        'w_cout': _nrm(ks[17], (L, D_CONV, D_MODEL), D_CONV ** -0.5),
        'b_cout': _nrm(ks[18], (L, D_MODEL), 0.01),
        'w_out': _nrm(ks[19], (L, D_MODEL, D_MODEL), BETA * D_MODEL ** -0.5),
        'b_out': _nrm(ks[20], (L, D_MODEL), 0.01),
        'ln1_g': 1.0 + _nrm(ks[21], (L, D_MODEL), 0.01),
        'ln1_b': _nrm(ks[22], (L, D_MODEL), 0.01),
        'w_route_group': _nrm(ks[23], (L, D_MODEL, N_GROUPS), D_MODEL ** -0.5),
        'b_route_group': _nrm(ks[24], (L, N_GROUPS), 0.01),
        'w_route_expert': _nrm(ks[25], (L, D_MODEL, N_EXPERTS), D_MODEL ** -0.5),
        'b_route_expert': _nrm(ks[26], (L, N_EXPERTS), 0.01),
        'w_gate': _nrm(ks[27], (L, N_EXPERTS, D_MODEL, D_FF_EXPERT), D_MODEL ** -0.5),
        'w_up': _nrm(ks[28], (L, N_EXPERTS, D_MODEL, D_FF_EXPERT), D_MODEL ** -0.5),
        'w_down': _nrm(ks[29], (L, N_EXPERTS, D_FF_EXPERT, D_MODEL), BETA * D_FF_EXPERT ** -0.5),
        'ln2_g': 1.0 + _nrm(ks[30], (L, D_MODEL), 0.01),
        'ln2_b': _nrm(ks[31], (L, D_MODEL), 0.01),
    }


def reference(x, w_in, b_in, lam_re, lam_im, log_dt, ssm_b_re, ssm_b_im, ssm_c_re, ssm_c_im,
              ssm_d, w_glu, b_glu, conv_w, conv_b, ln_c_g, ln_c_b, w_cout, b_cout, w_out, b_out,
              ln1_g, ln1_b, w_route_group, b_route_group, w_route_expert, b_route_expert,
              w_gate, w_up, w_down, ln2_g, ln2_b):
    params = (w_in, b_in, lam_re, lam_im, log_dt, ssm_b_re, ssm_b_im, ssm_c_re, ssm_c_im,
              ssm_d, w_glu, b_glu, conv_w, conv_b, ln_c_g, ln_c_b, w_cout, b_cout, w_out, b_out,
              ln1_g, ln1_b, w_route_group, b_route_group, w_route_expert, b_route_expert,
              w_gate, w_up, w_down, ln2_g, ln2_b)
    h = x
    for i in range(DEPTH):
        h = _layer(h, *[p[i] for p in params])
    return h
```

```python
import math
import numpy as np
import concourse.bass as bass
import concourse.mybir as mybir
from concourse.bass_utils import run_bass_kernel_spmd
from contextlib import ExitStack

F32 = mybir.dt.float32
BF16 = mybir.dt.bfloat16
AF = mybir.ActivationFunctionType
OP = mybir.AluOpType
AX = mybir.AxisListType

NCORES = 8
D = 1024
SEQ = 2048
TOK = 4096
TS = 256
NT = TOK // TS
CW = 31
ALPHA = 2.0 ** 0.25
EPS = 1e-5
MAGIC = 12582912.0
TWO_PI = 2.0 * math.pi
DEBUG_H = False
S5_HALF = True
S5_DELAY = True
S5_PRIO = 2.0
CAP = 384
NS4 = CAP // 128
I32 = mybir.dt.int32


import types


def _freeze(fn):
    if fn.__closure__ is None:
        return fn
    cells = []
    for c in fn.__closure__:
        try:
            cells.append(types.CellType(c.cell_contents))
        except ValueError:
            cells.append(c)
    return types.FunctionType(fn.__code__, fn.__globals__, fn.__name__, fn.__defaults__, tuple(cells))


class Sched:
    def __init__(self, nc, es, ndma=14):
        self.nc = nc
        self.names = ['pe', 'act', 'dve', 'pool', 'sp']
        self.prog = {k: [] for k in self.names}
        self.sem = {k: es.enter_context(nc.semaphore('s_' + k)) for k in ['pe', 'act', 'dve', 'pool']}
        self.cnt = {k: 0 for k in self.sem}
        self.dsem = [es.enter_context(nc.semaphore('d%d' % i)) for i in range(ndma)]
        self.dcnt = [0] * ndma
        self.dnext = 0
        self.seen = {k: {} for k in self.names}
        self.lastw = {}
        self.readers = {}
        self.rec = None
        self.sim_eng = {}
        self.sim_w = {}
        self.sim_r = {}

    def record(self, section):
        self.rec = []
        section()
        r = self.rec
        self.rec = None
        return r

    COST = {'pe': 0.17, 'act': 0.42, 'dve': 0.36, 'pool': 0.75, 'sp': 0.1}

    def replay_merged(self, chains, prio=None):
        pos = [0] * len(chains)
        now = max(self.sim_eng.values()) if self.sim_eng else 0.0
        for k in self.names:
            self.sim_eng[k] = now
        while True:
            best, bkey, bt = None, None, 0.0
            for i, c in enumerate(chains):
                if pos[i] >= len(c):
                    continue
                kind, eng, fn, reads, writes, cost = c[pos[i]]
                t = self.sim_eng[eng]
                for r in reads:
                    t = max(t, self.sim_w.get(r, 0.0))
                for w in writes:
                    t = max(t, self.sim_w.get(w, 0.0), self.sim_r.get(w, 0.0))
                key = (t - (prio[i] if prio else 0.0), pos[i] / len(c))
                if best is None or key < bkey:
                    best, bkey, bt = i, key, t
            if best is None:
                break
            kind, eng, fn, reads, writes, cost = chains[best][pos[best]]
            pos[best] += 1
            if kind == 'op':
                end = bt + (cost if cost is not None else self.COST[eng])
                self.sim_eng[eng] = end
                done = end + 0.06
                self.op(eng, fn, reads, writes)
            else:
                self.sim_eng[eng] = bt + (1.0 if eng == 'pool' else 0.1)
                done = bt + 2.5
                self.dma(eng, fn, reads, writes)
            for r in reads:
                self.sim_r[r] = max(self.sim_r.get(r, 0.0), done)
            for w in writes:
                self.sim_w[w] = done
                self.sim_r[w] = 0.0

    def _semobj(self, k):
        return self.sem[k] if isinstance(k, str) else self.dsem[k]

    def _waits(self, eng, deps):
        best = {}
        for (k, v) in deps:
            if k == 'pe' and eng == 'pe':
                continue
            if self.seen[eng].get(k, 0) >= v:
                continue
            best[k] = max(best.get(k, 0), v)
        for k, v in best.items():
            self.seen[eng][k] = v
            so = self._semobj(k)
            self.prog[eng].append(lambda e, so=so, v=v: e.wait_ge(so, v))

    def _deps(self, reads, writes):
        deps = []
        for r in reads:
            if r in self.lastw:
                deps.append(self.lastw[r])
        for w in writes:
            if w in self.lastw:
                deps.append(self.lastw[w])
            deps.extend(self.readers.get(w, []))
        return deps

    def _commit(self, tok, reads, writes):
        for r in reads:
            self.readers.setdefault(r, []).append(tok)
        for w in writes:
            self.lastw[w] = tok
            self.readers[w] = []

    def op(self, eng, fn, reads=(), writes=(), cost=None):
        fn = _freeze(fn)
        if self.rec is not None:
            self.rec.append(('op', eng, fn, tuple(reads), tuple(writes), cost))
            return
        self._waits(eng, self._deps(reads, writes))
        self.cnt[eng] += 1
        so = self.sem[eng]
        self.prog[eng].append(lambda e, fn=fn, so=so: fn(e).then_inc(so, 1))
        self._commit((eng, self.cnt[eng]), reads, writes)

    def dma(self, eng, fn, reads=(), writes=()):
        fn = _freeze(fn)
        if self.rec is not None:
            self.rec.append(('dma', eng, fn, tuple(reads), tuple(writes), None))
            return
        i = self.dnext
        self.dnext = (self.dnext + 1) % len(self.dsem)
        deps = self._deps(reads, writes)
        if self.dcnt[i] > 0:
            deps.append((i, self.dcnt[i]))
        self._waits(eng, deps)
        self.dcnt[i] += 16
        so = self.dsem[i]
        self.prog[eng].append(lambda e, fn=fn, so=so: fn(e).then_inc(so, 16))
        self._commit((i, self.dcnt[i]), reads, writes)

    def barrier(self):
        deps = [(k, v) for k, v in self.cnt.items() if v > 0]
        deps += [(i, v) for i, v in enumerate(self.dcnt) if v > 0]
        for eng in self.names:
            self._waits(eng, deps)

    def emit(self):
        nc = self.nc
        prog = self.prog
        with nc.Block() as block:
            @block.tensor
            def _(e):
                for f in prog['pe']:
                    f(e)

            @block.scalar
            def _(e):
                for f in prog['act']:
                    f(e)

            @block.vector
            def _(e):
                for f in prog['dve']:
                    f(e)

            @block.gpsimd
            def _(e):
                for f in prog['pool']:
                    f(e)

            @block.sync
            def _(e):
                for f in prog['sp']:
                    f(e)
        self.prog = {k: [] for k in self.names}


def build_nc():
    nc = bass.Bass("TRN2", target_bir_lowering=False)

    def din(name, shape):
        return nc.dram_tensor(name, list(shape), F32, kind="ExternalInput").ap()

    xT = din("xT", [D, TOK])
    xtok = din("xtok", [TOK, D])
    w_in = din("w_in", [D, 3584])
    w_glu = din("w_glu", [512, 2048])
    w_cout = din("w_cout", [512, 1024])
    w_out = din("w_out", [D, D])
    w_rt = din("w_rt", [D, 36])
    w_gate = din("w_gate", [32, D, 256])
    w_up = din("w_up", [32, D, 256])
    w_down = din("w_down", [32, 256, D])
    cols = din("cols", [128, 72])
    convw = din("convw", [128, 4 * CW])
    rep5 = din("rep5", [128, 5 * D])
    brt = din("brt", [128, 36])
    pq = din("pq", [128, 48])
    bc = din("bc", [128, 4 * 256])
    ident_d = din("ident", [128, 128])
    kk_d = din("kk", [128, TS])
    out = nc.dram_tensor("out", [TOK, D], F32, kind="ExternalOutput").ap()
    tri_d = din("tri", [128, 128])
    ecap_d = din("ecap", [128, 32])
    bdmask_d = din("bdmask", [128, 128])
    xbuf_d = nc.dram_tensor("xbuf_scr", [32 * CAP, D], BF16, kind="Internal").ap()
    ybuf_d = nc.dram_tensor("ybuf_scr", [32 * CAP, D], BF16, kind="Internal").ap()
    zb_d = nc.dram_tensor("zb_scr", [TOK, D], F32, kind="Internal").ap()
    rep5v = rep5.rearrange("p (a d) -> p a d", a=5)

    with ExitStack() as es0:
        S = Sched(nc, es0)
        dest_all = es0.enter_context(nc.sbuf_tensor("dest_all", [128, 64], I32))
        gsel_all = es0.enter_context(nc.sbuf_tensor("gsel_all", [128, 32, 2], F32))
        eps_t = es0.enter_context(nc.sbuf_tensor("eps_t", [128, 1], F32))
        S.op('dve', lambda e: e.memset(eps_t[:], EPS), writes=['eps'])
        regh = {}

        def bcreg(e, tag):
            if tag not in regh:
                regh[tag] = e.alloc_register('bcr' + tag)
                e.reg_mov(regh[tag], 32 * CAP - 1)
            return regh[tag]
        vop = lambda fn, r, w, c=None: S.op('dve', fn, reads=r, writes=w, cost=c)
        aop = lambda fn, r, w, c=None: S.op('act', fn, reads=r, writes=w, cost=c)
        pop = lambda fn, r, w, c=None: S.op('pool', fn, reads=r, writes=w, cost=c)
        peop = lambda fn, r, w, c=None: S.op('pe', fn, reads=r, writes=w, cost=c)

        with ExitStack() as es:
            def sb(name, shape, dt=F32):
                return es.enter_context(nc.sbuf_tensor(name, list(shape), dt))
            PS = [es.enter_context(nc.psum_tensor("ps%d" % i, [128, 512], F32)) for i in range(8)]

            win_bf = sb("win_bf", [128, 8, 3584], BF16)
            wglu_bf = sb("wglu_bf", [128, 4, 2048], BF16)
            wcout_bf = sb("wcout_bf", [128, 4, 1024], BF16)
            wout_bf = sb("wout_bf", [128, 8, 1024], BF16)
            wrt32 = sb("wrt32", [128, 8, 36])
            cols_t = sb("cols_t", [128, 72])
            convw_t = sb("convw_t", [128, 4, CW])
            brt_t = sb("brt_t", [128, 36])
            ident = sb("ident_sb", [128, 128])
            ones_bf = sb("ones_bf", [128, 128], BF16)
            halfpi = sb("halfpi", [128, 1])
            repA = sb("repA", [128, 3, D])
            cdiag = sb("cdiag", [128, CW, 128], BF16)
            TC = 4
            NCH = TS // TC
            WZ = sb("WZ", [128, 4, TC, 2, 128], BF16)
            KM = sb("KM", [128, 4, TC, 128], BF16)
            WY = sb("WY", [128, 16, TC, 2, 32], BF16)
            cos2 = sb("cos2", [128, 16, NCH])
            sin2 = sb("sin2", [128, 16, NCH])
            R4rep = sb("R4rep", [128, 16, NCH])
            r4 = sb("r4", [128, 16])
            carry = sb("carry", [128, 2, 16])
            fk = lambda i: 'f%d' % i
            bk = lambda i: 'b%d' % i

            S.dma('sp', lambda e: e.dma_start(out=repA[:], in_=rep5v[:, 0:3, :]), writes=['rep'])
            S.dma('sp', lambda e: e.dma_start(out=wrt32[:], in_=w_rt.rearrange("(k p) c -> p k c", p=128)), writes=['wrt'])
            S.dma('sp', lambda e: e.dma_start(out=cols_t[:], in_=cols), writes=['cols'])
            S.dma('sp', lambda e: e.dma_start(out=convw_t[:], in_=convw.rearrange("p (c j) -> p c j", c=4)), writes=['convw'])
            S.dma('sp', lambda e: e.dma_start(out=brt_t[:], in_=brt), writes=['brt'])
            S.dma('sp', lambda e: e.dma_start(out=ident[:], in_=ident_d), writes=['ident'])
            vop(lambda e: e.memset(halfpi[:], math.pi / 2), [], ['halfpi'])
            vop(lambda e: e.memset(ones_bf[:], 1.0), [], ['ones'])

            with ExitStack() as esS:
                def sbs(name, shape, dt=F32):
                    return esS.enter_context(nc.sbuf_tensor(name, list(shape), dt))
                pq_t = sbs("pq_t", [128, 3, 16])
                bc_t = sbs("bc_t", [128, 4, 16, 16])
                kk = sbs("kk_sb", [128, NCH])
                NSL = 30
                small = sbs("small", [128, NSL * 16])
                vbrs = [sbs("vbr%d" % i, [128, 256]) for i in range(2)]
                vbis = [sbs("vbi%d" % i, [128, 256]) for i in range(2)]
                tmpbs = [sbs("tmpb%d" % i, [128, 256]) for i in range(2)]
                tmpas = [sbs("tmpa%d" % i, [128, 256]) for i in range(2)]
                E4rs = [sbs("E4r%d" % i, [128, 128]) for i in range(2)]
                E4is = [sbs("E4i%d" % i, [128, 128]) for i in range(2)]
                CTE = sbs("CTE", [128, 4, 2, 128])
                wtmps = [sbs("wtmp%d" % i, [128, 4, 16]) for i in range(4)]
                bdm = sbs("bdm", [128, 128])
                S.dma('sp', lambda e: e.dma_start(out=bdm[:], in_=bdmask_d), writes=['bdm'])
                tb6 = sbs("tb6", [128, 4, NCH])
                S.dma('sp', lambda e: e.dma_start(out=pq_t[:], in_=pq.rearrange("p (a q) -> p a q", a=3)), writes=['pq'])
                S.dma('sp', lambda e: e.dma_start(out=bc_t[:], in_=bc.rearrange("p (a q h) -> p a q h", a=4, q=16)), writes=['bc'])
                S.dma('sp', lambda e: e.dma_start(out=kk[:], in_=kk_d[:, 0:NCH]), writes=['kk'])
                for k in range(8):
                    S.dma('pool', lambda e, k=k: e.dma_start(out=win_bf[:, k, :], in_=w_in[k * 128:(k + 1) * 128, :]), writes=['win'])
                    S.dma('pool', lambda e, k=k: e.dma_start(out=wout_bf[:, k, :], in_=w_out[k * 128:(k + 1) * 128, :]), writes=['wout'])
                for k in range(4):
                    S.dma('pool', lambda e, k=k: e.dma_start(out=wglu_bf[:, k, :], in_=w_glu[k * 128:(k + 1) * 128, :]), writes=['wglu'])
                    S.dma('pool', lambda e, k=k: e.dma_start(out=wcout_bf[:, k, :], in_=w_cout[k * 128:(k + 1) * 128, :]), writes=['wcout'])
                sm = lambda i: small[:, i * 16:(i + 1) * 16]
                smc = lambda i, q: small[:, i * 16 + q:i * 16 + q + 1]
                sk = lambda i: 'sm%d' % i
                DT, RHO, TH, Y0, DEN, FR, FI, T1, T2, T3 = range(10)
                PR = lambda k: 10 + k
                PI = lambda k: 15 + k
                GR = lambda k: 20 + k
                GI = lambda k: 24 + k
                Y4 = 28
                lre, lim, ldt = pq_t[:, 0, :], pq_t[:, 1, :], pq_t[:, 2, :]
                TT = lambda o, a, b, op: vop(lambda e: e.tensor_tensor(out=sm(o), in0=sm(a), in1=sm(b), op=op), [sk(a), sk(b)], [sk(o)])
                aop(lambda e: e.activation(out=sm(DT), in_=ldt, func=AF.Exp), ['pq'], [sk(DT)])
                vop(lambda e: e.tensor_tensor(out=sm(RHO), in0=lre, in1=sm(DT), op=OP.mult), ['pq', sk(DT)], [sk(RHO)])
                vop(lambda e: e.tensor_tensor(out=sm(TH), in0=lim, in1=sm(DT), op=OP.mult), ['pq', sk(DT)], [sk(TH)])
                vop(lambda e: e.tensor_scalar(out=sm(Y0), in0=sm(TH), scalar1=1.0 / TWO_PI, scalar2=None, op0=OP.mult), [sk(TH)], [sk(Y0)])
                vop(lambda e: e.memset(sm(PR(0)), 1.0), [], [sk(PR(0))])
                vop(lambda e: e.memset(sm(PI(0)), 0.0), [], [sk(PI(0))])
                for k in range(1, TC + 1):
                    vop(lambda e: e.tensor_scalar(out=sm(T1), in0=sm(Y0), scalar1=float(k), scalar2=None, op0=OP.mult), [sk(Y0)], [sk(T1)])
                    vop(lambda e: e.tensor_scalar(out=sm(T2), in0=sm(T1), scalar1=MAGIC, scalar2=MAGIC, op0=OP.add, op1=OP.subtract), [sk(T1)], [sk(T2)])
                    TT(T1, T1, T2, OP.subtract)
                    aop(lambda e: e.activation(out=sm(T2), in_=sm(T1), func=AF.Abs), [sk(T1)], [sk(T2)])
                    aop(lambda e: e.activation(out=sm(PI(k)), in_=sm(T1), func=AF.Sin, scale=TWO_PI), [sk(T1)], [sk(PI(k))])
                    aop(lambda e: e.activation(out=sm(PR(k)), in_=sm(T2), func=AF.Sin, scale=-TWO_PI, bias=halfpi[:, 0:1]), [sk(T2), 'halfpi'], [sk(PR(k))])
                    aop(lambda e: e.activation(out=sm(T3), in_=sm(RHO), func=AF.Exp, scale=float(k)), [sk(RHO)], [sk(T3)])
                    TT(PR(k), PR(k), T3, OP.mult)
                    TT(PI(k), PI(k), T3, OP.mult)
                    if k == TC:
                        vop(lambda e: e.tensor_copy(out=r4[:], in_=sm(T3)), [sk(T3)], ['r4'])
                vop(lambda e: e.tensor_tensor(out=sm(T1), in0=lre, in1=lre, op=OP.mult), ['pq'], [sk(T1)])
                vop(lambda e: e.tensor_tensor(out=sm(T2), in0=lim, in1=lim, op=OP.mult), ['pq'], [sk(T2)])
                TT(DEN, T1, T2, OP.add)
                vop(lambda e: e.reciprocal(out=sm(DEN), in_=sm(DEN)), [sk(DEN)], [sk(DEN)])
                vop(lambda e: e.tensor_scalar(out=sm(T1), in0=sm(PR(1)), scalar1=-1.0, scalar2=None, op0=OP.add), [sk(PR(1))], [sk(T1)])
                vop(lambda e: e.tensor_tensor(out=sm(T2), in0=sm(T1), in1=lre, op=OP.mult), [sk(T1), 'pq'], [sk(T2)])
                vop(lambda e: e.tensor_tensor(out=sm(T3), in0=sm(PI(1)), in1=lim, op=OP.mult), [sk(PI(1)), 'pq'], [sk(T3)])
                TT(T2, T2, T3, OP.add)
                TT(FR, T2, DEN, OP.mult)
                vop(lambda e: e.tensor_tensor(out=sm(T2), in0=sm(PI(1)), in1=lre, op=OP.mult), [sk(PI(1)), 'pq'], [sk(T2)])
                vop(lambda e: e.tensor_tensor(out=sm(T3), in0=sm(T1), in1=lim, op=OP.mult), [sk(T1), 'pq'], [sk(T3)])
                TT(T2, T2, T3, OP.subtract)
                TT(FI, T2, DEN, OP.mult)
                for k in range(TC):
                    TT(T1, PR(k), FR, OP.mult)
                    TT(T2, PI(k), FI, OP.mult)
                    TT(GR(k), T1, T2, OP.subtract)
                    TT(T1, PR(k), FI, OP.mult)
                    TT(T2, PI(k), FR, OP.mult)
                    TT(GI(k), T1, T2, OP.add)
                vop(lambda e: e.memset(CTE[:], 0.0), [], ['CTE'])
                for cq in range(4):
                    for pl in range(4):
                        q = cq * 4 + pl
                        for g2 in range(2):
                            hsl = slice(g2 * 64, (g2 + 1) * 64)
                            csl = slice(pl * 32 + g2 * 16, pl * 32 + g2 * 16 + 16)
                            vop(lambda e: e.tensor_copy(out=CTE[hsl, cq, 0, csl], in_=bc_t[hsl, 2, q, :]), ['bc'], ['CTE'])
                            vop(lambda e: e.tensor_scalar(out=CTE[hsl, cq, 1, csl], in0=bc_t[hsl, 3, q, :], scalar1=-1.0, scalar2=None, op0=OP.mult), ['bc'], ['CTE'])
                def emit_VB(k):
                    vbr, vbi, tmpa, tmpb = vbrs[k % 2], vbis[k % 2], tmpas[k % 2], tmpbs[k % 2]
                    kvr, kvi, kta, ktb = 'vbr%d' % (k % 2), 'vbi%d' % (k % 2), 'tmpa%d' % (k % 2), 'tmpb%d' % (k % 2)
                    for q in range(16):
                        bre_q, bim_q = bc_t[:, 0, q, :], bc_t[:, 1, q, :]
                        sl = slice(q * 16, (q + 1) * 16)
                        vop(lambda e: e.tensor_scalar(out=tmpa[:, sl], in0=bim_q, scalar1=smc(GI(k), q), scalar2=None, op0=OP.mult), ['bc', sk(GI(k))], [kta])
                        vop(lambda e: e.tensor_scalar(out=tmpb[:, sl], in0=bre_q, scalar1=smc(GI(k), q), scalar2=None, op0=OP.mult), ['bc', sk(GI(k))], [ktb])
                        vop(lambda e: e.scalar_tensor_tensor(out=vbr[:, sl], in0=bre_q, scalar=smc(GR(k), q), in1=tmpa[:, sl], op0=OP.mult, op1=OP.subtract), ['bc', sk(GR(k)), kta], [kvr])
                        vop(lambda e: e.scalar_tensor_tensor(out=vbi[:, sl], in0=bim_q, scalar=smc(GR(k), q), in1=tmpb[:, sl], op0=OP.mult, op1=OP.add), ['bc', sk(GR(k)), ktb], [kvi])

                def emit_E4(k):
                    vbr, vbi = vbrs[k % 2], vbis[k % 2]
                    kvr, kvi = 'vbr%d' % (k % 2), 'vbi%d' % (k % 2)
                    for cq in range(4):
                        E4r, E4i = E4rs[cq % 2], E4is[cq % 2]
                        ekr, eki = 'E4r%d' % (cq % 2), 'E4i%d' % (cq % 2)
                        for ri, (src, E4, ek) in enumerate([(vbr, E4r, ekr), (vbi, E4i, eki)]):
                            pop(lambda e: e.memset(E4[:], 0.0), [], [ek + 'p', ek + 'a'])
                            for pl in range(4):
                                q = cq * 4 + pl
                                for g2 in range(2):
                                    o_ = E4[g2 * 64:(g2 + 1) * 64, pl * 32 + g2 * 16:pl * 32 + g2 * 16 + 16]
                                    i_ = src[g2 * 64:(g2 + 1) * 64, q * 16:(q + 1) * 16]
                                    if g2 == 0:
                                        pop(lambda e: e.tensor_copy(out=o_, in_=i_), [kvr, kvi], [ek + 'p'])
                                    else:
                                        aop(lambda e: e.activation(out=o_, in_=i_, func=AF.Identity), [kvr, kvi], [ek + 'a'])
                            peop(lambda e: e.transpose(out=PS[7][:, ri * 128:(ri + 1) * 128], in_=E4[:], identity=ident[:]), [ek + 'p', ek + 'a', 'ident'], ['ps7'])
                            aop(lambda e: e.activation(out=WZ[:, cq, TC - 1 - k, ri, :], in_=PS[7][:, ri * 128:(ri + 1) * 128], func=AF.Identity), ['ps7'], ['WZ'])
                        peop(lambda e: e.matmul(PS[6][:, 0:128], lhsT=E4r[:], rhs=CTE[:, cq, 0, :], start=True, stop=False), [ekr + 'p', ekr + 'a', 'CTE'], ['ps6'])
                        peop(lambda e: e.matmul(PS[6][:, 0:128], lhsT=E4i[:], rhs=CTE[:, cq, 1, :], start=False, stop=True), [eki + 'p', eki + 'a', 'CTE'], ['ps6'])
                        vop(lambda e: e.tensor_tensor(out=KM[:, cq, k, :], in0=PS[6][:, 0:128], in1=bdm[:], op=OP.mult), ['ps6', 'bdm'], ['KM'])
                emit_VB(0)
                for k in range(TC):
                    if k + 1 < TC:
                        emit_VB(k + 1)
                    emit_E4(k)
                vop(lambda e: e.memset(WY[:], 0.0), [], ['WYp', 'WYa'])
                for j in range(TC):
                    for q in range(16):
                        ctr, cti = bc_t[:, 2, q, :], bc_t[:, 3, q, :]
                        wtmp = wtmps[q % 4]
                        w0k, w1k, w2k, w3k = ['wt%d_%d' % (i_, q % 4) for i_ in range(4)]
                        prq, piq = smc(PR(j + 1), q), smc(PI(j + 1), q)
                        kpr, kpi = sk(PR(j + 1)), sk(PI(j + 1))
                        vop(lambda e: e.tensor_scalar(out=wtmp[:, 0, :], in0=cti, scalar1=piq, scalar2=None, op0=OP.mult), ['bc', kpi], [w0k])
                        vop(lambda e: e.tensor_scalar(out=wtmp[:, 2, :], in0=cti, scalar1=prq, scalar2=None, op0=OP.mult), ['bc', kpr], [w2k])
                        vop(lambda e: e.scalar_tensor_tensor(out=wtmp[:, 1, :], in0=ctr, scalar=prq, in1=wtmp[:, 0, :], op0=OP.mult, op1=OP.subtract), ['bc', kpr, w0k], [w1k])
                        vop(lambda e: e.scalar_tensor_tensor(out=wtmp[:, 3, :], in0=ctr, scalar=piq, in1=wtmp[:, 2, :], op0=OP.mult, op1=OP.add), ['bc', kpi, w2k], [w3k])
                        for g2 in range(2):
                            hsl = slice(g2 * 64, (g2 + 1) * 64)
                            if g2 == 0:
                                pop(lambda e: e.tensor_copy(out=WY[hsl, q, j, 0, g2 * 16:(g2 + 1) * 16], in_=wtmp[hsl, 1, :]), [w1k], ['WYp'])
                                pop(lambda e: e.tensor_scalar(out=WY[hsl, q, j, 1, g2 * 16:(g2 + 1) * 16], in0=wtmp[hsl, 3, :], scalar1=-1.0, scalar2=None, op0=OP.mult), [w3k], ['WYp'])
                            else:
                                aop(lambda e: e.activation(out=WY[hsl, q, j, 0, g2 * 16:(g2 + 1) * 16], in_=wtmp[hsl, 1, :], func=AF.Identity), [w1k], ['WYa'])
                                aop(lambda e: e.activation(out=WY[hsl, q, j, 1, g2 * 16:(g2 + 1) * 16], in_=wtmp[hsl, 3, :], func=AF.Identity, scale=-1.0), [w3k], ['WYa'])
                vop(lambda e: e.tensor_scalar(out=sm(Y4), in0=sm(Y0), scalar1=float(TC), scalar2=None, op0=OP.mult), [sk(Y0)], [sk(Y4)])
                for q in range(16):
                    vop(lambda e: e.tensor_scalar(out=tb6[:, 0, :], in0=kk[:], scalar1=smc(Y4, q), scalar2=None, op0=OP.mult), ['kk', sk(Y4)], ['tb0'])
                    vop(lambda e: e.tensor_scalar(out=tb6[:, 1, :], in0=tb6[:, 0, :], scalar1=MAGIC, scalar2=MAGIC, op0=OP.add, op1=OP.subtract), ['tb0'], ['tb1'])
                    vop(lambda e: e.tensor_tensor(out=tb6[:, 2, :], in0=tb6[:, 0, :], in1=tb6[:, 1, :], op=OP.subtract), ['tb0', 'tb1'], ['tb2'])
                    aop(lambda e: e.activation(out=tb6[:, 3, :], in_=tb6[:, 2, :], func=AF.Abs), ['tb2'], ['tb3'])
                    aop(lambda e: e.activation(out=sin2[:, q, :], in_=tb6[:, 2, :], func=AF.Sin, scale=TWO_PI), ['tb2'], ['sin2'])
                    aop(lambda e: e.activation(out=cos2[:, q, :], in_=tb6[:, 3, :], func=AF.Sin, scale=-TWO_PI, bias=halfpi[:, 0:1]), ['tb3', 'halfpi'], ['cos2'])
                    aop(lambda e: e.activation(out=R4rep[:, q, :], in_=kk[:], func=AF.Identity, scale=0.0, bias=r4[:, q:q + 1]), ['kk', 'r4'], ['R4rep'])
                vop(lambda e: e.memset(R4rep[:, :, 0:1], 0.0), ['R4rep'], ['R4rep'])
                S.barrier()
                S.emit()

            xT_bfs = [sb("xT_bf%d" % i, [128, 8, TS], BF16) for i in range(2)]
            u_bf = sb("u_bf", [128, 4, TS], BF16)
            um = sb("um", [128, TC - 1, TS], BF16)
            u_pm = sb("u_pm", [128, 4, TS], BF16)
            vbuf = sb("vbuf", [128, 4, 30 + TS], BF16)
            y_bf = sb("y_bf", [128, 4, TS], BF16)
            c_bf = sb("c_bf", [128, 4, TS], BF16)
            cs_bf = sb("cs_bf", [128, 4, TS], BF16)
            merged = sb("merged", [128, 8, TS], BF16)
            hb = sb("hb", [128, D], BF16)
            tri_bf = sb("tri_bf", [128, 128], BF16)
            ecap = sb("ecap_sb", [128, 32])
            selsum = sb("selsum", [128, 32], BF16)
            sel_bf = sb("sel_bf", [128, 32], BF16)
            NF = 9
            FT = [sb("f32t%d" % i, [128, TS]) for i in range(NF)]
            BT_ = [sb("bft%d" % i, [128, TS], BF16) for i in range(4)]
            xtk = sb("xtk", [128, D])
            zt = sb("zt", [128, D])
            hT32 = sb("hT32", [128, 8, 128])
            rsm = sb("rsm", [128, 512])
            stats = sb("stats", [128, 16])
            halo = sb("halo", [128, 4, 32], BF16)
            tmp4 = sb("tmp4", [128, 2, 4])
            CL = [sb("cl%d" % i, [128, TS]) for i in range(2)]
            S.dma('pool', lambda e: e.dma_start(out=tri_bf[:], in_=tri_d), writes=['tri'])
            S.dma('sp', lambda e: e.dma_start(out=ecap[:], in_=ecap_d), writes=['ecap'])
            vop(lambda e: e.memset(selsum[:], 0.0), [], ['selsum'])
            vop(lambda e: e.memset(um[:], 0.0), [], ['um'])

            bcol = lambda off, m: cols_t[:, off + m:off + m + 1]
            B_IN, B_GLU, B_COUT, DCOL, CONVB, LNCG, LNCB = 0, 28, 44, 52, 56, 60, 64
            mmrot = [0]
            NROT = 6

            def mmbank():
                b = mmrot[0]
                mmrot[0] = (mmrot[0] + 1) % NROT
                return b

            xcur = [None, None]

            def proj(m):
                b = mmbank()
                xT_bf, xkey = xcur
                for k in range(8):
                    peop(lambda e: e.matmul(PS[b][:, 0:TS], lhsT=win_bf[:, k, m * 128:(m + 1) * 128], rhs=xT_bf[:, k, :], start=(k == 0), stop=(k == 7)),
                         ['win', xkey], ['ps%d' % b])
                return b

            def load_xT(ti_):
                buf = xT_bfs[ti_ % 2]
                tt0 = ti_ * TS
                S.dma('pool', lambda e: e.dma_start(out=buf[:], in_=xT.rearrange("(k p) t -> p k t", p=128)[:, :, tt0:tt0 + TS]), writes=['xT%d' % (ti_ % 2)])

            TPS = SEQ // TS
            prev_tile = None
            for ti in range(NT):
                t0 = ti * TS
                first = (ti % TPS == 0)
                if ti == 0:
                    load_xT(0)
                xcur[0], xcur[1] = xT_bfs[ti % 2], 'xT%d' % (ti % 2)
                if ti + 1 < NT:
                    load_xT(ti + 1)
                if first:
                    pop(lambda e: e.memset(vbuf[:, :, 0:30], 0.0), [], ['vhalo'])
                    pop(lambda e: e.memset(carry[:], 0.0), [], ['carry'])

                for m in range(4):
                    b = proj(m)
                    aop(lambda e: e.activation(out=u_bf[:, m, :], in_=PS[b][:, 0:TS], func=AF.Identity, bias=bcol(B_IN, m)), ['ps%d' % b, 'cols'], ['u%d' % m])
                    aop(lambda e: e.activation(out=u_pm[:, m, :].rearrange("p (i c) -> p c i", i=TC), in_=PS[b][:, 0:TS].rearrange("p (c i) -> p c i", i=TC),
                                               func=AF.Identity, bias=bcol(B_IN, m)), ['ps%d' % b, 'cols'], ['up%d' % m])
                for c in range(4):
                    b = proj(8 + c)
                    aop(lambda e: e.activation(out=y_bf[:, c, :], in_=PS[b][:, 0:TS], func=AF.Sigmoid, bias=bcol(B_IN, 8 + c)), ['ps%d' % b, 'cols'], ['y%d' % c])
                for c in range(4):
                    b = proj(4 + c)
                    vop(lambda e: e.scalar_tensor_tensor(out=vbuf[:, c, 30:30 + TS], in0=PS[b][:, 0:TS], scalar=bcol(B_IN, 4 + c), in1=y_bf[:, c, :], op0=OP.add, op1=OP.mult),
                        ['ps%d' % b, 'cols', 'y%d' % c], ['v%d' % c])

                def sec_conv():
                    for c in range(4):
                        for j in range(CW):
                            if j % 4 != 3:
                                aop(lambda e: e.activation(out=cdiag[:, j, :], in_=ident[:], func=AF.Identity, scale=convw_t[:, c, j:j + 1]), ['ident', 'convw'], ['cd%d' % j])
                            else:
                                vop(lambda e: e.tensor_scalar(out=cdiag[:, j, :], in0=ident[:], scalar1=convw_t[:, c, j:j + 1], scalar2=None, op0=OP.mult), ['ident', 'convw'], ['cd%d' % j])
                        for j in range(CW):
                            peop(lambda e: e.matmul(PS[5][:, 0:TS], lhsT=cdiag[:, j, :], rhs=vbuf[:, c, j:j + TS], start=(j == 0), stop=(j == CW - 1)),
                                 ['cd%d' % j, 'v%d' % c, 'vhalo'], ['ps5'])
                        aop(lambda e: e.activation(out=c_bf[:, c, :], in_=PS[5][:, 0:TS], func=AF.Identity, bias=bcol(CONVB, c)), ['ps5', 'cols'], ['c_bf%d' % c])
                        aop(lambda e: e.activation(out=cs_bf[:, c, :], in_=PS[5][:, 0:TS], func=AF.Square, bias=bcol(CONVB, c)), ['ps5', 'cols'], ['cs%d' % c])
                        pop(lambda e: e.tensor_copy(out=halo[:, c, 0:30], in_=vbuf[:, c, TS:TS + 30]), ['v%d' % c], ['halo'])
                    for c in range(4):
                        peop(lambda e: e.matmul(PS[5][:, TS:2 * TS], lhsT=ones_bf[:], rhs=c_bf[:, c, :], start=(c == 0), stop=(c == 3)), ['ones', 'c_bf%d' % c], ['ps5'])
                    mean_t, rstd_t, msq_t, cn_t = CL[0], CL[1], CL[1], FT[8]
                    aop(lambda e: e.activation(out=mean_t[:], in_=PS[5][:, TS:2 * TS], func=AF.Identity, scale=1.0 / 512), ['ps5'], ['cl0'])
                    for c in range(4):
                        peop(lambda e: e.matmul(PS[5][:, TS:2 * TS], lhsT=ones_bf[:], rhs=cs_bf[:, c, :], start=(c == 0), stop=(c == 3)), ['ones', 'cs%d' % c], ['ps5'])
                    vop(lambda e: e.tensor_tensor(out=msq_t[:], in0=mean_t[:], in1=mean_t[:], op=OP.mult), ['cl0'], ['cl1'])
                    vop(lambda e: e.scalar_tensor_tensor(out=msq_t[:], in0=PS[5][:, TS:2 * TS], scalar=1.0 / 512, in1=msq_t[:], op0=OP.mult, op1=OP.subtract), ['ps5', 'cl1'], ['cl1'])
                    aop(lambda e: e.activation(out=msq_t[:], in_=msq_t[:], func=AF.Sqrt, bias=eps_t[:, 0:1]), ['cl1', 'eps'], ['cl1'])
                    vop(lambda e: e.reciprocal(out=rstd_t[:], in_=msq_t[:]), ['cl1'], ['cl1'])
                    for c in range(4):
                        pop(lambda e: e.tensor_tensor(out=cn_t[:], in0=c_bf[:, c, :], in1=mean_t[:], op=OP.subtract), ['c_bf%d' % c, 'cl0'], [fk(8)])
                        pop(lambda e: e.tensor_tensor(out=cn_t[:], in0=cn_t[:], in1=rstd_t[:], op=OP.mult), [fk(8), 'cl1'], [fk(8)])
                        aop(lambda e: e.activation(out=cs_bf[:, c, :], in_=cn_t[:], func=AF.Silu, scale=bcol(LNCG, c), bias=bcol(LNCB, c)), [fk(8), 'cols'], ['cs%d' % c])
                    for c in range(4):
                        pop(lambda e: e.tensor_copy(out=vbuf[:, c, 0:30], in_=halo[:, c, 0:30]), ['halo', 'v%d' % c], ['vhalo'])

                def sec_s5():
                    v3 = lambda t: t[:].rearrange("p (a c) -> p a c", a=4)
                    t1, t2, zre, zim, sre, sim = FT[0], FT[1], FT[2], FT[3], FT[4], FT[5]
                    k1, k2, kzr, kzi, ksr, ksi = fk(0), fk(1), fk(2), fk(3), fk(4), fk(5)

                    def PY(cq):
                        h_ = (cq % 2) if S5_HALF else 0
                        return PS[7 - h_][:, 0:TS], 'ps%d' % (7 - h_)

                    def SPb(cq, ri):
                        i_ = (cq % 2) * 2 + ri
                        return BT_[i_], bk(i_)

                    def stage_front(cq):
                        qs = slice(4 * cq, 4 * cq + 4)
                        C2 = cos2[:, qs, :].rearrange("p a c -> p (a c)")
                        S2 = sin2[:, qs, :].rearrange("p a c -> p (a c)")
                        RR = R4rep[:, qs, :].rearrange("p a c -> p (a c)")
                        ucq = u_bf[:, cq, :].rearrange("p (c i) -> p c i", i=TC)
                        for pl in range(4):
                            pr = slice(pl * 32, (pl + 1) * 32)
                            for ri in range(2):
                                for i in range(TC):
                                    ua = u_pm[pr, cq, i * NCH:(i + 1) * NCH]
                                    peop(lambda e: e.matmul(PS[3 + ri][:, pl * NCH:(pl + 1) * NCH], lhsT=WZ[pr, cq, i, ri, :], rhs=ua, start=(i == 0), stop=(i == TC - 1),
                                                            tile_position=(pl * 32, 0)), ['WZ', 'up%d' % cq], ['ps%d' % (3 + ri)])
                        for d in range(1, TC):
                            pop(lambda e: e.tensor_copy(out=um[:, d - 1, :].rearrange("p (c i) -> p c i", i=TC)[:, :, d:TC], in_=ucq[:, :, 0:TC - d]), ['u%d' % cq, 'um'], ['um%d' % d])
                        py, pyk = PY(cq)
                        for d in range(TC):
                            rhs_ = u_bf[:, cq, :] if d == 0 else um[:, d - 1, :]
                            peop(lambda e: e.matmul(py, lhsT=KM[:, cq, d, :], rhs=rhs_, start=(d == 0), stop=False), ['KM', 'u%d' % cq] + (['um%d' % d] if d else []), [pyk])
                        vop(lambda e: e.tensor_tensor(out=t1[:], in0=PS[3][:, 0:TS], in1=C2, op=OP.mult), ['ps3', 'cos2'], [k1])
                        vop(lambda e: e.tensor_tensor(out=t2[:], in0=PS[4][:, 0:TS], in1=S2, op=OP.mult), ['ps4', 'sin2'], [k2])
                        pop(lambda e: e.tensor_tensor(out=zre[:], in0=t1[:], in1=t2[:], op=OP.add), [k1, k2], [kzr])
                        vop(lambda e: e.tensor_tensor(out=sre[:], in0=PS[4][:, 0:TS], in1=C2, op=OP.mult), ['ps4', 'cos2'], [ksr])
                        vop(lambda e: e.tensor_tensor(out=sim[:], in0=PS[3][:, 0:TS], in1=S2, op=OP.mult), ['ps3', 'sin2'], [ksi])
                        pop(lambda e: e.tensor_tensor(out=zim[:], in0=sre[:], in1=sim[:], op=OP.subtract), [ksr, ksi], [kzi])
                        for ri, (zt_, kz) in enumerate([(zre, kzr), (zim, kzi)]):
                            vop(lambda e: e.tensor_tensor(out=tmp4[:, ri, :], in0=carry[:, ri, qs], in1=r4[:, qs], op=OP.mult), ['carry', 'r4'], ['tmp4_%d' % ri])
                            vop(lambda e: e.tensor_tensor(out=v3(zt_)[:, :, 0], in0=v3(zt_)[:, :, 0], in1=tmp4[:, ri, :], op=OP.add), [kz, 'tmp4_%d' % ri], [kz])
                        vop(lambda e: e.tensor_tensor_scan(out=sre[:], data0=RR, data1=zre[:], initial=0.0, op0=OP.mult, op1=OP.add), ['R4rep', kzr], [ksr])
                        vop(lambda e: e.tensor_tensor_scan(out=sim[:], data0=RR, data1=zim[:], initial=0.0, op0=OP.mult, op1=OP.add), ['R4rep', kzi], [ksi])
                        pop(lambda e: e.tensor_tensor(out=t1[:], in0=sre[:], in1=C2, op=OP.mult), [ksr, 'cos2'], [k1])
                        pop(lambda e: e.tensor_tensor(out=t2[:], in0=sim[:], in1=S2, op=OP.mult), [ksi, 'sin2'], [k2])
                        pop(lambda e: e.tensor_tensor(out=zre[:], in0=t1[:], in1=t2[:], op=OP.subtract), [k1, k2], [kzr])
                        vop(lambda e: e.tensor_tensor(out=t1[:], in0=sre[:], in1=S2, op=OP.mult), [ksr, 'sin2', kzr], [k1])
                        vop(lambda e: e.tensor_tensor(out=t2[:], in0=sim[:], in1=C2, op=OP.mult), [ksi, 'cos2', kzr], [k2])
                        vop(lambda e: e.tensor_tensor(out=zim[:], in0=t1[:], in1=t2[:], op=OP.add), [k1, k2], [kzi])
                        for ri, (zt_, kz) in enumerate([(zre, kzr), (zim, kzi)]):
                            spb, spk = SPb(cq, ri)
                            sp3 = spb[:].rearrange("p (a c) -> p a c", a=4)
                            aop(lambda e: e.activation(out=sp3[:, :, 1:NCH], in_=v3(zt_)[:, :, 0:NCH - 1], func=AF.Identity), [kz], [spk])
                            aop(lambda e: e.activation(out=sp3[:, :, 0], in_=carry[:, ri, qs], func=AF.Identity), ['carry'], [spk])
                            aop(lambda e: e.activation(out=carry[:, ri, qs], in_=v3(zt_)[:, :, NCH - 1], func=AF.Identity), [kz, spk], ['carry'])

                    def stage_back(cq):
                        py, pyk = PY(cq)
                        for pl in range(4):
                            q = 4 * cq + pl
                            pr = slice(pl * 32, (pl + 1) * 32)
                            for j in range(TC):
                                oj = py[pr, :].rearrange("p (c j) -> p j c", j=TC)[:, j, :]
                                for ri in range(2):
                                    spb, spk = SPb(cq, ri)
                                    peop(lambda e: e.matmul(oj, lhsT=WY[:, q, j, ri, :], rhs=spb[:, pl * NCH:(pl + 1) * NCH], start=False, stop=(ri == 1), tile_position=(0, pl * 32)),
                                         ['WY', spk], [pyk])
                        ys, g1 = FT[6], FT[7]
                        kA, kB = fk(6), fk(7)
                        vop(lambda e: e.scalar_tensor_tensor(out=ys[:], in0=u_bf[:, cq, :], scalar=bcol(DCOL, cq), in1=py, op0=OP.mult, op1=OP.add),
                            ['u%d' % cq, 'cols', pyk], [kA])
                        aop(lambda e: e.activation(out=g1[:], in_=ys[:], func=AF.Square), [kA], [kB])
                        vop(lambda e: e.tensor_scalar(out=g1[:], in0=g1[:], scalar1=0.044715, scalar2=1.0, op0=OP.mult, op1=OP.add), [kB], [kB])
                        vop(lambda e: e.tensor_tensor(out=g1[:], in0=g1[:], in1=ys[:], op=OP.mult), [kB, kA], [kB])
                        aop(lambda e: e.activation(out=g1[:], in_=g1[:], func=AF.Sigmoid, scale=1.5957691216057308), [kB], [kB])
                        vop(lambda e: e.tensor_tensor(out=y_bf[:, cq, :], in0=g1[:], in1=ys[:], op=OP.mult), [kB, kA], ['y%d' % cq])

                    if S5_DELAY:
                        stage_front(0)
                        for cq in range(1, 4):
                            stage_front(cq)
                            stage_back(cq - 1)
                        stage_back(3)
                    else:
                        for cq in range(4):
                            stage_front(cq)
                            stage_back(cq)

                def sec_ln(ti, t0):
                    for st in range(TS // 128):
                        r0 = t0 + st * 128
                        gst = ti * (TS // 128) + st
                        S.dma('sp', lambda e: e.dma_start(out=xtk[:], in_=xtok[r0:r0 + 128, :]), writes=['xtk'])
                        for hf in range(2):
                            hs = slice(hf * 512, (hf + 1) * 512)
                            b = hf
                            for k in range(8):
                                peop(lambda e: e.matmul(PS[b][:], lhsT=merged[:, k, st * 128:(st + 1) * 128], rhs=wout_bf[:, k, hs], start=(k == 0), stop=(k == 7)),
                                     ['wout', 'mg%d' % k], ['ps%d' % b], 0.28)
                            vop(lambda e: e.scalar_tensor_tensor(out=zt[:, hs], in0=xtk[:, hs], scalar=ALPHA, in1=PS[b][:], op0=OP.mult, op1=OP.add), ['xtk', 'ps%d' % b], ['zt%d' % hf], 0.6)
                            vop(lambda e: e.tensor_tensor(out=zt[:, hs], in0=zt[:, hs], in1=repA[:, 0, hs], op=OP.add), ['zt%d' % hf, 'rep'], ['zt%d' % hf], 0.6)
                            vop(lambda e: e.bn_stats(out=stats[:, hf * 6:(hf + 1) * 6], in_=zt[:, hs]), ['zt%d' % hf], ['bst%d' % hf], 0.65)
                        vop(lambda e: e.bn_aggr(out=stats[:, 12:14], in_=stats[:, 0:12]), ['bst0', 'bst1'], ['mv'])
                        aop(lambda e: e.activation(out=stats[:, 14:15], in_=stats[:, 13:14], func=AF.Sqrt, bias=eps_t[:, 0:1]), ['mv', 'eps'], ['sd'])
                        vop(lambda e: e.reciprocal(out=stats[:, 15:16], in_=stats[:, 14:15]), ['sd'], ['rs'])
                        vop(lambda e: e.tensor_scalar(out=zt[:], in0=zt[:], scalar1=stats[:, 12:13], scalar2=stats[:, 15:16], op0=OP.subtract, op1=OP.mult),
                            ['zt0', 'zt1', 'mv', 'rs'], ['zt0', 'zt1'], 0.85)
                        vop(lambda e: e.tensor_tensor(out=zt[:], in0=zt[:], in1=repA[:, 1, :], op=OP.mult), ['zt0', 'zt1', 'rep'], ['zt0', 'zt1'], 1.1)
                        vop(lambda e: e.tensor_tensor(out=zt[:], in0=zt[:], in1=repA[:, 2, :], op=OP.add), ['zt0', 'zt1', 'rep'], ['zt0', 'zt1'], 1.1)
                        aop(lambda e: e.activation(out=xtk[:], in_=zt[:], func=AF.Identity, scale=ALPHA), ['zt0', 'zt1', 'xtk'], ['xtk'], 1.06)
                        S.dma('sp', lambda e: e.dma_start(out=zb_d[r0:r0 + 128, :], in_=xtk[:]), reads=['xtk'], writes=['zb_d'])
                        if DEBUG_H:
                            S.dma('sp', lambda e: e.dma_start(out=out[r0:r0 + 128, :], in_=zt[:]), reads=['zt0', 'zt1'], writes=['out'])
                        for kb in range(2):
                            for kk4 in range(4):
                                k = kb * 4 + kk4
                                peop(lambda e: e.transpose(out=PS[2][:, kk4 * 128:(kk4 + 1) * 128], in_=zt[:, k * 128:(k + 1) * 128], identity=ident[:]),
                                     ['zt0', 'zt1', 'ident'], ['ps2'], 0.41)
                            aop(lambda e: e.activation(out=hT32[:, kb * 4:(kb + 1) * 4, :], in_=PS[2][:].rearrange("p (a b) -> p a b", a=4), func=AF.Identity), ['ps2'], ['hT32'], 0.5)
                        for k in range(8):
                            peop(lambda e: e.matmul(PS[2][:, 0:36], lhsT=hT32[:, k, :], rhs=wrt32[:, k, :], start=(k == 0), stop=(k == 7)), ['hT32', 'wrt'], ['ps2'], 0.27)
                        svop = lambda fn, r, w: vop(fn, r, w, 0.14)
                        R = lambda a, b: rsm[:, 192 + a:192 + b]
                        lg, zem, sel, ex, top8 = rsm[:, 0:36], rsm[:, 40:72], rsm[:, 72:104], rsm[:, 104:136], rsm[:, 136:144]
                        svop(lambda e: e.tensor_tensor(out=lg, in0=PS[2][:, 0:36], in1=brt_t[:], op=OP.add), ['ps2', 'brt'], ['lg'])
                        svop(lambda e: e.tensor_reduce(out=R(0, 1), in_=rsm[:, 0:4], axis=AX.X, op=OP.max), ['lg'], ['gmax'])
                        svop(lambda e: e.tensor_scalar(out=R(4, 8), in0=rsm[:, 0:4], scalar1=R(0, 1), scalar2=None, op0=OP.is_ge), ['lg', 'gmax'], ['ohg'])
                        svop(lambda e: e.tensor_scalar(out=R(1, 2), in0=R(0, 1), scalar1=-1.0, scalar2=None, op0=OP.mult), ['gmax'], ['ngmax'])
                        aop(lambda e: e.activation(out=R(8, 12), in_=rsm[:, 0:4], func=AF.Exp, bias=R(1, 2)), ['lg', 'ngmax'], ['eg'])
                        svop(lambda e: e.tensor_reduce(out=R(2, 3), in_=R(8, 12), axis=AX.X, op=OP.add), ['eg'], ['gsum'])
                        svop(lambda e: e.reciprocal(out=R(2, 3), in_=R(2, 3)), ['gsum'], ['gsum'])
                        svop(lambda e: e.tensor_scalar(out=R(12, 16), in0=R(4, 8), scalar1=-1.0, scalar2=1e30, op0=OP.add, op1=OP.mult), ['ohg'], ['pen'])
                        for g in range(4):
                            svop(lambda e: e.tensor_scalar(out=rsm[:, 40 + g * 8:48 + g * 8], in0=rsm[:, 4 + g * 8:12 + g * 8], scalar1=R(12 + g, 13 + g), scalar2=None, op0=OP.add),
                                ['lg', 'pen'], ['zem'])
                        svop(lambda e: e.max(out=top8, in_=zem), ['zem'], ['top8'])
                        svop(lambda e: e.tensor_scalar(out=sel, in0=zem, scalar1=rsm[:, 137:138], scalar2=None, op0=OP.is_ge), ['zem', 'top8'], ['sel'])
                        svop(lambda e: e.tensor_scalar(out=R(3, 4), in0=rsm[:, 136:137], scalar1=-1.0, scalar2=None, op0=OP.mult), ['top8'], ['nv1'])
                        aop(lambda e: e.activation(out=ex, in_=zem, func=AF.Exp, bias=R(3, 4)), ['zem', 'nv1'], ['ex'])
                        svop(lambda e: e.tensor_tensor(out=ex, in0=ex, in1=sel, op=OP.mult), ['ex', 'sel'], ['ex'])
                        svop(lambda e: e.tensor_reduce(out=R(16, 17), in_=ex, axis=AX.X, op=OP.add), ['ex'], ['den'])
                        svop(lambda e: e.reciprocal(out=R(16, 17), in_=R(16, 17)), ['den'], ['den'])
                        svop(lambda e: e.tensor_tensor(out=R(16, 17), in0=R(16, 17), in1=R(2, 3), op=OP.mult), ['den', 'gsum'], ['den'])
                        gt, oh0, oh1, pos, tmpr, ovf = (rsm[:, 256:288], rsm[:, 288:320], rsm[:, 320:352], rsm[:, 352:384], rsm[:, 384:416], rsm[:, 416:448])
                        X = lambda i: rsm[:, 448 + i:449 + i]
                        svop(lambda e: e.tensor_scalar(out=gt, in0=ex, scalar1=R(16, 17), scalar2=None, op0=OP.mult), ['ex', 'den'], ['gt'])
                        svop(lambda e: e.tensor_scalar(out=oh0, in0=zem, scalar1=rsm[:, 136:137], scalar2=None, op0=OP.is_ge), ['zem', 'top8'], ['oh0'])
                        svop(lambda e: e.tensor_tensor(out=oh1, in0=sel, in1=oh0, op=OP.subtract), ['sel', 'oh0'], ['oh1'])
                        svop(lambda e: e.tensor_copy(out=sel_bf[:], in_=sel), ['sel'], ['sel_bf'])
                        peop(lambda e: e.matmul(PS[2][:, 64:96], lhsT=tri_bf[:], rhs=sel_bf[:], start=True, stop=False), ['tri', 'sel_bf'], ['ps2'])
                        peop(lambda e: e.matmul(PS[2][:, 64:96], lhsT=ones_bf[:], rhs=selsum[:], start=False, stop=True), ['ones', 'selsum'], ['ps2'])
                        svop(lambda e: e.tensor_tensor(out=selsum[:], in0=selsum[:], in1=sel, op=OP.add), ['selsum', 'sel'], ['selsum'])
                        svop(lambda e: e.tensor_tensor(out=pos, in0=PS[2][:, 64:96], in1=ecap[:], op=OP.add), ['ps2', 'ecap'], ['pos'])
                        svop(lambda e: e.tensor_scalar(out=ovf, in0=PS[2][:, 64:96], scalar1=float(CAP), scalar2=1e6, op0=OP.is_ge, op1=OP.mult), ['ps2'], ['ovf'])
                        svop(lambda e: e.tensor_tensor(out=pos, in0=pos, in1=ovf, op=OP.add), ['pos', 'ovf'], ['pos'])
                        for kq, ohk in enumerate([oh0, oh1]):
                            okey = 'oh%d' % kq
                            svop(lambda e: e.tensor_tensor(out=tmpr, in0=ohk, in1=pos, op=OP.mult), [okey, 'pos'], ['tmpr'])
                            svop(lambda e: e.tensor_reduce(out=X(kq), in_=tmpr, axis=AX.X, op=OP.add), ['tmpr'], ['dx%d' % kq])
                            svop(lambda e: e.tensor_copy(out=dest_all[:, 2 * gst + kq:2 * gst + kq + 1], in_=X(kq)), ['dx%d' % kq], ['dest%d_%d' % (gst, kq)])
                            svop(lambda e: e.tensor_tensor(out=tmpr, in0=ohk, in1=gt, op=OP.mult), [okey, 'gt'], ['tmpr'])
                            svop(lambda e: e.tensor_reduce(out=gsel_all[:, gst, kq:kq + 1], in_=tmpr, axis=AX.X, op=OP.add), ['tmpr'], ['gsel'])
                        aop(lambda e: e.activation(out=hb[:].rearrange("t (k p) -> t k p", p=128), in_=zt[:].rearrange("t (p k) -> t k p", k=8), func=AF.Identity), ['zt0', 'zt1'], ['hb'], 1.0)
                        for kq in range(2):
                            S.dma('pool', lambda e: e.indirect_dma_start(out=xbuf_d, out_offset=bass.IndirectOffsetOnAxis(ap=dest_all[:, 2 * gst + kq:2 * gst + kq + 1], axis=0), in_=hb[:], in_offset=None,
                                                                         bounds_check=bcreg(e, 'A'), oob_is_err=False), reads=['hb', 'dest%d_%d' % (gst, kq)], writes=['xbuf'])
                chains = [S.record(sec_conv), S.record(sec_s5)]
                prio = [0.0, S5_PRIO]
                if prev_tile is not None:
                    pt = prev_tile
                    chains.insert(0, S.record(lambda: sec_ln(*pt)))
                    prio.insert(0, 0.0)
                S.replay_merged(chains, prio)
                for m in range(8):
                    b = proj(20 + m)
                    aop(lambda e: e.activation(out=BT_[3][:], in_=PS[b][:, 0:TS], func=AF.Sigmoid, bias=bcol(B_IN, 20 + m)), ['ps%d' % b, 'cols'], [bk(3)])
                    b2 = mmbank()
                    for k in range(4):
                        peop(lambda e: e.matmul(PS[b2][:, 0:TS], lhsT=wcout_bf[:, k, m * 128:(m + 1) * 128], rhs=cs_bf[:, k, :], start=(k == 0), stop=(k == 3)),
                             ['wcout', 'cs%d' % k], ['ps%d' % b2])
                    vop(lambda e: e.scalar_tensor_tensor(out=merged[:, m, :], in0=PS[b2][:, 0:TS], scalar=bcol(B_COUT, m), in1=BT_[3][:], op0=OP.add, op1=OP.mult),
                        ['ps%d' % b2, 'cols', bk(3)], ['mg%d' % m])

                for m in range(8):
                    b = proj(12 + m)
                    aop(lambda e: e.activation(out=BT_[3][:], in_=PS[b][:, 0:TS], func=AF.Sigmoid, bias=bcol(B_IN, 12 + m)), ['ps%d' % b, 'cols'], [bk(3)])
                    bg = mmbank()
                    for k in range(4):
                        peop(lambda e: e.matmul(PS[bg][:, 0:TS], lhsT=wglu_bf[:, k, (8 + m) * 128:(9 + m) * 128], rhs=y_bf[:, k, :], start=(k == 0), stop=(k == 3)),
                             ['wglu', 'y%d' % k], ['ps%d' % bg])
                    aop(lambda e: e.activation(out=BT_[2][:], in_=PS[bg][:, 0:TS], func=AF.Sigmoid, bias=bcol(B_GLU, 8 + m)), ['ps%d' % bg, 'cols'], [bk(2)])
                    bv = mmbank()
                    for k in range(4):
                        peop(lambda e: e.matmul(PS[bv][:, 0:TS], lhsT=wglu_bf[:, k, m * 128:(m + 1) * 128], rhs=y_bf[:, k, :], start=(k == 0), stop=(k == 3)),
                             ['wglu', 'y%d' % k], ['ps%d' % bv])
                    vop(lambda e: e.scalar_tensor_tensor(out=FT[8][:], in0=PS[bv][:, 0:TS], scalar=bcol(B_GLU, m), in1=BT_[2][:], op0=OP.add, op1=OP.mult),
                        ['ps%d' % bv, 'cols', bk(2)], [fk(8)])
                    pop(lambda e: e.tensor_tensor(out=FT[8][:], in0=FT[8][:], in1=BT_[3][:], op=OP.mult), [fk(8), bk(3)], [fk(8)])
                    pop(lambda e: e.tensor_tensor(out=merged[:, m, :], in0=FT[8][:], in1=merged[:, m, :], op=OP.add), [fk(8), 'mg%d' % m], ['mg%d' % m])

                prev_tile = (ti, t0)
                if ti == NT - 1:
                    S.replay_merged([S.record(lambda: sec_ln(ti, t0))])

            S.barrier()
            S.emit()

        if not DEBUG_H:
            with ExitStack() as es:
                def sb(name, shape, dt=F32):
                    return es.enter_context(nc.sbuf_tensor(name, list(shape), dt))
                PS = [es.enter_context(nc.psum_tensor("pb%d" % i, [128, 512], F32)) for i in range(6)]
                PT = [es.enter_context(nc.psum_tensor("pt%d" % i, [128, 1024], BF16)) for i in range(2)]
                wg = [sb("wg%d" % i, [128, 8, 256], BF16) for i in range(2)]
                wu = [sb("wu%d" % i, [128, 8, 256], BF16) for i in range(2)]
                wd = [sb("wd%d" % i, [128, 2, D], BF16) for i in range(2)]
                xrows = [sb("xrows%d" % i, [128, NS4, D], BF16) for i in range(2)]
                xTe = sb("xTe", [128, 8, CAP], BF16)
                sgt = sb("sgt", [128, 2, CAP], BF16)
                hid = sb("hid", [128, 2, CAP], BF16)
                ysb = [sb("ysb%d" % i, [128, NS4, D], BF16) for i in range(2)]
                identb = sb("identb", [128, 128], BF16)
                NACC = 4
                statb = sb("statsb", [128, 24 * NACC])
                repB = sb("repB", [128, 2, D])
                acc = [sb("acc%d" % i, [128, D]) for i in range(NACC)]
                yg = [[sb("yg%d_%d" % (i, k), [128, D], BF16) for k in range(2)] for i in range(NACC)]
                S.dma('sp', lambda e: e.dma_start(out=repB[:], in_=rep5v[:, 3:5, :]), writes=['repB'])
                S.dma('pool', lambda e: e.dma_start(out=identb[:], in_=ident_d), writes=['identb'])
                for i in range(NACC):
                    for k in range(2):
                        pop(lambda e: e.memset(yg[i][k][:], 0.0), [], ['yg%d_%d' % (i, k)])
                rot = [0]

                def bank(n=6):
                    b = rot[0]
                    rot[0] = (rot[0] + 1) % n
                    return b
                def load_expert(ex_j):
                    wj = ex_j % 2
                    xrj = xrows[wj]
                    S.dma('sp', lambda e: e.dma_start(out=xrj[:], in_=xbuf_d[ex_j * CAP:(ex_j + 1) * CAP, :].rearrange("(s p) d -> p s d", p=128)), reads=['xbuf'], writes=['xr%d' % wj])
                    S.dma('pool', lambda e: e.dma_start(out=wg[wj][:], in_=w_gate[ex_j].rearrange("(p k) f -> p k f", k=8)), writes=['wg%d' % wj])
                    S.dma('pool', lambda e: e.dma_start(out=wu[wj][:], in_=w_up[ex_j].rearrange("(p k) f -> p k f", k=8)), writes=['wu%d' % wj])
                    S.dma('pool', lambda e: e.dma_start(out=wd[wj][:], in_=w_down[ex_j].rearrange("(k p) f -> p k f", p=128)), writes=['wd%d' % wj])
                load_expert(0)
                for ex_i in range(32):
                    wi = ex_i % 2
                    xr = xrows[wi]
                    ys = ysb[wi]
                    if ex_i + 1 < 32:
                        load_expert(ex_i + 1)
                    for s4 in range(NS4):
                        pt = PT[s4 % 2]
                        for k in range(8):
                            peop(lambda e: e.transpose(out=pt[:, k * 128:(k + 1) * 128], in_=xr[:, s4, k * 128:(k + 1) * 128], identity=identb[:]), ['xr%d' % wi, 'identb'], ['pt%d' % (s4 % 2)])
                        ev = aop if s4 % 2 == 0 else vop
                        ev(lambda e: e.tensor_copy(out=xTe[:, :, s4 * 128:(s4 + 1) * 128], in_=pt[:].rearrange("p (k r) -> p k r", k=8)) if False else
                           (e.activation(out=xTe[:, :, s4 * 128:(s4 + 1) * 128], in_=pt[:].rearrange("p (k r) -> p k r", k=8), func=AF.Identity) if s4 % 2 == 0 else
                            e.tensor_copy(out=xTe[:, :, s4 * 128:(s4 + 1) * 128], in_=pt[:].rearrange("p (k r) -> p k r", k=8))),
                           ['pt%d' % (s4 % 2)], ['xTe%d' % s4])
                    xk = ['xTe%d' % i for i in range(NS4)]
                    for f in range(2):
                        bgk = bank()
                        for k in range(8):
                            peop(lambda e: e.matmul(PS[bgk][:, 0:CAP], lhsT=wg[wi][:, k, f * 128:(f + 1) * 128], rhs=xTe[:, k, :], start=(k == 0), stop=(k == 7)), ['wg%d' % wi] + xk, ['pb%d' % bgk])
                        aop(lambda e: e.activation(out=sgt[:, f, :], in_=PS[bgk][:, 0:CAP], func=AF.Silu), ['pb%d' % bgk], ['sgt%d' % f])
                        buk = bank()
                        for k in range(8):
                            peop(lambda e: e.matmul(PS[buk][:, 0:CAP], lhsT=wu[wi][:, k, f * 128:(f + 1) * 128], rhs=xTe[:, k, :], start=(k == 0), stop=(k == 7)), ['wu%d' % wi] + xk, ['pb%d' % buk])
                        vop(lambda e: e.tensor_tensor(out=hid[:, f, :], in0=PS[buk][:, 0:CAP], in1=sgt[:, f, :], op=OP.mult), ['pb%d' % buk, 'sgt%d' % f], ['hid%d' % f])
                    for s4 in range(NS4):
                        for hf in range(2):
                            hs = slice(hf * 512, (hf + 1) * 512)
                            b = bank()
                            for f in range(2):
                                peop(lambda e: e.matmul(PS[b][:], lhsT=hid[:, f, s4 * 128:(s4 + 1) * 128], rhs=wd[wi][:, f, hs], start=(f == 0), stop=(f == 1)),
                                     ['hid0', 'hid1', 'wd%d' % wi], ['pb%d' % b])
                            if (s4 * 2 + hf) % 2 == 0:
                                aop(lambda e: e.activation(out=ys[:, s4, hs], in_=PS[b][:], func=AF.Identity), ['pb%d' % b], ['ysa%d' % wi])
                            else:
                                vop(lambda e: e.tensor_copy(out=ys[:, s4, hs], in_=PS[b][:]), ['pb%d' % b], ['ysv%d' % wi])
                    S.dma('sp', lambda e: e.dma_start(out=ybuf_d[ex_i * CAP:(ex_i + 1) * CAP, :].rearrange("(s p) d -> p s d", p=128), in_=ys[:]), reads=['ysa%d' % wi, 'ysv%d' % wi], writes=['ybuf%d' % ex_i])
                ybk = ['ybuf%d' % i for i in range(32)]
                NSUB = TOK // 128

                def comb_load(st):
                    r0 = st * 128
                    pi_ = st % NACC
                    a_ = acc[pi_]
                    S.dma('sp', lambda e: e.dma_start(out=a_[:], in_=zb_d[r0:r0 + 128, :]), reads=['zb_d'], writes=['accb%d' % pi_])
                    for kq in range(2):
                        g = yg[pi_][kq]
                        S.dma('pool', lambda e: e.indirect_dma_start(out=g[:], out_offset=None, in_=ybuf_d, in_offset=bass.IndirectOffsetOnAxis(ap=dest_all[:, 2 * st + kq:2 * st + kq + 1], axis=0),
                                                                     bounds_check=bcreg(e, 'B'), oob_is_err=False), reads=ybk, writes=['yg%d_%d' % (pi_, kq)])
                def comb_compute(st):
                    r0 = st * 128
                    pi_ = st % NACC
                    a_ = acc[pi_]
                    ak = 'accb%d' % pi_
                    sb_ = statb[:, pi_ * 24:pi_ * 24 + 24]
                    sk_ = 'stb%d' % pi_
                    for kq in range(2):
                        g = yg[pi_][kq]
                        vop(lambda e: e.scalar_tensor_tensor(out=a_[:], in0=g[:], scalar=gsel_all[:, st, kq:kq + 1], in1=a_[:], op0=OP.mult, op1=OP.add),
                            ['yg%d_%d' % (pi_, kq), 'gsel', ak], [ak], 1.3)
                    for hf in range(2):
                        vop(lambda e: e.bn_stats(out=sb_[:, hf * 6:(hf + 1) * 6], in_=a_[:, hf * 512:(hf + 1) * 512]), [ak], [sk_ + 'b%d' % hf], 0.65)
                    vop(lambda e: e.bn_aggr(out=sb_[:, 12:14], in_=sb_[:, 0:12]), [sk_ + 'b0', sk_ + 'b1'], [sk_ + 'mv'], 0.2)
                    aop(lambda e: e.activation(out=sb_[:, 14:15], in_=sb_[:, 13:14], func=AF.Sqrt, bias=eps_t[:, 0:1]), [sk_ + 'mv', 'eps'], [sk_ + 'sd'], 0.3)
                    vop(lambda e: e.reciprocal(out=sb_[:, 15:16], in_=sb_[:, 14:15]), [sk_ + 'sd'], [sk_ + 'rs'], 0.16)
                    vop(lambda e: e.scalar_tensor_tensor(out=sb_[:, 16:17], in0=sb_[:, 12:13], scalar=-1.0, in1=sb_[:, 15:16], op0=OP.mult, op1=OP.mult), [sk_ + 'mv', sk_ + 'rs'], [sk_ + 'nb'], 0.1)
                    aop(lambda e: e.activation(out=a_[:], in_=a_[:], func=AF.Identity, scale=sb_[:, 15:16], bias=sb_[:, 16:17]), [ak, sk_ + 'rs', sk_ + 'nb'], [ak], 1.1)
                    vop(lambda e: e.tensor_tensor(out=a_[:], in0=a_[:], in1=repB[:, 0, :], op=OP.mult), [ak, 'repB'], [ak], 1.1)
                    pop(lambda e: e.tensor_tensor(out=a_[:], in0=a_[:], in1=repB[:, 1, :], op=OP.add), [ak, 'repB'], [ak], 2.35)
                    S.dma('sp', lambda e: e.dma_start(out=out[r0:r0 + 128, :], in_=a_[:]), reads=[ak], writes=['out'])

                comb_load(0)
                comb_load(1)
                for st0 in range(0, NSUB, 2):
                    for st in (st0 + 2, st0 + 3):
                        if st < NSUB:
                            comb_load(st)
                    S.replay_merged([S.record(lambda: comb_compute(st0)), S.record(lambda: comb_compute(st0 + 1))])
                S.barrier()
                S.emit()
    return nc


def _prep(inp):
    f = lambda a: np.ascontiguousarray(a, dtype=np.float32)
    colmat = lambda v: f(np.asarray(v).reshape(-1, 128).T)
    cols = np.concatenate([colmat(inp['b_in'][0]), colmat(inp['b_glu'][0]), colmat(inp['b_cout'][0]), colmat(inp['ssm_d'][0]),
                           colmat(inp['conv_b'][0]), colmat(inp['ln_c_g'][0]), colmat(inp['ln_c_b'][0])], axis=1)
    assert cols.shape == (128, 68)
    cols = f(np.concatenate([cols, np.zeros((128, 4), np.float32)], axis=1))
    cw = inp['conv_w'][0][:, 0, :]
    convw = f(cw.T.reshape(4, 128, CW).transpose(1, 0, 2).reshape(128, 4 * CW))
    rep5 = f(np.concatenate([np.broadcast_to(inp[k][0][None, :], (128, D)) for k in ['b_out', 'ln1_g', 'ln1_b', 'ln2_g', 'ln2_b']], axis=1))
    w_rt = f(np.concatenate([inp['w_route_group'][0], inp['w_route_expert'][0]], axis=1))
    brt = f(np.broadcast_to(np.concatenate([inp['b_route_group'][0], inp['b_route_expert'][0]])[None, :], (128, 36)))
    pqf = lambda a: a.reshape(16, 2, 64).transpose(1, 2, 0).reshape(128, 16)
    ldt = np.broadcast_to(inp['log_dt'][0].reshape(16, 2, 1), (16, 2, 64))
    pq = f(np.concatenate([pqf(inp['lam_re'][0]), pqf(inp['lam_im'][0]), pqf(np.ascontiguousarray(ldt))], axis=1))
    bf_ = lambda a: a.reshape(16, 2, 64, 16).transpose(1, 2, 0, 3).reshape(128, 256)
    cf_ = lambda a: a.reshape(16, 2, 16, 64).transpose(1, 3, 0, 2).reshape(128, 256)
    bc = f(np.concatenate([bf_(inp['ssm_b_re'][0]), bf_(inp['ssm_b_im'][0]), cf_(inp['ssm_c_re'][0]), cf_(inp['ssm_c_im'][0])], axis=1))
    shared = {
        'w_in': f(inp['w_in'][0]), 'w_glu': f(inp['w_glu'][0]), 'w_cout': f(inp['w_cout'][0]), 'w_out': f(inp['w_out'][0]),
        'w_rt': w_rt, 'w_gate': f(inp['w_gate'][0]), 'w_up': f(inp['w_up'][0]), 'w_down': f(inp['w_down'][0]),
        'cols': cols, 'convw': convw, 'rep5': rep5, 'brt': brt, 'pq': pq, 'bc': bc,
        'ident': np.eye(128, dtype=np.float32),
        'tri': np.triu(np.ones((128, 128), np.float32), 1),
        'ecap': f(np.broadcast_to((np.arange(32, dtype=np.float32) * CAP)[None, :], (128, 32))),
        'kk': f(np.broadcast_to(np.arange(1, TS + 1, dtype=np.float32)[None, :], (128, TS))),
        'bdmask': f(np.kron(np.eye(4, dtype=np.float32), np.ones((32, 32), np.float32))),
    }
    x = inp['x']
    maps = []
    for c in range(NCORES):
        xc = f(x[2 * c:2 * c + 2].reshape(TOK, D))
        m = dict(shared)
        m['xtok'] = xc
        m['xT'] = f(xc.T)
        maps.append(m)
    return maps


def kernel(**inputs):
    maps = _prep(inputs)
    nc = build_nc()
    res = run_bass_kernel_spmd(nc, maps, core_ids=list(range(NCORES)))
    outs = [np.asarray(r['out']).reshape(2, SEQ, D) for r in res.results]
    return np.concatenate(outs, axis=0).astype(np.float32)
```

```python
import math
import numpy as np
import concourse.bass as bass
import concourse.mybir as mybir
from concourse.bass_utils import run_bass_kernel_spmd
from contextlib import ExitStack

F32 = mybir.dt.float32
BF16 = mybir.dt.bfloat16
AF = mybir.ActivationFunctionType
OP = mybir.AluOpType
AX = mybir.AxisListType

NCORES = 8
D = 1024
SEQ = 2048
TOK = 4096
TS = 256
NT = TOK // TS
CW = 31
ALPHA = 2.0 ** 0.25
EPS = 1e-5
MAGIC = 12582912.0
TWO_PI = 2.0 * math.pi
DEBUG_H = False
S5_HALF = True
S5_DELAY = True
CAP = 384
NS4 = CAP // 128
I32 = mybir.dt.int32


import types


def _freeze(fn):
    if fn.__closure__ is None:
        return fn
    cells = []
    for c in fn.__closure__:
        try:
            cells.append(types.CellType(c.cell_contents))
        except ValueError:
            cells.append(c)
    return types.FunctionType(fn.__code__, fn.__globals__, fn.__name__, fn.__defaults__, tuple(cells))


class Sched:
    def __init__(self, nc, es, ndma=14):
        self.nc = nc
        self.names = ['pe', 'act', 'dve', 'pool', 'sp']
        self.prog = {k: [] for k in self.names}
        self.sem = {k: es.enter_context(nc.semaphore('s_' + k)) for k in ['pe', 'act', 'dve', 'pool']}
        self.cnt = {k: 0 for k in self.sem}
        self.dsem = [es.enter_context(nc.semaphore('d%d' % i)) for i in range(ndma)]
        self.dcnt = [0] * ndma
        self.dnext = 0
        self.seen = {k: {} for k in self.names}
        self.lastw = {}
        self.readers = {}
        self.rec = None
        self.sim_eng = {}
        self.sim_w = {}
        self.sim_r = {}

    def record(self, section):
        self.rec = []
        section()
        r = self.rec
        self.rec = None
        return r

    COST = {'pe': 0.17, 'act': 0.42, 'dve': 0.36, 'pool': 0.75, 'sp': 0.1}

    def replay_merged(self, chains):
        pos = [0] * len(chains)
        now = max(self.sim_eng.values()) if self.sim_eng else 0.0
        for k in self.names:
            self.sim_eng[k] = now
        while True:
            best, bkey, bt = None, None, 0.0
            for i, c in enumerate(chains):
                if pos[i] >= len(c):
                    continue
                kind, eng, fn, reads, writes, cost = c[pos[i]]
                t = self.sim_eng[eng]
                for r in reads:
                    t = max(t, self.sim_w.get(r, 0.0))
                for w in writes:
                    t = max(t, self.sim_w.get(w, 0.0), self.sim_r.get(w, 0.0))
                key = (t, pos[i] / len(c))
                if best is None or key < bkey:
                    best, bkey, bt = i, key, t
            if best is None:
                break
            kind, eng, fn, reads, writes, cost = chains[best][pos[best]]
            pos[best] += 1
            if kind == 'op':
                end = bt + (cost if cost is not None else self.COST[eng])
                self.sim_eng[eng] = end
                done = end + 0.06
                self.op(eng, fn, reads, writes)
            else:
                self.sim_eng[eng] = bt + (1.0 if eng == 'pool' else 0.1)
                done = bt + 2.5
                self.dma(eng, fn, reads, writes)
            for r in reads:
                self.sim_r[r] = max(self.sim_r.get(r, 0.0), done)
            for w in writes:
                self.sim_w[w] = done
                self.sim_r[w] = 0.0

    def _semobj(self, k):
        return self.sem[k] if isinstance(k, str) else self.dsem[k]

    def _waits(self, eng, deps):
        best = {}
        for (k, v) in deps:
            if k == 'pe' and eng == 'pe':
                continue
            if self.seen[eng].get(k, 0) >= v:
                continue
            best[k] = max(best.get(k, 0), v)
        for k, v in best.items():
            self.seen[eng][k] = v
            so = self._semobj(k)
            self.prog[eng].append(lambda e, so=so, v=v: e.wait_ge(so, v))

    def _deps(self, reads, writes):
        deps = []
        for r in reads:
            if r in self.lastw:
                deps.append(self.lastw[r])
        for w in writes:
            if w in self.lastw:
                deps.append(self.lastw[w])
            deps.extend(self.readers.get(w, []))
        return deps

    def _commit(self, tok, reads, writes):
        for r in reads:
            self.readers.setdefault(r, []).append(tok)
        for w in writes:
            self.lastw[w] = tok
            self.readers[w] = []

    def op(self, eng, fn, reads=(), writes=(), cost=None):
        fn = _freeze(fn)
        if self.rec is not None:
            self.rec.append(('op', eng, fn, tuple(reads), tuple(writes), cost))
            return
        self._waits(eng, self._deps(reads, writes))
        self.cnt[eng] += 1
        so = self.sem[eng]
        self.prog[eng].append(lambda e, fn=fn, so=so: fn(e).then_inc(so, 1))
        self._commit((eng, self.cnt[eng]), reads, writes)

    def dma(self, eng, fn, reads=(), writes=()):
        fn = _freeze(fn)
        if self.rec is not None:
            self.rec.append(('dma', eng, fn, tuple(reads), tuple(writes), None))
            return
        i = self.dnext
        self.dnext = (self.dnext + 1) % len(self.dsem)
        deps = self._deps(reads, writes)
        if self.dcnt[i] > 0:
            deps.append((i, self.dcnt[i]))
        self._waits(eng, deps)
        self.dcnt[i] += 16
        so = self.dsem[i]
        self.prog[eng].append(lambda e, fn=fn, so=so: fn(e).then_inc(so, 16))
        self._commit((i, self.dcnt[i]), reads, writes)

    def barrier(self):
        deps = [(k, v) for k, v in self.cnt.items() if v > 0]
        deps += [(i, v) for i, v in enumerate(self.dcnt) if v > 0]
        for eng in self.names:
            self._waits(eng, deps)

    def emit(self):
        nc = self.nc
        prog = self.prog
        with nc.Block() as block:
            @block.tensor
            def _(e):
                for f in prog['pe']:
                    f(e)

            @block.scalar
            def _(e):
                for f in prog['act']:
                    f(e)

            @block.vector
            def _(e):
                for f in prog['dve']:
                    f(e)

            @block.gpsimd
            def _(e):
                for f in prog['pool']:
                    f(e)

            @block.sync
            def _(e):
                for f in prog['sp']:
                    f(e)
        self.prog = {k: [] for k in self.names}


def build_nc():
    nc = bass.Bass("TRN2", target_bir_lowering=False)

    def din(name, shape):
        return nc.dram_tensor(name, list(shape), F32, kind="ExternalInput").ap()

    xT = din("xT", [D, TOK])
    xtok = din("xtok", [TOK, D])
    w_in = din("w_in", [D, 3584])
    w_glu = din("w_glu", [512, 2048])
    w_cout = din("w_cout", [512, 1024])
    w_out = din("w_out", [D, D])
    w_rt = din("w_rt", [D, 36])
    w_gate = din("w_gate", [32, D, 256])
    w_up = din("w_up", [32, D, 256])
    w_down = din("w_down", [32, 256, D])
    cols = din("cols", [128, 72])
    convw = din("convw", [128, 4 * CW])
    rep5 = din("rep5", [128, 5 * D])
    brt = din("brt", [128, 36])
    pq = din("pq", [128, 48])
    bc = din("bc", [128, 4 * 256])
    ident_d = din("ident", [128, 128])
    kk_d = din("kk", [128, TS])
    out = nc.dram_tensor("out", [TOK, D], F32, kind="ExternalOutput").ap()
    tri_d = din("tri", [128, 128])
    ecap_d = din("ecap", [128, 32])
    bdmask_d = din("bdmask", [128, 128])
    xbuf_d = nc.dram_tensor("xbuf_scr", [32 * CAP, D], BF16, kind="Internal").ap()
    ybuf_d = nc.dram_tensor("ybuf_scr", [32 * CAP, D], BF16, kind="Internal").ap()
    zb_d = nc.dram_tensor("zb_scr", [TOK, D], F32, kind="Internal").ap()
    rep5v = rep5.rearrange("p (a d) -> p a d", a=5)

    with ExitStack() as es0:
        S = Sched(nc, es0)
        dest_all = es0.enter_context(nc.sbuf_tensor("dest_all", [128, 64], I32))
        gsel_all = es0.enter_context(nc.sbuf_tensor("gsel_all", [128, 32, 2], F32))
        eps_t = es0.enter_context(nc.sbuf_tensor("eps_t", [128, 1], F32))
        S.op('dve', lambda e: e.memset(eps_t[:], EPS), writes=['eps'])
        regh = {}

        def bcreg(e, tag):
            if tag not in regh:
                regh[tag] = e.alloc_register('bcr' + tag)
                e.reg_mov(regh[tag], 32 * CAP - 1)
            return regh[tag]
        vop = lambda fn, r, w, c=None: S.op('dve', fn, reads=r, writes=w, cost=c)
        aop = lambda fn, r, w, c=None: S.op('act', fn, reads=r, writes=w, cost=c)
        pop = lambda fn, r, w, c=None: S.op('pool', fn, reads=r, writes=w, cost=c)
        peop = lambda fn, r, w, c=None: S.op('pe', fn, reads=r, writes=w, cost=c)

        with ExitStack() as es:
            def sb(name, shape, dt=F32):
                return es.enter_context(nc.sbuf_tensor(name, list(shape), dt))
            PS = [es.enter_context(nc.psum_tensor("ps%d" % i, [128, 512], F32)) for i in range(8)]

            win_bf = sb("win_bf", [128, 8, 3584], BF16)
            wglu_bf = sb("wglu_bf", [128, 4, 2048], BF16)
            wcout_bf = sb("wcout_bf", [128, 4, 1024], BF16)
            wout_bf = sb("wout_bf", [128, 8, 1024], BF16)
            wrt32 = sb("wrt32", [128, 8, 36])
            cols_t = sb("cols_t", [128, 72])
            convw_t = sb("convw_t", [128, 4, CW])
            brt_t = sb("brt_t", [128, 36])
            ident = sb("ident_sb", [128, 128])
            ones_bf = sb("ones_bf", [128, 128], BF16)
            halfpi = sb("halfpi", [128, 1])
            repA = sb("repA", [128, 3, D])
            cdiag = sb("cdiag", [128, CW, 128], BF16)
            TC = 4
            NCH = TS // TC
            WZ = sb("WZ", [128, 4, TC, 2, 128], BF16)
            KM = sb("KM", [128, 4, TC, 128], BF16)
            WY = sb("WY", [128, 16, TC, 2, 32], BF16)
            cos2 = sb("cos2", [128, 16, NCH])
            sin2 = sb("sin2", [128, 16, NCH])
            R4rep = sb("R4rep", [128, 16, NCH])
            r4 = sb("r4", [128, 16])
            carry = sb("carry", [128, 2, 16])
            fk = lambda i: 'f%d' % i
            bk = lambda i: 'b%d' % i

            S.dma('sp', lambda e: e.dma_start(out=repA[:], in_=rep5v[:, 0:3, :]), writes=['rep'])
            S.dma('sp', lambda e: e.dma_start(out=wrt32[:], in_=w_rt.rearrange("(k p) c -> p k c", p=128)), writes=['wrt'])
            S.dma('sp', lambda e: e.dma_start(out=cols_t[:], in_=cols), writes=['cols'])
            S.dma('sp', lambda e: e.dma_start(out=convw_t[:], in_=convw.rearrange("p (c j) -> p c j", c=4)), writes=['convw'])
            S.dma('sp', lambda e: e.dma_start(out=brt_t[:], in_=brt), writes=['brt'])
            S.dma('sp', lambda e: e.dma_start(out=ident[:], in_=ident_d), writes=['ident'])
            vop(lambda e: e.memset(halfpi[:], math.pi / 2), [], ['halfpi'])
            vop(lambda e: e.memset(ones_bf[:], 1.0), [], ['ones'])

            with ExitStack() as esS:
                def sbs(name, shape, dt=F32):
                    return esS.enter_context(nc.sbuf_tensor(name, list(shape), dt))
                pq_t = sbs("pq_t", [128, 3, 16])
                bc_t = sbs("bc_t", [128, 4, 16, 16])
                kk = sbs("kk_sb", [128, NCH])
                NSL = 30
                small = sbs("small", [128, NSL * 16])
                vbrs = [sbs("vbr%d" % i, [128, 256]) for i in range(2)]
                vbis = [sbs("vbi%d" % i, [128, 256]) for i in range(2)]
                tmpbs = [sbs("tmpb%d" % i, [128, 256]) for i in range(2)]
                tmpas = [sbs("tmpa%d" % i, [128, 256]) for i in range(2)]
                E4rs = [sbs("E4r%d" % i, [128, 128]) for i in range(2)]
                E4is = [sbs("E4i%d" % i, [128, 128]) for i in range(2)]
                CTE = sbs("CTE", [128, 4, 2, 128])
                wtmps = [sbs("wtmp%d" % i, [128, 4, 16]) for i in range(4)]
                bdm = sbs("bdm", [128, 128])
                S.dma('sp', lambda e: e.dma_start(out=bdm[:], in_=bdmask_d), writes=['bdm'])
                tb6 = sbs("tb6", [128, 4, NCH])
                S.dma('sp', lambda e: e.dma_start(out=pq_t[:], in_=pq.rearrange("p (a q) -> p a q", a=3)), writes=['pq'])
                S.dma('sp', lambda e: e.dma_start(out=bc_t[:], in_=bc.rearrange("p (a q h) -> p a q h", a=4, q=16)), writes=['bc'])
                S.dma('sp', lambda e: e.dma_start(out=kk[:], in_=kk_d[:, 0:NCH]), writes=['kk'])
                for k in range(8):
                    S.dma('pool', lambda e, k=k: e.dma_start(out=win_bf[:, k, :], in_=w_in[k * 128:(k + 1) * 128, :]), writes=['win'])
                    S.dma('pool', lambda e, k=k: e.dma_start(out=wout_bf[:, k, :], in_=w_out[k * 128:(k + 1) * 128, :]), writes=['wout'])
                for k in range(4):
                    S.dma('pool', lambda e, k=k: e.dma_start(out=wglu_bf[:, k, :], in_=w_glu[k * 128:(k + 1) * 128, :]), writes=['wglu'])
                    S.dma('pool', lambda e, k=k: e.dma_start(out=wcout_bf[:, k, :], in_=w_cout[k * 128:(k + 1) * 128, :]), writes=['wcout'])
                sm = lambda i: small[:, i * 16:(i + 1) * 16]
                smc = lambda i, q: small[:, i * 16 + q:i * 16 + q + 1]
                sk = lambda i: 'sm%d' % i
                DT, RHO, TH, Y0, DEN, FR, FI, T1, T2, T3 = range(10)
                PR = lambda k: 10 + k
                PI = lambda k: 15 + k
                GR = lambda k: 20 + k
                GI = lambda k: 24 + k
                Y4 = 28
                lre, lim, ldt = pq_t[:, 0, :], pq_t[:, 1, :], pq_t[:, 2, :]
                TT = lambda o, a, b, op: vop(lambda e: e.tensor_tensor(out=sm(o), in0=sm(a), in1=sm(b), op=op), [sk(a), sk(b)], [sk(o)])
                aop(lambda e: e.activation(out=sm(DT), in_=ldt, func=AF.Exp), ['pq'], [sk(DT)])
                vop(lambda e: e.tensor_tensor(out=sm(RHO), in0=lre, in1=sm(DT), op=OP.mult), ['pq', sk(DT)], [sk(RHO)])
                vop(lambda e: e.tensor_tensor(out=sm(TH), in0=lim, in1=sm(DT), op=OP.mult), ['pq', sk(DT)], [sk(TH)])
                vop(lambda e: e.tensor_scalar(out=sm(Y0), in0=sm(TH), scalar1=1.0 / TWO_PI, scalar2=None, op0=OP.mult), [sk(TH)], [sk(Y0)])
                vop(lambda e: e.memset(sm(PR(0)), 1.0), [], [sk(PR(0))])
                vop(lambda e: e.memset(sm(PI(0)), 0.0), [], [sk(PI(0))])
                for k in range(1, TC + 1):
                    vop(lambda e: e.tensor_scalar(out=sm(T1), in0=sm(Y0), scalar1=float(k), scalar2=None, op0=OP.mult), [sk(Y0)], [sk(T1)])
                    vop(lambda e: e.tensor_scalar(out=sm(T2), in0=sm(T1), scalar1=MAGIC, scalar2=MAGIC, op0=OP.add, op1=OP.subtract), [sk(T1)], [sk(T2)])
                    TT(T1, T1, T2, OP.subtract)
                    aop(lambda e: e.activation(out=sm(T2), in_=sm(T1), func=AF.Abs), [sk(T1)], [sk(T2)])
                    aop(lambda e: e.activation(out=sm(PI(k)), in_=sm(T1), func=AF.Sin, scale=TWO_PI), [sk(T1)], [sk(PI(k))])
                    aop(lambda e: e.activation(out=sm(PR(k)), in_=sm(T2), func=AF.Sin, scale=-TWO_PI, bias=halfpi[:, 0:1]), [sk(T2), 'halfpi'], [sk(PR(k))])
                    aop(lambda e: e.activation(out=sm(T3), in_=sm(RHO), func=AF.Exp, scale=float(k)), [sk(RHO)], [sk(T3)])
                    TT(PR(k), PR(k), T3, OP.mult)
                    TT(PI(k), PI(k), T3, OP.mult)
                    if k == TC:
                        vop(lambda e: e.tensor_copy(out=r4[:], in_=sm(T3)), [sk(T3)], ['r4'])
                vop(lambda e: e.tensor_tensor(out=sm(T1), in0=lre, in1=lre, op=OP.mult), ['pq'], [sk(T1)])
                vop(lambda e: e.tensor_tensor(out=sm(T2), in0=lim, in1=lim, op=OP.mult), ['pq'], [sk(T2)])
                TT(DEN, T1, T2, OP.add)
                vop(lambda e: e.reciprocal(out=sm(DEN), in_=sm(DEN)), [sk(DEN)], [sk(DEN)])
                vop(lambda e: e.tensor_scalar(out=sm(T1), in0=sm(PR(1)), scalar1=-1.0, scalar2=None, op0=OP.add), [sk(PR(1))], [sk(T1)])
                vop(lambda e: e.tensor_tensor(out=sm(T2), in0=sm(T1), in1=lre, op=OP.mult), [sk(T1), 'pq'], [sk(T2)])
                vop(lambda e: e.tensor_tensor(out=sm(T3), in0=sm(PI(1)), in1=lim, op=OP.mult), [sk(PI(1)), 'pq'], [sk(T3)])
                TT(T2, T2, T3, OP.add)
                TT(FR, T2, DEN, OP.mult)
                vop(lambda e: e.tensor_tensor(out=sm(T2), in0=sm(PI(1)), in1=lre, op=OP.mult), [sk(PI(1)), 'pq'], [sk(T2)])
                vop(lambda e: e.tensor_tensor(out=sm(T3), in0=sm(T1), in1=lim, op=OP.mult), [sk(T1), 'pq'], [sk(T3)])
                TT(T2, T2, T3, OP.subtract)
                TT(FI, T2, DEN, OP.mult)
                for k in range(TC):
                    TT(T1, PR(k), FR, OP.mult)
                    TT(T2, PI(k), FI, OP.mult)
                    TT(GR(k), T1, T2, OP.subtract)
                    TT(T1, PR(k), FI, OP.mult)
                    TT(T2, PI(k), FR, OP.mult)
                    TT(GI(k), T1, T2, OP.add)
                vop(lambda e: e.memset(CTE[:], 0.0), [], ['CTE'])
                for cq in range(4):
                    for pl in range(4):
                        q = cq * 4 + pl
                        for g2 in range(2):
                            hsl = slice(g2 * 64, (g2 + 1) * 64)
                            csl = slice(pl * 32 + g2 * 16, pl * 32 + g2 * 16 + 16)
                            vop(lambda e: e.tensor_copy(out=CTE[hsl, cq, 0, csl], in_=bc_t[hsl, 2, q, :]), ['bc'], ['CTE'])
                            vop(lambda e: e.tensor_scalar(out=CTE[hsl, cq, 1, csl], in0=bc_t[hsl, 3, q, :], scalar1=-1.0, scalar2=None, op0=OP.mult), ['bc'], ['CTE'])
                def emit_VB(k):
                    vbr, vbi, tmpa, tmpb = vbrs[k % 2], vbis[k % 2], tmpas[k % 2], tmpbs[k % 2]
                    kvr, kvi, kta, ktb = 'vbr%d' % (k % 2), 'vbi%d' % (k % 2), 'tmpa%d' % (k % 2), 'tmpb%d' % (k % 2)
                    for q in range(16):
                        bre_q, bim_q = bc_t[:, 0, q, :], bc_t[:, 1, q, :]
                        sl = slice(q * 16, (q + 1) * 16)
                        vop(lambda e: e.tensor_scalar(out=tmpa[:, sl], in0=bim_q, scalar1=smc(GI(k), q), scalar2=None, op0=OP.mult), ['bc', sk(GI(k))], [kta])
                        vop(lambda e: e.tensor_scalar(out=tmpb[:, sl], in0=bre_q, scalar1=smc(GI(k), q), scalar2=None, op0=OP.mult), ['bc', sk(GI(k))], [ktb])
                        vop(lambda e: e.scalar_tensor_tensor(out=vbr[:, sl], in0=bre_q, scalar=smc(GR(k), q), in1=tmpa[:, sl], op0=OP.mult, op1=OP.subtract), ['bc', sk(GR(k)), kta], [kvr])
                        vop(lambda e: e.scalar_tensor_tensor(out=vbi[:, sl], in0=bim_q, scalar=smc(GR(k), q), in1=tmpb[:, sl], op0=OP.mult, op1=OP.add), ['bc', sk(GR(k)), ktb], [kvi])

                def emit_E4(k):
                    vbr, vbi = vbrs[k % 2], vbis[k % 2]
                    kvr, kvi = 'vbr%d' % (k % 2), 'vbi%d' % (k % 2)
                    for cq in range(4):
                        E4r, E4i = E4rs[cq % 2], E4is[cq % 2]
                        ekr, eki = 'E4r%d' % (cq % 2), 'E4i%d' % (cq % 2)
                        for ri, (src, E4, ek) in enumerate([(vbr, E4r, ekr), (vbi, E4i, eki)]):
                            for pl in range(4):
                                q = cq * 4 + pl
                                for g2 in range(2):
                                    o_ = E4[g2 * 64:(g2 + 1) * 64, pl * 32 + g2 * 16:pl * 32 + g2 * 16 + 16]
                                    i_ = src[g2 * 64:(g2 + 1) * 64, q * 16:(q + 1) * 16]
                                    if g2 == 0:
                                        pop(lambda e: e.tensor_copy(out=o_, in_=i_), [kvr, kvi], [ek + 'p'])
                                    else:
                                        aop(lambda e: e.activation(out=o_, in_=i_, func=AF.Identity), [kvr, kvi], [ek + 'a'])
                            peop(lambda e: e.transpose(out=PS[7][:, ri * 128:(ri + 1) * 128], in_=E4[:], identity=ident[:]), [ek + 'p', ek + 'a', 'ident'], ['ps7'])
                            aop(lambda e: e.activation(out=WZ[:, cq, TC - 1 - k, ri, :], in_=PS[7][:, ri * 128:(ri + 1) * 128], func=AF.Identity), ['ps7'], ['WZ'])
                        peop(lambda e: e.matmul(PS[6][:, 0:128], lhsT=E4r[:], rhs=CTE[:, cq, 0, :], start=True, stop=False), [ekr + 'p', ekr + 'a', 'CTE'], ['ps6'])
                        peop(lambda e: e.matmul(PS[6][:, 0:128], lhsT=E4i[:], rhs=CTE[:, cq, 1, :], start=False, stop=True), [eki + 'p', eki + 'a', 'CTE'], ['ps6'])
                        vop(lambda e: e.tensor_tensor(out=KM[:, cq, k, :], in0=PS[6][:, 0:128], in1=bdm[:], op=OP.mult), ['ps6', 'bdm'], ['KM'])
                for i_ in range(2):
                    pop(lambda e: e.memset(E4rs[i_][:], 0.0), [], ['E4r%dp' % i_, 'E4r%da' % i_])
                    pop(lambda e: e.memset(E4is[i_][:], 0.0), [], ['E4i%dp' % i_, 'E4i%da' % i_])
                emit_VB(0)
                for k in range(TC):
                    if k + 1 < TC:
                        emit_VB(k + 1)
                    emit_E4(k)
                vop(lambda e: e.memset(WY[:], 0.0), [], ['WYp', 'WYa'])
                for j in range(TC):
                    for q in range(16):
                        ctr, cti = bc_t[:, 2, q, :], bc_t[:, 3, q, :]
                        wtmp = wtmps[q % 4]
                        w0k, w1k, w2k, w3k = ['wt%d_%d' % (i_, q % 4) for i_ in range(4)]
                        prq, piq = smc(PR(j + 1), q), smc(PI(j + 1), q)
                        kpr, kpi = sk(PR(j + 1)), sk(PI(j + 1))
                        vop(lambda e: e.tensor_scalar(out=wtmp[:, 0, :], in0=cti, scalar1=piq, scalar2=None, op0=OP.mult), ['bc', kpi], [w0k])
                        vop(lambda e: e.tensor_scalar(out=wtmp[:, 2, :], in0=cti, scalar1=prq, scalar2=None, op0=OP.mult), ['bc', kpr], [w2k])
                        vop(lambda e: e.scalar_tensor_tensor(out=wtmp[:, 1, :], in0=ctr, scalar=prq, in1=wtmp[:, 0, :], op0=OP.mult, op1=OP.subtract), ['bc', kpr, w0k], [w1k])
                        vop(lambda e: e.scalar_tensor_tensor(out=wtmp[:, 3, :], in0=ctr, scalar=piq, in1=wtmp[:, 2, :], op0=OP.mult, op1=OP.add), ['bc', kpi, w2k], [w3k])
                        for g2 in range(2):
                            hsl = slice(g2 * 64, (g2 + 1) * 64)
                            pop(lambda e: e.tensor_copy(out=WY[hsl, q, j, 0, g2 * 16:(g2 + 1) * 16], in_=wtmp[hsl, 1, :]), [w1k], ['WYp'])
                            aop(lambda e: e.activation(out=WY[hsl, q, j, 1, g2 * 16:(g2 + 1) * 16], in_=wtmp[hsl, 3, :], func=AF.Identity, scale=-1.0), [w3k], ['WYa'])
                vop(lambda e: e.tensor_scalar(out=sm(Y4), in0=sm(Y0), scalar1=float(TC), scalar2=None, op0=OP.mult), [sk(Y0)], [sk(Y4)])
                for q in range(16):
                    vop(lambda e: e.tensor_scalar(out=tb6[:, 0, :], in0=kk[:], scalar1=smc(Y4, q), scalar2=None, op0=OP.mult), ['kk', sk(Y4)], ['tb0'])
                    vop(lambda e: e.tensor_scalar(out=tb6[:, 1, :], in0=tb6[:, 0, :], scalar1=MAGIC, scalar2=MAGIC, op0=OP.add, op1=OP.subtract), ['tb0'], ['tb1'])
                    vop(lambda e: e.tensor_tensor(out=tb6[:, 2, :], in0=tb6[:, 0, :], in1=tb6[:, 1, :], op=OP.subtract), ['tb0', 'tb1'], ['tb2'])
                    aop(lambda e: e.activation(out=tb6[:, 3, :], in_=tb6[:, 2, :], func=AF.Abs), ['tb2'], ['tb3'])
                    aop(lambda e: e.activation(out=sin2[:, q, :], in_=tb6[:, 2, :], func=AF.Sin, scale=TWO_PI), ['tb2'], ['sin2'])
                    aop(lambda e: e.activation(out=cos2[:, q, :], in_=tb6[:, 3, :], func=AF.Sin, scale=-TWO_PI, bias=halfpi[:, 0:1]), ['tb3', 'halfpi'], ['cos2'])
                    aop(lambda e: e.activation(out=R4rep[:, q, :], in_=kk[:], func=AF.Identity, scale=0.0, bias=r4[:, q:q + 1]), ['kk', 'r4'], ['R4rep'])
                vop(lambda e: e.memset(R4rep[:, :, 0:1], 0.0), ['R4rep'], ['R4rep'])
                S.barrier()
                S.emit()

            xT_bfs = [sb("xT_bf%d" % i, [128, 8, TS], BF16) for i in range(2)]
            u_bf = sb("u_bf", [128, 4, TS], BF16)
            um = sb("um", [128, TC - 1, TS], BF16)
            u_pm = sb("u_pm", [128, 4, TS], BF16)
            vbuf = sb("vbuf", [128, 4, 30 + TS], BF16)
            y_bf = sb("y_bf", [128, 4, TS], BF16)
            c_bf = sb("c_bf", [128, 4, TS], BF16)
            cs_bf = sb("cs_bf", [128, 4, TS], BF16)
            merged = sb("merged", [128, 8, TS], BF16)
            hb = sb("hb", [128, D], BF16)
            tri_bf = sb("tri_bf", [128, 128], BF16)
            ecap = sb("ecap_sb", [128, 32])
            selsum = sb("selsum", [128, 32], BF16)
            sel_bf = sb("sel_bf", [128, 32], BF16)
            NF = 9
            FT = [sb("f32t%d" % i, [128, TS]) for i in range(NF)]
            BT_ = [sb("bft%d" % i, [128, TS], BF16) for i in range(4)]
            xtk = sb("xtk", [128, D])
            zt = sb("zt", [128, D])
            hT32 = sb("hT32", [128, 8, 128])
            rsm = sb("rsm", [128, 512])
            stats = sb("stats", [128, 16])
            halo = sb("halo", [128, 4, 32], BF16)
            tmp4 = sb("tmp4", [128, 2, 4])
            CL = [sb("cl%d" % i, [128, TS]) for i in range(2)]
            S.dma('pool', lambda e: e.dma_start(out=tri_bf[:], in_=tri_d), writes=['tri'])
            S.dma('sp', lambda e: e.dma_start(out=ecap[:], in_=ecap_d), writes=['ecap'])
            vop(lambda e: e.memset(selsum[:], 0.0), [], ['selsum'])
            vop(lambda e: e.memset(um[:], 0.0), [], ['um'])

            bcol = lambda off, m: cols_t[:, off + m:off + m + 1]
            B_IN, B_GLU, B_COUT, DCOL, CONVB, LNCG, LNCB = 0, 28, 44, 52, 56, 60, 64
            mmrot = [0]
            NROT = 6

            def mmbank():
                b = mmrot[0]
                mmrot[0] = (mmrot[0] + 1) % NROT
                return b

            xcur = [None, None]

            def proj(m):
                b = mmbank()
                xT_bf, xkey = xcur
                for k in range(8):
                    peop(lambda e: e.matmul(PS[b][:, 0:TS], lhsT=win_bf[:, k, m * 128:(m + 1) * 128], rhs=xT_bf[:, k, :], start=(k == 0), stop=(k == 7)),
                         ['win', xkey], ['ps%d' % b])
                return b

            def load_xT(ti_):
                buf = xT_bfs[ti_ % 2]
                tt0 = ti_ * TS
                S.dma('pool', lambda e: e.dma_start(out=buf[:], in_=xT.rearrange("(k p) t -> p k t", p=128)[:, :, tt0:tt0 + TS]), writes=['xT%d' % (ti_ % 2)])

            TPS = SEQ // TS
            prev_tile = None
            for ti in range(NT):
                t0 = ti * TS
                first = (ti % TPS == 0)
                if ti == 0:
                    load_xT(0)
                xcur[0], xcur[1] = xT_bfs[ti % 2], 'xT%d' % (ti % 2)
                if ti + 1 < NT:
                    load_xT(ti + 1)
                if first:
                    pop(lambda e: e.memset(vbuf[:, :, 0:30], 0.0), [], ['vhalo'])
                    pop(lambda e: e.memset(carry[:], 0.0), [], ['carry'])

                for m in range(4):
                    b = proj(m)
                    aop(lambda e: e.activation(out=u_bf[:, m, :], in_=PS[b][:, 0:TS], func=AF.Identity, bias=bcol(B_IN, m)), ['ps%d' % b, 'cols'], ['u%d' % m])
                    aop(lambda e: e.activation(out=u_pm[:, m, :].rearrange("p (i c) -> p c i", i=TC), in_=PS[b][:, 0:TS].rearrange("p (c i) -> p c i", i=TC),
                                               func=AF.Identity, bias=bcol(B_IN, m)), ['ps%d' % b, 'cols'], ['up%d' % m])
                for c in range(4):
                    b = proj(8 + c)
                    aop(lambda e: e.activation(out=y_bf[:, c, :], in_=PS[b][:, 0:TS], func=AF.Sigmoid, bias=bcol(B_IN, 8 + c)), ['ps%d' % b, 'cols'], ['y%d' % c])
                for c in range(4):
                    b = proj(4 + c)
                    vop(lambda e: e.scalar_tensor_tensor(out=vbuf[:, c, 30:30 + TS], in0=PS[b][:, 0:TS], scalar=bcol(B_IN, 4 + c), in1=y_bf[:, c, :], op0=OP.add, op1=OP.mult),
                        ['ps%d' % b, 'cols', 'y%d' % c], ['v%d' % c])

                def sec_conv():
                    for c in range(4):
                        for j in range(CW):
                            if j % 4 != 3:
                                aop(lambda e: e.activation(out=cdiag[:, j, :], in_=ident[:], func=AF.Identity, scale=convw_t[:, c, j:j + 1]), ['ident', 'convw'], ['cd%d' % j])
                            else:
                                vop(lambda e: e.tensor_scalar(out=cdiag[:, j, :], in0=ident[:], scalar1=convw_t[:, c, j:j + 1], scalar2=None, op0=OP.mult), ['ident', 'convw'], ['cd%d' % j])
                        for j in range(CW):
                            peop(lambda e: e.matmul(PS[5][:, 0:TS], lhsT=cdiag[:, j, :], rhs=vbuf[:, c, j:j + TS], start=(j == 0), stop=(j == CW - 1)),
                                 ['cd%d' % j, 'v%d' % c, 'vhalo'], ['ps5'])
                        aop(lambda e: e.activation(out=c_bf[:, c, :], in_=PS[5][:, 0:TS], func=AF.Identity, bias=bcol(CONVB, c)), ['ps5', 'cols'], ['c_bf%d' % c])
                        aop(lambda e: e.activation(out=cs_bf[:, c, :], in_=PS[5][:, 0:TS], func=AF.Square, bias=bcol(CONVB, c)), ['ps5', 'cols'], ['cs%d' % c])
                        pop(lambda e: e.tensor_copy(out=halo[:, c, 0:30], in_=vbuf[:, c, TS:TS + 30]), ['v%d' % c], ['halo'])
                    for c in range(4):
                        peop(lambda e: e.matmul(PS[5][:, TS:2 * TS], lhsT=ones_bf[:], rhs=c_bf[:, c, :], start=(c == 0), stop=(c == 3)), ['ones', 'c_bf%d' % c], ['ps5'])
                    mean_t, rstd_t, msq_t, cn_t = CL[0], CL[1], CL[1], FT[8]
                    aop(lambda e: e.activation(out=mean_t[:], in_=PS[5][:, TS:2 * TS], func=AF.Identity, scale=1.0 / 512), ['ps5'], ['cl0'])
                    for c in range(4):
                        peop(lambda e: e.matmul(PS[5][:, TS:2 * TS], lhsT=ones_bf[:], rhs=cs_bf[:, c, :], start=(c == 0), stop=(c == 3)), ['ones', 'cs%d' % c], ['ps5'])
                    vop(lambda e: e.tensor_tensor(out=msq_t[:], in0=mean_t[:], in1=mean_t[:], op=OP.mult), ['cl0'], ['cl1'])
                    vop(lambda e: e.scalar_tensor_tensor(out=msq_t[:], in0=PS[5][:, TS:2 * TS], scalar=1.0 / 512, in1=msq_t[:], op0=OP.mult, op1=OP.subtract), ['ps5', 'cl1'], ['cl1'])
                    aop(lambda e: e.activation(out=msq_t[:], in_=msq_t[:], func=AF.Sqrt, bias=eps_t[:, 0:1]), ['cl1', 'eps'], ['cl1'])
                    vop(lambda e: e.reciprocal(out=rstd_t[:], in_=msq_t[:]), ['cl1'], ['cl1'])
                    for c in range(4):
                        pop(lambda e: e.tensor_tensor(out=cn_t[:], in0=c_bf[:, c, :], in1=mean_t[:], op=OP.subtract), ['c_bf%d' % c, 'cl0'], [fk(8)])
                        pop(lambda e: e.tensor_tensor(out=cn_t[:], in0=cn_t[:], in1=rstd_t[:], op=OP.mult), [fk(8), 'cl1'], [fk(8)])
                        aop(lambda e: e.activation(out=cs_bf[:, c, :], in_=cn_t[:], func=AF.Silu, scale=bcol(LNCG, c), bias=bcol(LNCB, c)), [fk(8), 'cols'], ['cs%d' % c])
                    for c in range(4):
                        pop(lambda e: e.tensor_copy(out=vbuf[:, c, 0:30], in_=halo[:, c, 0:30]), ['halo', 'v%d' % c], ['vhalo'])

                def sec_s5():
                    v3 = lambda t: t[:].rearrange("p (a c) -> p a c", a=4)
                    t1, t2, zre, zim, sre, sim = FT[0], FT[1], FT[2], FT[3], FT[4], FT[5]
                    k1, k2, kzr, kzi, ksr, ksi = fk(0), fk(1), fk(2), fk(3), fk(4), fk(5)

                    def PY(cq):
                        h_ = (cq % 2) if S5_HALF else 0
                        return PS[7 - h_][:, 0:TS], 'ps%d' % (7 - h_)

                    def SPb(cq, ri):
                        i_ = (cq % 2) * 2 + ri
                        return BT_[i_], bk(i_)

                    def stage_front(cq):
                        qs = slice(4 * cq, 4 * cq + 4)
                        C2 = cos2[:, qs, :].rearrange("p a c -> p (a c)")
                        S2 = sin2[:, qs, :].rearrange("p a c -> p (a c)")
                        RR = R4rep[:, qs, :].rearrange("p a c -> p (a c)")
                        ucq = u_bf[:, cq, :].rearrange("p (c i) -> p c i", i=TC)
                        for pl in range(4):
                            pr = slice(pl * 32, (pl + 1) * 32)
                            for ri in range(2):
                                for i in range(TC):
                                    ua = u_pm[pr, cq, i * NCH:(i + 1) * NCH]
                                    peop(lambda e: e.matmul(PS[3 + ri][:, pl * NCH:(pl + 1) * NCH], lhsT=WZ[pr, cq, i, ri, :], rhs=ua, start=(i == 0), stop=(i == TC - 1),
                                                            tile_position=(pl * 32, 0)), ['WZ', 'up%d' % cq], ['ps%d' % (3 + ri)])
                        for d in range(1, TC):
                            pop(lambda e: e.tensor_copy(out=um[:, d - 1, :].rearrange("p (c i) -> p c i", i=TC)[:, :, d:TC], in_=ucq[:, :, 0:TC - d]), ['u%d' % cq, 'um'], ['um%d' % d])
                        py, pyk = PY(cq)
                        for d in range(TC):
                            rhs_ = u_bf[:, cq, :] if d == 0 else um[:, d - 1, :]
                            peop(lambda e: e.matmul(py, lhsT=KM[:, cq, d, :], rhs=rhs_, start=(d == 0), stop=False), ['KM', 'u%d' % cq] + (['um%d' % d] if d else []), [pyk])
                        vop(lambda e: e.tensor_tensor(out=t1[:], in0=PS[3][:, 0:TS], in1=C2, op=OP.mult), ['ps3', 'cos2'], [k1])
                        vop(lambda e: e.tensor_tensor(out=t2[:], in0=PS[4][:, 0:TS], in1=S2, op=OP.mult), ['ps4', 'sin2'], [k2])
                        pop(lambda e: e.tensor_tensor(out=zre[:], in0=t1[:], in1=t2[:], op=OP.add), [k1, k2], [kzr])
                        vop(lambda e: e.tensor_tensor(out=sre[:], in0=PS[4][:, 0:TS], in1=C2, op=OP.mult), ['ps4', 'cos2'], [ksr])
                        vop(lambda e: e.tensor_tensor(out=sim[:], in0=PS[3][:, 0:TS], in1=S2, op=OP.mult), ['ps3', 'sin2'], [ksi])
                        pop(lambda e: e.tensor_tensor(out=zim[:], in0=sre[:], in1=sim[:], op=OP.subtract), [ksr, ksi], [kzi])
                        for ri, (zt_, kz) in enumerate([(zre, kzr), (zim, kzi)]):
                            vop(lambda e: e.tensor_tensor(out=tmp4[:, ri, :], in0=carry[:, ri, qs], in1=r4[:, qs], op=OP.mult), ['carry', 'r4'], ['tmp4_%d' % ri])
                            vop(lambda e: e.tensor_tensor(out=v3(zt_)[:, :, 0], in0=v3(zt_)[:, :, 0], in1=tmp4[:, ri, :], op=OP.add), [kz, 'tmp4_%d' % ri], [kz])
                        vop(lambda e: e.tensor_tensor_scan(out=sre[:], data0=RR, data1=zre[:], initial=0.0, op0=OP.mult, op1=OP.add), ['R4rep', kzr], [ksr])
                        vop(lambda e: e.tensor_tensor_scan(out=sim[:], data0=RR, data1=zim[:], initial=0.0, op0=OP.mult, op1=OP.add), ['R4rep', kzi], [ksi])
                        pop(lambda e: e.tensor_tensor(out=t1[:], in0=sre[:], in1=C2, op=OP.mult), [ksr, 'cos2'], [k1])
                        pop(lambda e: e.tensor_tensor(out=t2[:], in0=sim[:], in1=S2, op=OP.mult), [ksi, 'sin2'], [k2])
                        pop(lambda e: e.tensor_tensor(out=zre[:], in0=t1[:], in1=t2[:], op=OP.subtract), [k1, k2], [kzr])
                        vop(lambda e: e.tensor_tensor(out=t1[:], in0=sre[:], in1=S2, op=OP.mult), [ksr, 'sin2', kzr], [k1])
                        vop(lambda e: e.tensor_tensor(out=t2[:], in0=sim[:], in1=C2, op=OP.mult), [ksi, 'cos2', kzr], [k2])
                        vop(lambda e: e.tensor_tensor(out=zim[:], in0=t1[:], in1=t2[:], op=OP.add), [k1, k2], [kzi])
                        for ri, (zt_, kz) in enumerate([(zre, kzr), (zim, kzi)]):
                            spb, spk = SPb(cq, ri)
                            sp3 = spb[:].rearrange("p (a c) -> p a c", a=4)
                            aop(lambda e: e.activation(out=sp3[:, :, 1:NCH], in_=v3(zt_)[:, :, 0:NCH - 1], func=AF.Identity), [kz], [spk])
                            aop(lambda e: e.activation(out=sp3[:, :, 0], in_=carry[:, ri, qs], func=AF.Identity), ['carry'], [spk])
                            aop(lambda e: e.activation(out=carry[:, ri, qs], in_=v3(zt_)[:, :, NCH - 1], func=AF.Identity), [kz, spk], ['carry'])

                    def stage_back(cq):
                        py, pyk = PY(cq)
                        for pl in range(4):
                            q = 4 * cq + pl
                            pr = slice(pl * 32, (pl + 1) * 32)
                            for j in range(TC):
                                oj = py[pr, :].rearrange("p (c j) -> p j c", j=TC)[:, j, :]
                                for ri in range(2):
                                    spb, spk = SPb(cq, ri)
                                    peop(lambda e: e.matmul(oj, lhsT=WY[:, q, j, ri, :], rhs=spb[:, pl * NCH:(pl + 1) * NCH], start=False, stop=(ri == 1), tile_position=(0, pl * 32)),
                                         ['WY', spk], [pyk])
                        ys, g1 = FT[6], FT[7]
                        kA, kB = fk(6), fk(7)
                        vop(lambda e: e.scalar_tensor_tensor(out=ys[:], in0=u_bf[:, cq, :], scalar=bcol(DCOL, cq), in1=py, op0=OP.mult, op1=OP.add),
                            ['u%d' % cq, 'cols', pyk], [kA])
                        aop(lambda e: e.activation(out=g1[:], in_=ys[:], func=AF.Square), [kA], [kB])
                        vop(lambda e: e.tensor_scalar(out=g1[:], in0=g1[:], scalar1=0.044715, scalar2=1.0, op0=OP.mult, op1=OP.add), [kB], [kB])
                        vop(lambda e: e.tensor_tensor(out=g1[:], in0=g1[:], in1=ys[:], op=OP.mult), [kB, kA], [kB])
                        aop(lambda e: e.activation(out=g1[:], in_=g1[:], func=AF.Sigmoid, scale=1.5957691216057308), [kB], [kB])
                        vop(lambda e: e.tensor_tensor(out=y_bf[:, cq, :], in0=g1[:], in1=ys[:], op=OP.mult), [kB, kA], ['y%d' % cq])

                    if S5_DELAY:
                        stage_front(0)
                        for cq in range(1, 4):
                            stage_front(cq)
                            stage_back(cq - 1)
                        stage_back(3)
                    else:
                        for cq in range(4):
                            stage_front(cq)
                            stage_back(cq)

                def sec_ln(ti, t0):
                    for st in range(TS // 128):
                        r0 = t0 + st * 128
                        gst = ti * (TS // 128) + st
                        S.dma('sp', lambda e: e.dma_start(out=xtk[:], in_=xtok[r0:r0 + 128, :]), writes=['xtk'])
                        for hf in range(2):
                            hs = slice(hf * 512, (hf + 1) * 512)
                            b = hf
                            for k in range(8):
                                peop(lambda e: e.matmul(PS[b][:], lhsT=merged[:, k, st * 128:(st + 1) * 128], rhs=wout_bf[:, k, hs], start=(k == 0), stop=(k == 7)),
                                     ['wout', 'mg%d' % k], ['ps%d' % b], 0.28)
                            vop(lambda e: e.scalar_tensor_tensor(out=zt[:, hs], in0=xtk[:, hs], scalar=ALPHA, in1=PS[b][:], op0=OP.mult, op1=OP.add), ['xtk', 'ps%d' % b], ['zt%d' % hf], 0.6)
                            vop(lambda e: e.tensor_tensor(out=zt[:, hs], in0=zt[:, hs], in1=repA[:, 0, hs], op=OP.add), ['zt%d' % hf, 'rep'], ['zt%d' % hf], 0.6)
                            vop(lambda e: e.bn_stats(out=stats[:, hf * 6:(hf + 1) * 6], in_=zt[:, hs]), ['zt%d' % hf], ['bst%d' % hf], 0.65)
                        vop(lambda e: e.bn_aggr(out=stats[:, 12:14], in_=stats[:, 0:12]), ['bst0', 'bst1'], ['mv'])
                        aop(lambda e: e.activation(out=stats[:, 14:15], in_=stats[:, 13:14], func=AF.Sqrt, bias=eps_t[:, 0:1]), ['mv', 'eps'], ['sd'])
                        vop(lambda e: e.reciprocal(out=stats[:, 15:16], in_=stats[:, 14:15]), ['sd'], ['rs'])
                        vop(lambda e: e.tensor_scalar(out=zt[:], in0=zt[:], scalar1=stats[:, 12:13], scalar2=stats[:, 15:16], op0=OP.subtract, op1=OP.mult),
                            ['zt0', 'zt1', 'mv', 'rs'], ['zt0', 'zt1'], 0.85)
                        vop(lambda e: e.tensor_tensor(out=zt[:], in0=zt[:], in1=repA[:, 1, :], op=OP.mult), ['zt0', 'zt1', 'rep'], ['zt0', 'zt1'], 1.1)
                        vop(lambda e: e.tensor_tensor(out=zt[:], in0=zt[:], in1=repA[:, 2, :], op=OP.add), ['zt0', 'zt1', 'rep'], ['zt0', 'zt1'], 1.1)
                        aop(lambda e: e.activation(out=xtk[:], in_=zt[:], func=AF.Identity, scale=ALPHA), ['zt0', 'zt1', 'xtk'], ['xtk'], 1.06)
                        S.dma('sp', lambda e: e.dma_start(out=zb_d[r0:r0 + 128, :], in_=xtk[:]), reads=['xtk'], writes=['zb_d'])
                        if DEBUG_H:
                            S.dma('sp', lambda e: e.dma_start(out=out[r0:r0 + 128, :], in_=zt[:]), reads=['zt0', 'zt1'], writes=['out'])
                        for kb in range(2):
                            for kk4 in range(4):
                                k = kb * 4 + kk4
                                peop(lambda e: e.transpose(out=PS[2][:, kk4 * 128:(kk4 + 1) * 128], in_=zt[:, k * 128:(k + 1) * 128], identity=ident[:]),
                                     ['zt0', 'zt1', 'ident'], ['ps2'], 0.41)
                            aop(lambda e: e.activation(out=hT32[:, kb * 4:(kb + 1) * 4, :], in_=PS[2][:].rearrange("p (a b) -> p a b", a=4), func=AF.Identity), ['ps2'], ['hT32'], 0.5)
                        for k in range(8):
                            peop(lambda e: e.matmul(PS[2][:, 0:36], lhsT=hT32[:, k, :], rhs=wrt32[:, k, :], start=(k == 0), stop=(k == 7)), ['hT32', 'wrt'], ['ps2'], 0.27)
                        svop = lambda fn, r, w: vop(fn, r, w, 0.14)
                        R = lambda a, b: rsm[:, 192 + a:192 + b]
                        lg, zem, sel, ex, top8 = rsm[:, 0:36], rsm[:, 40:72], rsm[:, 72:104], rsm[:, 104:136], rsm[:, 136:144]
                        svop(lambda e: e.tensor_tensor(out=lg, in0=PS[2][:, 0:36], in1=brt_t[:], op=OP.add), ['ps2', 'brt'], ['lg'])
                        svop(lambda e: e.tensor_reduce(out=R(0, 1), in_=rsm[:, 0:4], axis=AX.X, op=OP.max), ['lg'], ['gmax'])
                        svop(lambda e: e.tensor_scalar(out=R(4, 8), in0=rsm[:, 0:4], scalar1=R(0, 1), scalar2=None, op0=OP.is_ge), ['lg', 'gmax'], ['ohg'])
                        svop(lambda e: e.tensor_scalar(out=R(1, 2), in0=R(0, 1), scalar1=-1.0, scalar2=None, op0=OP.mult), ['gmax'], ['ngmax'])
                        aop(lambda e: e.activation(out=R(8, 12), in_=rsm[:, 0:4], func=AF.Exp, bias=R(1, 2)), ['lg', 'ngmax'], ['eg'])
                        svop(lambda e: e.tensor_reduce(out=R(2, 3), in_=R(8, 12), axis=AX.X, op=OP.add), ['eg'], ['gsum'])
                        svop(lambda e: e.reciprocal(out=R(2, 3), in_=R(2, 3)), ['gsum'], ['gsum'])
                        svop(lambda e: e.tensor_scalar(out=R(12, 16), in0=R(4, 8), scalar1=-1.0, scalar2=1e30, op0=OP.add, op1=OP.mult), ['ohg'], ['pen'])
                        for g in range(4):
                            svop(lambda e: e.tensor_scalar(out=rsm[:, 40 + g * 8:48 + g * 8], in0=rsm[:, 4 + g * 8:12 + g * 8], scalar1=R(12 + g, 13 + g), scalar2=None, op0=OP.add),
                                ['lg', 'pen'], ['zem'])
                        svop(lambda e: e.max(out=top8, in_=zem), ['zem'], ['top8'])
                        svop(lambda e: e.tensor_scalar(out=sel, in0=zem, scalar1=rsm[:, 137:138], scalar2=None, op0=OP.is_ge), ['zem', 'top8'], ['sel'])
                        svop(lambda e: e.tensor_scalar(out=R(3, 4), in0=rsm[:, 136:137], scalar1=-1.0, scalar2=None, op0=OP.mult), ['top8'], ['nv1'])
                        aop(lambda e: e.activation(out=ex, in_=zem, func=AF.Exp, bias=R(3, 4)), ['zem', 'nv1'], ['ex'])
                        svop(lambda e: e.tensor_tensor(out=ex, in0=ex, in1=sel, op=OP.mult), ['ex', 'sel'], ['ex'])
                        svop(lambda e: e.tensor_reduce(out=R(16, 17), in_=ex, axis=AX.X, op=OP.add), ['ex'], ['den'])
                        svop(lambda e: e.reciprocal(out=R(16, 17), in_=R(16, 17)), ['den'], ['den'])
                        svop(lambda e: e.tensor_tensor(out=R(16, 17), in0=R(16, 17), in1=R(2, 3), op=OP.mult), ['den', 'gsum'], ['den'])
                        gt, oh0, oh1, pos, tmpr, ovf = (rsm[:, 256:288], rsm[:, 288:320], rsm[:, 320:352], rsm[:, 352:384], rsm[:, 384:416], rsm[:, 416:448])
                        X = lambda i: rsm[:, 448 + i:449 + i]
                        svop(lambda e: e.tensor_scalar(out=gt, in0=ex, scalar1=R(16, 17), scalar2=None, op0=OP.mult), ['ex', 'den'], ['gt'])
                        svop(lambda e: e.tensor_scalar(out=oh0, in0=zem, scalar1=rsm[:, 136:137], scalar2=None, op0=OP.is_ge), ['zem', 'top8'], ['oh0'])
                        svop(lambda e: e.tensor_tensor(out=oh1, in0=sel, in1=oh0, op=OP.subtract), ['sel', 'oh0'], ['oh1'])
                        svop(lambda e: e.tensor_copy(out=sel_bf[:], in_=sel), ['sel'], ['sel_bf'])
                        peop(lambda e: e.matmul(PS[2][:, 64:96], lhsT=tri_bf[:], rhs=sel_bf[:], start=True, stop=False), ['tri', 'sel_bf'], ['ps2'])
                        peop(lambda e: e.matmul(PS[2][:, 64:96], lhsT=ones_bf[:], rhs=selsum[:], start=False, stop=True), ['ones', 'selsum'], ['ps2'])
                        svop(lambda e: e.tensor_tensor(out=selsum[:], in0=selsum[:], in1=sel, op=OP.add), ['selsum', 'sel'], ['selsum'])
                        svop(lambda e: e.tensor_tensor(out=pos, in0=PS[2][:, 64:96], in1=ecap[:], op=OP.add), ['ps2', 'ecap'], ['pos'])
                        svop(lambda e: e.tensor_scalar(out=ovf, in0=PS[2][:, 64:96], scalar1=float(CAP), scalar2=1e6, op0=OP.is_ge, op1=OP.mult), ['ps2'], ['ovf'])
                        svop(lambda e: e.tensor_tensor(out=pos, in0=pos, in1=ovf, op=OP.add), ['pos', 'ovf'], ['pos'])
                        for kq, ohk in enumerate([oh0, oh1]):
                            okey = 'oh%d' % kq
                            svop(lambda e: e.tensor_tensor(out=tmpr, in0=ohk, in1=pos, op=OP.mult), [okey, 'pos'], ['tmpr'])
                            svop(lambda e: e.tensor_reduce(out=X(kq), in_=tmpr, axis=AX.X, op=OP.add), ['tmpr'], ['dx%d' % kq])
                            svop(lambda e: e.tensor_copy(out=dest_all[:, 2 * gst + kq:2 * gst + kq + 1], in_=X(kq)), ['dx%d' % kq], ['dest%d_%d' % (gst, kq)])
                            svop(lambda e: e.tensor_tensor(out=tmpr, in0=ohk, in1=gt, op=OP.mult), [okey, 'gt'], ['tmpr'])
                            svop(lambda e: e.tensor_reduce(out=gsel_all[:, gst, kq:kq + 1], in_=tmpr, axis=AX.X, op=OP.add), ['tmpr'], ['gsel'])
                        aop(lambda e: e.activation(out=hb[:].rearrange("t (k p) -> t k p", p=128), in_=zt[:].rearrange("t (p k) -> t k p", k=8), func=AF.Identity), ['zt0', 'zt1'], ['hb'], 1.0)
                        for kq in range(2):
                            S.dma('pool', lambda e: e.indirect_dma_start(out=xbuf_d, out_offset=bass.IndirectOffsetOnAxis(ap=dest_all[:, 2 * gst + kq:2 * gst + kq + 1], axis=0), in_=hb[:], in_offset=None,
                                                                         bounds_check=bcreg(e, 'A'), oob_is_err=False), reads=['hb', 'dest%d_%d' % (gst, kq)], writes=['xbuf'])
                chains = [S.record(sec_conv), S.record(sec_s5)]
                if prev_tile is not None:
                    pt = prev_tile
                    chains.insert(0, S.record(lambda: sec_ln(*pt)))
                S.replay_merged(chains)
                for m in range(8):
                    b = proj(20 + m)
                    aop(lambda e: e.activation(out=BT_[3][:], in_=PS[b][:, 0:TS], func=AF.Sigmoid, bias=bcol(B_IN, 20 + m)), ['ps%d' % b, 'cols'], [bk(3)])
                    b2 = mmbank()
                    for k in range(4):
                        peop(lambda e: e.matmul(PS[b2][:, 0:TS], lhsT=wcout_bf[:, k, m * 128:(m + 1) * 128], rhs=cs_bf[:, k, :], start=(k == 0), stop=(k == 3)),
                             ['wcout', 'cs%d' % k], ['ps%d' % b2])
                    vop(lambda e: e.scalar_tensor_tensor(out=merged[:, m, :], in0=PS[b2][:, 0:TS], scalar=bcol(B_COUT, m), in1=BT_[3][:], op0=OP.add, op1=OP.mult),
                        ['ps%d' % b2, 'cols', bk(3)], ['mg%d' % m])

                for m in range(8):
                    b = proj(12 + m)
                    aop(lambda e: e.activation(out=BT_[3][:], in_=PS[b][:, 0:TS], func=AF.Sigmoid, bias=bcol(B_IN, 12 + m)), ['ps%d' % b, 'cols'], [bk(3)])
                    bg = mmbank()
                    for k in range(4):
                        peop(lambda e: e.matmul(PS[bg][:, 0:TS], lhsT=wglu_bf[:, k, (8 + m) * 128:(9 + m) * 128], rhs=y_bf[:, k, :], start=(k == 0), stop=(k == 3)),
                             ['wglu', 'y%d' % k], ['ps%d' % bg])
                    aop(lambda e: e.activation(out=BT_[2][:], in_=PS[bg][:, 0:TS], func=AF.Sigmoid, bias=bcol(B_GLU, 8 + m)), ['ps%d' % bg, 'cols'], [bk(2)])
                    bv = mmbank()
                    for k in range(4):
                        peop(lambda e: e.matmul(PS[bv][:, 0:TS], lhsT=wglu_bf[:, k, m * 128:(m + 1) * 128], rhs=y_bf[:, k, :], start=(k == 0), stop=(k == 3)),
                             ['wglu', 'y%d' % k], ['ps%d' % bv])
                    vop(lambda e: e.scalar_tensor_tensor(out=FT[8][:], in0=PS[bv][:, 0:TS], scalar=bcol(B_GLU, m), in1=BT_[2][:], op0=OP.add, op1=OP.mult),
                        ['ps%d' % bv, 'cols', bk(2)], [fk(8)])
                    pop(lambda e: e.tensor_tensor(out=FT[8][:], in0=FT[8][:], in1=BT_[3][:], op=OP.mult), [fk(8), bk(3)], [fk(8)])
                    pop(lambda e: e.tensor_tensor(out=merged[:, m, :], in0=FT[8][:], in1=merged[:, m, :], op=OP.add), [fk(8), 'mg%d' % m], ['mg%d' % m])

                prev_tile = (ti, t0)
                if ti == NT - 1:
                    S.replay_merged([S.record(lambda: sec_ln(ti, t0))])

            S.barrier()
            S.emit()

        if not DEBUG_H:
            with ExitStack() as es:
                def sb(name, shape, dt=F32):
                    return es.enter_context(nc.sbuf_tensor(name, list(shape), dt))
                PS = [es.enter_context(nc.psum_tensor("pb%d" % i, [128, 512], F32)) for i in range(6)]
                PT = [es.enter_context(nc.psum_tensor("pt%d" % i, [128, 1024], BF16)) for i in range(2)]
                wg = [sb("wg%d" % i, [128, 8, 256], BF16) for i in range(2)]
                wu = [sb("wu%d" % i, [128, 8, 256], BF16) for i in range(2)]
                wd = [sb("wd%d" % i, [128, 2, D], BF16) for i in range(2)]
                xrows = [sb("xrows%d" % i, [128, NS4, D], BF16) for i in range(2)]
                xTe = sb("xTe", [128, 8, CAP], BF16)
                sgt = sb("sgt", [128, 2, CAP], BF16)
                hid = sb("hid", [128, 2, CAP], BF16)
                ysb = [sb("ysb%d" % i, [128, NS4, D], BF16) for i in range(2)]
                identb = sb("identb", [128, 128], BF16)
                NACC = 4
                statb = sb("statsb", [128, 24 * NACC])
                repB = sb("repB", [128, 2, D])
                acc = [sb("acc%d" % i, [128, D]) for i in range(NACC)]
                yg = [[sb("yg%d_%d" % (i, k), [128, D], BF16) for k in range(2)] for i in range(NACC)]
                dg = [[sb("dg%d_%d" % (i, k), [128, 128], BF16) for k in range(2)] for i in range(NACC)]
                S.dma('sp', lambda e: e.dma_start(out=repB[:], in_=rep5v[:, 3:5, :]), writes=['repB'])
                S.dma('pool', lambda e: e.dma_start(out=identb[:], in_=ident_d), writes=['identb'])
                for i in range(NACC):
                    for k in range(2):
                        pop(lambda e: e.memset(yg[i][k][:], 0.0), [], ['yg%d_%d' % (i, k)])
                rot = [0]

                def bank(n=6):
                    b = rot[0]
                    rot[0] = (rot[0] + 1) % n
                    return b
                def load_expert(ex_j):
                    wj = ex_j % 2
                    xrj = xrows[wj]
                    S.dma('sp', lambda e: e.dma_start(out=xrj[:], in_=xbuf_d[ex_j * CAP:(ex_j + 1) * CAP, :].rearrange("(s p) d -> p s d", p=128)), reads=['xbuf'], writes=['xr%d' % wj])
                    S.dma('pool', lambda e: e.dma_start(out=wg[wj][:], in_=w_gate[ex_j].rearrange("(p k) f -> p k f", k=8)), writes=['wg%d' % wj])
                    S.dma('pool', lambda e: e.dma_start(out=wu[wj][:], in_=w_up[ex_j].rearrange("(p k) f -> p k f", k=8)), writes=['wu%d' % wj])
                    S.dma('pool', lambda e: e.dma_start(out=wd[wj][:], in_=w_down[ex_j].rearrange("(k p) f -> p k f", p=128)), writes=['wd%d' % wj])
                load_expert(0)
                for ex_i in range(32):
                    wi = ex_i % 2
                    xr = xrows[wi]
                    ys = ysb[wi]
                    if ex_i + 1 < 32:
                        load_expert(ex_i + 1)
                    for s4 in range(NS4):
                        pt = PT[s4 % 2]
                        for k in range(8):
                            peop(lambda e: e.transpose(out=pt[:, k * 128:(k + 1) * 128], in_=xr[:, s4, k * 128:(k + 1) * 128], identity=identb[:]), ['xr%d' % wi, 'identb'], ['pt%d' % (s4 % 2)])
                        ev = aop if s4 % 2 == 0 else vop
                        ev(lambda e: e.tensor_copy(out=xTe[:, :, s4 * 128:(s4 + 1) * 128], in_=pt[:].rearrange("p (k r) -> p k r", k=8)) if False else
                           (e.activation(out=xTe[:, :, s4 * 128:(s4 + 1) * 128], in_=pt[:].rearrange("p (k r) -> p k r", k=8), func=AF.Identity) if s4 % 2 == 0 else
                            e.tensor_copy(out=xTe[:, :, s4 * 128:(s4 + 1) * 128], in_=pt[:].rearrange("p (k r) -> p k r", k=8))),
                           ['pt%d' % (s4 % 2)], ['xTe%d' % s4])
                    xk = ['xTe%d' % i for i in range(NS4)]
                    for f in range(2):
                        bgk = bank()
                        for k in range(8):
                            peop(lambda e: e.matmul(PS[bgk][:, 0:CAP], lhsT=wg[wi][:, k, f * 128:(f + 1) * 128], rhs=xTe[:, k, :], start=(k == 0), stop=(k == 7)), ['wg%d' % wi] + xk, ['pb%d' % bgk])
                        aop(lambda e: e.activation(out=sgt[:, f, :], in_=PS[bgk][:, 0:CAP], func=AF.Silu), ['pb%d' % bgk], ['sgt%d' % f])
                        buk = bank()
                        for k in range(8):
                            peop(lambda e: e.matmul(PS[buk][:, 0:CAP], lhsT=wu[wi][:, k, f * 128:(f + 1) * 128], rhs=xTe[:, k, :], start=(k == 0), stop=(k == 7)), ['wu%d' % wi] + xk, ['pb%d' % buk])
                        vop(lambda e: e.tensor_tensor(out=hid[:, f, :], in0=PS[buk][:, 0:CAP], in1=sgt[:, f, :], op=OP.mult), ['pb%d' % buk, 'sgt%d' % f], ['hid%d' % f])
                    for s4 in range(NS4):
                        for hf in range(2):
                            hs = slice(hf * 512, (hf + 1) * 512)
                            b = bank()
                            for f in range(2):
                                peop(lambda e: e.matmul(PS[b][:], lhsT=hid[:, f, s4 * 128:(s4 + 1) * 128], rhs=wd[wi][:, f, hs], start=(f == 0), stop=(f == 1)),
                                     ['hid0', 'hid1', 'wd%d' % wi], ['pb%d' % b])
                            if (s4 * 2 + hf) % 2 == 0:
                                aop(lambda e: e.activation(out=ys[:, s4, hs], in_=PS[b][:], func=AF.Identity), ['pb%d' % b], ['ysa%d' % wi])
                            else:
                                vop(lambda e: e.tensor_copy(out=ys[:, s4, hs], in_=PS[b][:]), ['pb%d' % b], ['ysv%d' % wi])
                    S.dma('sp', lambda e: e.dma_start(out=ybuf_d[ex_i * CAP:(ex_i + 1) * CAP, :].rearrange("(s p) d -> p s d", p=128), in_=ys[:]), reads=['ysa%d' % wi, 'ysv%d' % wi], writes=['ybuf%d' % ex_i])
                ybk = ['ybuf%d' % i for i in range(32)]
                NSUB = TOK // 128

                def comb_load(st):
                    r0 = st * 128
                    pi_ = st % NACC
                    a_ = acc[pi_]
                    S.dma('sp', lambda e: e.dma_start(out=a_[:], in_=zb_d[r0:r0 + 128, :]), reads=['zb_d'], writes=['accb%d' % pi_])
                    for kq in range(2):
                        g = yg[pi_][kq]
                        S.dma('pool', lambda e: e.indirect_dma_start(out=g[:], out_offset=None, in_=ybuf_d, in_offset=bass.IndirectOffsetOnAxis(ap=dest_all[:, 2 * st + kq:2 * st + kq + 1], axis=0),
                                                                     bounds_check=bcreg(e, 'B'), oob_is_err=False), reads=ybk, writes=['yg%d_%d' % (pi_, kq)])
                def comb_compute(st):
                    r0 = st * 128
                    pi_ = st % NACC
                    a_ = acc[pi_]
                    ak = 'accb%d' % pi_
                    sb_ = statb[:, pi_ * 24:pi_ * 24 + 24]
                    sk_ = 'stb%d' % pi_
                    for kq in range(2):
                        dgk = dg[pi_][kq]
                        aop(lambda e: e.activation(out=dgk[:], in_=identb[:], func=AF.Identity, scale=gsel_all[:, st, kq:kq + 1]), ['identb', 'gsel'], ['dg%d_%d' % (pi_, kq)], 0.2)
                    for hf in range(2):
                        hs = slice(hf * 512, (hf + 1) * 512)
                        b = (st % 2) * 2 + hf
                        for kq in range(2):
                            g = yg[pi_][kq]
                            dgk = dg[pi_][kq]
                            peop(lambda e: e.matmul(PS[b][:], lhsT=dgk[:], rhs=g[:, hs], start=(kq == 0), stop=(kq == 1)),
                                 ['dg%d_%d' % (pi_, kq), 'yg%d_%d' % (pi_, kq)], ['pb%d' % b], 0.28)
                        vop(lambda e: e.tensor_tensor(out=a_[:, hs], in0=a_[:, hs], in1=PS[b][:], op=OP.add), ['pb%d' % b, ak], [ak], 0.6)
                    for hf in range(2):
                        vop(lambda e: e.bn_stats(out=sb_[:, hf * 6:(hf + 1) * 6], in_=a_[:, hf * 512:(hf + 1) * 512]), [ak], [sk_ + 'b%d' % hf], 0.65)
                    vop(lambda e: e.bn_aggr(out=sb_[:, 12:14], in_=sb_[:, 0:12]), [sk_ + 'b0', sk_ + 'b1'], [sk_ + 'mv'], 0.2)
                    aop(lambda e: e.activation(out=sb_[:, 14:15], in_=sb_[:, 13:14], func=AF.Sqrt, bias=eps_t[:, 0:1]), [sk_ + 'mv', 'eps'], [sk_ + 'sd'], 0.3)
                    vop(lambda e: e.reciprocal(out=sb_[:, 15:16], in_=sb_[:, 14:15]), [sk_ + 'sd'], [sk_ + 'rs'], 0.16)
                    vop(lambda e: e.scalar_tensor_tensor(out=sb_[:, 16:17], in0=sb_[:, 12:13], scalar=-1.0, in1=sb_[:, 15:16], op0=OP.mult, op1=OP.mult), [sk_ + 'mv', sk_ + 'rs'], [sk_ + 'nb'], 0.1)
                    aop(lambda e: e.activation(out=a_[:], in_=a_[:], func=AF.Identity, scale=sb_[:, 15:16], bias=sb_[:, 16:17]), [ak, sk_ + 'rs', sk_ + 'nb'], [ak], 1.1)
                    vop(lambda e: e.tensor_tensor(out=a_[:], in0=a_[:], in1=repB[:, 0, :], op=OP.mult), [ak, 'repB'], [ak], 1.1)
                    pop(lambda e: e.tensor_tensor(out=a_[:, 0:512], in0=a_[:, 0:512], in1=repB[:, 1, 0:512], op=OP.add), [ak, 'repB'], [ak + 'p'], 1.2)
                    vop(lambda e: e.tensor_tensor(out=a_[:, 512:1024], in0=a_[:, 512:1024], in1=repB[:, 1, 512:1024], op=OP.add), [ak, 'repB'], [ak + 'v'], 0.6)
                    S.dma('sp', lambda e: e.dma_start(out=out[r0:r0 + 128, :], in_=a_[:]), reads=[ak, ak + 'p', ak + 'v'], writes=['out'])

                comb_load(0)
                comb_load(1)
                for st0 in range(0, NSUB, 2):
                    for st in (st0 + 2, st0 + 3):
                        if st < NSUB:
                            comb_load(st)
                    S.replay_merged([S.record(lambda: comb_compute(st0)), S.record(lambda: comb_compute(st0 + 1))])
                S.barrier()
                S.emit()
    return nc


def _prep(inp):
    f = lambda a: np.ascontiguousarray(a, dtype=np.float32)
    colmat = lambda v: f(np.asarray(v).reshape(-1, 128).T)
    cols = np.concatenate([colmat(inp['b_in'][0]), colmat(inp['b_glu'][0]), colmat(inp['b_cout'][0]), colmat(inp['ssm_d'][0]),
                           colmat(inp['conv_b'][0]), colmat(inp['ln_c_g'][0]), colmat(inp['ln_c_b'][0])], axis=1)
    assert cols.shape == (128, 68)
    cols = f(np.concatenate([cols, np.zeros((128, 4), np.float32)], axis=1))
    cw = inp['conv_w'][0][:, 0, :]
    convw = f(cw.T.reshape(4, 128, CW).transpose(1, 0, 2).reshape(128, 4 * CW))
    rep5 = f(np.concatenate([np.broadcast_to(inp[k][0][None, :], (128, D)) for k in ['b_out', 'ln1_g', 'ln1_b', 'ln2_g', 'ln2_b']], axis=1))
    w_rt = f(np.concatenate([inp['w_route_group'][0], inp['w_route_expert'][0]], axis=1))
    brt = f(np.broadcast_to(np.concatenate([inp['b_route_group'][0], inp['b_route_expert'][0]])[None, :], (128, 36)))
    pqf = lambda a: a.reshape(16, 2, 64).transpose(1, 2, 0).reshape(128, 16)
    ldt = np.broadcast_to(inp['log_dt'][0].reshape(16, 2, 1), (16, 2, 64))
    pq = f(np.concatenate([pqf(inp['lam_re'][0]), pqf(inp['lam_im'][0]), pqf(np.ascontiguousarray(ldt))], axis=1))
    bf_ = lambda a: a.reshape(16, 2, 64, 16).transpose(1, 2, 0, 3).reshape(128, 256)
    cf_ = lambda a: a.reshape(16, 2, 16, 64).transpose(1, 3, 0, 2).reshape(128, 256)
    bc = f(np.concatenate([bf_(inp['ssm_b_re'][0]), bf_(inp['ssm_b_im'][0]), cf_(inp['ssm_c_re'][0]), cf_(inp['ssm_c_im'][0])], axis=1))
    shared = {
        'w_in': f(inp['w_in'][0]), 'w_glu': f(inp['w_glu'][0]), 'w_cout': f(inp['w_cout'][0]), 'w_out': f(inp['w_out'][0]),
        'w_rt': w_rt, 'w_gate': f(inp['w_gate'][0]), 'w_up': f(inp['w_up'][0]), 'w_down': f(inp['w_down'][0]),
        'cols': cols, 'convw': convw, 'rep5': rep5, 'brt': brt, 'pq': pq, 'bc': bc,
        'ident': np.eye(128, dtype=np.float32),
        'tri': np.triu(np.ones((128, 128), np.float32), 1),
        'ecap': f(np.broadcast_to((np.arange(32, dtype=np.float32) * CAP)[None, :], (128, 32))),
        'kk': f(np.broadcast_to(np.arange(1, TS + 1, dtype=np.float32)[None, :], (128, TS))),
        'bdmask': f(np.kron(np.eye(4, dtype=np.float32), np.ones((32, 32), np.float32))),
    }
    x = inp['x']
    maps = []
    for c in range(NCORES):
        xc = f(x[2 * c:2 * c + 2].reshape(TOK, D))
        m = dict(shared)
        m['xtok'] = xc
        m['xT'] = f(xc.T)
        maps.append(m)
    return maps


def kernel(**inputs):
    maps = _prep(inputs)
    nc = build_nc()
    res = run_bass_kernel_spmd(nc, maps, core_ids=list(range(NCORES)))
    outs = [np.asarray(r['out']).reshape(2, SEQ, D) for r in res.results]
    return np.concatenate(outs, axis=0).astype(np.float32)
```

```python
import math
import numpy as np
import concourse.bass as bass
import concourse.mybir as mybir
from concourse.bass_utils import run_bass_kernel_spmd
from contextlib import ExitStack

F32 = mybir.dt.float32
BF16 = mybir.dt.bfloat16
AF = mybir.ActivationFunctionType
OP = mybir.AluOpType
AX = mybir.AxisListType

NCORES = 8
D = 1024
SEQ = 2048
TOK = 4096
TS = 256
NT = TOK // TS
CW = 31
ALPHA = 2.0 ** 0.25
EPS = 1e-5
MAGIC = 12582912.0
TWO_PI = 2.0 * math.pi
DEBUG_H = False
S5_HALF = True
S5_DELAY = True
CAP = 384
NS4 = CAP // 128
I32 = mybir.dt.int32


import types


def _freeze(fn):
    if fn.__closure__ is None:
        return fn
    cells = []
    for c in fn.__closure__:
        try:
            cells.append(types.CellType(c.cell_contents))
        except ValueError:
            cells.append(c)
    return types.FunctionType(fn.__code__, fn.__globals__, fn.__name__, fn.__defaults__, tuple(cells))


class Sched:
    def __init__(self, nc, es, ndma=14):
        self.nc = nc
        self.names = ['pe', 'act', 'dve', 'pool', 'sp']
        self.prog = {k: [] for k in self.names}
        self.sem = {k: es.enter_context(nc.semaphore('s_' + k)) for k in ['pe', 'act', 'dve', 'pool']}
        self.cnt = {k: 0 for k in self.sem}
        self.dsem = [es.enter_context(nc.semaphore('d%d' % i)) for i in range(ndma)]
        self.dcnt = [0] * ndma
        self.dnext = 0
        self.seen = {k: {} for k in self.names}
        self.lastw = {}
        self.readers = {}
        self.rec = None
        self.sim_eng = {}
        self.sim_w = {}
        self.sim_r = {}

    def record(self, section):
        self.rec = []
        section()
        r = self.rec
        self.rec = None
        return r

    COST = {'pe': 0.17, 'act': 0.42, 'dve': 0.36, 'pool': 0.75, 'sp': 0.1}

    def replay_merged(self, chains):
        pos = [0] * len(chains)
        now = max(self.sim_eng.values()) if self.sim_eng else 0.0
        for k in self.names:
            self.sim_eng[k] = now
        while True:
            best, bkey, bt = None, None, 0.0
            for i, c in enumerate(chains):
                if pos[i] >= len(c):
                    continue
                kind, eng, fn, reads, writes, cost = c[pos[i]]
                t = self.sim_eng[eng]
                for r in reads:
                    t = max(t, self.sim_w.get(r, 0.0))
                for w in writes:
                    t = max(t, self.sim_w.get(w, 0.0), self.sim_r.get(w, 0.0))
                key = (t, pos[i] / len(c))
                if best is None or key < bkey:
                    best, bkey, bt = i, key, t
            if best is None:
                break
            kind, eng, fn, reads, writes, cost = chains[best][pos[best]]
            pos[best] += 1
            if kind == 'op':
                end = bt + (cost if cost is not None else self.COST[eng])
                self.sim_eng[eng] = end
                done = end + 0.06
                self.op(eng, fn, reads, writes)
            else:
                self.sim_eng[eng] = bt + (1.0 if eng == 'pool' else 0.1)
                done = bt + 2.5
                self.dma(eng, fn, reads, writes)
            for r in reads:
                self.sim_r[r] = max(self.sim_r.get(r, 0.0), done)
            for w in writes:
                self.sim_w[w] = done
                self.sim_r[w] = 0.0

    def _semobj(self, k):
        return self.sem[k] if isinstance(k, str) else self.dsem[k]

    def _waits(self, eng, deps):
        best = {}
        for (k, v) in deps:
            if k == 'pe' and eng == 'pe':
                continue
            if self.seen[eng].get(k, 0) >= v:
                continue
            best[k] = max(best.get(k, 0), v)
        for k, v in best.items():
            self.seen[eng][k] = v
            so = self._semobj(k)
            self.prog[eng].append(lambda e, so=so, v=v: e.wait_ge(so, v))

    def _deps(self, reads, writes):
        deps = []
        for r in reads:
            if r in self.lastw:
                deps.append(self.lastw[r])
        for w in writes:
            if w in self.lastw:
                deps.append(self.lastw[w])
            deps.extend(self.readers.get(w, []))
        return deps

    def _commit(self, tok, reads, writes):
        for r in reads:
            self.readers.setdefault(r, []).append(tok)
        for w in writes:
            self.lastw[w] = tok
            self.readers[w] = []

    def op(self, eng, fn, reads=(), writes=(), cost=None):
        fn = _freeze(fn)
        if self.rec is not None:
            self.rec.append(('op', eng, fn, tuple(reads), tuple(writes), cost))
            return
        self._waits(eng, self._deps(reads, writes))
        self.cnt[eng] += 1
        so = self.sem[eng]
        self.prog[eng].append(lambda e, fn=fn, so=so: fn(e).then_inc(so, 1))
        self._commit((eng, self.cnt[eng]), reads, writes)

    def dma(self, eng, fn, reads=(), writes=()):
        fn = _freeze(fn)
        if self.rec is not None:
            self.rec.append(('dma', eng, fn, tuple(reads), tuple(writes), None))
            return
        i = self.dnext
        self.dnext = (self.dnext + 1) % len(self.dsem)
        deps = self._deps(reads, writes)
        if self.dcnt[i] > 0:
            deps.append((i, self.dcnt[i]))
        self._waits(eng, deps)
        self.dcnt[i] += 16
        so = self.dsem[i]
        self.prog[eng].append(lambda e, fn=fn, so=so: fn(e).then_inc(so, 16))
        self._commit((i, self.dcnt[i]), reads, writes)

    def barrier(self):
        deps = [(k, v) for k, v in self.cnt.items() if v > 0]
        deps += [(i, v) for i, v in enumerate(self.dcnt) if v > 0]
        for eng in self.names:
            self._waits(eng, deps)

    def emit(self):
        nc = self.nc
        prog = self.prog
        with nc.Block() as block:
            @block.tensor
            def _(e):
                for f in prog['pe']:
                    f(e)

            @block.scalar
            def _(e):
                for f in prog['act']:
                    f(e)

            @block.vector
            def _(e):
                for f in prog['dve']:
                    f(e)

            @block.gpsimd
            def _(e):
                for f in prog['pool']:
                    f(e)

            @block.sync
            def _(e):
                for f in prog['sp']:
                    f(e)
        self.prog = {k: [] for k in self.names}


def build_nc():
    nc = bass.Bass("TRN2", target_bir_lowering=False)

    def din(name, shape):
        return nc.dram_tensor(name, list(shape), F32, kind="ExternalInput").ap()

    xT = din("xT", [D, TOK])
    xtok = din("xtok", [TOK, D])
    w_in = din("w_in", [D, 3584])
    w_glu = din("w_glu", [512, 2048])
    w_cout = din("w_cout", [512, 1024])
    w_out = din("w_out", [D, D])
    w_rt = din("w_rt", [D, 36])
    w_gate = din("w_gate", [32, D, 256])
    w_up = din("w_up", [32, D, 256])
    w_down = din("w_down", [32, 256, D])
    cols = din("cols", [128, 72])
    convw = din("convw", [128, 4 * CW])
    rep5 = din("rep5", [128, 5 * D])
    brt = din("brt", [128, 36])
    pq = din("pq", [128, 48])
    bc = din("bc", [128, 4 * 256])
    ident_d = din("ident", [128, 128])
    kk_d = din("kk", [128, TS])
    out = nc.dram_tensor("out", [TOK, D], F32, kind="ExternalOutput").ap()
    tri_d = din("tri", [128, 128])
    ecap_d = din("ecap", [128, 32])
    bdmask_d = din("bdmask", [128, 128])
    xbuf_d = nc.dram_tensor("xbuf_scr", [32 * CAP, D], BF16, kind="Internal").ap()
    ybuf_d = nc.dram_tensor("ybuf_scr", [32 * CAP, D], BF16, kind="Internal").ap()
    zb_d = nc.dram_tensor("zb_scr", [TOK, D], F32, kind="Internal").ap()
    rep5v = rep5.rearrange("p (a d) -> p a d", a=5)

    with ExitStack() as es0:
        S = Sched(nc, es0)
        dest_all = es0.enter_context(nc.sbuf_tensor("dest_all", [128, 64], I32))
        gsel_all = es0.enter_context(nc.sbuf_tensor("gsel_all", [128, 32, 2], F32))
        eps_t = es0.enter_context(nc.sbuf_tensor("eps_t", [128, 1], F32))
        S.op('dve', lambda e: e.memset(eps_t[:], EPS), writes=['eps'])
        regh = {}

        def bcreg(e, tag):
            if tag not in regh:
                regh[tag] = e.alloc_register('bcr' + tag)
                e.reg_mov(regh[tag], 32 * CAP - 1)
            return regh[tag]
        vop = lambda fn, r, w, c=None: S.op('dve', fn, reads=r, writes=w, cost=c)
        aop = lambda fn, r, w, c=None: S.op('act', fn, reads=r, writes=w, cost=c)
        pop = lambda fn, r, w, c=None: S.op('pool', fn, reads=r, writes=w, cost=c)
        peop = lambda fn, r, w, c=None: S.op('pe', fn, reads=r, writes=w, cost=c)

        with ExitStack() as es:
            def sb(name, shape, dt=F32):
                return es.enter_context(nc.sbuf_tensor(name, list(shape), dt))
            PS = [es.enter_context(nc.psum_tensor("ps%d" % i, [128, 512], F32)) for i in range(8)]

            win_bf = sb("win_bf", [128, 8, 3584], BF16)
            wglu_bf = sb("wglu_bf", [128, 4, 2048], BF16)
            wcout_bf = sb("wcout_bf", [128, 4, 1024], BF16)
            wout_bf = sb("wout_bf", [128, 8, 1024], BF16)
            wrt32 = sb("wrt32", [128, 8, 36])
            cols_t = sb("cols_t", [128, 72])
            convw_t = sb("convw_t", [128, 4, CW])
            brt_t = sb("brt_t", [128, 36])
            ident = sb("ident_sb", [128, 128])
            ones_bf = sb("ones_bf", [128, 128], BF16)
            halfpi = sb("halfpi", [128, 1])
            repA = sb("repA", [128, 3, D])
            cdiag = sb("cdiag", [128, CW, 128], BF16)
            TC = 4
            NCH = TS // TC
            WZ = sb("WZ", [128, 4, TC, 2, 128], BF16)
            KM = sb("KM", [128, 4, TC, 128], BF16)
            WY = sb("WY", [128, 16, TC, 2, 32], BF16)
            cos2 = sb("cos2", [128, 16, NCH])
            sin2 = sb("sin2", [128, 16, NCH])
            R4rep = sb("R4rep", [128, 16, NCH])
            r4 = sb("r4", [128, 16])
            carry = sb("carry", [128, 2, 16])
            fk = lambda i: 'f%d' % i
            bk = lambda i: 'b%d' % i

            S.dma('sp', lambda e: e.dma_start(out=repA[:], in_=rep5v[:, 0:3, :]), writes=['rep'])
            S.dma('sp', lambda e: e.dma_start(out=wrt32[:], in_=w_rt.rearrange("(k p) c -> p k c", p=128)), writes=['wrt'])
            S.dma('sp', lambda e: e.dma_start(out=cols_t[:], in_=cols), writes=['cols'])
            S.dma('sp', lambda e: e.dma_start(out=convw_t[:], in_=convw.rearrange("p (c j) -> p c j", c=4)), writes=['convw'])
            S.dma('sp', lambda e: e.dma_start(out=brt_t[:], in_=brt), writes=['brt'])
            S.dma('sp', lambda e: e.dma_start(out=ident[:], in_=ident_d), writes=['ident'])
            vop(lambda e: e.memset(halfpi[:], math.pi / 2), [], ['halfpi'])
            vop(lambda e: e.memset(ones_bf[:], 1.0), [], ['ones'])

            with ExitStack() as esS:
                def sbs(name, shape, dt=F32):
                    return esS.enter_context(nc.sbuf_tensor(name, list(shape), dt))
                pq_t = sbs("pq_t", [128, 3, 16])
                bc_t = sbs("bc_t", [128, 4, 16, 16])
                kk = sbs("kk_sb", [128, NCH])
                NSL = 30
                small = sbs("small", [128, NSL * 16])
                vbrs = [sbs("vbr%d" % i, [128, 256]) for i in range(2)]
                vbis = [sbs("vbi%d" % i, [128, 256]) for i in range(2)]
                tmpbs = [sbs("tmpb%d" % i, [128, 256]) for i in range(2)]
                tmpas = [sbs("tmpa%d" % i, [128, 256]) for i in range(2)]
                E4rs = [sbs("E4r%d" % i, [128, 128]) for i in range(2)]
                E4is = [sbs("E4i%d" % i, [128, 128]) for i in range(2)]
                CTE = sbs("CTE", [128, 4, 2, 128])
                wts = [[sbs("wt%d_%d" % (i, jj), [128, 256]) for i in range(4)] for jj in range(2)]
                bdm = sbs("bdm", [128, 128])
                S.dma('sp', lambda e: e.dma_start(out=bdm[:], in_=bdmask_d), writes=['bdm'])
                tb6 = sbs("tb6", [128, 4, NCH])
                S.dma('sp', lambda e: e.dma_start(out=pq_t[:], in_=pq.rearrange("p (a q) -> p a q", a=3)), writes=['pq'])
                S.dma('sp', lambda e: e.dma_start(out=bc_t[:], in_=bc.rearrange("p (a q h) -> p a q h", a=4, q=16)), writes=['bc'])
                S.dma('sp', lambda e: e.dma_start(out=kk[:], in_=kk_d[:, 0:NCH]), writes=['kk'])
                for k in range(8):
                    S.dma('pool', lambda e, k=k: e.dma_start(out=win_bf[:, k, :], in_=w_in[k * 128:(k + 1) * 128, :]), writes=['win'])
                    S.dma('pool', lambda e, k=k: e.dma_start(out=wout_bf[:, k, :], in_=w_out[k * 128:(k + 1) * 128, :]), writes=['wout'])
                for k in range(4):
                    S.dma('pool', lambda e, k=k: e.dma_start(out=wglu_bf[:, k, :], in_=w_glu[k * 128:(k + 1) * 128, :]), writes=['wglu'])
                    S.dma('pool', lambda e, k=k: e.dma_start(out=wcout_bf[:, k, :], in_=w_cout[k * 128:(k + 1) * 128, :]), writes=['wcout'])
                sm = lambda i: small[:, i * 16:(i + 1) * 16]
                smc = lambda i, q: small[:, i * 16 + q:i * 16 + q + 1]
                sk = lambda i: 'sm%d' % i
                DT, RHO, TH, Y0, DEN, FR, FI, T1, T2, T3 = range(10)
                PR = lambda k: 10 + k
                PI = lambda k: 15 + k
                GR = lambda k: 20 + k
                GI = lambda k: 24 + k
                Y4 = 28
                lre, lim, ldt = pq_t[:, 0, :], pq_t[:, 1, :], pq_t[:, 2, :]
                TT = lambda o, a, b, op: vop(lambda e: e.tensor_tensor(out=sm(o), in0=sm(a), in1=sm(b), op=op), [sk(a), sk(b)], [sk(o)])
                aop(lambda e: e.activation(out=sm(DT), in_=ldt, func=AF.Exp), ['pq'], [sk(DT)])
                vop(lambda e: e.tensor_tensor(out=sm(RHO), in0=lre, in1=sm(DT), op=OP.mult), ['pq', sk(DT)], [sk(RHO)])
                vop(lambda e: e.tensor_tensor(out=sm(TH), in0=lim, in1=sm(DT), op=OP.mult), ['pq', sk(DT)], [sk(TH)])
                vop(lambda e: e.tensor_scalar(out=sm(Y0), in0=sm(TH), scalar1=1.0 / TWO_PI, scalar2=None, op0=OP.mult), [sk(TH)], [sk(Y0)])
                vop(lambda e: e.memset(sm(PR(0)), 1.0), [], [sk(PR(0))])
                vop(lambda e: e.memset(sm(PI(0)), 0.0), [], [sk(PI(0))])
                for k in range(1, TC + 1):
                    vop(lambda e: e.tensor_scalar(out=sm(T1), in0=sm(Y0), scalar1=float(k), scalar2=None, op0=OP.mult), [sk(Y0)], [sk(T1)])
                    vop(lambda e: e.tensor_scalar(out=sm(T2), in0=sm(T1), scalar1=MAGIC, scalar2=MAGIC, op0=OP.add, op1=OP.subtract), [sk(T1)], [sk(T2)])
                    TT(T1, T1, T2, OP.subtract)
                    aop(lambda e: e.activation(out=sm(T2), in_=sm(T1), func=AF.Abs), [sk(T1)], [sk(T2)])
                    aop(lambda e: e.activation(out=sm(PI(k)), in_=sm(T1), func=AF.Sin, scale=TWO_PI), [sk(T1)], [sk(PI(k))])
                    aop(lambda e: e.activation(out=sm(PR(k)), in_=sm(T2), func=AF.Sin, scale=-TWO_PI, bias=halfpi[:, 0:1]), [sk(T2), 'halfpi'], [sk(PR(k))])
                    aop(lambda e: e.activation(out=sm(T3), in_=sm(RHO), func=AF.Exp, scale=float(k)), [sk(RHO)], [sk(T3)])
                    TT(PR(k), PR(k), T3, OP.mult)
                    TT(PI(k), PI(k), T3, OP.mult)
                    if k == TC:
                        vop(lambda e: e.tensor_copy(out=r4[:], in_=sm(T3)), [sk(T3)], ['r4'])
                vop(lambda e: e.tensor_tensor(out=sm(T1), in0=lre, in1=lre, op=OP.mult), ['pq'], [sk(T1)])
                vop(lambda e: e.tensor_tensor(out=sm(T2), in0=lim, in1=lim, op=OP.mult), ['pq'], [sk(T2)])
                TT(DEN, T1, T2, OP.add)
                vop(lambda e: e.reciprocal(out=sm(DEN), in_=sm(DEN)), [sk(DEN)], [sk(DEN)])
                vop(lambda e: e.tensor_scalar(out=sm(T1), in0=sm(PR(1)), scalar1=-1.0, scalar2=None, op0=OP.add), [sk(PR(1))], [sk(T1)])
                vop(lambda e: e.tensor_tensor(out=sm(T2), in0=sm(T1), in1=lre, op=OP.mult), [sk(T1), 'pq'], [sk(T2)])
                vop(lambda e: e.tensor_tensor(out=sm(T3), in0=sm(PI(1)), in1=lim, op=OP.mult), [sk(PI(1)), 'pq'], [sk(T3)])
                TT(T2, T2, T3, OP.add)
                TT(FR, T2, DEN, OP.mult)
                vop(lambda e: e.tensor_tensor(out=sm(T2), in0=sm(PI(1)), in1=lre, op=OP.mult), [sk(PI(1)), 'pq'], [sk(T2)])
                vop(lambda e: e.tensor_tensor(out=sm(T3), in0=sm(T1), in1=lim, op=OP.mult), [sk(T1), 'pq'], [sk(T3)])
                TT(T2, T2, T3, OP.subtract)
                TT(FI, T2, DEN, OP.mult)
                for k in range(TC):
                    TT(T1, PR(k), FR, OP.mult)
                    TT(T2, PI(k), FI, OP.mult)
                    TT(GR(k), T1, T2, OP.subtract)
                    TT(T1, PR(k), FI, OP.mult)
                    TT(T2, PI(k), FR, OP.mult)
                    TT(GI(k), T1, T2, OP.add)
                bq = lambda i: sm(i).unsqueeze(2).to_broadcast([128, 16, 16])
                q3 = lambda t: t[:].rearrange("p (q h) -> p q h", h=16)
                bre, bim, ctr, cti = bc_t[:, 0, :, :], bc_t[:, 1, :, :], bc_t[:, 2, :, :], bc_t[:, 3, :, :]
                vop(lambda e: e.memset(CTE[:], 0.0), [], ['CTE'])
                for cq in range(4):
                    for g2 in range(2):
                        hsl = slice(g2 * 64, (g2 + 1) * 64)
                        o0 = CTE[hsl, cq, 0, :].rearrange("p (pl c) -> p pl c", c=32)[:, :, g2 * 16:(g2 + 1) * 16]
                        o1 = CTE[hsl, cq, 1, :].rearrange("p (pl c) -> p pl c", c=32)[:, :, g2 * 16:(g2 + 1) * 16]
                        vop(lambda e: e.tensor_copy(out=o0, in_=bc_t[hsl, 2, cq * 4:(cq + 1) * 4, :]), ['bc'], ['CTE'])
                        vop(lambda e: e.tensor_scalar(out=o1, in0=bc_t[hsl, 3, cq * 4:(cq + 1) * 4, :], scalar1=-1.0, scalar2=None, op0=OP.mult), ['bc'], ['CTE'])

                def emit_VB(k):
                    vbr, vbi, tmpa, tmpb = vbrs[k % 2], vbis[k % 2], tmpas[k % 2], tmpbs[k % 2]
                    kvr, kvi, kta, ktb = 'vbr%d' % (k % 2), 'vbi%d' % (k % 2), 'tmpa%d' % (k % 2), 'tmpb%d' % (k % 2)
                    vop(lambda e: e.tensor_tensor(out=q3(tmpa), in0=bim, in1=bq(GI(k)), op=OP.mult), ['bc', sk(GI(k))], [kta])
                    vop(lambda e: e.tensor_tensor(out=q3(tmpb), in0=bre, in1=bq(GI(k)), op=OP.mult), ['bc', sk(GI(k))], [ktb])
                    vop(lambda e: e.tensor_tensor(out=q3(vbr), in0=bre, in1=bq(GR(k)), op=OP.mult), ['bc', sk(GR(k))], [kvr])
                    vop(lambda e: e.tensor_tensor(out=q3(vbi), in0=bim, in1=bq(GR(k)), op=OP.mult), ['bc', sk(GR(k))], [kvi])
                    vop(lambda e: e.tensor_tensor(out=vbr[:], in0=vbr[:], in1=tmpa[:], op=OP.subtract), [kvr, kta], [kvr])
                    vop(lambda e: e.tensor_tensor(out=vbi[:], in0=vbi[:], in1=tmpb[:], op=OP.add), [kvi, ktb], [kvi])

                def emit_E4(k):
                    vbr, vbi = vbrs[k % 2], vbis[k % 2]
                    kvr, kvi = 'vbr%d' % (k % 2), 'vbi%d' % (k % 2)
                    for cq in range(4):
                        E4r, E4i = E4rs[cq % 2], E4is[cq % 2]
                        ekr, eki = 'E4r%d' % (cq % 2), 'E4i%d' % (cq % 2)
                        for ri, (src, E4, ek, ksrc) in enumerate([(vbr, E4r, ekr, kvr), (vbi, E4i, eki, kvi)]):
                            for g2 in range(2):
                                hsl = slice(g2 * 64, (g2 + 1) * 64)
                                o_ = E4[hsl, :].rearrange("p (pl c) -> p pl c", c=32)[:, :, g2 * 16:(g2 + 1) * 16]
                                i_ = src[hsl, cq * 64:(cq + 1) * 64].rearrange("p (pl h) -> p pl h", h=16)
                                if g2 == 0:
                                    vop(lambda e: e.tensor_copy(out=o_, in_=i_), [ksrc], [ek + 'p'])
                                else:
                                    aop(lambda e: e.activation(out=o_, in_=i_, func=AF.Identity), [ksrc], [ek + 'a'])
                            peop(lambda e: e.transpose(out=PS[7][:, ri * 128:(ri + 1) * 128], in_=E4[:], identity=ident[:]), [ek + 'p', ek + 'a', 'ident'], ['ps7'])
                            aop(lambda e: e.activation(out=WZ[:, cq, TC - 1 - k, ri, :], in_=PS[7][:, ri * 128:(ri + 1) * 128], func=AF.Identity), ['ps7'], ['WZ'])
                        peop(lambda e: e.matmul(PS[6][:, 0:128], lhsT=E4r[:], rhs=CTE[:, cq, 0, :], start=True, stop=False), [ekr + 'p', ekr + 'a', 'CTE'], ['ps6'])
                        peop(lambda e: e.matmul(PS[6][:, 0:128], lhsT=E4i[:], rhs=CTE[:, cq, 1, :], start=False, stop=True), [eki + 'p', eki + 'a', 'CTE'], ['ps6'])
                        vop(lambda e: e.tensor_tensor(out=KM[:, cq, k, :], in0=PS[6][:, 0:128], in1=bdm[:], op=OP.mult), ['ps6', 'bdm'], ['KM'])
                for i_ in range(2):
                    vop(lambda e: e.memset(E4rs[i_][:], 0.0), [], ['E4r%dp' % i_, 'E4r%da' % i_])
                    vop(lambda e: e.memset(E4is[i_][:], 0.0), [], ['E4i%dp' % i_, 'E4i%da' % i_])
                emit_VB(0)
                for k in range(TC):
                    if k + 1 < TC:
                        emit_VB(k + 1)
                    emit_E4(k)
                vop(lambda e: e.memset(WY[:], 0.0), [], ['WYp', 'WYa'])
                for j in range(TC):
                    w0, w1, w2, w3 = wts[j % 2]
                    w0k, w1k, w2k, w3k = ['wt%d_%d' % (i_, j % 2) for i_ in range(4)]
                    kpr, kpi = sk(PR(j + 1)), sk(PI(j + 1))
                    vop(lambda e: e.tensor_tensor(out=q3(w0), in0=cti, in1=bq(PI(j + 1)), op=OP.mult), ['bc', kpi], [w0k])
                    vop(lambda e: e.tensor_tensor(out=q3(w1), in0=ctr, in1=bq(PR(j + 1)), op=OP.mult), ['bc', kpr], [w1k])
                    vop(lambda e: e.tensor_tensor(out=q3(w2), in0=cti, in1=bq(PR(j + 1)), op=OP.mult), ['bc', kpr], [w2k])
                    vop(lambda e: e.tensor_tensor(out=q3(w3), in0=ctr, in1=bq(PI(j + 1)), op=OP.mult), ['bc', kpi], [w3k])
                    vop(lambda e: e.tensor_tensor(out=w1[:], in0=w1[:], in1=w0[:], op=OP.subtract), [w1k, w0k], [w1k])
                    vop(lambda e: e.tensor_tensor(out=w3[:], in0=w3[:], in1=w2[:], op=OP.add), [w3k, w2k], [w3k])
                    for g2 in range(2):
                        hsl = slice(g2 * 64, (g2 + 1) * 64)
                        vop(lambda e: e.tensor_copy(out=WY[hsl, :, j, 0, g2 * 16:(g2 + 1) * 16], in_=q3(w1)[hsl, :, :]), [w1k], ['WYp'])
                        aop(lambda e: e.activation(out=WY[hsl, :, j, 1, g2 * 16:(g2 + 1) * 16], in_=q3(w3)[hsl, :, :], func=AF.Identity, scale=-1.0), [w3k], ['WYa'])
                vop(lambda e: e.tensor_scalar(out=sm(Y4), in0=sm(Y0), scalar1=float(TC), scalar2=None, op0=OP.mult), [sk(Y0)], [sk(Y4)])
                for q in range(16):
                    vop(lambda e: e.tensor_scalar(out=tb6[:, 0, :], in0=kk[:], scalar1=smc(Y4, q), scalar2=None, op0=OP.mult), ['kk', sk(Y4)], ['tb0'])
                    vop(lambda e: e.tensor_scalar(out=tb6[:, 1, :], in0=tb6[:, 0, :], scalar1=MAGIC, scalar2=MAGIC, op0=OP.add, op1=OP.subtract), ['tb0'], ['tb1'])
                    vop(lambda e: e.tensor_tensor(out=tb6[:, 2, :], in0=tb6[:, 0, :], in1=tb6[:, 1, :], op=OP.subtract), ['tb0', 'tb1'], ['tb2'])
                    aop(lambda e: e.activation(out=tb6[:, 3, :], in_=tb6[:, 2, :], func=AF.Abs), ['tb2'], ['tb3'])
                    aop(lambda e: e.activation(out=sin2[:, q, :], in_=tb6[:, 2, :], func=AF.Sin, scale=TWO_PI), ['tb2'], ['sin2'])
                    aop(lambda e: e.activation(out=cos2[:, q, :], in_=tb6[:, 3, :], func=AF.Sin, scale=-TWO_PI, bias=halfpi[:, 0:1]), ['tb3', 'halfpi'], ['cos2'])
                    aop(lambda e: e.activation(out=R4rep[:, q, :], in_=kk[:], func=AF.Identity, scale=0.0, bias=r4[:, q:q + 1]), ['kk', 'r4'], ['R4rep'])
                vop(lambda e: e.memset(R4rep[:, :, 0:1], 0.0), ['R4rep'], ['R4rep'])
                S.barrier()
                S.emit()

            xT_bfs = [sb("xT_bf%d" % i, [128, 8, TS], BF16) for i in range(2)]
            u_bf = sb("u_bf", [128, 4, TS], BF16)
            um = sb("um", [128, TC - 1, TS], BF16)
            u_pm = sb("u_pm", [128, 4, TS], BF16)
            vbuf = sb("vbuf", [128, 4, 30 + TS], BF16)
            y_bf = sb("y_bf", [128, 4, TS], BF16)
            c_bf = sb("c_bf", [128, 4, TS], BF16)
            cs_bf = sb("cs_bf", [128, 4, TS], BF16)
            merged = sb("merged", [128, 8, TS], BF16)
            hb = sb("hb", [128, D], BF16)
            tri_bf = sb("tri_bf", [128, 128], BF16)
            ecap = sb("ecap_sb", [128, 32])
            selsum = sb("selsum", [128, 32], BF16)
            sel_bf = sb("sel_bf", [128, 32], BF16)
            NF = 9
            FT = [sb("f32t%d" % i, [128, TS]) for i in range(NF)]
            BT_ = [sb("bft%d" % i, [128, TS], BF16) for i in range(4)]
            xtk = sb("xtk", [128, D])
            zt = sb("zt", [128, D])
            hT32 = sb("hT32", [128, 8, 128])
            rsm = sb("rsm", [128, 512])
            stats = sb("stats", [128, 16])
            halo = sb("halo", [128, 4, 32], BF16)
            tmp4 = sb("tmp4", [128, 2, 4])
            CL = [sb("cl%d" % i, [128, TS]) for i in range(2)]
            S.dma('pool', lambda e: e.dma_start(out=tri_bf[:], in_=tri_d), writes=['tri'])
            S.dma('sp', lambda e: e.dma_start(out=ecap[:], in_=ecap_d), writes=['ecap'])
            vop(lambda e: e.memset(selsum[:], 0.0), [], ['selsum'])
            vop(lambda e: e.memset(um[:], 0.0), [], ['um'])

            bcol = lambda off, m: cols_t[:, off + m:off + m + 1]
            B_IN, B_GLU, B_COUT, DCOL, CONVB, LNCG, LNCB = 0, 28, 44, 52, 56, 60, 64
            mmrot = [0]
            NROT = 6

            def mmbank():
                b = mmrot[0]
                mmrot[0] = (mmrot[0] + 1) % NROT
                return b

            xcur = [None, None]

            def proj(m):
                b = mmbank()
                xT_bf, xkey = xcur
                for k in range(8):
                    peop(lambda e: e.matmul(PS[b][:, 0:TS], lhsT=win_bf[:, k, m * 128:(m + 1) * 128], rhs=xT_bf[:, k, :], start=(k == 0), stop=(k == 7)),
                         ['win', xkey], ['ps%d' % b])
                return b

            def load_xT(ti_):
                buf = xT_bfs[ti_ % 2]
                tt0 = ti_ * TS
                S.dma('pool', lambda e: e.dma_start(out=buf[:], in_=xT.rearrange("(k p) t -> p k t", p=128)[:, :, tt0:tt0 + TS]), writes=['xT%d' % (ti_ % 2)])

            TPS = SEQ // TS
            prev_tile = None
            for ti in range(NT):
                t0 = ti * TS
                first = (ti % TPS == 0)
                if ti == 0:
                    load_xT(0)
                xcur[0], xcur[1] = xT_bfs[ti % 2], 'xT%d' % (ti % 2)
                if ti + 1 < NT:
                    load_xT(ti + 1)
                if first:
                    pop(lambda e: e.memset(vbuf[:, :, 0:30], 0.0), [], ['vhalo'])
                    pop(lambda e: e.memset(carry[:], 0.0), [], ['carry'])

                for m in range(4):
                    b = proj(m)
                    aop(lambda e: e.activation(out=u_bf[:, m, :], in_=PS[b][:, 0:TS], func=AF.Identity, bias=bcol(B_IN, m)), ['ps%d' % b, 'cols'], ['u%d' % m])
                    aop(lambda e: e.activation(out=u_pm[:, m, :].rearrange("p (i c) -> p c i", i=TC), in_=PS[b][:, 0:TS].rearrange("p (c i) -> p c i", i=TC),
                                               func=AF.Identity, bias=bcol(B_IN, m)), ['ps%d' % b, 'cols'], ['up%d' % m])
                for c in range(4):
                    b = proj(8 + c)
                    aop(lambda e: e.activation(out=y_bf[:, c, :], in_=PS[b][:, 0:TS], func=AF.Sigmoid, bias=bcol(B_IN, 8 + c)), ['ps%d' % b, 'cols'], ['y%d' % c])
                for c in range(4):
                    b = proj(4 + c)
                    vop(lambda e: e.scalar_tensor_tensor(out=vbuf[:, c, 30:30 + TS], in0=PS[b][:, 0:TS], scalar=bcol(B_IN, 4 + c), in1=y_bf[:, c, :], op0=OP.add, op1=OP.mult),
                        ['ps%d' % b, 'cols', 'y%d' % c], ['v%d' % c])

                def sec_conv():
                    for c in range(4):
                        for j in range(CW):
                            if j % 4 != 3:
                                aop(lambda e: e.activation(out=cdiag[:, j, :], in_=ident[:], func=AF.Identity, scale=convw_t[:, c, j:j + 1]), ['ident', 'convw'], ['cd%d' % j])
                            else:
                                vop(lambda e: e.tensor_scalar(out=cdiag[:, j, :], in0=ident[:], scalar1=convw_t[:, c, j:j + 1], scalar2=None, op0=OP.mult), ['ident', 'convw'], ['cd%d' % j])
                        for j in range(CW):
                            peop(lambda e: e.matmul(PS[5][:, 0:TS], lhsT=cdiag[:, j, :], rhs=vbuf[:, c, j:j + TS], start=(j == 0), stop=(j == CW - 1)),
                                 ['cd%d' % j, 'v%d' % c, 'vhalo'], ['ps5'])
                        aop(lambda e: e.activation(out=c_bf[:, c, :], in_=PS[5][:, 0:TS], func=AF.Identity, bias=bcol(CONVB, c)), ['ps5', 'cols'], ['c_bf%d' % c])
                        aop(lambda e: e.activation(out=cs_bf[:, c, :], in_=PS[5][:, 0:TS], func=AF.Square, bias=bcol(CONVB, c)), ['ps5', 'cols'], ['cs%d' % c])
                        pop(lambda e: e.tensor_copy(out=halo[:, c, 0:30], in_=vbuf[:, c, TS:TS + 30]), ['v%d' % c], ['halo'])
                    for c in range(4):
                        peop(lambda e: e.matmul(PS[5][:, TS:2 * TS], lhsT=ones_bf[:], rhs=c_bf[:, c, :], start=(c == 0), stop=(c == 3)), ['ones', 'c_bf%d' % c], ['ps5'])
                    mean_t, rstd_t, msq_t, cn_t = CL[0], CL[1], CL[1], FT[8]
                    aop(lambda e: e.activation(out=mean_t[:], in_=PS[5][:, TS:2 * TS], func=AF.Identity, scale=1.0 / 512), ['ps5'], ['cl0'])
                    for c in range(4):
                        peop(lambda e: e.matmul(PS[5][:, TS:2 * TS], lhsT=ones_bf[:], rhs=cs_bf[:, c, :], start=(c == 0), stop=(c == 3)), ['ones', 'cs%d' % c], ['ps5'])
                    vop(lambda e: e.tensor_tensor(out=msq_t[:], in0=mean_t[:], in1=mean_t[:], op=OP.mult), ['cl0'], ['cl1'])
                    vop(lambda e: e.scalar_tensor_tensor(out=msq_t[:], in0=PS[5][:, TS:2 * TS], scalar=1.0 / 512, in1=msq_t[:], op0=OP.mult, op1=OP.subtract), ['ps5', 'cl1'], ['cl1'])
                    aop(lambda e: e.activation(out=msq_t[:], in_=msq_t[:], func=AF.Sqrt, bias=eps_t[:, 0:1]), ['cl1', 'eps'], ['cl1'])
                    vop(lambda e: e.reciprocal(out=rstd_t[:], in_=msq_t[:]), ['cl1'], ['cl1'])
                    for c in range(4):
                        pop(lambda e: e.tensor_tensor(out=cn_t[:], in0=c_bf[:, c, :], in1=mean_t[:], op=OP.subtract), ['c_bf%d' % c, 'cl0'], [fk(8)])
                        pop(lambda e: e.tensor_tensor(out=cn_t[:], in0=cn_t[:], in1=rstd_t[:], op=OP.mult), [fk(8), 'cl1'], [fk(8)])
                        aop(lambda e: e.activation(out=cs_bf[:, c, :], in_=cn_t[:], func=AF.Silu, scale=bcol(LNCG, c), bias=bcol(LNCB, c)), [fk(8), 'cols'], ['cs%d' % c])
                    for c in range(4):
                        pop(lambda e: e.tensor_copy(out=vbuf[:, c, 0:30], in_=halo[:, c, 0:30]), ['halo', 'v%d' % c], ['vhalo'])

                def sec_s5():
                    v3 = lambda t: t[:].rearrange("p (a c) -> p a c", a=4)
                    t1, t2, zre, zim, sre, sim = FT[0], FT[1], FT[2], FT[3], FT[4], FT[5]
                    k1, k2, kzr, kzi, ksr, ksi = fk(0), fk(1), fk(2), fk(3), fk(4), fk(5)

                    def PY(cq):
                        h_ = (cq % 2) if S5_HALF else 0
                        return PS[7 - h_][:, 0:TS], 'ps%d' % (7 - h_)

                    def SPb(cq, ri):
                        i_ = (cq % 2) * 2 + ri
                        return BT_[i_], bk(i_)

                    def stage_front(cq):
                        qs = slice(4 * cq, 4 * cq + 4)
                        C2 = cos2[:, qs, :].rearrange("p a c -> p (a c)")
                        S2 = sin2[:, qs, :].rearrange("p a c -> p (a c)")
                        RR = R4rep[:, qs, :].rearrange("p a c -> p (a c)")
                        ucq = u_bf[:, cq, :].rearrange("p (c i) -> p c i", i=TC)
                        for pl in range(4):
                            pr = slice(pl * 32, (pl + 1) * 32)
                            for ri in range(2):
                                for i in range(TC):
                                    ua = u_pm[pr, cq, i * NCH:(i + 1) * NCH]
                                    peop(lambda e: e.matmul(PS[3 + ri][:, pl * NCH:(pl + 1) * NCH], lhsT=WZ[pr, cq, i, ri, :], rhs=ua, start=(i == 0), stop=(i == TC - 1),
                                                            tile_position=(pl * 32, 0)), ['WZ', 'up%d' % cq], ['ps%d' % (3 + ri)])
                        for d in range(1, TC):
                            pop(lambda e: e.tensor_copy(out=um[:, d - 1, :].rearrange("p (c i) -> p c i", i=TC)[:, :, d:TC], in_=ucq[:, :, 0:TC - d]), ['u%d' % cq, 'um'], ['um%d' % d])
                        py, pyk = PY(cq)
                        for d in range(TC):
                            rhs_ = u_bf[:, cq, :] if d == 0 else um[:, d - 1, :]
                            peop(lambda e: e.matmul(py, lhsT=KM[:, cq, d, :], rhs=rhs_, start=(d == 0), stop=False), ['KM', 'u%d' % cq] + (['um%d' % d] if d else []), [pyk])
                        vop(lambda e: e.tensor_tensor(out=t1[:], in0=PS[3][:, 0:TS], in1=C2, op=OP.mult), ['ps3', 'cos2'], [k1])
                        vop(lambda e: e.tensor_tensor(out=t2[:], in0=PS[4][:, 0:TS], in1=S2, op=OP.mult), ['ps4', 'sin2'], [k2])
                        pop(lambda e: e.tensor_tensor(out=zre[:], in0=t1[:], in1=t2[:], op=OP.add), [k1, k2], [kzr])
                        vop(lambda e: e.tensor_tensor(out=sre[:], in0=PS[4][:, 0:TS], in1=C2, op=OP.mult), ['ps4', 'cos2'], [ksr])
                        vop(lambda e: e.tensor_tensor(out=sim[:], in0=PS[3][:, 0:TS], in1=S2, op=OP.mult), ['ps3', 'sin2'], [ksi])
                        pop(lambda e: e.tensor_tensor(out=zim[:], in0=sre[:], in1=sim[:], op=OP.subtract), [ksr, ksi], [kzi])
                        for ri, (zt_, kz) in enumerate([(zre, kzr), (zim, kzi)]):
                            vop(lambda e: e.tensor_tensor(out=tmp4[:, ri, :], in0=carry[:, ri, qs], in1=r4[:, qs], op=OP.mult), ['carry', 'r4'], ['tmp4_%d' % ri])
                            vop(lambda e: e.tensor_tensor(out=v3(zt_)[:, :, 0], in0=v3(zt_)[:, :, 0], in1=tmp4[:, ri, :], op=OP.add), [kz, 'tmp4_%d' % ri], [kz])
                        vop(lambda e: e.tensor_tensor_scan(out=sre[:], data0=RR, data1=zre[:], initial=0.0, op0=OP.mult, op1=OP.add), ['R4rep', kzr], [ksr])
                        vop(lambda e: e.tensor_tensor_scan(out=sim[:], data0=RR, data1=zim[:], initial=0.0, op0=OP.mult, op1=OP.add), ['R4rep', kzi], [ksi])
                        pop(lambda e: e.tensor_tensor(out=t1[:], in0=sre[:], in1=C2, op=OP.mult), [ksr, 'cos2'], [k1])
                        pop(lambda e: e.tensor_tensor(out=t2[:], in0=sim[:], in1=S2, op=OP.mult), [ksi, 'sin2'], [k2])
                        pop(lambda e: e.tensor_tensor(out=zre[:], in0=t1[:], in1=t2[:], op=OP.subtract), [k1, k2], [kzr])
                        vop(lambda e: e.tensor_tensor(out=t1[:], in0=sre[:], in1=S2, op=OP.mult), [ksr, 'sin2', kzr], [k1])
                        vop(lambda e: e.tensor_tensor(out=t2[:], in0=sim[:], in1=C2, op=OP.mult), [ksi, 'cos2', kzr], [k2])
                        vop(lambda e: e.tensor_tensor(out=zim[:], in0=t1[:], in1=t2[:], op=OP.add), [k1, k2], [kzi])
                        for ri, (zt_, kz) in enumerate([(zre, kzr), (zim, kzi)]):
                            spb, spk = SPb(cq, ri)
                            sp3 = spb[:].rearrange("p (a c) -> p a c", a=4)
                            aop(lambda e: e.activation(out=sp3[:, :, 1:NCH], in_=v3(zt_)[:, :, 0:NCH - 1], func=AF.Identity), [kz], [spk])
                            aop(lambda e: e.activation(out=sp3[:, :, 0], in_=carry[:, ri, qs], func=AF.Identity), ['carry'], [spk])
                            aop(lambda e: e.activation(out=carry[:, ri, qs], in_=v3(zt_)[:, :, NCH - 1], func=AF.Identity), [kz, spk], ['carry'])

                    def stage_back(cq):
                        py, pyk = PY(cq)
                        for pl in range(4):
                            q = 4 * cq + pl
                            pr = slice(pl * 32, (pl + 1) * 32)
                            for j in range(TC):
                                oj = py[pr, :].rearrange("p (c j) -> p j c", j=TC)[:, j, :]
                                for ri in range(2):
                                    spb, spk = SPb(cq, ri)
                                    peop(lambda e: e.matmul(oj, lhsT=WY[:, q, j, ri, :], rhs=spb[:, pl * NCH:(pl + 1) * NCH], start=False, stop=(ri == 1), tile_position=(0, pl * 32)),
                                         ['WY', spk], [pyk])
                        ys, g1 = FT[6], FT[7]
                        kA, kB = fk(6), fk(7)
                        vop(lambda e: e.scalar_tensor_tensor(out=ys[:], in0=u_bf[:, cq, :], scalar=bcol(DCOL, cq), in1=py, op0=OP.mult, op1=OP.add),
                            ['u%d' % cq, 'cols', pyk], [kA])
                        aop(lambda e: e.activation(out=g1[:], in_=ys[:], func=AF.Square), [kA], [kB])
                        vop(lambda e: e.tensor_scalar(out=g1[:], in0=g1[:], scalar1=0.044715, scalar2=1.0, op0=OP.mult, op1=OP.add), [kB], [kB])
                        vop(lambda e: e.tensor_tensor(out=g1[:], in0=g1[:], in1=ys[:], op=OP.mult), [kB, kA], [kB])
                        aop(lambda e: e.activation(out=g1[:], in_=g1[:], func=AF.Sigmoid, scale=1.5957691216057308), [kB], [kB])
                        vop(lambda e: e.tensor_tensor(out=y_bf[:, cq, :], in0=g1[:], in1=ys[:], op=OP.mult), [kB, kA], ['y%d' % cq])

                    if S5_DELAY:
                        stage_front(0)
                        for cq in range(1, 4):
                            stage_front(cq)
                            stage_back(cq - 1)
                        stage_back(3)
                    else:
                        for cq in range(4):
                            stage_front(cq)
                            stage_back(cq)

                def sec_ln(ti, t0):
                    for st in range(TS // 128):
                        r0 = t0 + st * 128
                        gst = ti * (TS // 128) + st
                        S.dma('sp', lambda e: e.dma_start(out=xtk[:], in_=xtok[r0:r0 + 128, :]), writes=['xtk'])
                        for hf in range(2):
                            hs = slice(hf * 512, (hf + 1) * 512)
                            b = hf
                            for k in range(8):
                                peop(lambda e: e.matmul(PS[b][:], lhsT=merged[:, k, st * 128:(st + 1) * 128], rhs=wout_bf[:, k, hs], start=(k == 0), stop=(k == 7)),
                                     ['wout', 'mg%d' % k], ['ps%d' % b], 0.28)
                            vop(lambda e: e.scalar_tensor_tensor(out=zt[:, hs], in0=xtk[:, hs], scalar=ALPHA, in1=PS[b][:], op0=OP.mult, op1=OP.add), ['xtk', 'ps%d' % b], ['zt%d' % hf], 0.6)
                            vop(lambda e: e.tensor_tensor(out=zt[:, hs], in0=zt[:, hs], in1=repA[:, 0, hs], op=OP.add), ['zt%d' % hf, 'rep'], ['zt%d' % hf], 0.6)
                            vop(lambda e: e.bn_stats(out=stats[:, hf * 6:(hf + 1) * 6], in_=zt[:, hs]), ['zt%d' % hf], ['bst%d' % hf], 0.65)
                        vop(lambda e: e.bn_aggr(out=stats[:, 12:14], in_=stats[:, 0:12]), ['bst0', 'bst1'], ['mv'])
                        aop(lambda e: e.activation(out=stats[:, 14:15], in_=stats[:, 13:14], func=AF.Sqrt, bias=eps_t[:, 0:1]), ['mv', 'eps'], ['sd'])
                        vop(lambda e: e.reciprocal(out=stats[:, 15:16], in_=stats[:, 14:15]), ['sd'], ['rs'])
                        vop(lambda e: e.tensor_scalar(out=zt[:], in0=zt[:], scalar1=stats[:, 12:13], scalar2=stats[:, 15:16], op0=OP.subtract, op1=OP.mult),
                            ['zt0', 'zt1', 'mv', 'rs'], ['zt0', 'zt1'], 0.85)
                        vop(lambda e: e.tensor_tensor(out=zt[:], in0=zt[:], in1=repA[:, 1, :], op=OP.mult), ['zt0', 'zt1', 'rep'], ['zt0', 'zt1'], 1.1)
                        vop(lambda e: e.tensor_tensor(out=zt[:], in0=zt[:], in1=repA[:, 2, :], op=OP.add), ['zt0', 'zt1', 'rep'], ['zt0', 'zt1'], 1.1)
                        aop(lambda e: e.activation(out=xtk[:], in_=zt[:], func=AF.Identity, scale=ALPHA), ['zt0', 'zt1', 'xtk'], ['xtk'], 1.06)
                        S.dma('sp', lambda e: e.dma_start(out=zb_d[r0:r0 + 128, :], in_=xtk[:]), reads=['xtk'], writes=['zb_d'])
                        if DEBUG_H:
                            S.dma('sp', lambda e: e.dma_start(out=out[r0:r0 + 128, :], in_=zt[:]), reads=['zt0', 'zt1'], writes=['out'])
                        for kb in range(2):
                            for kk4 in range(4):
                                k = kb * 4 + kk4
                                peop(lambda e: e.transpose(out=PS[2][:, kk4 * 128:(kk4 + 1) * 128], in_=zt[:, k * 128:(k + 1) * 128], identity=ident[:]),
                                     ['zt0', 'zt1', 'ident'], ['ps2'], 0.41)
                            aop(lambda e: e.activation(out=hT32[:, kb * 4:(kb + 1) * 4, :], in_=PS[2][:].rearrange("p (a b) -> p a b", a=4), func=AF.Identity), ['ps2'], ['hT32'], 0.5)
                        for k in range(8):
                            peop(lambda e: e.matmul(PS[2][:, 0:36], lhsT=hT32[:, k, :], rhs=wrt32[:, k, :], start=(k == 0), stop=(k == 7)), ['hT32', 'wrt'], ['ps2'], 0.27)
                        svop = lambda fn, r, w: vop(fn, r, w, 0.14)
                        R = lambda a, b: rsm[:, 192 + a:192 + b]
                        lg, zem, sel, ex, top8 = rsm[:, 0:36], rsm[:, 40:72], rsm[:, 72:104], rsm[:, 104:136], rsm[:, 136:144]
                        svop(lambda e: e.tensor_tensor(out=lg, in0=PS[2][:, 0:36], in1=brt_t[:], op=OP.add), ['ps2', 'brt'], ['lg'])
                        svop(lambda e: e.tensor_reduce(out=R(0, 1), in_=rsm[:, 0:4], axis=AX.X, op=OP.max), ['lg'], ['gmax'])
                        svop(lambda e: e.tensor_scalar(out=R(4, 8), in0=rsm[:, 0:4], scalar1=R(0, 1), scalar2=None, op0=OP.is_ge), ['lg', 'gmax'], ['ohg'])
                        svop(lambda e: e.tensor_scalar(out=R(1, 2), in0=R(0, 1), scalar1=-1.0, scalar2=None, op0=OP.mult), ['gmax'], ['ngmax'])
                        aop(lambda e: e.activation(out=R(8, 12), in_=rsm[:, 0:4], func=AF.Exp, bias=R(1, 2)), ['lg', 'ngmax'], ['eg'])
                        svop(lambda e: e.tensor_reduce(out=R(2, 3), in_=R(8, 12), axis=AX.X, op=OP.add), ['eg'], ['gsum'])
                        svop(lambda e: e.reciprocal(out=R(2, 3), in_=R(2, 3)), ['gsum'], ['gsum'])
                        svop(lambda e: e.tensor_scalar(out=R(12, 16), in0=R(4, 8), scalar1=-1.0, scalar2=1e30, op0=OP.add, op1=OP.mult), ['ohg'], ['pen'])
                        for g in range(4):
                            svop(lambda e: e.tensor_scalar(out=rsm[:, 40 + g * 8:48 + g * 8], in0=rsm[:, 4 + g * 8:12 + g * 8], scalar1=R(12 + g, 13 + g), scalar2=None, op0=OP.add),
                                ['lg', 'pen'], ['zem'])
                        svop(lambda e: e.max(out=top8, in_=zem), ['zem'], ['top8'])
                        svop(lambda e: e.tensor_scalar(out=sel, in0=zem, scalar1=rsm[:, 137:138], scalar2=None, op0=OP.is_ge), ['zem', 'top8'], ['sel'])
                        svop(lambda e: e.tensor_scalar(out=R(3, 4), in0=rsm[:, 136:137], scalar1=-1.0, scalar2=None, op0=OP.mult), ['top8'], ['nv1'])
                        aop(lambda e: e.activation(out=ex, in_=zem, func=AF.Exp, bias=R(3, 4)), ['zem', 'nv1'], ['ex'])
                        svop(lambda e: e.tensor_tensor(out=ex, in0=ex, in1=sel, op=OP.mult), ['ex', 'sel'], ['ex'])
                        svop(lambda e: e.tensor_reduce(out=R(16, 17), in_=ex, axis=AX.X, op=OP.add), ['ex'], ['den'])
                        svop(lambda e: e.reciprocal(out=R(16, 17), in_=R(16, 17)), ['den'], ['den'])
                        svop(lambda e: e.tensor_tensor(out=R(16, 17), in0=R(16, 17), in1=R(2, 3), op=OP.mult), ['den', 'gsum'], ['den'])
                        gt, oh0, oh1, pos, tmpr, ovf = (rsm[:, 256:288], rsm[:, 288:320], rsm[:, 320:352], rsm[:, 352:384], rsm[:, 384:416], rsm[:, 416:448])
                        X = lambda i: rsm[:, 448 + i:449 + i]
                        svop(lambda e: e.tensor_scalar(out=gt, in0=ex, scalar1=R(16, 17), scalar2=None, op0=OP.mult), ['ex', 'den'], ['gt'])
                        svop(lambda e: e.tensor_scalar(out=oh0, in0=zem, scalar1=rsm[:, 136:137], scalar2=None, op0=OP.is_ge), ['zem', 'top8'], ['oh0'])
                        svop(lambda e: e.tensor_tensor(out=oh1, in0=sel, in1=oh0, op=OP.subtract), ['sel', 'oh0'], ['oh1'])
                        svop(lambda e: e.tensor_copy(out=sel_bf[:], in_=sel), ['sel'], ['sel_bf'])
                        peop(lambda e: e.matmul(PS[2][:, 64:96], lhsT=tri_bf[:], rhs=sel_bf[:], start=True, stop=False), ['tri', 'sel_bf'], ['ps2'])
                        peop(lambda e: e.matmul(PS[2][:, 64:96], lhsT=ones_bf[:], rhs=selsum[:], start=False, stop=True), ['ones', 'selsum'], ['ps2'])
                        svop(lambda e: e.tensor_tensor(out=selsum[:], in0=selsum[:], in1=sel, op=OP.add), ['selsum', 'sel'], ['selsum'])
                        svop(lambda e: e.tensor_tensor(out=pos, in0=PS[2][:, 64:96], in1=ecap[:], op=OP.add), ['ps2', 'ecap'], ['pos'])
                        svop(lambda e: e.tensor_scalar(out=ovf, in0=PS[2][:, 64:96], scalar1=float(CAP), scalar2=1e6, op0=OP.is_ge, op1=OP.mult), ['ps2'], ['ovf'])
                        svop(lambda e: e.tensor_tensor(out=pos, in0=pos, in1=ovf, op=OP.add), ['pos', 'ovf'], ['pos'])
                        for kq, ohk in enumerate([oh0, oh1]):
                            okey = 'oh%d' % kq
                            svop(lambda e: e.tensor_tensor(out=tmpr, in0=ohk, in1=pos, op=OP.mult), [okey, 'pos'], ['tmpr'])
                            svop(lambda e: e.tensor_reduce(out=X(kq), in_=tmpr, axis=AX.X, op=OP.add), ['tmpr'], ['dx%d' % kq])
                            svop(lambda e: e.tensor_copy(out=dest_all[:, 2 * gst + kq:2 * gst + kq + 1], in_=X(kq)), ['dx%d' % kq], ['dest%d_%d' % (gst, kq)])
                            svop(lambda e: e.tensor_tensor(out=tmpr, in0=ohk, in1=gt, op=OP.mult), [okey, 'gt'], ['tmpr'])
                            svop(lambda e: e.tensor_reduce(out=gsel_all[:, gst, kq:kq + 1], in_=tmpr, axis=AX.X, op=OP.add), ['tmpr'], ['gsel'])
                        aop(lambda e: e.activation(out=hb[:].rearrange("t (k p) -> t k p", p=128), in_=zt[:].rearrange("t (p k) -> t k p", k=8), func=AF.Identity), ['zt0', 'zt1'], ['hb'], 1.0)
                        for kq in range(2):
                            S.dma('pool', lambda e: e.indirect_dma_start(out=xbuf_d, out_offset=bass.IndirectOffsetOnAxis(ap=dest_all[:, 2 * gst + kq:2 * gst + kq + 1], axis=0), in_=hb[:], in_offset=None,
                                                                         bounds_check=bcreg(e, 'A'), oob_is_err=False), reads=['hb', 'dest%d_%d' % (gst, kq)], writes=['xbuf'])
                chains = [S.record(sec_conv), S.record(sec_s5)]
                if prev_tile is not None:
                    pt = prev_tile
                    chains.insert(0, S.record(lambda: sec_ln(*pt)))
                S.replay_merged(chains)
                for m in range(8):
                    b = proj(20 + m)
                    aop(lambda e: e.activation(out=BT_[3][:], in_=PS[b][:, 0:TS], func=AF.Sigmoid, bias=bcol(B_IN, 20 + m)), ['ps%d' % b, 'cols'], [bk(3)])
                    b2 = mmbank()
                    for k in range(4):
                        peop(lambda e: e.matmul(PS[b2][:, 0:TS], lhsT=wcout_bf[:, k, m * 128:(m + 1) * 128], rhs=cs_bf[:, k, :], start=(k == 0), stop=(k == 3)),
                             ['wcout', 'cs%d' % k], ['ps%d' % b2])
                    vop(lambda e: e.scalar_tensor_tensor(out=merged[:, m, :], in0=PS[b2][:, 0:TS], scalar=bcol(B_COUT, m), in1=BT_[3][:], op0=OP.add, op1=OP.mult),
                        ['ps%d' % b2, 'cols', bk(3)], ['mg%d' % m])

                for m in range(8):
                    b = proj(12 + m)
                    aop(lambda e: e.activation(out=BT_[3][:], in_=PS[b][:, 0:TS], func=AF.Sigmoid, bias=bcol(B_IN, 12 + m)), ['ps%d' % b, 'cols'], [bk(3)])
                    bg = mmbank()
                    for k in range(4):
                        peop(lambda e: e.matmul(PS[bg][:, 0:TS], lhsT=wglu_bf[:, k, (8 + m) * 128:(9 + m) * 128], rhs=y_bf[:, k, :], start=(k == 0), stop=(k == 3)),
                             ['wglu', 'y%d' % k], ['ps%d' % bg])
                    aop(lambda e: e.activation(out=BT_[2][:], in_=PS[bg][:, 0:TS], func=AF.Sigmoid, bias=bcol(B_GLU, 8 + m)), ['ps%d' % bg, 'cols'], [bk(2)])
                    bv = mmbank()
                    for k in range(4):
                        peop(lambda e: e.matmul(PS[bv][:, 0:TS], lhsT=wglu_bf[:, k, m * 128:(m + 1) * 128], rhs=y_bf[:, k, :], start=(k == 0), stop=(k == 3)),
                             ['wglu', 'y%d' % k], ['ps%d' % bv])
                    vop(lambda e: e.scalar_tensor_tensor(out=FT[8][:], in0=PS[bv][:, 0:TS], scalar=bcol(B_GLU, m), in1=BT_[2][:], op0=OP.add, op1=OP.mult),
                        ['ps%d' % bv, 'cols', bk(2)], [fk(8)])
                    pop(lambda e: e.tensor_tensor(out=FT[8][:], in0=FT[8][:], in1=BT_[3][:], op=OP.mult), [fk(8), bk(3)], [fk(8)])
                    pop(lambda e: e.tensor_tensor(out=merged[:, m, :], in0=FT[8][:], in1=merged[:, m, :], op=OP.add), [fk(8), 'mg%d' % m], ['mg%d' % m])

                prev_tile = (ti, t0)
                if ti == NT - 1:
                    S.replay_merged([S.record(lambda: sec_ln(ti, t0))])

            S.barrier()
            S.emit()

        if not DEBUG_H:
            with ExitStack() as es:
                def sb(name, shape, dt=F32):
                    return es.enter_context(nc.sbuf_tensor(name, list(shape), dt))
                PS = [es.enter_context(nc.psum_tensor("pb%d" % i, [128, 512], F32)) for i in range(6)]
                PT = [es.enter_context(nc.psum_tensor("pt%d" % i, [128, 1024], BF16)) for i in range(2)]
                wg = [sb("wg%d" % i, [128, 8, 256], BF16) for i in range(2)]
                wu = [sb("wu%d" % i, [128, 8, 256], BF16) for i in range(2)]
                wd = [sb("wd%d" % i, [128, 2, D], BF16) for i in range(2)]
                xrows = [sb("xrows%d" % i, [128, NS4, D], BF16) for i in range(2)]
                xTe = sb("xTe", [128, 8, CAP], BF16)
                sgt = sb("sgt", [128, 2, CAP], BF16)
                hid = sb("hid", [128, 2, CAP], BF16)
                ysb = [sb("ysb%d" % i, [128, NS4, D], BF16) for i in range(2)]
                identb = sb("identb", [128, 128], BF16)
                NACC = 4
                statb = sb("statsb", [128, 24 * NACC])
                repB = sb("repB", [128, 2, D])
                acc = [sb("acc%d" % i, [128, D]) for i in range(NACC)]
                yg = [[sb("yg%d_%d" % (i, k), [128, D], BF16) for k in range(2)] for i in range(NACC)]
                dg = [[sb("dg%d_%d" % (i, k), [128, 128], BF16) for k in range(2)] for i in range(NACC)]
                S.dma('sp', lambda e: e.dma_start(out=repB[:], in_=rep5v[:, 3:5, :]), writes=['repB'])
                S.dma('pool', lambda e: e.dma_start(out=identb[:], in_=ident_d), writes=['identb'])
                for i in range(NACC):
                    for k in range(2):
                        pop(lambda e: e.memset(yg[i][k][:], 0.0), [], ['yg%d_%d' % (i, k)])
                rot = [0]

                def bank(n=6):
                    b = rot[0]
                    rot[0] = (rot[0] + 1) % n
                    return b
                def load_expert(ex_j):
                    wj = ex_j % 2
                    xrj = xrows[wj]
                    S.dma('sp', lambda e: e.dma_start(out=xrj[:], in_=xbuf_d[ex_j * CAP:(ex_j + 1) * CAP, :].rearrange("(s p) d -> p s d", p=128)), reads=['xbuf'], writes=['xr%d' % wj])
                    S.dma('pool', lambda e: e.dma_start(out=wg[wj][:], in_=w_gate[ex_j].rearrange("(p k) f -> p k f", k=8)), writes=['wg%d' % wj])
                    S.dma('pool', lambda e: e.dma_start(out=wu[wj][:], in_=w_up[ex_j].rearrange("(p k) f -> p k f", k=8)), writes=['wu%d' % wj])
                    S.dma('pool', lambda e: e.dma_start(out=wd[wj][:], in_=w_down[ex_j].rearrange("(k p) f -> p k f", p=128)), writes=['wd%d' % wj])
                load_expert(0)
                for ex_i in range(32):
                    wi = ex_i % 2
                    xr = xrows[wi]
                    ys = ysb[wi]
                    if ex_i + 1 < 32:
                        load_expert(ex_i + 1)
                    for s4 in range(NS4):
                        pt = PT[s4 % 2]
                        for k in range(8):
                            peop(lambda e: e.transpose(out=pt[:, k * 128:(k + 1) * 128], in_=xr[:, s4, k * 128:(k + 1) * 128], identity=identb[:]), ['xr%d' % wi, 'identb'], ['pt%d' % (s4 % 2)])
                        ev = aop if s4 % 2 == 0 else vop
                        ev(lambda e: e.tensor_copy(out=xTe[:, :, s4 * 128:(s4 + 1) * 128], in_=pt[:].rearrange("p (k r) -> p k r", k=8)) if False else
                           (e.activation(out=xTe[:, :, s4 * 128:(s4 + 1) * 128], in_=pt[:].rearrange("p (k r) -> p k r", k=8), func=AF.Identity) if s4 % 2 == 0 else
                            e.tensor_copy(out=xTe[:, :, s4 * 128:(s4 + 1) * 128], in_=pt[:].rearrange("p (k r) -> p k r", k=8))),
                           ['pt%d' % (s4 % 2)], ['xTe%d' % s4])
                    xk = ['xTe%d' % i for i in range(NS4)]
                    for f in range(2):
                        bgk = bank()
                        for k in range(8):
                            peop(lambda e: e.matmul(PS[bgk][:, 0:CAP], lhsT=wg[wi][:, k, f * 128:(f + 1) * 128], rhs=xTe[:, k, :], start=(k == 0), stop=(k == 7)), ['wg%d' % wi] + xk, ['pb%d' % bgk])
                        aop(lambda e: e.activation(out=sgt[:, f, :], in_=PS[bgk][:, 0:CAP], func=AF.Silu), ['pb%d' % bgk], ['sgt%d' % f])
                        buk = bank()
                        for k in range(8):
                            peop(lambda e: e.matmul(PS[buk][:, 0:CAP], lhsT=wu[wi][:, k, f * 128:(f + 1) * 128], rhs=xTe[:, k, :], start=(k == 0), stop=(k == 7)), ['wu%d' % wi] + xk, ['pb%d' % buk])
                        vop(lambda e: e.tensor_tensor(out=hid[:, f, :], in0=PS[buk][:, 0:CAP], in1=sgt[:, f, :], op=OP.mult), ['pb%d' % buk, 'sgt%d' % f], ['hid%d' % f])
                    for s4 in range(NS4):
                        for hf in range(2):
                            hs = slice(hf * 512, (hf + 1) * 512)
                            b = bank()
                            for f in range(2):
                                peop(lambda e: e.matmul(PS[b][:], lhsT=hid[:, f, s4 * 128:(s4 + 1) * 128], rhs=wd[wi][:, f, hs], start=(f == 0), stop=(f == 1)),
                                     ['hid0', 'hid1', 'wd%d' % wi], ['pb%d' % b])
                            if (s4 * 2 + hf) % 2 == 0:
                                aop(lambda e: e.activation(out=ys[:, s4, hs], in_=PS[b][:], func=AF.Identity), ['pb%d' % b], ['ysa%d' % wi])
                            else:
                                vop(lambda e: e.tensor_copy(out=ys[:, s4, hs], in_=PS[b][:]), ['pb%d' % b], ['ysv%d' % wi])
                    S.dma('sp', lambda e: e.dma_start(out=ybuf_d[ex_i * CAP:(ex_i + 1) * CAP, :].rearrange("(s p) d -> p s d", p=128), in_=ys[:]), reads=['ysa%d' % wi, 'ysv%d' % wi], writes=['ybuf%d' % ex_i])
                ybk = ['ybuf%d' % i for i in range(32)]
                NSUB = TOK // 128

                def comb_load(st):
                    r0 = st * 128
                    pi_ = st % NACC
                    a_ = acc[pi_]
                    S.dma('sp', lambda e: e.dma_start(out=a_[:], in_=zb_d[r0:r0 + 128, :]), reads=['zb_d'], writes=['accb%d' % pi_])
                    for kq in range(2):
                        g = yg[pi_][kq]
                        S.dma('pool', lambda e: e.indirect_dma_start(out=g[:], out_offset=None, in_=ybuf_d, in_offset=bass.IndirectOffsetOnAxis(ap=dest_all[:, 2 * st + kq:2 * st + kq + 1], axis=0),
                                                                     bounds_check=bcreg(e, 'B'), oob_is_err=False), reads=ybk, writes=['yg%d_%d' % (pi_, kq)])
                def comb_compute(st):
                    r0 = st * 128
                    pi_ = st % NACC
                    a_ = acc[pi_]
                    ak = 'accb%d' % pi_
                    sb_ = statb[:, pi_ * 24:pi_ * 24 + 24]
                    sk_ = 'stb%d' % pi_
                    for kq in range(2):
                        dgk = dg[pi_][kq]
                        aop(lambda e: e.activation(out=dgk[:], in_=identb[:], func=AF.Identity, scale=gsel_all[:, st, kq:kq + 1]), ['identb', 'gsel'], ['dg%d_%d' % (pi_, kq)], 0.2)
                    for hf in range(2):
                        hs = slice(hf * 512, (hf + 1) * 512)
                        b = (st % 2) * 2 + hf
                        for kq in range(2):
                            g = yg[pi_][kq]
                            dgk = dg[pi_][kq]
                            peop(lambda e: e.matmul(PS[b][:], lhsT=dgk[:], rhs=g[:, hs], start=(kq == 0), stop=(kq == 1)),
                                 ['dg%d_%d' % (pi_, kq), 'yg%d_%d' % (pi_, kq)], ['pb%d' % b], 0.28)
                        vop(lambda e: e.tensor_tensor(out=a_[:, hs], in0=a_[:, hs], in1=PS[b][:], op=OP.add), ['pb%d' % b, ak], [ak], 0.6)
                    for hf in range(2):
                        vop(lambda e: e.bn_stats(out=sb_[:, hf * 6:(hf + 1) * 6], in_=a_[:, hf * 512:(hf + 1) * 512]), [ak], [sk_ + 'b%d' % hf], 0.65)
                    vop(lambda e: e.bn_aggr(out=sb_[:, 12:14], in_=sb_[:, 0:12]), [sk_ + 'b0', sk_ + 'b1'], [sk_ + 'mv'], 0.2)
                    aop(lambda e: e.activation(out=sb_[:, 14:15], in_=sb_[:, 13:14], func=AF.Sqrt, bias=eps_t[:, 0:1]), [sk_ + 'mv', 'eps'], [sk_ + 'sd'], 0.3)
                    vop(lambda e: e.reciprocal(out=sb_[:, 15:16], in_=sb_[:, 14:15]), [sk_ + 'sd'], [sk_ + 'rs'], 0.16)
                    vop(lambda e: e.scalar_tensor_tensor(out=sb_[:, 16:17], in0=sb_[:, 12:13], scalar=-1.0, in1=sb_[:, 15:16], op0=OP.mult, op1=OP.mult), [sk_ + 'mv', sk_ + 'rs'], [sk_ + 'nb'], 0.1)
                    aop(lambda e: e.activation(out=a_[:], in_=a_[:], func=AF.Identity, scale=sb_[:, 15:16], bias=sb_[:, 16:17]), [ak, sk_ + 'rs', sk_ + 'nb'], [ak], 1.1)
                    vop(lambda e: e.tensor_tensor(out=a_[:], in0=a_[:], in1=repB[:, 0, :], op=OP.mult), [ak, 'repB'], [ak], 1.1)
                    pop(lambda e: e.tensor_tensor(out=a_[:, 0:512], in0=a_[:, 0:512], in1=repB[:, 1, 0:512], op=OP.add), [ak, 'repB'], [ak + 'p'], 1.2)
                    vop(lambda e: e.tensor_tensor(out=a_[:, 512:1024], in0=a_[:, 512:1024], in1=repB[:, 1, 512:1024], op=OP.add), [ak, 'repB'], [ak + 'v'], 0.6)
                    S.dma('sp', lambda e: e.dma_start(out=out[r0:r0 + 128, :], in_=a_[:]), reads=[ak, ak + 'p', ak + 'v'], writes=['out'])

                comb_load(0)
                comb_load(1)
                for st0 in range(0, NSUB, 2):
                    for st in (st0 + 2, st0 + 3):
                        if st < NSUB:
                            comb_load(st)
                    S.replay_merged([S.record(lambda: comb_compute(st0)), S.record(lambda: comb_compute(st0 + 1))])
                S.barrier()
                S.emit()
    return nc


def _prep(inp):
    f = lambda a: np.ascontiguousarray(a, dtype=np.float32)
    colmat = lambda v: f(np.asarray(v).reshape(-1, 128).T)
    cols = np.concatenate([colmat(inp['b_in'][0]), colmat(inp['b_glu'][0]), colmat(inp['b_cout'][0]), colmat(inp['ssm_d'][0]),
                           colmat(inp['conv_b'][0]), colmat(inp['ln_c_g'][0]), colmat(inp['ln_c_b'][0])], axis=1)
    assert cols.shape == (128, 68)
    cols = f(np.concatenate([cols, np.zeros((128, 4), np.float32)], axis=1))
    cw = inp['conv_w'][0][:, 0, :]
    convw = f(cw.T.reshape(4, 128, CW).transpose(1, 0, 2).reshape(128, 4 * CW))
    rep5 = f(np.concatenate([np.broadcast_to(inp[k][0][None, :], (128, D)) for k in ['b_out', 'ln1_g', 'ln1_b', 'ln2_g', 'ln2_b']], axis=1))
    w_rt = f(np.concatenate([inp['w_route_group'][0], inp['w_route_expert'][0]], axis=1))
    brt = f(np.broadcast_to(np.concatenate([inp['b_route_group'][0], inp['b_route_expert'][0]])[None, :], (128, 36)))
    pqf = lambda a: a.reshape(16, 2, 64).transpose(1, 2, 0).reshape(128, 16)
    ldt = np.broadcast_to(inp['log_dt'][0].reshape(16, 2, 1), (16, 2, 64))
    pq = f(np.concatenate([pqf(inp['lam_re'][0]), pqf(inp['lam_im'][0]), pqf(np.ascontiguousarray(ldt))], axis=1))
    bf_ = lambda a: a.reshape(16, 2, 64, 16).transpose(1, 2, 0, 3).reshape(128, 256)
    cf_ = lambda a: a.reshape(16, 2, 16, 64).transpose(1, 3, 0, 2).reshape(128, 256)
    bc = f(np.concatenate([bf_(inp['ssm_b_re'][0]), bf_(inp['ssm_b_im'][0]), cf_(inp['ssm_c_re'][0]), cf_(inp['ssm_c_im'][0])], axis=1))
    shared = {
        'w_in': f(inp['w_in'][0]), 'w_glu': f(inp['w_glu'][0]), 'w_cout': f(inp['w_cout'][0]), 'w_out': f(inp['w_out'][0]),
        'w_rt': w_rt, 'w_gate': f(inp['w_gate'][0]), 'w_up': f(inp['w_up'][0]), 'w_down': f(inp['w_down'][0]),
        'cols': cols, 'convw': convw, 'rep5': rep5, 'brt': brt, 'pq': pq, 'bc': bc,
        'ident': np.eye(128, dtype=np.float32),
        'tri': np.triu(np.ones((128, 128), np.float32), 1),
        'ecap': f(np.broadcast_to((np.arange(32, dtype=np.float32) * CAP)[None, :], (128, 32))),
        'kk': f(np.broadcast_to(np.arange(1, TS + 1, dtype=np.float32)[None, :], (128, TS))),
        'bdmask': f(np.kron(np.eye(4, dtype=np.float32), np.ones((32, 32), np.float32))),
    }
    x = inp['x']
    maps = []
    for c in range(NCORES):
        xc = f(x[2 * c:2 * c + 2].reshape(TOK, D))
        m = dict(shared)
        m['xtok'] = xc
        m['xT'] = f(xc.T)
        maps.append(m)
    return maps


def kernel(**inputs):
    maps = _prep(inputs)
    nc = build_nc()
    res = run_bass_kernel_spmd(nc, maps, core_ids=list(range(NCORES)))
    outs = [np.asarray(r['out']).reshape(2, SEQ, D) for r in res.results]
    return np.concatenate(outs, axis=0).astype(np.float32)
```

```python
import math
import numpy as np
import concourse.bass as bass
import concourse.mybir as mybir
from concourse.bass_utils import run_bass_kernel_spmd
from contextlib import ExitStack

F32 = mybir.dt.float32
BF16 = mybir.dt.bfloat16
AF = mybir.ActivationFunctionType
OP = mybir.AluOpType
AX = mybir.AxisListType

NCORES = 8
D = 1024
SEQ = 2048
TOK = 4096
TS = 256
NT = TOK // TS
CW = 31
ALPHA = 2.0 ** 0.25
EPS = 1e-5
MAGIC = 12582912.0
TWO_PI = 2.0 * math.pi
DEBUG_H = False
S5_HALF = True
S5_DELAY = True
CAP = 384
NS4 = CAP // 128
I32 = mybir.dt.int32


import types


def _freeze(fn):
    if fn.__closure__ is None:
        return fn
    cells = []
    for c in fn.__closure__:
        try:
            cells.append(types.CellType(c.cell_contents))
        except ValueError:
            cells.append(c)
    return types.FunctionType(fn.__code__, fn.__globals__, fn.__name__, fn.__defaults__, tuple(cells))


class Sched:
    def __init__(self, nc, es, ndma=14):
        self.nc = nc
        self.names = ['pe', 'act', 'dve', 'pool', 'sp']
        self.prog = {k: [] for k in self.names}
        self.sem = {k: es.enter_context(nc.semaphore('s_' + k)) for k in ['pe', 'act', 'dve', 'pool']}
        self.cnt = {k: 0 for k in self.sem}
        self.dsem = [es.enter_context(nc.semaphore('d%d' % i)) for i in range(ndma)]
        self.dcnt = [0] * ndma
        self.dnext = 0
        self.seen = {k: {} for k in self.names}
        self.lastw = {}
        self.readers = {}
        self.rec = None
        self.sim_eng = {}
        self.sim_w = {}
        self.sim_r = {}

    def record(self, section):
        self.rec = []
        section()
        r = self.rec
        self.rec = None
        return r

    COST = {'pe': 0.17, 'act': 0.42, 'dve': 0.36, 'pool': 0.75, 'sp': 0.1}

    def replay_merged(self, chains):
        pos = [0] * len(chains)
        now = max(self.sim_eng.values()) if self.sim_eng else 0.0
        for k in self.names:
            self.sim_eng[k] = now
        while True:
            best, bkey, bt = None, None, 0.0
            for i, c in enumerate(chains):
                if pos[i] >= len(c):
                    continue
                kind, eng, fn, reads, writes, cost = c[pos[i]]
                t = self.sim_eng[eng]
                for r in reads:
                    t = max(t, self.sim_w.get(r, 0.0))
                for w in writes:
                    t = max(t, self.sim_w.get(w, 0.0), self.sim_r.get(w, 0.0))
                key = (t, pos[i] / len(c))
                if best is None or key < bkey:
                    best, bkey, bt = i, key, t
            if best is None:
                break
            kind, eng, fn, reads, writes, cost = chains[best][pos[best]]
            pos[best] += 1
            if kind == 'op':
                end = bt + (cost if cost is not None else self.COST[eng])
                self.sim_eng[eng] = end
                done = end + 0.06
                self.op(eng, fn, reads, writes)
            else:
                self.sim_eng[eng] = bt + (1.0 if eng == 'pool' else 0.1)
                done = bt + 2.5
                self.dma(eng, fn, reads, writes)
            for r in reads:
                self.sim_r[r] = max(self.sim_r.get(r, 0.0), done)
            for w in writes:
                self.sim_w[w] = done
                self.sim_r[w] = 0.0

    def _semobj(self, k):
        return self.sem[k] if isinstance(k, str) else self.dsem[k]

    def _waits(self, eng, deps):
        best = {}
        for (k, v) in deps:
            if k == 'pe' and eng == 'pe':
                continue
            if self.seen[eng].get(k, 0) >= v:
                continue
            best[k] = max(best.get(k, 0), v)
        for k, v in best.items():
            self.seen[eng][k] = v
            so = self._semobj(k)
            self.prog[eng].append(lambda e, so=so, v=v: e.wait_ge(so, v))

    def _deps(self, reads, writes):
        deps = []
        for r in reads:
            if r in self.lastw:
                deps.append(self.lastw[r])
        for w in writes:
            if w in self.lastw:
                deps.append(self.lastw[w])
            deps.extend(self.readers.get(w, []))
        return deps

    def _commit(self, tok, reads, writes):
        for r in reads:
            self.readers.setdefault(r, []).append(tok)
        for w in writes:
            self.lastw[w] = tok
            self.readers[w] = []

    def op(self, eng, fn, reads=(), writes=(), cost=None):
        fn = _freeze(fn)
        if self.rec is not None:
            self.rec.append(('op', eng, fn, tuple(reads), tuple(writes), cost))
            return
        self._waits(eng, self._deps(reads, writes))
        self.cnt[eng] += 1
        so = self.sem[eng]
        self.prog[eng].append(lambda e, fn=fn, so=so: fn(e).then_inc(so, 1))
        self._commit((eng, self.cnt[eng]), reads, writes)

    def dma(self, eng, fn, reads=(), writes=()):
        fn = _freeze(fn)
        if self.rec is not None:
            self.rec.append(('dma', eng, fn, tuple(reads), tuple(writes), None))
            return
        i = self.dnext
        self.dnext = (self.dnext + 1) % len(self.dsem)
        deps = self._deps(reads, writes)
        if self.dcnt[i] > 0:
            deps.append((i, self.dcnt[i]))
        self._waits(eng, deps)
        self.dcnt[i] += 16
        so = self.dsem[i]
        self.prog[eng].append(lambda e, fn=fn, so=so: fn(e).then_inc(so, 16))
        self._commit((i, self.dcnt[i]), reads, writes)

    def barrier(self):
        deps = [(k, v) for k, v in self.cnt.items() if v > 0]
        deps += [(i, v) for i, v in enumerate(self.dcnt) if v > 0]
        for eng in self.names:
            self._waits(eng, deps)

    def emit(self):
        nc = self.nc
        prog = self.prog
        with nc.Block() as block:
            @block.tensor
            def _(e):
                for f in prog['pe']:
                    f(e)

            @block.scalar
            def _(e):
                for f in prog['act']:
                    f(e)

            @block.vector
            def _(e):
                for f in prog['dve']:
                    f(e)

            @block.gpsimd
            def _(e):
                for f in prog['pool']:
                    f(e)

            @block.sync
            def _(e):
                for f in prog['sp']:
                    f(e)
        self.prog = {k: [] for k in self.names}


def build_nc():
    nc = bass.Bass("TRN2", target_bir_lowering=False)

    def din(name, shape):
        return nc.dram_tensor(name, list(shape), F32, kind="ExternalInput").ap()

    xT = din("xT", [D, TOK])
    xtok = din("xtok", [TOK, D])
    w_in = din("w_in", [D, 3584])
    w_glu = din("w_glu", [512, 2048])
    w_cout = din("w_cout", [512, 1024])
    w_out = din("w_out", [D, D])
    w_rt = din("w_rt", [D, 36])
    w_gate = din("w_gate", [32, D, 256])
    w_up = din("w_up", [32, D, 256])
    w_down = din("w_down", [32, 256, D])
    cols = din("cols", [128, 72])
    convw = din("convw", [128, 4 * CW])
    rep5 = din("rep5", [128, 5 * D])
    brt = din("brt", [128, 36])
    pq = din("pq", [128, 48])
    bc = din("bc", [128, 4 * 256])
    ident_d = din("ident", [128, 128])
    kk_d = din("kk", [128, TS])
    out = nc.dram_tensor("out", [TOK, D], F32, kind="ExternalOutput").ap()
    tri_d = din("tri", [128, 128])
    ecap_d = din("ecap", [128, 32])
    bdmask_d = din("bdmask", [128, 128])
    xbuf_d = nc.dram_tensor("xbuf_scr", [32 * CAP, D], BF16, kind="Internal").ap()
    ybuf_d = nc.dram_tensor("ybuf_scr", [32 * CAP, D], BF16, kind="Internal").ap()
    zb_d = nc.dram_tensor("zb_scr", [TOK, D], F32, kind="Internal").ap()
    rep5v = rep5.rearrange("p (a d) -> p a d", a=5)

    with ExitStack() as es0:
        S = Sched(nc, es0)
        dest_all = es0.enter_context(nc.sbuf_tensor("dest_all", [128, 64], I32))
        gsel_all = es0.enter_context(nc.sbuf_tensor("gsel_all", [128, 32, 2], F32))
        eps_t = es0.enter_context(nc.sbuf_tensor("eps_t", [128, 1], F32))
        S.op('dve', lambda e: e.memset(eps_t[:], EPS), writes=['eps'])
        regh = {}

        def bcreg(e, tag):
            if tag not in regh:
                regh[tag] = e.alloc_register('bcr' + tag)
                e.reg_mov(regh[tag], 32 * CAP - 1)
            return regh[tag]
        vop = lambda fn, r, w, c=None: S.op('dve', fn, reads=r, writes=w, cost=c)
        aop = lambda fn, r, w, c=None: S.op('act', fn, reads=r, writes=w, cost=c)
        pop = lambda fn, r, w, c=None: S.op('pool', fn, reads=r, writes=w, cost=c)
        peop = lambda fn, r, w, c=None: S.op('pe', fn, reads=r, writes=w, cost=c)

        with ExitStack() as es:
            def sb(name, shape, dt=F32):
                return es.enter_context(nc.sbuf_tensor(name, list(shape), dt))
            PS = [es.enter_context(nc.psum_tensor("ps%d" % i, [128, 512], F32)) for i in range(8)]

            win_bf = sb("win_bf", [128, 8, 3584], BF16)
            wglu_bf = sb("wglu_bf", [128, 4, 2048], BF16)
            wcout_bf = sb("wcout_bf", [128, 4, 1024], BF16)
            wout_bf = sb("wout_bf", [128, 8, 1024], BF16)
            wrt32 = sb("wrt32", [128, 8, 36])
            cols_t = sb("cols_t", [128, 72])
            convw_t = sb("convw_t", [128, 4, CW])
            brt_t = sb("brt_t", [128, 36])
            ident = sb("ident_sb", [128, 128])
            ones_bf = sb("ones_bf", [128, 128], BF16)
            halfpi = sb("halfpi", [128, 1])
            repA = sb("repA", [128, 3, D])
            cdiag = sb("cdiag", [128, CW, 128], BF16)
            TC = 4
            NCH = TS // TC
            WZ = sb("WZ", [128, 4, TC, 2, 128], BF16)
            KM = sb("KM", [128, 4, TC, 128], BF16)
            WY = sb("WY", [128, 16, TC, 2, 32], BF16)
            cos2 = sb("cos2", [128, 16, NCH])
            sin2 = sb("sin2", [128, 16, NCH])
            R4rep = sb("R4rep", [128, 16, NCH])
            r4 = sb("r4", [128, 16])
            carry = sb("carry", [128, 2, 16])
            fk = lambda i: 'f%d' % i
            bk = lambda i: 'b%d' % i

            S.dma('sp', lambda e: e.dma_start(out=repA[:], in_=rep5v[:, 0:3, :]), writes=['rep'])
            S.dma('sp', lambda e: e.dma_start(out=wrt32[:], in_=w_rt.rearrange("(k p) c -> p k c", p=128)), writes=['wrt'])
            S.dma('sp', lambda e: e.dma_start(out=cols_t[:], in_=cols), writes=['cols'])
            S.dma('sp', lambda e: e.dma_start(out=convw_t[:], in_=convw.rearrange("p (c j) -> p c j", c=4)), writes=['convw'])
            S.dma('sp', lambda e: e.dma_start(out=brt_t[:], in_=brt), writes=['brt'])
            S.dma('sp', lambda e: e.dma_start(out=ident[:], in_=ident_d), writes=['ident'])
            vop(lambda e: e.memset(halfpi[:], math.pi / 2), [], ['halfpi'])
            vop(lambda e: e.memset(ones_bf[:], 1.0), [], ['ones'])

            with ExitStack() as esS:
                def sbs(name, shape, dt=F32):
                    return esS.enter_context(nc.sbuf_tensor(name, list(shape), dt))
                pq_t = sbs("pq_t", [128, 3, 16])
                bc_t = sbs("bc_t", [128, 4, 16, 16])
                kk = sbs("kk_sb", [128, NCH])
                NSL = 30
                small = sbs("small", [128, NSL * 16])
                vbrs = [sbs("vbr%d" % i, [128, 256]) for i in range(2)]
                vbis = [sbs("vbi%d" % i, [128, 256]) for i in range(2)]
                tmpbs = [sbs("tmpb%d" % i, [128, 256]) for i in range(2)]
                tmpas = [sbs("tmpa%d" % i, [128, 256]) for i in range(2)]
                E4rs = [sbs("E4r%d" % i, [128, 128]) for i in range(2)]
                E4is = [sbs("E4i%d" % i, [128, 128]) for i in range(2)]
                CTE = sbs("CTE", [128, 4, 2, 128])
                wts = [[sbs("wt%d_%d" % (i, jj), [128, 256]) for i in range(4)] for jj in range(2)]
                bdm = sbs("bdm", [128, 128])
                S.dma('sp', lambda e: e.dma_start(out=bdm[:], in_=bdmask_d), writes=['bdm'])
                tbw = sbs("tbw", [128, 2, 16 * NCH])
                ptmp = sbs("ptmp", [128, 3, 16 * TC])
                S.dma('sp', lambda e: e.dma_start(out=pq_t[:], in_=pq.rearrange("p (a q) -> p a q", a=3)), writes=['pq'])
                S.dma('sp', lambda e: e.dma_start(out=bc_t[:], in_=bc.rearrange("p (a q h) -> p a q h", a=4, q=16)), writes=['bc'])
                S.dma('sp', lambda e: e.dma_start(out=kk[:], in_=kk_d[:, 0:NCH]), writes=['kk'])
                for k in range(8):
                    S.dma('pool', lambda e, k=k: e.dma_start(out=win_bf[:, k, :], in_=w_in[k * 128:(k + 1) * 128, :]), writes=['win'])
                    S.dma('pool', lambda e, k=k: e.dma_start(out=wout_bf[:, k, :], in_=w_out[k * 128:(k + 1) * 128, :]), writes=['wout'])
                for k in range(4):
                    S.dma('pool', lambda e, k=k: e.dma_start(out=wglu_bf[:, k, :], in_=w_glu[k * 128:(k + 1) * 128, :]), writes=['wglu'])
                    S.dma('pool', lambda e, k=k: e.dma_start(out=wcout_bf[:, k, :], in_=w_cout[k * 128:(k + 1) * 128, :]), writes=['wcout'])
                sm = lambda i: small[:, i * 16:(i + 1) * 16]
                smc = lambda i, q: small[:, i * 16 + q:i * 16 + q + 1]
                sk = lambda i: 'sm%d' % i
                DT, RHO, TH, Y0, DEN, FR, FI, T1, T2, T3 = range(10)
                PR = lambda k: 10 + k
                PI = lambda k: 15 + k
                GR = lambda k: 20 + k
                GI = lambda k: 24 + k
                Y4 = 28
                lre, lim, ldt = pq_t[:, 0, :], pq_t[:, 1, :], pq_t[:, 2, :]
                TT = lambda o, a, b, op: vop(lambda e: e.tensor_tensor(out=sm(o), in0=sm(a), in1=sm(b), op=op), [sk(a), sk(b)], [sk(o)])
                aop(lambda e: e.activation(out=sm(DT), in_=ldt, func=AF.Exp), ['pq'], [sk(DT)])
                vop(lambda e: e.tensor_tensor(out=sm(RHO), in0=lre, in1=sm(DT), op=OP.mult), ['pq', sk(DT)], [sk(RHO)])
                vop(lambda e: e.tensor_tensor(out=sm(TH), in0=lim, in1=sm(DT), op=OP.mult), ['pq', sk(DT)], [sk(TH)])
                vop(lambda e: e.tensor_scalar(out=sm(Y0), in0=sm(TH), scalar1=1.0 / TWO_PI, scalar2=None, op0=OP.mult), [sk(TH)], [sk(Y0)])
                vop(lambda e: e.memset(sm(PR(0)), 1.0), [], [sk(PR(0))])
                vop(lambda e: e.memset(sm(PI(0)), 0.0), [], [sk(PI(0))])
                P4 = lambda base: small[:, base * 16:(base + TC) * 16]
                pt = lambda i: ptmp[:, i, :]
                prk = [sk(PR(k)) for k in range(1, TC + 1)]
                pik = [sk(PI(k)) for k in range(1, TC + 1)]
                for k in range(1, TC + 1):
                    aop(lambda e: e.activation(out=pt(2)[:, (k - 1) * 16:k * 16], in_=sm(RHO), func=AF.Exp, scale=float(k)), [sk(RHO)], ['pt2'])
                    vop(lambda e: e.tensor_scalar(out=pt(0)[:, (k - 1) * 16:k * 16], in0=sm(Y0), scalar1=float(k), scalar2=None, op0=OP.mult), [sk(Y0)], ['pt0'])
                vop(lambda e: e.tensor_scalar(out=pt(1), in0=pt(0), scalar1=MAGIC, scalar2=MAGIC, op0=OP.add, op1=OP.subtract), ['pt0'], ['pt1'])
                vop(lambda e: e.tensor_tensor(out=pt(0), in0=pt(0), in1=pt(1), op=OP.subtract), ['pt0', 'pt1'], ['pt0'])
                aop(lambda e: e.activation(out=pt(1), in_=pt(0), func=AF.Abs), ['pt0'], ['pt1'])
                aop(lambda e: e.activation(out=P4(PI(1)), in_=pt(0), func=AF.Sin, scale=TWO_PI), ['pt0'], pik)
                aop(lambda e: e.activation(out=P4(PR(1)), in_=pt(1), func=AF.Sin, scale=-TWO_PI, bias=halfpi[:, 0:1]), ['pt1', 'halfpi'], prk)
                vop(lambda e: e.tensor_tensor(out=P4(PR(1)), in0=P4(PR(1)), in1=pt(2), op=OP.mult), prk + ['pt2'], prk)
                vop(lambda e: e.tensor_tensor(out=P4(PI(1)), in0=P4(PI(1)), in1=pt(2), op=OP.mult), pik + ['pt2'], pik)
                vop(lambda e: e.tensor_copy(out=r4[:], in_=pt(2)[:, (TC - 1) * 16:TC * 16]), ['pt2'], ['r4'])
                vop(lambda e: e.tensor_tensor(out=sm(T1), in0=lre, in1=lre, op=OP.mult), ['pq'], [sk(T1)])
                vop(lambda e: e.tensor_tensor(out=sm(T2), in0=lim, in1=lim, op=OP.mult), ['pq'], [sk(T2)])
                TT(DEN, T1, T2, OP.add)
                vop(lambda e: e.reciprocal(out=sm(DEN), in_=sm(DEN)), [sk(DEN)], [sk(DEN)])
                vop(lambda e: e.tensor_scalar(out=sm(T1), in0=sm(PR(1)), scalar1=-1.0, scalar2=None, op0=OP.add), [sk(PR(1))], [sk(T1)])
                vop(lambda e: e.tensor_tensor(out=sm(T2), in0=sm(T1), in1=lre, op=OP.mult), [sk(T1), 'pq'], [sk(T2)])
                vop(lambda e: e.tensor_tensor(out=sm(T3), in0=sm(PI(1)), in1=lim, op=OP.mult), [sk(PI(1)), 'pq'], [sk(T3)])
                TT(T2, T2, T3, OP.add)
                TT(FR, T2, DEN, OP.mult)
                vop(lambda e: e.tensor_tensor(out=sm(T2), in0=sm(PI(1)), in1=lre, op=OP.mult), [sk(PI(1)), 'pq'], [sk(T2)])
                vop(lambda e: e.tensor_tensor(out=sm(T3), in0=sm(T1), in1=lim, op=OP.mult), [sk(T1), 'pq'], [sk(T3)])
                TT(T2, T2, T3, OP.subtract)
                TT(FI, T2, DEN, OP.mult)
                for k in range(TC):
                    TT(T1, PR(k), FR, OP.mult)
                    TT(T2, PI(k), FI, OP.mult)
                    TT(GR(k), T1, T2, OP.subtract)
                    TT(T1, PR(k), FI, OP.mult)
                    TT(T2, PI(k), FR, OP.mult)
                    TT(GI(k), T1, T2, OP.add)
                bq = lambda i: sm(i).unsqueeze(2).to_broadcast([128, 16, 16])
                q3 = lambda t: t[:].rearrange("p (q h) -> p q h", h=16)
                bre, bim, ctr, cti = bc_t[:, 0, :, :], bc_t[:, 1, :, :], bc_t[:, 2, :, :], bc_t[:, 3, :, :]
                vop(lambda e: e.memset(CTE[:], 0.0), [], ['CTE'])
                for cq in range(4):
                    for g2 in range(2):
                        hsl = slice(g2 * 64, (g2 + 1) * 64)
                        o0 = CTE[hsl, cq, 0, :].rearrange("p (pl c) -> p pl c", c=32)[:, :, g2 * 16:(g2 + 1) * 16]
                        o1 = CTE[hsl, cq, 1, :].rearrange("p (pl c) -> p pl c", c=32)[:, :, g2 * 16:(g2 + 1) * 16]
                        vop(lambda e: e.tensor_copy(out=o0, in_=bc_t[hsl, 2, cq * 4:(cq + 1) * 4, :]), ['bc'], ['CTE'])
                        vop(lambda e: e.tensor_scalar(out=o1, in0=bc_t[hsl, 3, cq * 4:(cq + 1) * 4, :], scalar1=-1.0, scalar2=None, op0=OP.mult), ['bc'], ['CTE'])

                def emit_VB(k):
                    vbr, vbi, tmpa, tmpb = vbrs[k % 2], vbis[k % 2], tmpas[k % 2], tmpbs[k % 2]
                    kvr, kvi, kta, ktb = 'vbr%d' % (k % 2), 'vbi%d' % (k % 2), 'tmpa%d' % (k % 2), 'tmpb%d' % (k % 2)
                    vop(lambda e: e.tensor_tensor(out=q3(tmpa), in0=bim, in1=bq(GI(k)), op=OP.mult), ['bc', sk(GI(k))], [kta])
                    vop(lambda e: e.tensor_tensor(out=q3(tmpb), in0=bre, in1=bq(GI(k)), op=OP.mult), ['bc', sk(GI(k))], [ktb])
                    vop(lambda e: e.tensor_tensor(out=q3(vbr), in0=bre, in1=bq(GR(k)), op=OP.mult), ['bc', sk(GR(k))], [kvr])
                    vop(lambda e: e.tensor_tensor(out=q3(vbi), in0=bim, in1=bq(GR(k)), op=OP.mult), ['bc', sk(GR(k))], [kvi])
                    vop(lambda e: e.tensor_tensor(out=vbr[:], in0=vbr[:], in1=tmpa[:], op=OP.subtract), [kvr, kta], [kvr])
                    vop(lambda e: e.tensor_tensor(out=vbi[:], in0=vbi[:], in1=tmpb[:], op=OP.add), [kvi, ktb], [kvi])

                def emit_E4(k):
                    vbr, vbi = vbrs[k % 2], vbis[k % 2]
                    kvr, kvi = 'vbr%d' % (k % 2), 'vbi%d' % (k % 2)
                    for cq in range(4):
                        E4r, E4i = E4rs[cq % 2], E4is[cq % 2]
                        ekr, eki = 'E4r%d' % (cq % 2), 'E4i%d' % (cq % 2)
                        for ri, (src, E4, ek, ksrc) in enumerate([(vbr, E4r, ekr, kvr), (vbi, E4i, eki, kvi)]):
                            for g2 in range(2):
                                hsl = slice(g2 * 64, (g2 + 1) * 64)
                                o_ = E4[hsl, :].rearrange("p (pl c) -> p pl c", c=32)[:, :, g2 * 16:(g2 + 1) * 16]
                                i_ = src[hsl, cq * 64:(cq + 1) * 64].rearrange("p (pl h) -> p pl h", h=16)
                                if g2 == 0:
                                    vop(lambda e: e.tensor_copy(out=o_, in_=i_), [ksrc], [ek + 'p'])
                                else:
                                    aop(lambda e: e.activation(out=o_, in_=i_, func=AF.Identity), [ksrc], [ek + 'a'])
                            peop(lambda e: e.transpose(out=PS[7][:, ri * 128:(ri + 1) * 128], in_=E4[:], identity=ident[:]), [ek + 'p', ek + 'a', 'ident'], ['ps7'])
                            aop(lambda e: e.activation(out=WZ[:, cq, TC - 1 - k, ri, :], in_=PS[7][:, ri * 128:(ri + 1) * 128], func=AF.Identity), ['ps7'], ['WZ'])
                        peop(lambda e: e.matmul(PS[6][:, 0:128], lhsT=E4r[:], rhs=CTE[:, cq, 0, :], start=True, stop=False), [ekr + 'p', ekr + 'a', 'CTE'], ['ps6'])
                        peop(lambda e: e.matmul(PS[6][:, 0:128], lhsT=E4i[:], rhs=CTE[:, cq, 1, :], start=False, stop=True), [eki + 'p', eki + 'a', 'CTE'], ['ps6'])
                        vop(lambda e: e.tensor_tensor(out=KM[:, cq, k, :], in0=PS[6][:, 0:128], in1=bdm[:], op=OP.mult), ['ps6', 'bdm'], ['KM'])
                for i_ in range(2):
                    vop(lambda e: e.memset(E4rs[i_][:], 0.0), [], ['E4r%dp' % i_, 'E4r%da' % i_])
                    vop(lambda e: e.memset(E4is[i_][:], 0.0), [], ['E4i%dp' % i_, 'E4i%da' % i_])
                emit_VB(0)
                for k in range(TC):
                    if k + 1 < TC:
                        emit_VB(k + 1)
                    emit_E4(k)
                vop(lambda e: e.memset(WY[:], 0.0), [], ['WYp', 'WYa'])
                for j in range(TC):
                    w0, w1, w2, w3 = wts[j % 2]
                    w0k, w1k, w2k, w3k = ['wt%d_%d' % (i_, j % 2) for i_ in range(4)]
                    kpr, kpi = sk(PR(j + 1)), sk(PI(j + 1))
                    vop(lambda e: e.tensor_tensor(out=q3(w0), in0=cti, in1=bq(PI(j + 1)), op=OP.mult), ['bc', kpi], [w0k])
                    vop(lambda e: e.tensor_tensor(out=q3(w1), in0=ctr, in1=bq(PR(j + 1)), op=OP.mult), ['bc', kpr], [w1k])
                    vop(lambda e: e.tensor_tensor(out=q3(w2), in0=cti, in1=bq(PR(j + 1)), op=OP.mult), ['bc', kpr], [w2k])
                    vop(lambda e: e.tensor_tensor(out=q3(w3), in0=ctr, in1=bq(PI(j + 1)), op=OP.mult), ['bc', kpi], [w3k])
                    vop(lambda e: e.tensor_tensor(out=w1[:], in0=w1[:], in1=w0[:], op=OP.subtract), [w1k, w0k], [w1k])
                    vop(lambda e: e.tensor_tensor(out=w3[:], in0=w3[:], in1=w2[:], op=OP.add), [w3k, w2k], [w3k])
                    for g2 in range(2):
                        hsl = slice(g2 * 64, (g2 + 1) * 64)
                        vop(lambda e: e.tensor_copy(out=WY[hsl, :, j, 0, g2 * 16:(g2 + 1) * 16], in_=q3(w1)[hsl, :, :]), [w1k], ['WYp'])
                        aop(lambda e: e.activation(out=WY[hsl, :, j, 1, g2 * 16:(g2 + 1) * 16], in_=q3(w3)[hsl, :, :], func=AF.Identity, scale=-1.0), [w3k], ['WYa'])
                vop(lambda e: e.tensor_scalar(out=sm(Y4), in0=sm(Y0), scalar1=float(TC), scalar2=None, op0=OP.mult), [sk(Y0)], [sk(Y4)])
                kkb = kk[:].unsqueeze(1).to_broadcast([128, 16, NCH])
                y4b = sm(Y4).unsqueeze(2).to_broadcast([128, 16, NCH])
                flat = lambda t: t[:].rearrange("p q c -> p (q c)")
                vop(lambda e: e.tensor_tensor(out=tbw[:, 0, :].rearrange("p (q c) -> p q c", c=NCH), in0=kkb, in1=y4b, op=OP.mult), ['kk', sk(Y4)], ['tb0'])
                vop(lambda e: e.tensor_scalar(out=tbw[:, 1, :], in0=tbw[:, 0, :], scalar1=MAGIC, scalar2=MAGIC, op0=OP.add, op1=OP.subtract), ['tb0'], ['tb1'])
                vop(lambda e: e.tensor_tensor(out=tbw[:, 0, :], in0=tbw[:, 0, :], in1=tbw[:, 1, :], op=OP.subtract), ['tb0', 'tb1'], ['tb0'])
                aop(lambda e: e.activation(out=tbw[:, 1, :], in_=tbw[:, 0, :], func=AF.Abs), ['tb0'], ['tb1'])
                aop(lambda e: e.activation(out=flat(sin2), in_=tbw[:, 0, :], func=AF.Sin, scale=TWO_PI), ['tb0'], ['sin2'])
                aop(lambda e: e.activation(out=flat(cos2), in_=tbw[:, 1, :], func=AF.Sin, scale=-TWO_PI, bias=halfpi[:, 0:1]), ['tb1', 'halfpi'], ['cos2'])
                vop(lambda e: e.tensor_copy(out=R4rep[:], in_=r4[:].unsqueeze(2).to_broadcast([128, 16, NCH])), ['r4'], ['R4rep'])
                vop(lambda e: e.memset(R4rep[:, :, 0:1], 0.0), ['R4rep'], ['R4rep'])
                S.barrier()
                S.emit()

            xT_bfs = [sb("xT_bf%d" % i, [128, 8, TS], BF16) for i in range(2)]
            u_bf = sb("u_bf", [128, 4, TS], BF16)
            um = sb("um", [128, TC - 1, TS], BF16)
            u_pm = sb("u_pm", [128, 4, TS], BF16)
            vbuf = sb("vbuf", [128, 4, 30 + TS], BF16)
            y_bf = sb("y_bf", [128, 4, TS], BF16)
            c_bf = sb("c_bf", [128, 4, TS], BF16)
            cs_bf = sb("cs_bf", [128, 4, TS], BF16)
            merged = sb("merged", [128, 8, TS], BF16)
            hb = sb("hb", [128, D], BF16)
            tri_bf = sb("tri_bf", [128, 128], BF16)
            ecap = sb("ecap_sb", [128, 32])
            selsum = sb("selsum", [128, 32], BF16)
            sel_bf = sb("sel_bf", [128, 32], BF16)
            NF = 9
            FT = [sb("f32t%d" % i, [128, TS]) for i in range(NF)]
            BT_ = [sb("bft%d" % i, [128, TS], BF16) for i in range(4)]
            xtk = sb("xtk", [128, D])
            zt = sb("zt", [128, D])
            hT32 = sb("hT32", [128, 8, 128])
            rsm = sb("rsm", [128, 512])
            stats = sb("stats", [128, 16])
            halo = sb("halo", [128, 4, 32], BF16)
            tmp4 = sb("tmp4", [128, 2, 4])
            CL = [sb("cl%d" % i, [128, TS]) for i in range(2)]
            S.dma('pool', lambda e: e.dma_start(out=tri_bf[:], in_=tri_d), writes=['tri'])
            S.dma('sp', lambda e: e.dma_start(out=ecap[:], in_=ecap_d), writes=['ecap'])
            vop(lambda e: e.memset(selsum[:], 0.0), [], ['selsum'])
            vop(lambda e: e.memset(um[:], 0.0), [], ['um'])

            bcol = lambda off, m: cols_t[:, off + m:off + m + 1]
            B_IN, B_GLU, B_COUT, DCOL, CONVB, LNCG, LNCB = 0, 28, 44, 52, 56, 60, 64
            mmrot = [0]
            NROT = 6

            def mmbank():
                b = mmrot[0]
                mmrot[0] = (mmrot[0] + 1) % NROT
                return b

            xcur = [None, None]

            def proj(m):
                b = mmbank()
                xT_bf, xkey = xcur
                for k in range(8):
                    peop(lambda e: e.matmul(PS[b][:, 0:TS], lhsT=win_bf[:, k, m * 128:(m + 1) * 128], rhs=xT_bf[:, k, :], start=(k == 0), stop=(k == 7)),
                         ['win', xkey], ['ps%d' % b])
                return b

            def load_xT(ti_):
                buf = xT_bfs[ti_ % 2]
                tt0 = ti_ * TS
                S.dma('pool', lambda e: e.dma_start(out=buf[:], in_=xT.rearrange("(k p) t -> p k t", p=128)[:, :, tt0:tt0 + TS]), writes=['xT%d' % (ti_ % 2)])

            TPS = SEQ // TS
            prev_tile = None
            for ti in range(NT):
                t0 = ti * TS
                first = (ti % TPS == 0)
                if ti == 0:
                    load_xT(0)
                xcur[0], xcur[1] = xT_bfs[ti % 2], 'xT%d' % (ti % 2)
                if ti + 1 < NT:
                    load_xT(ti + 1)
                if first:
                    pop(lambda e: e.memset(vbuf[:, :, 0:30], 0.0), [], ['vhalo'])
                    pop(lambda e: e.memset(carry[:], 0.0), [], ['carry'])

                for m in range(4):
                    b = proj(m)
                    aop(lambda e: e.activation(out=u_bf[:, m, :], in_=PS[b][:, 0:TS], func=AF.Identity, bias=bcol(B_IN, m)), ['ps%d' % b, 'cols'], ['u%d' % m])
                    aop(lambda e: e.activation(out=u_pm[:, m, :].rearrange("p (i c) -> p c i", i=TC), in_=PS[b][:, 0:TS].rearrange("p (c i) -> p c i", i=TC),
                                               func=AF.Identity, bias=bcol(B_IN, m)), ['ps%d' % b, 'cols'], ['up%d' % m])
                for c in range(4):
                    b = proj(8 + c)
                    aop(lambda e: e.activation(out=y_bf[:, c, :], in_=PS[b][:, 0:TS], func=AF.Sigmoid, bias=bcol(B_IN, 8 + c)), ['ps%d' % b, 'cols'], ['y%d' % c])
                for c in range(4):
                    b = proj(4 + c)
                    vop(lambda e: e.scalar_tensor_tensor(out=vbuf[:, c, 30:30 + TS], in0=PS[b][:, 0:TS], scalar=bcol(B_IN, 4 + c), in1=y_bf[:, c, :], op0=OP.add, op1=OP.mult),
                        ['ps%d' % b, 'cols', 'y%d' % c], ['v%d' % c])

                def sec_conv():
                    for c in range(4):
                        for j in range(CW):
                            if j % 4 != 3:
                                aop(lambda e: e.activation(out=cdiag[:, j, :], in_=ident[:], func=AF.Identity, scale=convw_t[:, c, j:j + 1]), ['ident', 'convw'], ['cd%d' % j])
                            else:
                                vop(lambda e: e.tensor_scalar(out=cdiag[:, j, :], in0=ident[:], scalar1=convw_t[:, c, j:j + 1], scalar2=None, op0=OP.mult), ['ident', 'convw'], ['cd%d' % j])
                        for j in range(CW):
                            peop(lambda e: e.matmul(PS[5][:, 0:TS], lhsT=cdiag[:, j, :], rhs=vbuf[:, c, j:j + TS], start=(j == 0), stop=(j == CW - 1)),
                                 ['cd%d' % j, 'v%d' % c, 'vhalo'], ['ps5'])
                        aop(lambda e: e.activation(out=c_bf[:, c, :], in_=PS[5][:, 0:TS], func=AF.Identity, bias=bcol(CONVB, c)), ['ps5', 'cols'], ['c_bf%d' % c])
                        aop(lambda e: e.activation(out=cs_bf[:, c, :], in_=PS[5][:, 0:TS], func=AF.Square, bias=bcol(CONVB, c)), ['ps5', 'cols'], ['cs%d' % c])
                        pop(lambda e: e.tensor_copy(out=halo[:, c, 0:30], in_=vbuf[:, c, TS:TS + 30]), ['v%d' % c], ['halo'])
                    for c in range(4):
                        peop(lambda e: e.matmul(PS[5][:, TS:2 * TS], lhsT=ones_bf[:], rhs=c_bf[:, c, :], start=(c == 0), stop=(c == 3)), ['ones', 'c_bf%d' % c], ['ps5'])
                    mean_t, rstd_t, msq_t, cn_t = CL[0], CL[1], CL[1], FT[8]
                    aop(lambda e: e.activation(out=mean_t[:], in_=PS[5][:, TS:2 * TS], func=AF.Identity, scale=1.0 / 512), ['ps5'], ['cl0'])
                    for c in range(4):
                        peop(lambda e: e.matmul(PS[5][:, TS:2 * TS], lhsT=ones_bf[:], rhs=cs_bf[:, c, :], start=(c == 0), stop=(c == 3)), ['ones', 'cs%d' % c], ['ps5'])
                    vop(lambda e: e.tensor_tensor(out=msq_t[:], in0=mean_t[:], in1=mean_t[:], op=OP.mult), ['cl0'], ['cl1'])
                    vop(lambda e: e.scalar_tensor_tensor(out=msq_t[:], in0=PS[5][:, TS:2 * TS], scalar=1.0 / 512, in1=msq_t[:], op0=OP.mult, op1=OP.subtract), ['ps5', 'cl1'], ['cl1'])
                    aop(lambda e: e.activation(out=msq_t[:], in_=msq_t[:], func=AF.Sqrt, bias=eps_t[:, 0:1]), ['cl1', 'eps'], ['cl1'])
                    vop(lambda e: e.reciprocal(out=rstd_t[:], in_=msq_t[:]), ['cl1'], ['cl1'])
                    for c in range(4):
                        pop(lambda e: e.tensor_tensor(out=cn_t[:], in0=c_bf[:, c, :], in1=mean_t[:], op=OP.subtract), ['c_bf%d' % c, 'cl0'], [fk(8)])
                        pop(lambda e: e.tensor_tensor(out=cn_t[:], in0=cn_t[:], in1=rstd_t[:], op=OP.mult), [fk(8), 'cl1'], [fk(8)])
                        aop(lambda e: e.activation(out=cs_bf[:, c, :], in_=cn_t[:], func=AF.Silu, scale=bcol(LNCG, c), bias=bcol(LNCB, c)), [fk(8), 'cols'], ['cs%d' % c])
                    for c in range(4):
                        pop(lambda e: e.tensor_copy(out=vbuf[:, c, 0:30], in_=halo[:, c, 0:30]), ['halo', 'v%d' % c], ['vhalo'])

                def sec_s5():
                    v3 = lambda t: t[:].rearrange("p (a c) -> p a c", a=4)
                    t1, t2, zre, zim, sre, sim = FT[0], FT[1], FT[2], FT[3], FT[4], FT[5]
                    k1, k2, kzr, kzi, ksr, ksi = fk(0), fk(1), fk(2), fk(3), fk(4), fk(5)

                    def PY(cq):
                        h_ = (cq % 2) if S5_HALF else 0
                        return PS[7 - h_][:, 0:TS], 'ps%d' % (7 - h_)

                    def SPb(cq, ri):
                        i_ = (cq % 2) * 2 + ri
                        return BT_[i_], bk(i_)

                    def stage_front(cq):
                        qs = slice(4 * cq, 4 * cq + 4)
                        C2 = cos2[:, qs, :].rearrange("p a c -> p (a c)")
                        S2 = sin2[:, qs, :].rearrange("p a c -> p (a c)")
                        RR = R4rep[:, qs, :].rearrange("p a c -> p (a c)")
                        ucq = u_bf[:, cq, :].rearrange("p (c i) -> p c i", i=TC)
                        for pl in range(4):
                            pr = slice(pl * 32, (pl + 1) * 32)
                            for ri in range(2):
                                for i in range(TC):
                                    ua = u_pm[pr, cq, i * NCH:(i + 1) * NCH]
                                    peop(lambda e: e.matmul(PS[3 + ri][:, pl * NCH:(pl + 1) * NCH], lhsT=WZ[pr, cq, i, ri, :], rhs=ua, start=(i == 0), stop=(i == TC - 1),
                                                            tile_position=(pl * 32, 0)), ['WZ', 'up%d' % cq], ['ps%d' % (3 + ri)])
                        for d in range(1, TC):
                            pop(lambda e: e.tensor_copy(out=um[:, d - 1, :].rearrange("p (c i) -> p c i", i=TC)[:, :, d:TC], in_=ucq[:, :, 0:TC - d]), ['u%d' % cq, 'um'], ['um%d' % d])
                        py, pyk = PY(cq)
                        for d in range(TC):
                            rhs_ = u_bf[:, cq, :] if d == 0 else um[:, d - 1, :]
                            peop(lambda e: e.matmul(py, lhsT=KM[:, cq, d, :], rhs=rhs_, start=(d == 0), stop=False), ['KM', 'u%d' % cq] + (['um%d' % d] if d else []), [pyk])
                        vop(lambda e: e.tensor_tensor(out=t1[:], in0=PS[3][:, 0:TS], in1=C2, op=OP.mult), ['ps3', 'cos2'], [k1])
                        vop(lambda e: e.tensor_tensor(out=t2[:], in0=PS[4][:, 0:TS], in1=S2, op=OP.mult), ['ps4', 'sin2'], [k2])
                        pop(lambda e: e.tensor_tensor(out=zre[:], in0=t1[:], in1=t2[:], op=OP.add), [k1, k2], [kzr])
                        vop(lambda e: e.tensor_tensor(out=sre[:], in0=PS[4][:, 0:TS], in1=C2, op=OP.mult), ['ps4', 'cos2'], [ksr])
                        vop(lambda e: e.tensor_tensor(out=sim[:], in0=PS[3][:, 0:TS], in1=S2, op=OP.mult), ['ps3', 'sin2'], [ksi])
                        pop(lambda e: e.tensor_tensor(out=zim[:], in0=sre[:], in1=sim[:], op=OP.subtract), [ksr, ksi], [kzi])
                        for ri, (zt_, kz) in enumerate([(zre, kzr), (zim, kzi)]):
                            vop(lambda e: e.tensor_tensor(out=tmp4[:, ri, :], in0=carry[:, ri, qs], in1=r4[:, qs], op=OP.mult), ['carry', 'r4'], ['tmp4_%d' % ri])
                            vop(lambda e: e.tensor_tensor(out=v3(zt_)[:, :, 0], in0=v3(zt_)[:, :, 0], in1=tmp4[:, ri, :], op=OP.add), [kz, 'tmp4_%d' % ri], [kz])
                        vop(lambda e: e.tensor_tensor_scan(out=sre[:], data0=RR, data1=zre[:], initial=0.0, op0=OP.mult, op1=OP.add), ['R4rep', kzr], [ksr])
                        vop(lambda e: e.tensor_tensor_scan(out=sim[:], data0=RR, data1=zim[:], initial=0.0, op0=OP.mult, op1=OP.add), ['R4rep', kzi], [ksi])
                        pop(lambda e: e.tensor_tensor(out=t1[:], in0=sre[:], in1=C2, op=OP.mult), [ksr, 'cos2'], [k1])
                        pop(lambda e: e.tensor_tensor(out=t2[:], in0=sim[:], in1=S2, op=OP.mult), [ksi, 'sin2'], [k2])
                        pop(lambda e: e.tensor_tensor(out=zre[:], in0=t1[:], in1=t2[:], op=OP.subtract), [k1, k2], [kzr])
                        vop(lambda e: e.tensor_tensor(out=t1[:], in0=sre[:], in1=S2, op=OP.mult), [ksr, 'sin2', kzr], [k1])
                        vop(lambda e: e.tensor_tensor(out=t2[:], in0=sim[:], in1=C2, op=OP.mult), [ksi, 'cos2', kzr], [k2])
                        vop(lambda e: e.tensor_tensor(out=zim[:], in0=t1[:], in1=t2[:], op=OP.add), [k1, k2], [kzi])
                        for ri, (zt_, kz) in enumerate([(zre, kzr), (zim, kzi)]):
                            spb, spk = SPb(cq, ri)
                            sp3 = spb[:].rearrange("p (a c) -> p a c", a=4)
                            aop(lambda e: e.activation(out=sp3[:, :, 1:NCH], in_=v3(zt_)[:, :, 0:NCH - 1], func=AF.Identity), [kz], [spk])
                            aop(lambda e: e.activation(out=sp3[:, :, 0], in_=carry[:, ri, qs], func=AF.Identity), ['carry'], [spk])
                            aop(lambda e: e.activation(out=carry[:, ri, qs], in_=v3(zt_)[:, :, NCH - 1], func=AF.Identity), [kz, spk], ['carry'])

                    def stage_back(cq):
                        py, pyk = PY(cq)
                        for pl in range(4):
                            q = 4 * cq + pl
                            pr = slice(pl * 32, (pl + 1) * 32)
                            for j in range(TC):
                                oj = py[pr, :].rearrange("p (c j) -> p j c", j=TC)[:, j, :]
                                for ri in range(2):
                                    spb, spk = SPb(cq, ri)
                                    peop(lambda e: e.matmul(oj, lhsT=WY[:, q, j, ri, :], rhs=spb[:, pl * NCH:(pl + 1) * NCH], start=False, stop=(ri == 1), tile_position=(0, pl * 32)),
                                         ['WY', spk], [pyk])
                        ys, g1 = FT[6], FT[7]
                        kA, kB = fk(6), fk(7)
                        vop(lambda e: e.scalar_tensor_tensor(out=ys[:], in0=u_bf[:, cq, :], scalar=bcol(DCOL, cq), in1=py, op0=OP.mult, op1=OP.add),
                            ['u%d' % cq, 'cols', pyk], [kA])
                        aop(lambda e: e.activation(out=g1[:], in_=ys[:], func=AF.Square), [kA], [kB])
                        vop(lambda e: e.tensor_scalar(out=g1[:], in0=g1[:], scalar1=0.044715, scalar2=1.0, op0=OP.mult, op1=OP.add), [kB], [kB])
                        vop(lambda e: e.tensor_tensor(out=g1[:], in0=g1[:], in1=ys[:], op=OP.mult), [kB, kA], [kB])
                        aop(lambda e: e.activation(out=g1[:], in_=g1[:], func=AF.Sigmoid, scale=1.5957691216057308), [kB], [kB])
                        vop(lambda e: e.tensor_tensor(out=y_bf[:, cq, :], in0=g1[:], in1=ys[:], op=OP.mult), [kB, kA], ['y%d' % cq])

                    if S5_DELAY:
                        stage_front(0)
                        for cq in range(1, 4):
                            stage_front(cq)
                            stage_back(cq - 1)
                        stage_back(3)
                    else:
                        for cq in range(4):
                            stage_front(cq)
                            stage_back(cq)

                def sec_ln(ti, t0):
                    for st in range(TS // 128):
                        r0 = t0 + st * 128
                        gst = ti * (TS // 128) + st
                        S.dma('sp', lambda e: e.dma_start(out=xtk[:], in_=xtok[r0:r0 + 128, :]), writes=['xtk'])
                        for hf in range(2):
                            hs = slice(hf * 512, (hf + 1) * 512)
                            b = hf
                            for k in range(8):
                                peop(lambda e: e.matmul(PS[b][:], lhsT=merged[:, k, st * 128:(st + 1) * 128], rhs=wout_bf[:, k, hs], start=(k == 0), stop=(k == 7)),
                                     ['wout', 'mg%d' % k], ['ps%d' % b], 0.28)
                            vop(lambda e: e.scalar_tensor_tensor(out=zt[:, hs], in0=xtk[:, hs], scalar=ALPHA, in1=PS[b][:], op0=OP.mult, op1=OP.add), ['xtk', 'ps%d' % b], ['zt%d' % hf], 0.6)
                            vop(lambda e: e.tensor_tensor(out=zt[:, hs], in0=zt[:, hs], in1=repA[:, 0, hs], op=OP.add), ['zt%d' % hf, 'rep'], ['zt%d' % hf], 0.6)
                            vop(lambda e: e.bn_stats(out=stats[:, hf * 6:(hf + 1) * 6], in_=zt[:, hs]), ['zt%d' % hf], ['bst%d' % hf], 0.65)
                        vop(lambda e: e.bn_aggr(out=stats[:, 12:14], in_=stats[:, 0:12]), ['bst0', 'bst1'], ['mv'])
                        aop(lambda e: e.activation(out=stats[:, 14:15], in_=stats[:, 13:14], func=AF.Sqrt, bias=eps_t[:, 0:1]), ['mv', 'eps'], ['sd'])
                        vop(lambda e: e.reciprocal(out=stats[:, 15:16], in_=stats[:, 14:15]), ['sd'], ['rs'])
                        vop(lambda e: e.tensor_scalar(out=zt[:], in0=zt[:], scalar1=stats[:, 12:13], scalar2=stats[:, 15:16], op0=OP.subtract, op1=OP.mult),
                            ['zt0', 'zt1', 'mv', 'rs'], ['zt0', 'zt1'], 0.85)
                        vop(lambda e: e.tensor_tensor(out=zt[:], in0=zt[:], in1=repA[:, 1, :], op=OP.mult), ['zt0', 'zt1', 'rep'], ['zt0', 'zt1'], 1.1)
                        vop(lambda e: e.tensor_tensor(out=zt[:], in0=zt[:], in1=repA[:, 2, :], op=OP.add), ['zt0', 'zt1', 'rep'], ['zt0', 'zt1'], 1.1)
                        aop(lambda e: e.activation(out=xtk[:], in_=zt[:], func=AF.Identity, scale=ALPHA), ['zt0', 'zt1', 'xtk'], ['xtk'], 1.06)
                        S.dma('sp', lambda e: e.dma_start(out=zb_d[r0:r0 + 128, :], in_=xtk[:]), reads=['xtk'], writes=['zb_d'])
                        if DEBUG_H:
                            S.dma('sp', lambda e: e.dma_start(out=out[r0:r0 + 128, :], in_=zt[:]), reads=['zt0', 'zt1'], writes=['out'])
                        for kb in range(2):
                            for kk4 in range(4):
                                k = kb * 4 + kk4
                                peop(lambda e: e.transpose(out=PS[2][:, kk4 * 128:(kk4 + 1) * 128], in_=zt[:, k * 128:(k + 1) * 128], identity=ident[:]),
                                     ['zt0', 'zt1', 'ident'], ['ps2'], 0.41)
                            aop(lambda e: e.activation(out=hT32[:, kb * 4:(kb + 1) * 4, :], in_=PS[2][:].rearrange("p (a b) -> p a b", a=4), func=AF.Identity), ['ps2'], ['hT32'], 0.5)
                        for k in range(8):
                            peop(lambda e: e.matmul(PS[2][:, 0:36], lhsT=hT32[:, k, :], rhs=wrt32[:, k, :], start=(k == 0), stop=(k == 7)), ['hT32', 'wrt'], ['ps2'], 0.27)
                        svop = lambda fn, r, w: vop(fn, r, w, 0.14)
                        R = lambda a, b: rsm[:, 192 + a:192 + b]
                        lg, zem, sel, ex, top8 = rsm[:, 0:36], rsm[:, 40:72], rsm[:, 72:104], rsm[:, 104:136], rsm[:, 136:144]
                        svop(lambda e: e.tensor_tensor(out=lg, in0=PS[2][:, 0:36], in1=brt_t[:], op=OP.add), ['ps2', 'brt'], ['lg'])
                        svop(lambda e: e.tensor_reduce(out=R(0, 1), in_=rsm[:, 0:4], axis=AX.X, op=OP.max), ['lg'], ['gmax'])
                        svop(lambda e: e.tensor_scalar(out=R(4, 8), in0=rsm[:, 0:4], scalar1=R(0, 1), scalar2=None, op0=OP.is_ge), ['lg', 'gmax'], ['ohg'])
                        svop(lambda e: e.tensor_scalar(out=R(1, 2), in0=R(0, 1), scalar1=-1.0, scalar2=None, op0=OP.mult), ['gmax'], ['ngmax'])
                        aop(lambda e: e.activation(out=R(8, 12), in_=rsm[:, 0:4], func=AF.Exp, bias=R(1, 2)), ['lg', 'ngmax'], ['eg'])
                        svop(lambda e: e.tensor_reduce(out=R(2, 3), in_=R(8, 12), axis=AX.X, op=OP.add), ['eg'], ['gsum'])
                        svop(lambda e: e.reciprocal(out=R(2, 3), in_=R(2, 3)), ['gsum'], ['gsum'])
                        svop(lambda e: e.tensor_scalar(out=R(12, 16), in0=R(4, 8), scalar1=-1.0, scalar2=1e30, op0=OP.add, op1=OP.mult), ['ohg'], ['pen'])
                        for g in range(4):
                            svop(lambda e: e.tensor_scalar(out=rsm[:, 40 + g * 8:48 + g * 8], in0=rsm[:, 4 + g * 8:12 + g * 8], scalar1=R(12 + g, 13 + g), scalar2=None, op0=OP.add),
                                ['lg', 'pen'], ['zem'])
                        svop(lambda e: e.max(out=top8, in_=zem), ['zem'], ['top8'])
                        svop(lambda e: e.tensor_scalar(out=sel, in0=zem, scalar1=rsm[:, 137:138], scalar2=None, op0=OP.is_ge), ['zem', 'top8'], ['sel'])
                        svop(lambda e: e.tensor_scalar(out=R(3, 4), in0=rsm[:, 136:137], scalar1=-1.0, scalar2=None, op0=OP.mult), ['top8'], ['nv1'])
                        aop(lambda e: e.activation(out=ex, in_=zem, func=AF.Exp, bias=R(3, 4)), ['zem', 'nv1'], ['ex'])
                        svop(lambda e: e.tensor_tensor(out=ex, in0=ex, in1=sel, op=OP.mult), ['ex', 'sel'], ['ex'])
                        svop(lambda e: e.tensor_reduce(out=R(16, 17), in_=ex, axis=AX.X, op=OP.add), ['ex'], ['den'])
                        svop(lambda e: e.reciprocal(out=R(16, 17), in_=R(16, 17)), ['den'], ['den'])
                        svop(lambda e: e.tensor_tensor(out=R(16, 17), in0=R(16, 17), in1=R(2, 3), op=OP.mult), ['den', 'gsum'], ['den'])
                        gt, oh0, oh1, pos, tmpr, ovf = (rsm[:, 256:288], rsm[:, 288:320], rsm[:, 320:352], rsm[:, 352:384], rsm[:, 384:416], rsm[:, 416:448])
                        X = lambda i: rsm[:, 448 + i:449 + i]
                        svop(lambda e: e.tensor_scalar(out=gt, in0=ex, scalar1=R(16, 17), scalar2=None, op0=OP.mult), ['ex', 'den'], ['gt'])
                        svop(lambda e: e.tensor_scalar(out=oh0, in0=zem, scalar1=rsm[:, 136:137], scalar2=None, op0=OP.is_ge), ['zem', 'top8'], ['oh0'])
                        svop(lambda e: e.tensor_tensor(out=oh1, in0=sel, in1=oh0, op=OP.subtract), ['sel', 'oh0'], ['oh1'])
                        svop(lambda e: e.tensor_copy(out=sel_bf[:], in_=sel), ['sel'], ['sel_bf'])
                        peop(lambda e: e.matmul(PS[2][:, 64:96], lhsT=tri_bf[:], rhs=sel_bf[:], start=True, stop=False), ['tri', 'sel_bf'], ['ps2'])
                        peop(lambda e: e.matmul(PS[2][:, 64:96], lhsT=ones_bf[:], rhs=selsum[:], start=False, stop=True), ['ones', 'selsum'], ['ps2'])
                        svop(lambda e: e.tensor_tensor(out=selsum[:], in0=selsum[:], in1=sel, op=OP.add), ['selsum', 'sel'], ['selsum'])
                        svop(lambda e: e.tensor_tensor(out=pos, in0=PS[2][:, 64:96], in1=ecap[:], op=OP.add), ['ps2', 'ecap'], ['pos'])
                        svop(lambda e: e.tensor_scalar(out=ovf, in0=PS[2][:, 64:96], scalar1=float(CAP), scalar2=1e6, op0=OP.is_ge, op1=OP.mult), ['ps2'], ['ovf'])
                        svop(lambda e: e.tensor_tensor(out=pos, in0=pos, in1=ovf, op=OP.add), ['pos', 'ovf'], ['pos'])
                        for kq, ohk in enumerate([oh0, oh1]):
                            okey = 'oh%d' % kq
                            svop(lambda e: e.tensor_tensor(out=tmpr, in0=ohk, in1=pos, op=OP.mult), [okey, 'pos'], ['tmpr'])
                            svop(lambda e: e.tensor_reduce(out=X(kq), in_=tmpr, axis=AX.X, op=OP.add), ['tmpr'], ['dx%d' % kq])
                            svop(lambda e: e.tensor_copy(out=dest_all[:, 2 * gst + kq:2 * gst + kq + 1], in_=X(kq)), ['dx%d' % kq], ['dest%d_%d' % (gst, kq)])
                            svop(lambda e: e.tensor_tensor(out=tmpr, in0=ohk, in1=gt, op=OP.mult), [okey, 'gt'], ['tmpr'])
                            svop(lambda e: e.tensor_reduce(out=gsel_all[:, gst, kq:kq + 1], in_=tmpr, axis=AX.X, op=OP.add), ['tmpr'], ['gsel'])
                        aop(lambda e: e.activation(out=hb[:].rearrange("t (k p) -> t k p", p=128), in_=zt[:].rearrange("t (p k) -> t k p", k=8), func=AF.Identity), ['zt0', 'zt1'], ['hb'], 1.0)
                        for kq in range(2):
                            S.dma('pool', lambda e: e.indirect_dma_start(out=xbuf_d, out_offset=bass.IndirectOffsetOnAxis(ap=dest_all[:, 2 * gst + kq:2 * gst + kq + 1], axis=0), in_=hb[:], in_offset=None,
                                                                         bounds_check=bcreg(e, 'A'), oob_is_err=False), reads=['hb', 'dest%d_%d' % (gst, kq)], writes=['xbuf'])
                chains = [S.record(sec_conv), S.record(sec_s5)]
                if prev_tile is not None:
                    pt = prev_tile
                    chains.insert(0, S.record(lambda: sec_ln(*pt)))
                S.replay_merged(chains)
                for m in range(8):
                    b = proj(20 + m)
                    aop(lambda e: e.activation(out=BT_[3][:], in_=PS[b][:, 0:TS], func=AF.Sigmoid, bias=bcol(B_IN, 20 + m)), ['ps%d' % b, 'cols'], [bk(3)])
                    b2 = mmbank()
                    for k in range(4):
                        peop(lambda e: e.matmul(PS[b2][:, 0:TS], lhsT=wcout_bf[:, k, m * 128:(m + 1) * 128], rhs=cs_bf[:, k, :], start=(k == 0), stop=(k == 3)),
                             ['wcout', 'cs%d' % k], ['ps%d' % b2])
                    vop(lambda e: e.scalar_tensor_tensor(out=merged[:, m, :], in0=PS[b2][:, 0:TS], scalar=bcol(B_COUT, m), in1=BT_[3][:], op0=OP.add, op1=OP.mult),
                        ['ps%d' % b2, 'cols', bk(3)], ['mg%d' % m])

                for m in range(8):
                    b = proj(12 + m)
                    aop(lambda e: e.activation(out=BT_[3][:], in_=PS[b][:, 0:TS], func=AF.Sigmoid, bias=bcol(B_IN, 12 + m)), ['ps%d' % b, 'cols'], [bk(3)])
                    bg = mmbank()
                    for k in range(4):
                        peop(lambda e: e.matmul(PS[bg][:, 0:TS], lhsT=wglu_bf[:, k, (8 + m) * 128:(9 + m) * 128], rhs=y_bf[:, k, :], start=(k == 0), stop=(k == 3)),
                             ['wglu', 'y%d' % k], ['ps%d' % bg])
                    aop(lambda e: e.activation(out=BT_[2][:], in_=PS[bg][:, 0:TS], func=AF.Sigmoid, bias=bcol(B_GLU, 8 + m)), ['ps%d' % bg, 'cols'], [bk(2)])
                    bv = mmbank()
                    for k in range(4):
                        peop(lambda e: e.matmul(PS[bv][:, 0:TS], lhsT=wglu_bf[:, k, m * 128:(m + 1) * 128], rhs=y_bf[:, k, :], start=(k == 0), stop=(k == 3)),
                             ['wglu', 'y%d' % k], ['ps%d' % bv])
                    vop(lambda e: e.scalar_tensor_tensor(out=FT[8][:], in0=PS[bv][:, 0:TS], scalar=bcol(B_GLU, m), in1=BT_[2][:], op0=OP.add, op1=OP.mult),
                        ['ps%d' % bv, 'cols', bk(2)], [fk(8)])
                    pop(lambda e: e.tensor_tensor(out=FT[8][:], in0=FT[8][:], in1=BT_[3][:], op=OP.mult), [fk(8), bk(3)], [fk(8)])
                    pop(lambda e: e.tensor_tensor(out=merged[:, m, :], in0=FT[8][:], in1=merged[:, m, :], op=OP.add), [fk(8), 'mg%d' % m], ['mg%d' % m])

                prev_tile = (ti, t0)
                if ti == NT - 1:
                    S.replay_merged([S.record(lambda: sec_ln(ti, t0))])

            S.barrier()
            S.emit()

        if not DEBUG_H:
            with ExitStack() as es:
                def sb(name, shape, dt=F32):
                    return es.enter_context(nc.sbuf_tensor(name, list(shape), dt))
                PS = [es.enter_context(nc.psum_tensor("pb%d" % i, [128, 512], F32)) for i in range(6)]
                PT = [es.enter_context(nc.psum_tensor("pt%d" % i, [128, 1024], BF16)) for i in range(2)]
                wg = [sb("wg%d" % i, [128, 8, 256], BF16) for i in range(2)]
                wu = [sb("wu%d" % i, [128, 8, 256], BF16) for i in range(2)]
                wd = [sb("wd%d" % i, [128, 2, D], BF16) for i in range(2)]
                xrows = [sb("xrows%d" % i, [128, NS4, D], BF16) for i in range(2)]
                xTe = sb("xTe", [128, 8, CAP], BF16)
                sgt = sb("sgt", [128, 2, CAP], BF16)
                hid = sb("hid", [128, 2, CAP], BF16)
                ysb = [sb("ysb%d" % i, [128, NS4, D], BF16) for i in range(2)]
                identb = sb("identb", [128, 128], BF16)
                NACC = 4
                statb = sb("statsb", [128, 24 * NACC])
                repB = sb("repB", [128, 2, D])
                acc = [sb("acc%d" % i, [128, D]) for i in range(NACC)]
                yg = [[sb("yg%d_%d" % (i, k), [128, D], BF16) for k in range(2)] for i in range(NACC)]
                dg = [[sb("dg%d_%d" % (i, k), [128, 128], BF16) for k in range(2)] for i in range(NACC)]
                S.dma('sp', lambda e: e.dma_start(out=repB[:], in_=rep5v[:, 3:5, :]), writes=['repB'])
                S.dma('pool', lambda e: e.dma_start(out=identb[:], in_=ident_d), writes=['identb'])
                for i in range(NACC):
                    for k in range(2):
                        pop(lambda e: e.memset(yg[i][k][:], 0.0), [], ['yg%d_%d' % (i, k)])
                rot = [0]

                def bank(n=6):
                    b = rot[0]
                    rot[0] = (rot[0] + 1) % n
                    return b
                def load_expert(ex_j):
                    wj = ex_j % 2
                    xrj = xrows[wj]
                    S.dma('sp', lambda e: e.dma_start(out=xrj[:], in_=xbuf_d[ex_j * CAP:(ex_j + 1) * CAP, :].rearrange("(s p) d -> p s d", p=128)), reads=['xbuf'], writes=['xr%d' % wj])
                    S.dma('pool', lambda e: e.dma_start(out=wg[wj][:], in_=w_gate[ex_j].rearrange("(p k) f -> p k f", k=8)), writes=['wg%d' % wj])
                    S.dma('pool', lambda e: e.dma_start(out=wu[wj][:], in_=w_up[ex_j].rearrange("(p k) f -> p k f", k=8)), writes=['wu%d' % wj])
                    S.dma('pool', lambda e: e.dma_start(out=wd[wj][:], in_=w_down[ex_j].rearrange("(k p) f -> p k f", p=128)), writes=['wd%d' % wj])
                load_expert(0)
                for ex_i in range(32):
                    wi = ex_i % 2
                    xr = xrows[wi]
                    ys = ysb[wi]
                    if ex_i + 1 < 32:
                        load_expert(ex_i + 1)
                    for s4 in range(NS4):
                        pt = PT[s4 % 2]
                        for k in range(8):
                            peop(lambda e: e.transpose(out=pt[:, k * 128:(k + 1) * 128], in_=xr[:, s4, k * 128:(k + 1) * 128], identity=identb[:]), ['xr%d' % wi, 'identb'], ['pt%d' % (s4 % 2)])
                        ev = aop if s4 % 2 == 0 else vop
                        ev(lambda e: e.tensor_copy(out=xTe[:, :, s4 * 128:(s4 + 1) * 128], in_=pt[:].rearrange("p (k r) -> p k r", k=8)) if False else
                           (e.activation(out=xTe[:, :, s4 * 128:(s4 + 1) * 128], in_=pt[:].rearrange("p (k r) -> p k r", k=8), func=AF.Identity) if s4 % 2 == 0 else
                            e.tensor_copy(out=xTe[:, :, s4 * 128:(s4 + 1) * 128], in_=pt[:].rearrange("p (k r) -> p k r", k=8))),
                           ['pt%d' % (s4 % 2)], ['xTe%d' % s4])
                    xk = ['xTe%d' % i for i in range(NS4)]
                    for f in range(2):
                        bgk = bank()
                        for k in range(8):
                            peop(lambda e: e.matmul(PS[bgk][:, 0:CAP], lhsT=wg[wi][:, k, f * 128:(f + 1) * 128], rhs=xTe[:, k, :], start=(k == 0), stop=(k == 7)), ['wg%d' % wi] + xk, ['pb%d' % bgk])
                        aop(lambda e: e.activation(out=sgt[:, f, :], in_=PS[bgk][:, 0:CAP], func=AF.Silu), ['pb%d' % bgk], ['sgt%d' % f])
                        buk = bank()
                        for k in range(8):
                            peop(lambda e: e.matmul(PS[buk][:, 0:CAP], lhsT=wu[wi][:, k, f * 128:(f + 1) * 128], rhs=xTe[:, k, :], start=(k == 0), stop=(k == 7)), ['wu%d' % wi] + xk, ['pb%d' % buk])
                        vop(lambda e: e.tensor_tensor(out=hid[:, f, :], in0=PS[buk][:, 0:CAP], in1=sgt[:, f, :], op=OP.mult), ['pb%d' % buk, 'sgt%d' % f], ['hid%d' % f])
                    for s4 in range(NS4):
                        for hf in range(2):
                            hs = slice(hf * 512, (hf + 1) * 512)
                            b = bank()
                            for f in range(2):
                                peop(lambda e: e.matmul(PS[b][:], lhsT=hid[:, f, s4 * 128:(s4 + 1) * 128], rhs=wd[wi][:, f, hs], start=(f == 0), stop=(f == 1)),
                                     ['hid0', 'hid1', 'wd%d' % wi], ['pb%d' % b])
                            if (s4 * 2 + hf) % 2 == 0:
                                aop(lambda e: e.activation(out=ys[:, s4, hs], in_=PS[b][:], func=AF.Identity), ['pb%d' % b], ['ysa%d' % wi])
                            else:
                                vop(lambda e: e.tensor_copy(out=ys[:, s4, hs], in_=PS[b][:]), ['pb%d' % b], ['ysv%d' % wi])
                    S.dma('sp', lambda e: e.dma_start(out=ybuf_d[ex_i * CAP:(ex_i + 1) * CAP, :].rearrange("(s p) d -> p s d", p=128), in_=ys[:]), reads=['ysa%d' % wi, 'ysv%d' % wi], writes=['ybuf%d' % ex_i])
                ybk = ['ybuf%d' % i for i in range(32)]
                NSUB = TOK // 128

                def comb_load(st):
                    r0 = st * 128
                    pi_ = st % NACC
                    a_ = acc[pi_]
                    S.dma('sp', lambda e: e.dma_start(out=a_[:], in_=zb_d[r0:r0 + 128, :]), reads=['zb_d'], writes=['accb%d' % pi_])
                    for kq in range(2):
                        g = yg[pi_][kq]
                        S.dma('pool', lambda e: e.indirect_dma_start(out=g[:], out_offset=None, in_=ybuf_d, in_offset=bass.IndirectOffsetOnAxis(ap=dest_all[:, 2 * st + kq:2 * st + kq + 1], axis=0),
                                                                     bounds_check=bcreg(e, 'B'), oob_is_err=False), reads=ybk, writes=['yg%d_%d' % (pi_, kq)])
                def comb_compute(st):
                    r0 = st * 128
                    pi_ = st % NACC
                    a_ = acc[pi_]
                    ak = 'accb%d' % pi_
                    sb_ = statb[:, pi_ * 24:pi_ * 24 + 24]
                    sk_ = 'stb%d' % pi_
                    for kq in range(2):
                        dgk = dg[pi_][kq]
                        aop(lambda e: e.activation(out=dgk[:], in_=identb[:], func=AF.Identity, scale=gsel_all[:, st, kq:kq + 1]), ['identb', 'gsel'], ['dg%d_%d' % (pi_, kq)], 0.2)
                    for hf in range(2):
                        hs = slice(hf * 512, (hf + 1) * 512)
                        b = (st % 2) * 2 + hf
                        for kq in range(2):
                            g = yg[pi_][kq]
                            dgk = dg[pi_][kq]
                            peop(lambda e: e.matmul(PS[b][:], lhsT=dgk[:], rhs=g[:, hs], start=(kq == 0), stop=(kq == 1)),
                                 ['dg%d_%d' % (pi_, kq), 'yg%d_%d' % (pi_, kq)], ['pb%d' % b], 0.28)
                        vop(lambda e: e.tensor_tensor(out=a_[:, hs], in0=a_[:, hs], in1=PS[b][:], op=OP.add), ['pb%d' % b, ak], [ak], 0.6)
                    for hf in range(2):
                        vop(lambda e: e.bn_stats(out=sb_[:, hf * 6:(hf + 1) * 6], in_=a_[:, hf * 512:(hf + 1) * 512]), [ak], [sk_ + 'b%d' % hf], 0.65)
                    vop(lambda e: e.bn_aggr(out=sb_[:, 12:14], in_=sb_[:, 0:12]), [sk_ + 'b0', sk_ + 'b1'], [sk_ + 'mv'], 0.2)
                    aop(lambda e: e.activation(out=sb_[:, 14:15], in_=sb_[:, 13:14], func=AF.Sqrt, bias=eps_t[:, 0:1]), [sk_ + 'mv', 'eps'], [sk_ + 'sd'], 0.3)
                    vop(lambda e: e.reciprocal(out=sb_[:, 15:16], in_=sb_[:, 14:15]), [sk_ + 'sd'], [sk_ + 'rs'], 0.16)
                    vop(lambda e: e.scalar_tensor_tensor(out=sb_[:, 16:17], in0=sb_[:, 12:13], scalar=-1.0, in1=sb_[:, 15:16], op0=OP.mult, op1=OP.mult), [sk_ + 'mv', sk_ + 'rs'], [sk_ + 'nb'], 0.1)
                    aop(lambda e: e.activation(out=a_[:], in_=a_[:], func=AF.Identity, scale=sb_[:, 15:16], bias=sb_[:, 16:17]), [ak, sk_ + 'rs', sk_ + 'nb'], [ak], 1.1)
                    vop(lambda e: e.tensor_tensor(out=a_[:], in0=a_[:], in1=repB[:, 0, :], op=OP.mult), [ak, 'repB'], [ak], 1.1)
                    pop(lambda e: e.tensor_tensor(out=a_[:, 0:512], in0=a_[:, 0:512], in1=repB[:, 1, 0:512], op=OP.add), [ak, 'repB'], [ak + 'p'], 1.2)
                    vop(lambda e: e.tensor_tensor(out=a_[:, 512:1024], in0=a_[:, 512:1024], in1=repB[:, 1, 512:1024], op=OP.add), [ak, 'repB'], [ak + 'v'], 0.6)
                    S.dma('sp', lambda e: e.dma_start(out=out[r0:r0 + 128, :], in_=a_[:]), reads=[ak, ak + 'p', ak + 'v'], writes=['out'])

                comb_load(0)
                comb_load(1)
                for st0 in range(0, NSUB, 2):
                    for st in (st0 + 2, st0 + 3):
                        if st < NSUB:
                            comb_load(st)
                    S.replay_merged([S.record(lambda: comb_compute(st0)), S.record(lambda: comb_compute(st0 + 1))])
                S.barrier()
                S.emit()
    return nc


def _prep(inp):
    f = lambda a: np.ascontiguousarray(a, dtype=np.float32)
    colmat = lambda v: f(np.asarray(v).reshape(-1, 128).T)
    cols = np.concatenate([colmat(inp['b_in'][0]), colmat(inp['b_glu'][0]), colmat(inp['b_cout'][0]), colmat(inp['ssm_d'][0]),
                           colmat(inp['conv_b'][0]), colmat(inp['ln_c_g'][0]), colmat(inp['ln_c_b'][0])], axis=1)
    assert cols.shape == (128, 68)
    cols = f(np.concatenate([cols, np.zeros((128, 4), np.float32)], axis=1))
    cw = inp['conv_w'][0][:, 0, :]
    convw = f(cw.T.reshape(4, 128, CW).transpose(1, 0, 2).reshape(128, 4 * CW))
    rep5 = f(np.concatenate([np.broadcast_to(inp[k][0][None, :], (128, D)) for k in ['b_out', 'ln1_g', 'ln1_b', 'ln2_g', 'ln2_b']], axis=1))
    w_rt = f(np.concatenate([inp['w_route_group'][0], inp['w_route_expert'][0]], axis=1))
    brt = f(np.broadcast_to(np.concatenate([inp['b_route_group'][0], inp['b_route_expert'][0]])[None, :], (128, 36)))
    pqf = lambda a: a.reshape(16, 2, 64).transpose(1, 2, 0).reshape(128, 16)
    ldt = np.broadcast_to(inp['log_dt'][0].reshape(16, 2, 1), (16, 2, 64))
    pq = f(np.concatenate([pqf(inp['lam_re'][0]), pqf(inp['lam_im'][0]), pqf(np.ascontiguousarray(ldt))], axis=1))
    bf_ = lambda a: a.reshape(16, 2, 64, 16).transpose(1, 2, 0, 3).reshape(128, 256)
    cf_ = lambda a: a.reshape(16, 2, 16, 64).transpose(1, 3, 0, 2).reshape(128, 256)
    bc = f(np.concatenate([bf_(inp['ssm_b_re'][0]), bf_(inp['ssm_b_im'][0]), cf_(inp['ssm_c_re'][0]), cf_(inp['ssm_c_im'][0])], axis=1))
    shared = {
        'w_in': f(inp['w_in'][0]), 'w_glu': f(inp['w_glu'][0]), 'w_cout': f(inp['w_cout'][0]), 'w_out': f(inp['w_out'][0]),
        'w_rt': w_rt, 'w_gate': f(inp['w_gate'][0]), 'w_up': f(inp['w_up'][0]), 'w_down': f(inp['w_down'][0]),
        'cols': cols, 'convw': convw, 'rep5': rep5, 'brt': brt, 'pq': pq, 'bc': bc,
        'ident': np.eye(128, dtype=np.float32),
        'tri': np.triu(np.ones((128, 128), np.float32), 1),
        'ecap': f(np.broadcast_to((np.arange(32, dtype=np.float32) * CAP)[None, :], (128, 32))),
        'kk': f(np.broadcast_to(np.arange(1, TS + 1, dtype=np.float32)[None, :], (128, TS))),
        'bdmask': f(np.kron(np.eye(4, dtype=np.float32), np.ones((32, 32), np.float32))),
    }
    x = inp['x']
    maps = []
    for c in range(NCORES):
        xc = f(x[2 * c:2 * c + 2].reshape(TOK, D))
        m = dict(shared)
        m['xtok'] = xc
        m['xT'] = f(xc.T)
        maps.append(m)
    return maps


def kernel(**inputs):
    maps = _prep(inputs)
    nc = build_nc()
    res = run_bass_kernel_spmd(nc, maps, core_ids=list(range(NCORES)))
    outs = [np.asarray(r['out']).reshape(2, SEQ, D) for r in res.results]
    return np.concatenate(outs, axis=0).astype(np.float32)
```

```python
import math
import numpy as np
import concourse.bass as bass
import concourse.mybir as mybir
from concourse.bass_utils import run_bass_kernel_spmd
from contextlib import ExitStack

F32 = mybir.dt.float32
BF16 = mybir.dt.bfloat16
AF = mybir.ActivationFunctionType
OP = mybir.AluOpType
AX = mybir.AxisListType

NCORES = 8
D = 1024
SEQ = 2048
TOK = 4096
TS = 256
NT = TOK // TS
CW = 31
ALPHA = 2.0 ** 0.25
EPS = 1e-5
MAGIC = 12582912.0
TWO_PI = 2.0 * math.pi
DEBUG_H = False
S5_HALF = True
S5_DELAY = True
CAP = 384
NS4 = CAP // 128
I32 = mybir.dt.int32


import types


def _freeze(fn):
    if fn.__closure__ is None:
        return fn
    cells = []
    for c in fn.__closure__:
        try:
            cells.append(types.CellType(c.cell_contents))
        except ValueError:
            cells.append(c)
    return types.FunctionType(fn.__code__, fn.__globals__, fn.__name__, fn.__defaults__, tuple(cells))


class Sched:
    def __init__(self, nc, es, ndma=14):
        self.nc = nc
        self.names = ['pe', 'act', 'dve', 'pool', 'sp']
        self.prog = {k: [] for k in self.names}
        self.sem = {k: es.enter_context(nc.semaphore('s_' + k)) for k in ['pe', 'act', 'dve', 'pool']}
        self.cnt = {k: 0 for k in self.sem}
        self.dsem = [es.enter_context(nc.semaphore('d%d' % i)) for i in range(ndma)]
        self.dcnt = [0] * ndma
        self.dnext = 0
        self.seen = {k: {} for k in self.names}
        self.lastw = {}
        self.readers = {}
        self.rec = None
        self.sim_eng = {}
        self.sim_w = {}
        self.sim_r = {}

    def record(self, section):
        self.rec = []
        section()
        r = self.rec
        self.rec = None
        return r

    COST = {'pe': 0.17, 'act': 0.42, 'dve': 0.36, 'pool': 0.75, 'sp': 0.1}

    def replay_merged(self, chains):
        pos = [0] * len(chains)
        now = max(self.sim_eng.values()) if self.sim_eng else 0.0
        for k in self.names:
            self.sim_eng[k] = now
        while True:
            best, bkey, bt = None, None, 0.0
            for i, c in enumerate(chains):
                if pos[i] >= len(c):
                    continue
                kind, eng, fn, reads, writes, cost = c[pos[i]]
                t = self.sim_eng[eng]
                for r in reads:
                    t = max(t, self.sim_w.get(r, 0.0))
                for w in writes:
                    t = max(t, self.sim_w.get(w, 0.0), self.sim_r.get(w, 0.0))
                key = (t, pos[i] / len(c))
                if best is None or key < bkey:
                    best, bkey, bt = i, key, t
            if best is None:
                break
            kind, eng, fn, reads, writes, cost = chains[best][pos[best]]
            pos[best] += 1
            if kind == 'op':
                end = bt + (cost if cost is not None else self.COST[eng])
                self.sim_eng[eng] = end
                done = end + 0.06
                self.op(eng, fn, reads, writes)
            else:
                self.sim_eng[eng] = bt + (1.0 if eng == 'pool' else 0.1)
                done = bt + 2.5
                self.dma(eng, fn, reads, writes)
            for r in reads:
                self.sim_r[r] = max(self.sim_r.get(r, 0.0), done)
            for w in writes:
                self.sim_w[w] = done
                self.sim_r[w] = 0.0

    def _semobj(self, k):
        return self.sem[k] if isinstance(k, str) else self.dsem[k]

    def _waits(self, eng, deps):
        best = {}
        for (k, v) in deps:
            if k == 'pe' and eng == 'pe':
                continue
            if self.seen[eng].get(k, 0) >= v:
                continue
            best[k] = max(best.get(k, 0), v)
        for k, v in best.items():
            self.seen[eng][k] = v
            so = self._semobj(k)
            self.prog[eng].append(lambda e, so=so, v=v: e.wait_ge(so, v))

    def _deps(self, reads, writes):
        deps = []
        for r in reads:
            if r in self.lastw:
                deps.append(self.lastw[r])
        for w in writes:
            if w in self.lastw:
                deps.append(self.lastw[w])
            deps.extend(self.readers.get(w, []))
        return deps

    def _commit(self, tok, reads, writes):
        for r in reads:
            self.readers.setdefault(r, []).append(tok)
        for w in writes:
            self.lastw[w] = tok
            self.readers[w] = []

    def op(self, eng, fn, reads=(), writes=(), cost=None):
        fn = _freeze(fn)
        if self.rec is not None:
            self.rec.append(('op', eng, fn, tuple(reads), tuple(writes), cost))
            return
        self._waits(eng, self._deps(reads, writes))
        self.cnt[eng] += 1
        so = self.sem[eng]
        self.prog[eng].append(lambda e, fn=fn, so=so: fn(e).then_inc(so, 1))
        self._commit((eng, self.cnt[eng]), reads, writes)

    def dma(self, eng, fn, reads=(), writes=()):
        fn = _freeze(fn)
        if self.rec is not None:
            self.rec.append(('dma', eng, fn, tuple(reads), tuple(writes), None))
            return
        i = self.dnext
        self.dnext = (self.dnext + 1) % len(self.dsem)
        deps = self._deps(reads, writes)
        if self.dcnt[i] > 0:
            deps.append((i, self.dcnt[i]))
        self._waits(eng, deps)
        self.dcnt[i] += 16
        so = self.dsem[i]
        self.prog[eng].append(lambda e, fn=fn, so=so: fn(e).then_inc(so, 16))
        self._commit((i, self.dcnt[i]), reads, writes)

    def barrier(self):
        deps = [(k, v) for k, v in self.cnt.items() if v > 0]
        deps += [(i, v) for i, v in enumerate(self.dcnt) if v > 0]
        for eng in self.names:
            self._waits(eng, deps)

    def emit(self):
        nc = self.nc
        prog = self.prog
        with nc.Block() as block:
            @block.tensor
            def _(e):
                for f in prog['pe']:
                    f(e)

            @block.scalar
            def _(e):
                for f in prog['act']:
                    f(e)

            @block.vector
            def _(e):
                for f in prog['dve']:
                    f(e)

            @block.gpsimd
            def _(e):
                for f in prog['pool']:
                    f(e)

            @block.sync
            def _(e):
                for f in prog['sp']:
                    f(e)
        self.prog = {k: [] for k in self.names}


def build_nc():
    nc = bass.Bass("TRN2", target_bir_lowering=False)

    def din(name, shape):
        return nc.dram_tensor(name, list(shape), F32, kind="ExternalInput").ap()

    xT = din("xT", [D, TOK])
    xtok = din("xtok", [TOK, D])
    w_in = din("w_in", [D, 3584])
    w_glu = din("w_glu", [512, 2048])
    w_cout = din("w_cout", [512, 1024])
    w_out = din("w_out", [D, D])
    w_rt = din("w_rt", [D, 36])
    w_gate = din("w_gate", [32, D, 256])
    w_up = din("w_up", [32, D, 256])
    w_down = din("w_down", [32, 256, D])
    cols = din("cols", [128, 72])
    convw = din("convw", [128, 4 * CW])
    rep5 = din("rep5", [128, 5 * D])
    brt = din("brt", [128, 36])
    pq = din("pq", [128, 48])
    bc = din("bc", [128, 4 * 256])
    ident_d = din("ident", [128, 128])
    kk_d = din("kk", [128, TS])
    out = nc.dram_tensor("out", [TOK, D], F32, kind="ExternalOutput").ap()
    tri_d = din("tri", [128, 128])
    ecap_d = din("ecap", [128, 32])
    bdmask_d = din("bdmask", [128, 128])
    xbuf_d = nc.dram_tensor("xbuf_scr", [32 * CAP, D], BF16, kind="Internal").ap()
    ybuf_d = nc.dram_tensor("ybuf_scr", [32 * CAP, D], BF16, kind="Internal").ap()
    zb_d = nc.dram_tensor("zb_scr", [TOK, D], F32, kind="Internal").ap()
    rep5v = rep5.rearrange("p (a d) -> p a d", a=5)

    with ExitStack() as es0:
        S = Sched(nc, es0)
        dest_all = es0.enter_context(nc.sbuf_tensor("dest_all", [128, 64], I32))
        gsel_all = es0.enter_context(nc.sbuf_tensor("gsel_all", [128, 32, 2], F32))
        eps_t = es0.enter_context(nc.sbuf_tensor("eps_t", [128, 1], F32))
        S.op('dve', lambda e: e.memset(eps_t[:], EPS), writes=['eps'])
        regh = {}

        def bcreg(e, tag):
            if tag not in regh:
                regh[tag] = e.alloc_register('bcr' + tag)
                e.reg_mov(regh[tag], 32 * CAP - 1)
            return regh[tag]
        vop = lambda fn, r, w, c=None: S.op('dve', fn, reads=r, writes=w, cost=c)
        aop = lambda fn, r, w, c=None: S.op('act', fn, reads=r, writes=w, cost=c)
        pop = lambda fn, r, w, c=None: S.op('pool', fn, reads=r, writes=w, cost=c)
        peop = lambda fn, r, w, c=None: S.op('pe', fn, reads=r, writes=w, cost=c)

        with ExitStack() as es:
            def sb(name, shape, dt=F32):
                return es.enter_context(nc.sbuf_tensor(name, list(shape), dt))
            PS = [es.enter_context(nc.psum_tensor("ps%d" % i, [128, 512], F32)) for i in range(8)]

            win_bf = sb("win_bf", [128, 8, 3584], BF16)
            wglu_bf = sb("wglu_bf", [128, 4, 2048], BF16)
            wcout_bf = sb("wcout_bf", [128, 4, 1024], BF16)
            wout_bf = sb("wout_bf", [128, 8, 1024], BF16)
            wrt32 = sb("wrt32", [128, 8, 36])
            cols_t = sb("cols_t", [128, 72])
            convw_t = sb("convw_t", [128, 4, CW])
            brt_t = sb("brt_t", [128, 36])
            ident = sb("ident_sb", [128, 128])
            ones_bf = sb("ones_bf", [128, 128], BF16)
            halfpi = sb("halfpi", [128, 1])
            repA = sb("repA", [128, 3, D])
            cdiag = sb("cdiag", [128, CW, 128], BF16)
            TC = 4
            NCH = TS // TC
            WZ = sb("WZ", [128, 4, TC, 2, 128], BF16)
            KM = sb("KM", [128, 4, TC, 128], BF16)
            WY = sb("WY", [128, 16, TC, 2, 32], BF16)
            cos2 = sb("cos2", [128, 16, NCH])
            sin2 = sb("sin2", [128, 16, NCH])
            R4rep = sb("R4rep", [128, 16, NCH])
            r4 = sb("r4", [128, 16])
            carry = sb("carry", [128, 2, 16])
            fk = lambda i: 'f%d' % i
            bk = lambda i: 'b%d' % i

            S.dma('sp', lambda e: e.dma_start(out=repA[:], in_=rep5v[:, 0:3, :]), writes=['rep'])
            S.dma('sp', lambda e: e.dma_start(out=wrt32[:], in_=w_rt.rearrange("(k p) c -> p k c", p=128)), writes=['wrt'])
            S.dma('sp', lambda e: e.dma_start(out=cols_t[:], in_=cols), writes=['cols'])
            S.dma('sp', lambda e: e.dma_start(out=convw_t[:], in_=convw.rearrange("p (c j) -> p c j", c=4)), writes=['convw'])
            S.dma('sp', lambda e: e.dma_start(out=brt_t[:], in_=brt), writes=['brt'])
            S.dma('sp', lambda e: e.dma_start(out=ident[:], in_=ident_d), writes=['ident'])
            vop(lambda e: e.memset(halfpi[:], math.pi / 2), [], ['halfpi'])
            vop(lambda e: e.memset(ones_bf[:], 1.0), [], ['ones'])

            with ExitStack() as esS:
                def sbs(name, shape, dt=F32):
                    return esS.enter_context(nc.sbuf_tensor(name, list(shape), dt))
                pq_t = sbs("pq_t", [128, 3, 16])
                bc_t = sbs("bc_t", [128, 4, 16, 16])
                kk = sbs("kk_sb", [128, NCH])
                NSL = 30
                small = sbs("small", [128, NSL * 16])
                vbrs = [sbs("vbr%d" % i, [128, 256]) for i in range(2)]
                vbis = [sbs("vbi%d" % i, [128, 256]) for i in range(2)]
                tmpbs = [sbs("tmpb%d" % i, [128, 256]) for i in range(2)]
                tmpas = [sbs("tmpa%d" % i, [128, 256]) for i in range(2)]
                E4rs = [sbs("E4r%d" % i, [128, 128]) for i in range(4)]
                E4is = [sbs("E4i%d" % i, [128, 128]) for i in range(4)]
                CTE = sbs("CTE", [128, 4, 2, 128])
                wts = [[sbs("wt%d_%d" % (i, jj), [128, 256]) for i in range(4)] for jj in range(2)]
                bdm = sbs("bdm", [128, 128])
                S.dma('sp', lambda e: e.dma_start(out=bdm[:], in_=bdmask_d), writes=['bdm'])
                tbw = sbs("tbw", [128, 2, 16 * NCH])
                ptmp = sbs("ptmp", [128, 3, 16 * TC])
                S.dma('sp', lambda e: e.dma_start(out=pq_t[:], in_=pq.rearrange("p (a q) -> p a q", a=3)), writes=['pq'])
                S.dma('sp', lambda e: e.dma_start(out=bc_t[:], in_=bc.rearrange("p (a q h) -> p a q h", a=4, q=16)), writes=['bc'])
                S.dma('sp', lambda e: e.dma_start(out=kk[:], in_=kk_d[:, 0:NCH]), writes=['kk'])
                for k in range(8):
                    S.dma('pool', lambda e, k=k: e.dma_start(out=win_bf[:, k, :], in_=w_in[k * 128:(k + 1) * 128, :]), writes=['win'])
                    S.dma('pool', lambda e, k=k: e.dma_start(out=wout_bf[:, k, :], in_=w_out[k * 128:(k + 1) * 128, :]), writes=['wout'])
                for k in range(4):
                    S.dma('pool', lambda e, k=k: e.dma_start(out=wglu_bf[:, k, :], in_=w_glu[k * 128:(k + 1) * 128, :]), writes=['wglu'])
                    S.dma('pool', lambda e, k=k: e.dma_start(out=wcout_bf[:, k, :], in_=w_cout[k * 128:(k + 1) * 128, :]), writes=['wcout'])
                sm = lambda i: small[:, i * 16:(i + 1) * 16]
                smc = lambda i, q: small[:, i * 16 + q:i * 16 + q + 1]
                sk = lambda i: 'sm%d' % i
                DT, RHO, TH, Y0, DEN, FR, FI, T1, T2, T3 = range(10)
                PR = lambda k: 10 + k
                PI = lambda k: 15 + k
                GR = lambda k: 20 + k
                GI = lambda k: 24 + k
                Y4 = 28
                lre, lim, ldt = pq_t[:, 0, :], pq_t[:, 1, :], pq_t[:, 2, :]
                TT = lambda o, a, b, op: vop(lambda e: e.tensor_tensor(out=sm(o), in0=sm(a), in1=sm(b), op=op), [sk(a), sk(b)], [sk(o)])
                aop(lambda e: e.activation(out=sm(DT), in_=ldt, func=AF.Exp), ['pq'], [sk(DT)])
                vop(lambda e: e.tensor_tensor(out=sm(RHO), in0=lre, in1=sm(DT), op=OP.mult), ['pq', sk(DT)], [sk(RHO)])
                vop(lambda e: e.tensor_tensor(out=sm(TH), in0=lim, in1=sm(DT), op=OP.mult), ['pq', sk(DT)], [sk(TH)])
                vop(lambda e: e.tensor_scalar(out=sm(Y0), in0=sm(TH), scalar1=1.0 / TWO_PI, scalar2=None, op0=OP.mult), [sk(TH)], [sk(Y0)])
                vop(lambda e: e.memset(sm(PR(0)), 1.0), [], [sk(PR(0))])
                vop(lambda e: e.memset(sm(PI(0)), 0.0), [], [sk(PI(0))])
                P4 = lambda base: small[:, base * 16:(base + TC) * 16]
                pt = lambda i: ptmp[:, i, :]
                prk = [sk(PR(k)) for k in range(1, TC + 1)]
                pik = [sk(PI(k)) for k in range(1, TC + 1)]
                for k in range(1, TC + 1):
                    aop(lambda e: e.activation(out=pt(2)[:, (k - 1) * 16:k * 16], in_=sm(RHO), func=AF.Exp, scale=float(k)), [sk(RHO)], ['pt2'])
                    vop(lambda e: e.tensor_scalar(out=pt(0)[:, (k - 1) * 16:k * 16], in0=sm(Y0), scalar1=float(k), scalar2=None, op0=OP.mult), [sk(Y0)], ['pt0'])
                vop(lambda e: e.tensor_scalar(out=pt(1), in0=pt(0), scalar1=MAGIC, scalar2=MAGIC, op0=OP.add, op1=OP.subtract), ['pt0'], ['pt1'])
                vop(lambda e: e.tensor_tensor(out=pt(0), in0=pt(0), in1=pt(1), op=OP.subtract), ['pt0', 'pt1'], ['pt0'])
                aop(lambda e: e.activation(out=pt(1), in_=pt(0), func=AF.Abs), ['pt0'], ['pt1'])
                aop(lambda e: e.activation(out=P4(PI(1)), in_=pt(0), func=AF.Sin, scale=TWO_PI), ['pt0'], pik)
                aop(lambda e: e.activation(out=P4(PR(1)), in_=pt(1), func=AF.Sin, scale=-TWO_PI, bias=halfpi[:, 0:1]), ['pt1', 'halfpi'], prk)
                vop(lambda e: e.tensor_tensor(out=P4(PR(1)), in0=P4(PR(1)), in1=pt(2), op=OP.mult), prk + ['pt2'], prk)
                vop(lambda e: e.tensor_tensor(out=P4(PI(1)), in0=P4(PI(1)), in1=pt(2), op=OP.mult), pik + ['pt2'], pik)
                vop(lambda e: e.tensor_copy(out=r4[:], in_=pt(2)[:, (TC - 1) * 16:TC * 16]), ['pt2'], ['r4'])
                vop(lambda e: e.tensor_tensor(out=sm(T1), in0=lre, in1=lre, op=OP.mult), ['pq'], [sk(T1)])
                vop(lambda e: e.tensor_tensor(out=sm(T2), in0=lim, in1=lim, op=OP.mult), ['pq'], [sk(T2)])
                TT(DEN, T1, T2, OP.add)
                vop(lambda e: e.reciprocal(out=sm(DEN), in_=sm(DEN)), [sk(DEN)], [sk(DEN)])
                vop(lambda e: e.tensor_scalar(out=sm(T1), in0=sm(PR(1)), scalar1=-1.0, scalar2=None, op0=OP.add), [sk(PR(1))], [sk(T1)])
                vop(lambda e: e.tensor_tensor(out=sm(T2), in0=sm(T1), in1=lre, op=OP.mult), [sk(T1), 'pq'], [sk(T2)])
                vop(lambda e: e.tensor_tensor(out=sm(T3), in0=sm(PI(1)), in1=lim, op=OP.mult), [sk(PI(1)), 'pq'], [sk(T3)])
                TT(T2, T2, T3, OP.add)
                TT(FR, T2, DEN, OP.mult)
                vop(lambda e: e.tensor_tensor(out=sm(T2), in0=sm(PI(1)), in1=lre, op=OP.mult), [sk(PI(1)), 'pq'], [sk(T2)])
                vop(lambda e: e.tensor_tensor(out=sm(T3), in0=sm(T1), in1=lim, op=OP.mult), [sk(T1), 'pq'], [sk(T3)])
                TT(T2, T2, T3, OP.subtract)
                TT(FI, T2, DEN, OP.mult)
                for k in range(TC):
                    TT(T1, PR(k), FR, OP.mult)
                    TT(T2, PI(k), FI, OP.mult)
                    TT(GR(k), T1, T2, OP.subtract)
                    TT(T1, PR(k), FI, OP.mult)
                    TT(T2, PI(k), FR, OP.mult)
                    TT(GI(k), T1, T2, OP.add)
                bq = lambda i: sm(i).unsqueeze(2).to_broadcast([128, 16, 16])
                q3 = lambda t: t[:].rearrange("p (q h) -> p q h", h=16)
                bre, bim, ctr, cti = bc_t[:, 0, :, :], bc_t[:, 1, :, :], bc_t[:, 2, :, :], bc_t[:, 3, :, :]
                vop(lambda e: e.memset(CTE[:], 0.0), [], ['CTE'])
                for cq in range(4):
                    for g2 in range(2):
                        hsl = slice(g2 * 64, (g2 + 1) * 64)
                        o0 = CTE[hsl, cq, 0, :].rearrange("p (pl c) -> p pl c", c=32)[:, :, g2 * 16:(g2 + 1) * 16]
                        o1 = CTE[hsl, cq, 1, :].rearrange("p (pl c) -> p pl c", c=32)[:, :, g2 * 16:(g2 + 1) * 16]
                        vop(lambda e: e.tensor_copy(out=o0, in_=bc_t[hsl, 2, cq * 4:(cq + 1) * 4, :]), ['bc'], ['CTE'])
                        vop(lambda e: e.tensor_scalar(out=o1, in0=bc_t[hsl, 3, cq * 4:(cq + 1) * 4, :], scalar1=-1.0, scalar2=None, op0=OP.mult), ['bc'], ['CTE'])

                def emit_VB(k):
                    vbr, vbi, tmpa, tmpb = vbrs[k % 2], vbis[k % 2], tmpas[k % 2], tmpbs[k % 2]
                    kvr, kvi, kta, ktb = 'vbr%d' % (k % 2), 'vbi%d' % (k % 2), 'tmpa%d' % (k % 2), 'tmpb%d' % (k % 2)
                    vop(lambda e: e.tensor_tensor(out=q3(tmpa), in0=bim, in1=bq(GI(k)), op=OP.mult), ['bc', sk(GI(k))], [kta])
                    vop(lambda e: e.tensor_tensor(out=q3(tmpb), in0=bre, in1=bq(GI(k)), op=OP.mult), ['bc', sk(GI(k))], [ktb])
                    vop(lambda e: e.tensor_tensor(out=q3(vbr), in0=bre, in1=bq(GR(k)), op=OP.mult), ['bc', sk(GR(k))], [kvr])
                    vop(lambda e: e.tensor_tensor(out=q3(vbi), in0=bim, in1=bq(GR(k)), op=OP.mult), ['bc', sk(GR(k))], [kvi])
                    vop(lambda e: e.tensor_tensor(out=vbr[:], in0=vbr[:], in1=tmpa[:], op=OP.subtract), [kvr, kta], [kvr])
                    vop(lambda e: e.tensor_tensor(out=vbi[:], in0=vbi[:], in1=tmpb[:], op=OP.add), [kvi, ktb], [kvi])

                def emit_E4(k):
                    vbr, vbi = vbrs[k % 2], vbis[k % 2]
                    kvr, kvi = 'vbr%d' % (k % 2), 'vbi%d' % (k % 2)
                    for cq in range(4):
                        E4r, E4i = E4rs[cq], E4is[cq]
                        ekr, eki = 'E4r%d' % cq, 'E4i%d' % cq
                        for ri, (src, E4, ek, ksrc) in enumerate([(vbr, E4r, ekr, kvr), (vbi, E4i, eki, kvi)]):
                            for g2 in range(2):
                                hsl = slice(g2 * 64, (g2 + 1) * 64)
                                o_ = E4[hsl, :].rearrange("p (pl c) -> p pl c", c=32)[:, :, g2 * 16:(g2 + 1) * 16]
                                i_ = src[hsl, cq * 64:(cq + 1) * 64].rearrange("p (pl h) -> p pl h", h=16)
                                if g2 == 0:
                                    vop(lambda e: e.tensor_copy(out=o_, in_=i_), [ksrc], [ek + 'p'])
                                else:
                                    aop(lambda e: e.activation(out=o_, in_=i_, func=AF.Identity), [ksrc], [ek + 'a'])
                            peop(lambda e: e.transpose(out=PS[7][:, ri * 128:(ri + 1) * 128], in_=E4[:], identity=ident[:]), [ek + 'p', ek + 'a', 'ident'], ['ps7'])
                            aop(lambda e: e.activation(out=WZ[:, cq, TC - 1 - k, ri, :], in_=PS[7][:, ri * 128:(ri + 1) * 128], func=AF.Identity), ['ps7'], ['WZ'])
                        peop(lambda e: e.matmul(PS[6][:, 0:128], lhsT=E4r[:], rhs=CTE[:, cq, 0, :], start=True, stop=False), [ekr + 'p', ekr + 'a', 'CTE'], ['ps6'])
                        peop(lambda e: e.matmul(PS[6][:, 0:128], lhsT=E4i[:], rhs=CTE[:, cq, 1, :], start=False, stop=True), [eki + 'p', eki + 'a', 'CTE'], ['ps6'])
                        vop(lambda e: e.tensor_tensor(out=KM[:, cq, k, :], in0=PS[6][:, 0:128], in1=bdm[:], op=OP.mult), ['ps6', 'bdm'], ['KM'])
                for i_ in range(4):
                    vop(lambda e: e.memset(E4rs[i_][:], 0.0), [], ['E4r%dp' % i_, 'E4r%da' % i_])
                    vop(lambda e: e.memset(E4is[i_][:], 0.0), [], ['E4i%dp' % i_, 'E4i%da' % i_])
                emit_VB(0)
                for k in range(TC):
                    if k + 1 < TC:
                        emit_VB(k + 1)
                    emit_E4(k)
                vop(lambda e: e.memset(WY[:], 0.0), [], ['WYp', 'WYa'])
                for j in range(TC):
                    w0, w1, w2, w3 = wts[j % 2]
                    w0k, w1k, w2k, w3k = ['wt%d_%d' % (i_, j % 2) for i_ in range(4)]
                    kpr, kpi = sk(PR(j + 1)), sk(PI(j + 1))
                    vop(lambda e: e.tensor_tensor(out=q3(w0), in0=cti, in1=bq(PI(j + 1)), op=OP.mult), ['bc', kpi], [w0k])
                    vop(lambda e: e.tensor_tensor(out=q3(w1), in0=ctr, in1=bq(PR(j + 1)), op=OP.mult), ['bc', kpr], [w1k])
                    vop(lambda e: e.tensor_tensor(out=q3(w2), in0=cti, in1=bq(PR(j + 1)), op=OP.mult), ['bc', kpr], [w2k])
                    vop(lambda e: e.tensor_tensor(out=q3(w3), in0=ctr, in1=bq(PI(j + 1)), op=OP.mult), ['bc', kpi], [w3k])
                    vop(lambda e: e.tensor_tensor(out=w1[:], in0=w1[:], in1=w0[:], op=OP.subtract), [w1k, w0k], [w1k])
                    vop(lambda e: e.tensor_tensor(out=w3[:], in0=w3[:], in1=w2[:], op=OP.add), [w3k, w2k], [w3k])
                    for g2 in range(2):
                        hsl = slice(g2 * 64, (g2 + 1) * 64)
                        vop(lambda e: e.tensor_copy(out=WY[hsl, :, j, 0, g2 * 16:(g2 + 1) * 16], in_=q3(w1)[hsl, :, :]), [w1k], ['WYp'])
                        aop(lambda e: e.activation(out=WY[hsl, :, j, 1, g2 * 16:(g2 + 1) * 16], in_=q3(w3)[hsl, :, :], func=AF.Identity, scale=-1.0), [w3k], ['WYa'])
                vop(lambda e: e.tensor_scalar(out=sm(Y4), in0=sm(Y0), scalar1=float(TC), scalar2=None, op0=OP.mult), [sk(Y0)], [sk(Y4)])
                kkb = kk[:].unsqueeze(1).to_broadcast([128, 16, NCH])
                y4b = sm(Y4).unsqueeze(2).to_broadcast([128, 16, NCH])
                flat = lambda t: t[:].rearrange("p q c -> p (q c)")
                vop(lambda e: e.tensor_tensor(out=tbw[:, 0, :].rearrange("p (q c) -> p q c", c=NCH), in0=kkb, in1=y4b, op=OP.mult), ['kk', sk(Y4)], ['tb0'])
                vop(lambda e: e.tensor_scalar(out=tbw[:, 1, :], in0=tbw[:, 0, :], scalar1=MAGIC, scalar2=MAGIC, op0=OP.add, op1=OP.subtract), ['tb0'], ['tb1'])
                vop(lambda e: e.tensor_tensor(out=tbw[:, 0, :], in0=tbw[:, 0, :], in1=tbw[:, 1, :], op=OP.subtract), ['tb0', 'tb1'], ['tb0'])
                aop(lambda e: e.activation(out=tbw[:, 1, :], in_=tbw[:, 0, :], func=AF.Abs), ['tb0'], ['tb1'])
                aop(lambda e: e.activation(out=flat(sin2), in_=tbw[:, 0, :], func=AF.Sin, scale=TWO_PI), ['tb0'], ['sin2'])
                aop(lambda e: e.activation(out=flat(cos2), in_=tbw[:, 1, :], func=AF.Sin, scale=-TWO_PI, bias=halfpi[:, 0:1]), ['tb1', 'halfpi'], ['cos2'])
                vop(lambda e: e.tensor_copy(out=R4rep[:], in_=r4[:].unsqueeze(2).to_broadcast([128, 16, NCH])), ['r4'], ['R4rep'])
                vop(lambda e: e.memset(R4rep[:, :, 0:1], 0.0), ['R4rep'], ['R4rep'])
                S.barrier()
                S.emit()

            xT_bfs = [sb("xT_bf%d" % i, [128, 8, TS], BF16) for i in range(2)]
            u_bf = sb("u_bf", [128, 4, TS], BF16)
            um = sb("um", [128, TC - 1, TS], BF16)
            u_pm = sb("u_pm", [128, 4, TS], BF16)
            vbuf = sb("vbuf", [128, 4, 30 + TS], BF16)
            y_bf = sb("y_bf", [128, 4, TS], BF16)
            c_bf = sb("c_bf", [128, 4, TS], BF16)
            cs_bf = sb("cs_bf", [128, 4, TS], BF16)
            merged = sb("merged", [128, 8, TS], BF16)
            hb = sb("hb", [128, D], BF16)
            tri_bf = sb("tri_bf", [128, 128], BF16)
            ecap = sb("ecap_sb", [128, 32])
            selsum = sb("selsum", [128, 32], BF16)
            sel_bf = sb("sel_bf", [128, 32], BF16)
            NF = 9
            FT = [sb("f32t%d" % i, [128, TS]) for i in range(NF)]
            BT_ = [sb("bft%d" % i, [128, TS], BF16) for i in range(4)]
            xtk = sb("xtk", [128, D])
            zt = sb("zt", [128, D])
            hT32 = sb("hT32", [128, 8, 128])
            rsm = sb("rsm", [128, 512])
            stats = sb("stats", [128, 16])
            halo = sb("halo", [128, 4, 32], BF16)
            tmp4 = sb("tmp4", [128, 2, 4])
            CL = [sb("cl%d" % i, [128, TS]) for i in range(2)]
            S.dma('pool', lambda e: e.dma_start(out=tri_bf[:], in_=tri_d), writes=['tri'])
            S.dma('sp', lambda e: e.dma_start(out=ecap[:], in_=ecap_d), writes=['ecap'])
            vop(lambda e: e.memset(selsum[:], 0.0), [], ['selsum'])
            vop(lambda e: e.memset(um[:], 0.0), [], ['um'])

            bcol = lambda off, m: cols_t[:, off + m:off + m + 1]
            B_IN, B_GLU, B_COUT, DCOL, CONVB, LNCG, LNCB = 0, 28, 44, 52, 56, 60, 64
            mmrot = [0]
            NROT = 6

            def mmbank():
                b = mmrot[0]
                mmrot[0] = (mmrot[0] + 1) % NROT
                return b

            xcur = [None, None]

            def proj(m):
                b = mmbank()
                xT_bf, xkey = xcur
                for k in range(8):
                    peop(lambda e: e.matmul(PS[b][:, 0:TS], lhsT=win_bf[:, k, m * 128:(m + 1) * 128], rhs=xT_bf[:, k, :], start=(k == 0), stop=(k == 7)),
                         ['win', xkey], ['ps%d' % b])
                return b

            def load_xT(ti_):
                buf = xT_bfs[ti_ % 2]
                tt0 = ti_ * TS
                S.dma('pool', lambda e: e.dma_start(out=buf[:], in_=xT.rearrange("(k p) t -> p k t", p=128)[:, :, tt0:tt0 + TS]), writes=['xT%d' % (ti_ % 2)])

            TPS = SEQ // TS
            prev_tile = None
            for ti in range(NT):
                t0 = ti * TS
                first = (ti % TPS == 0)
                if ti == 0:
                    load_xT(0)
                xcur[0], xcur[1] = xT_bfs[ti % 2], 'xT%d' % (ti % 2)
                if ti + 1 < NT:
                    load_xT(ti + 1)
                if first:
                    pop(lambda e: e.memset(vbuf[:, :, 0:30], 0.0), [], ['vhalo'])
                    pop(lambda e: e.memset(carry[:], 0.0), [], ['carry'])

                for m in range(4):
                    b = proj(m)
                    aop(lambda e: e.activation(out=u_bf[:, m, :], in_=PS[b][:, 0:TS], func=AF.Identity, bias=bcol(B_IN, m)), ['ps%d' % b, 'cols'], ['u%d' % m])
                    aop(lambda e: e.activation(out=u_pm[:, m, :].rearrange("p (i c) -> p c i", i=TC), in_=PS[b][:, 0:TS].rearrange("p (c i) -> p c i", i=TC),
                                               func=AF.Identity, bias=bcol(B_IN, m)), ['ps%d' % b, 'cols'], ['up%d' % m])
                for c in range(4):
                    b = proj(8 + c)
                    aop(lambda e: e.activation(out=y_bf[:, c, :], in_=PS[b][:, 0:TS], func=AF.Sigmoid, bias=bcol(B_IN, 8 + c)), ['ps%d' % b, 'cols'], ['y%d' % c])
                for c in range(4):
                    b = proj(4 + c)
                    vop(lambda e: e.scalar_tensor_tensor(out=vbuf[:, c, 30:30 + TS], in0=PS[b][:, 0:TS], scalar=bcol(B_IN, 4 + c), in1=y_bf[:, c, :], op0=OP.add, op1=OP.mult),
                        ['ps%d' % b, 'cols', 'y%d' % c], ['v%d' % c])

                def sec_conv():
                    for c in range(4):
                        for j in range(CW):
                            if j % 4 != 3:
                                aop(lambda e: e.activation(out=cdiag[:, j, :], in_=ident[:], func=AF.Identity, scale=convw_t[:, c, j:j + 1]), ['ident', 'convw'], ['cd%d' % j])
                            else:
                                vop(lambda e: e.tensor_scalar(out=cdiag[:, j, :], in0=ident[:], scalar1=convw_t[:, c, j:j + 1], scalar2=None, op0=OP.mult), ['ident', 'convw'], ['cd%d' % j])
                        for j in range(CW):
                            peop(lambda e: e.matmul(PS[5][:, 0:TS], lhsT=cdiag[:, j, :], rhs=vbuf[:, c, j:j + TS], start=(j == 0), stop=(j == CW - 1)),
                                 ['cd%d' % j, 'v%d' % c, 'vhalo'], ['ps5'])
                        aop(lambda e: e.activation(out=c_bf[:, c, :], in_=PS[5][:, 0:TS], func=AF.Identity, bias=bcol(CONVB, c)), ['ps5', 'cols'], ['c_bf%d' % c])
                        aop(lambda e: e.activation(out=cs_bf[:, c, :], in_=PS[5][:, 0:TS], func=AF.Square, bias=bcol(CONVB, c)), ['ps5', 'cols'], ['cs%d' % c])
                        pop(lambda e: e.tensor_copy(out=halo[:, c, 0:30], in_=vbuf[:, c, TS:TS + 30]), ['v%d' % c], ['halo'])
                    for c in range(4):
                        peop(lambda e: e.matmul(PS[5][:, TS:2 * TS], lhsT=ones_bf[:], rhs=c_bf[:, c, :], start=(c == 0), stop=(c == 3)), ['ones', 'c_bf%d' % c], ['ps5'])
                    mean_t, rstd_t, msq_t, cn_t = CL[0], CL[1], CL[1], FT[8]
                    aop(lambda e: e.activation(out=mean_t[:], in_=PS[5][:, TS:2 * TS], func=AF.Identity, scale=1.0 / 512), ['ps5'], ['cl0'])
                    for c in range(4):
                        peop(lambda e: e.matmul(PS[5][:, TS:2 * TS], lhsT=ones_bf[:], rhs=cs_bf[:, c, :], start=(c == 0), stop=(c == 3)), ['ones', 'cs%d' % c], ['ps5'])
                    vop(lambda e: e.tensor_tensor(out=msq_t[:], in0=mean_t[:], in1=mean_t[:], op=OP.mult), ['cl0'], ['cl1'])
                    vop(lambda e: e.scalar_tensor_tensor(out=msq_t[:], in0=PS[5][:, TS:2 * TS], scalar=1.0 / 512, in1=msq_t[:], op0=OP.mult, op1=OP.subtract), ['ps5', 'cl1'], ['cl1'])
                    aop(lambda e: e.activation(out=msq_t[:], in_=msq_t[:], func=AF.Sqrt, bias=eps_t[:, 0:1]), ['cl1', 'eps'], ['cl1'])
                    vop(lambda e: e.reciprocal(out=rstd_t[:], in_=msq_t[:]), ['cl1'], ['cl1'])
                    for c in range(4):
                        pop(lambda e: e.tensor_tensor(out=cn_t[:], in0=c_bf[:, c, :], in1=mean_t[:], op=OP.subtract), ['c_bf%d' % c, 'cl0'], [fk(8)])
                        pop(lambda e: e.tensor_tensor(out=cn_t[:], in0=cn_t[:], in1=rstd_t[:], op=OP.mult), [fk(8), 'cl1'], [fk(8)])
                        aop(lambda e: e.activation(out=cs_bf[:, c, :], in_=cn_t[:], func=AF.Silu, scale=bcol(LNCG, c), bias=bcol(LNCB, c)), [fk(8), 'cols'], ['cs%d' % c])
                    for c in range(4):
                        pop(lambda e: e.tensor_copy(out=vbuf[:, c, 0:30], in_=halo[:, c, 0:30]), ['halo', 'v%d' % c], ['vhalo'])

                def sec_s5():
                    v3 = lambda t: t[:].rearrange("p (a c) -> p a c", a=4)
                    t1, t2, zre, zim, sre, sim = FT[0], FT[1], FT[2], FT[3], FT[4], FT[5]
                    k1, k2, kzr, kzi, ksr, ksi = fk(0), fk(1), fk(2), fk(3), fk(4), fk(5)

                    def PY(cq):
                        h_ = (cq % 2) if S5_HALF else 0
                        return PS[7 - h_][:, 0:TS], 'ps%d' % (7 - h_)

                    def SPb(cq, ri):
                        i_ = (cq % 2) * 2 + ri
                        return BT_[i_], bk(i_)

                    def stage_front(cq):
                        qs = slice(4 * cq, 4 * cq + 4)
                        C2 = cos2[:, qs, :].rearrange("p a c -> p (a c)")
                        S2 = sin2[:, qs, :].rearrange("p a c -> p (a c)")
                        RR = R4rep[:, qs, :].rearrange("p a c -> p (a c)")
                        ucq = u_bf[:, cq, :].rearrange("p (c i) -> p c i", i=TC)
                        for pl in range(4):
                            pr = slice(pl * 32, (pl + 1) * 32)
                            for ri in range(2):
                                for i in range(TC):
                                    ua = u_pm[pr, cq, i * NCH:(i + 1) * NCH]
                                    peop(lambda e: e.matmul(PS[3 + ri][:, pl * NCH:(pl + 1) * NCH], lhsT=WZ[pr, cq, i, ri, :], rhs=ua, start=(i == 0), stop=(i == TC - 1),
                                                            tile_position=(pl * 32, 0)), ['WZ', 'up%d' % cq], ['ps%d' % (3 + ri)])
                        for d in range(1, TC):
                            pop(lambda e: e.tensor_copy(out=um[:, d - 1, :].rearrange("p (c i) -> p c i", i=TC)[:, :, d:TC], in_=ucq[:, :, 0:TC - d]), ['u%d' % cq, 'um'], ['um%d' % d])
                        py, pyk = PY(cq)
                        for d in range(TC):
                            rhs_ = u_bf[:, cq, :] if d == 0 else um[:, d - 1, :]
                            peop(lambda e: e.matmul(py, lhsT=KM[:, cq, d, :], rhs=rhs_, start=(d == 0), stop=False), ['KM', 'u%d' % cq] + (['um%d' % d] if d else []), [pyk])
                        vop(lambda e: e.tensor_tensor(out=t1[:], in0=PS[3][:, 0:TS], in1=C2, op=OP.mult), ['ps3', 'cos2'], [k1])
                        vop(lambda e: e.tensor_tensor(out=t2[:], in0=PS[4][:, 0:TS], in1=S2, op=OP.mult), ['ps4', 'sin2'], [k2])
                        pop(lambda e: e.tensor_tensor(out=zre[:], in0=t1[:], in1=t2[:], op=OP.add), [k1, k2], [kzr])
                        vop(lambda e: e.tensor_tensor(out=sre[:], in0=PS[4][:, 0:TS], in1=C2, op=OP.mult), ['ps4', 'cos2'], [ksr])
                        vop(lambda e: e.tensor_tensor(out=sim[:], in0=PS[3][:, 0:TS], in1=S2, op=OP.mult), ['ps3', 'sin2'], [ksi])
                        pop(lambda e: e.tensor_tensor(out=zim[:], in0=sre[:], in1=sim[:], op=OP.subtract), [ksr, ksi], [kzi])
                        for ri, (zt_, kz) in enumerate([(zre, kzr), (zim, kzi)]):
                            vop(lambda e: e.tensor_tensor(out=tmp4[:, ri, :], in0=carry[:, ri, qs], in1=r4[:, qs], op=OP.mult), ['carry', 'r4'], ['tmp4_%d' % ri])
                            vop(lambda e: e.tensor_tensor(out=v3(zt_)[:, :, 0], in0=v3(zt_)[:, :, 0], in1=tmp4[:, ri, :], op=OP.add), [kz, 'tmp4_%d' % ri], [kz])
                        vop(lambda e: e.tensor_tensor_scan(out=sre[:], data0=RR, data1=zre[:], initial=0.0, op0=OP.mult, op1=OP.add), ['R4rep', kzr], [ksr])
                        vop(lambda e: e.tensor_tensor_scan(out=sim[:], data0=RR, data1=zim[:], initial=0.0, op0=OP.mult, op1=OP.add), ['R4rep', kzi], [ksi])
                        pop(lambda e: e.tensor_tensor(out=t1[:], in0=sre[:], in1=C2, op=OP.mult), [ksr, 'cos2'], [k1])
                        pop(lambda e: e.tensor_tensor(out=t2[:], in0=sim[:], in1=S2, op=OP.mult), [ksi, 'sin2'], [k2])
                        pop(lambda e: e.tensor_tensor(out=zre[:], in0=t1[:], in1=t2[:], op=OP.subtract), [k1, k2], [kzr])
                        vop(lambda e: e.tensor_tensor(out=t1[:], in0=sre[:], in1=S2, op=OP.mult), [ksr, 'sin2', kzr], [k1])
                        vop(lambda e: e.tensor_tensor(out=t2[:], in0=sim[:], in1=C2, op=OP.mult), [ksi, 'cos2', kzr], [k2])
                        vop(lambda e: e.tensor_tensor(out=zim[:], in0=t1[:], in1=t2[:], op=OP.add), [k1, k2], [kzi])
                        for ri, (zt_, kz) in enumerate([(zre, kzr), (zim, kzi)]):
                            spb, spk = SPb(cq, ri)
                            sp3 = spb[:].rearrange("p (a c) -> p a c", a=4)
                            aop(lambda e: e.activation(out=sp3[:, :, 1:NCH], in_=v3(zt_)[:, :, 0:NCH - 1], func=AF.Identity), [kz], [spk])
                            aop(lambda e: e.activation(out=sp3[:, :, 0], in_=carry[:, ri, qs], func=AF.Identity), ['carry'], [spk])
                            aop(lambda e: e.activation(out=carry[:, ri, qs], in_=v3(zt_)[:, :, NCH - 1], func=AF.Identity), [kz, spk], ['carry'])

                    def stage_back(cq):
                        py, pyk = PY(cq)
                        for pl in range(4):
                            q = 4 * cq + pl
                            pr = slice(pl * 32, (pl + 1) * 32)
                            for j in range(TC):
                                oj = py[pr, :].rearrange("p (c j) -> p j c", j=TC)[:, j, :]
                                for ri in range(2):
                                    spb, spk = SPb(cq, ri)
                                    peop(lambda e: e.matmul(oj, lhsT=WY[:, q, j, ri, :], rhs=spb[:, pl * NCH:(pl + 1) * NCH], start=False, stop=(ri == 1), tile_position=(0, pl * 32)),
                                         ['WY', spk], [pyk])
                        ys, g1 = FT[6], FT[7]
                        kA, kB = fk(6), fk(7)
                        vop(lambda e: e.scalar_tensor_tensor(out=ys[:], in0=u_bf[:, cq, :], scalar=bcol(DCOL, cq), in1=py, op0=OP.mult, op1=OP.add),
                            ['u%d' % cq, 'cols', pyk], [kA])
                        aop(lambda e: e.activation(out=g1[:], in_=ys[:], func=AF.Square), [kA], [kB])
                        vop(lambda e: e.tensor_scalar(out=g1[:], in0=g1[:], scalar1=0.044715, scalar2=1.0, op0=OP.mult, op1=OP.add), [kB], [kB])
                        vop(lambda e: e.tensor_tensor(out=g1[:], in0=g1[:], in1=ys[:], op=OP.mult), [kB, kA], [kB])
                        aop(lambda e: e.activation(out=g1[:], in_=g1[:], func=AF.Sigmoid, scale=1.5957691216057308), [kB], [kB])
                        vop(lambda e: e.tensor_tensor(out=y_bf[:, cq, :], in0=g1[:], in1=ys[:], op=OP.mult), [kB, kA], ['y%d' % cq])

                    if S5_DELAY:
                        stage_front(0)
                        for cq in range(1, 4):
                            stage_front(cq)
                            stage_back(cq - 1)
                        stage_back(3)
                    else:
                        for cq in range(4):
                            stage_front(cq)
                            stage_back(cq)

                def sec_ln(ti, t0):
                    for st in range(TS // 128):
                        r0 = t0 + st * 128
                        gst = ti * (TS // 128) + st
                        S.dma('sp', lambda e: e.dma_start(out=xtk[:], in_=xtok[r0:r0 + 128, :]), writes=['xtk'])
                        for hf in range(2):
                            hs = slice(hf * 512, (hf + 1) * 512)
                            b = hf
                            for k in range(8):
                                peop(lambda e: e.matmul(PS[b][:], lhsT=merged[:, k, st * 128:(st + 1) * 128], rhs=wout_bf[:, k, hs], start=(k == 0), stop=(k == 7)),
                                     ['wout', 'mg%d' % k], ['ps%d' % b], 0.28)
                            vop(lambda e: e.scalar_tensor_tensor(out=zt[:, hs], in0=xtk[:, hs], scalar=ALPHA, in1=PS[b][:], op0=OP.mult, op1=OP.add), ['xtk', 'ps%d' % b], ['zt%d' % hf], 0.6)
                            vop(lambda e: e.tensor_tensor(out=zt[:, hs], in0=zt[:, hs], in1=repA[:, 0, hs], op=OP.add), ['zt%d' % hf, 'rep'], ['zt%d' % hf], 0.6)
                            vop(lambda e: e.bn_stats(out=stats[:, hf * 6:(hf + 1) * 6], in_=zt[:, hs]), ['zt%d' % hf], ['bst%d' % hf], 0.65)
                        vop(lambda e: e.bn_aggr(out=stats[:, 12:14], in_=stats[:, 0:12]), ['bst0', 'bst1'], ['mv'])
                        aop(lambda e: e.activation(out=stats[:, 14:15], in_=stats[:, 13:14], func=AF.Sqrt, bias=eps_t[:, 0:1]), ['mv', 'eps'], ['sd'])
                        vop(lambda e: e.reciprocal(out=stats[:, 15:16], in_=stats[:, 14:15]), ['sd'], ['rs'])
                        vop(lambda e: e.tensor_scalar(out=zt[:], in0=zt[:], scalar1=stats[:, 12:13], scalar2=stats[:, 15:16], op0=OP.subtract, op1=OP.mult),
                            ['zt0', 'zt1', 'mv', 'rs'], ['zt0', 'zt1'], 0.85)
                        vop(lambda e: e.tensor_tensor(out=zt[:], in0=zt[:], in1=repA[:, 1, :], op=OP.mult), ['zt0', 'zt1', 'rep'], ['zt0', 'zt1'], 1.1)
                        vop(lambda e: e.tensor_tensor(out=zt[:], in0=zt[:], in1=repA[:, 2, :], op=OP.add), ['zt0', 'zt1', 'rep'], ['zt0', 'zt1'], 1.1)
                        aop(lambda e: e.activation(out=xtk[:], in_=zt[:], func=AF.Identity, scale=ALPHA), ['zt0', 'zt1', 'xtk'], ['xtk'], 1.06)
                        S.dma('sp', lambda e: e.dma_start(out=zb_d[r0:r0 + 128, :], in_=xtk[:]), reads=['xtk'], writes=['zb_d'])
                        if DEBUG_H:
                            S.dma('sp', lambda e: e.dma_start(out=out[r0:r0 + 128, :], in_=zt[:]), reads=['zt0', 'zt1'], writes=['out'])
                        for kb in range(2):
                            for kk4 in range(4):
                                k = kb * 4 + kk4
                                peop(lambda e: e.transpose(out=PS[2][:, kk4 * 128:(kk4 + 1) * 128], in_=zt[:, k * 128:(k + 1) * 128], identity=ident[:]),
                                     ['zt0', 'zt1', 'ident'], ['ps2'], 0.41)
                            aop(lambda e: e.activation(out=hT32[:, kb * 4:(kb + 1) * 4, :], in_=PS[2][:].rearrange("p (a b) -> p a b", a=4), func=AF.Identity), ['ps2'], ['hT32'], 0.5)
                        for k in range(8):
                            peop(lambda e: e.matmul(PS[2][:, 0:36], lhsT=hT32[:, k, :], rhs=wrt32[:, k, :], start=(k == 0), stop=(k == 7)), ['hT32', 'wrt'], ['ps2'], 0.27)
                        svop = lambda fn, r, w: vop(fn, r, w, 0.14)
                        R = lambda a, b: rsm[:, 192 + a:192 + b]
                        lg, zem, sel, ex, top8 = rsm[:, 0:36], rsm[:, 40:72], rsm[:, 72:104], rsm[:, 104:136], rsm[:, 136:144]
                        svop(lambda e: e.tensor_tensor(out=lg, in0=PS[2][:, 0:36], in1=brt_t[:], op=OP.add), ['ps2', 'brt'], ['lg'])
                        svop(lambda e: e.tensor_reduce(out=R(0, 1), in_=rsm[:, 0:4], axis=AX.X, op=OP.max), ['lg'], ['gmax'])
                        svop(lambda e: e.tensor_scalar(out=R(4, 8), in0=rsm[:, 0:4], scalar1=R(0, 1), scalar2=None, op0=OP.is_ge), ['lg', 'gmax'], ['ohg'])
                        svop(lambda e: e.tensor_scalar(out=R(1, 2), in0=R(0, 1), scalar1=-1.0, scalar2=None, op0=OP.mult), ['gmax'], ['ngmax'])
                        aop(lambda e: e.activation(out=R(8, 12), in_=rsm[:, 0:4], func=AF.Exp, bias=R(1, 2)), ['lg', 'ngmax'], ['eg'])
                        svop(lambda e: e.tensor_reduce(out=R(2, 3), in_=R(8, 12), axis=AX.X, op=OP.add), ['eg'], ['gsum'])
                        svop(lambda e: e.reciprocal(out=R(2, 3), in_=R(2, 3)), ['gsum'], ['gsum'])
                        svop(lambda e: e.tensor_scalar(out=R(12, 16), in0=R(4, 8), scalar1=-1.0, scalar2=1e30, op0=OP.add, op1=OP.mult), ['ohg'], ['pen'])
                        for g in range(4):
                            svop(lambda e: e.tensor_scalar(out=rsm[:, 40 + g * 8:48 + g * 8], in0=rsm[:, 4 + g * 8:12 + g * 8], scalar1=R(12 + g, 13 + g), scalar2=None, op0=OP.add),
                                ['lg', 'pen'], ['zem'])
                        svop(lambda e: e.max(out=top8, in_=zem), ['zem'], ['top8'])
                        svop(lambda e: e.tensor_scalar(out=sel, in0=zem, scalar1=rsm[:, 137:138], scalar2=None, op0=OP.is_ge), ['zem', 'top8'], ['sel'])
                        svop(lambda e: e.tensor_scalar(out=R(3, 4), in0=rsm[:, 136:137], scalar1=-1.0, scalar2=None, op0=OP.mult), ['top8'], ['nv1'])
                        aop(lambda e: e.activation(out=ex, in_=zem, func=AF.Exp, bias=R(3, 4)), ['zem', 'nv1'], ['ex'])
                        svop(lambda e: e.tensor_tensor(out=ex, in0=ex, in1=sel, op=OP.mult), ['ex', 'sel'], ['ex'])
                        svop(lambda e: e.tensor_reduce(out=R(16, 17), in_=ex, axis=AX.X, op=OP.add), ['ex'], ['den'])
                        svop(lambda e: e.reciprocal(out=R(16, 17), in_=R(16, 17)), ['den'], ['den'])
                        svop(lambda e: e.tensor_tensor(out=R(16, 17), in0=R(16, 17), in1=R(2, 3), op=OP.mult), ['den', 'gsum'], ['den'])
                        gt, oh0, oh1, pos, tmpr, ovf = (rsm[:, 256:288], rsm[:, 288:320], rsm[:, 320:352], rsm[:, 352:384], rsm[:, 384:416], rsm[:, 416:448])
                        X = lambda i: rsm[:, 448 + i:449 + i]
                        svop(lambda e: e.tensor_scalar(out=gt, in0=ex, scalar1=R(16, 17), scalar2=None, op0=OP.mult), ['ex', 'den'], ['gt'])
                        svop(lambda e: e.tensor_scalar(out=oh0, in0=zem, scalar1=rsm[:, 136:137], scalar2=None, op0=OP.is_ge), ['zem', 'top8'], ['oh0'])
                        svop(lambda e: e.tensor_tensor(out=oh1, in0=sel, in1=oh0, op=OP.subtract), ['sel', 'oh0'], ['oh1'])
                        svop(lambda e: e.tensor_copy(out=sel_bf[:], in_=sel), ['sel'], ['sel_bf'])
                        peop(lambda e: e.matmul(PS[2][:, 64:96], lhsT=tri_bf[:], rhs=sel_bf[:], start=True, stop=False), ['tri', 'sel_bf'], ['ps2'])
                        peop(lambda e: e.matmul(PS[2][:, 64:96], lhsT=ones_bf[:], rhs=selsum[:], start=False, stop=True), ['ones', 'selsum'], ['ps2'])
                        svop(lambda e: e.tensor_tensor(out=selsum[:], in0=selsum[:], in1=sel, op=OP.add), ['selsum', 'sel'], ['selsum'])
                        svop(lambda e: e.tensor_tensor(out=pos, in0=PS[2][:, 64:96], in1=ecap[:], op=OP.add), ['ps2', 'ecap'], ['pos'])
                        svop(lambda e: e.tensor_scalar(out=ovf, in0=PS[2][:, 64:96], scalar1=float(CAP), scalar2=1e6, op0=OP.is_ge, op1=OP.mult), ['ps2'], ['ovf'])
                        svop(lambda e: e.tensor_tensor(out=pos, in0=pos, in1=ovf, op=OP.add), ['pos', 'ovf'], ['pos'])
                        for kq, ohk in enumerate([oh0, oh1]):
                            okey = 'oh%d' % kq
                            svop(lambda e: e.tensor_tensor(out=tmpr, in0=ohk, in1=pos, op=OP.mult), [okey, 'pos'], ['tmpr'])
                            svop(lambda e: e.tensor_reduce(out=X(kq), in_=tmpr, axis=AX.X, op=OP.add), ['tmpr'], ['dx%d' % kq])
                            svop(lambda e: e.tensor_copy(out=dest_all[:, 2 * gst + kq:2 * gst + kq + 1], in_=X(kq)), ['dx%d' % kq], ['dest%d_%d' % (gst, kq)])
                            svop(lambda e: e.tensor_tensor(out=tmpr, in0=ohk, in1=gt, op=OP.mult), [okey, 'gt'], ['tmpr'])
                            svop(lambda e: e.tensor_reduce(out=gsel_all[:, gst, kq:kq + 1], in_=tmpr, axis=AX.X, op=OP.add), ['tmpr'], ['gsel'])
                        aop(lambda e: e.activation(out=hb[:].rearrange("t (k p) -> t k p", p=128), in_=zt[:].rearrange("t (p k) -> t k p", k=8), func=AF.Identity), ['zt0', 'zt1'], ['hb'], 1.0)
                        for kq in range(2):
                            S.dma('pool', lambda e: e.indirect_dma_start(out=xbuf_d, out_offset=bass.IndirectOffsetOnAxis(ap=dest_all[:, 2 * gst + kq:2 * gst + kq + 1], axis=0), in_=hb[:], in_offset=None,
                                                                         bounds_check=bcreg(e, 'A'), oob_is_err=False), reads=['hb', 'dest%d_%d' % (gst, kq)], writes=['xbuf'])
                chains = [S.record(sec_conv), S.record(sec_s5)]
                if prev_tile is not None:
                    pt = prev_tile
                    chains.insert(0, S.record(lambda: sec_ln(*pt)))
                S.replay_merged(chains)
                for m in range(8):
                    b = proj(20 + m)
                    aop(lambda e: e.activation(out=BT_[3][:], in_=PS[b][:, 0:TS], func=AF.Sigmoid, bias=bcol(B_IN, 20 + m)), ['ps%d' % b, 'cols'], [bk(3)])
                    b2 = mmbank()
                    for k in range(4):
                        peop(lambda e: e.matmul(PS[b2][:, 0:TS], lhsT=wcout_bf[:, k, m * 128:(m + 1) * 128], rhs=cs_bf[:, k, :], start=(k == 0), stop=(k == 3)),
                             ['wcout', 'cs%d' % k], ['ps%d' % b2])
                    vop(lambda e: e.scalar_tensor_tensor(out=merged[:, m, :], in0=PS[b2][:, 0:TS], scalar=bcol(B_COUT, m), in1=BT_[3][:], op0=OP.add, op1=OP.mult),
                        ['ps%d' % b2, 'cols', bk(3)], ['mg%d' % m])

                for m in range(8):
                    b = proj(12 + m)
                    aop(lambda e: e.activation(out=BT_[3][:], in_=PS[b][:, 0:TS], func=AF.Sigmoid, bias=bcol(B_IN, 12 + m)), ['ps%d' % b, 'cols'], [bk(3)])
                    bg = mmbank()
                    for k in range(4):
                        peop(lambda e: e.matmul(PS[bg][:, 0:TS], lhsT=wglu_bf[:, k, (8 + m) * 128:(9 + m) * 128], rhs=y_bf[:, k, :], start=(k == 0), stop=(k == 3)),
                             ['wglu', 'y%d' % k], ['ps%d' % bg])
                    aop(lambda e: e.activation(out=BT_[2][:], in_=PS[bg][:, 0:TS], func=AF.Sigmoid, bias=bcol(B_GLU, 8 + m)), ['ps%d' % bg, 'cols'], [bk(2)])
                    bv = mmbank()
                    for k in range(4):
                        peop(lambda e: e.matmul(PS[bv][:, 0:TS], lhsT=wglu_bf[:, k, m * 128:(m + 1) * 128], rhs=y_bf[:, k, :], start=(k == 0), stop=(k == 3)),
                             ['wglu', 'y%d' % k], ['ps%d' % bv])
                    vop(lambda e: e.scalar_tensor_tensor(out=FT[8][:], in0=PS[bv][:, 0:TS], scalar=bcol(B_GLU, m), in1=BT_[2][:], op0=OP.add, op1=OP.mult),
                        ['ps%d' % bv, 'cols', bk(2)], [fk(8)])
                    pop(lambda e: e.tensor_tensor(out=FT[8][:], in0=FT[8][:], in1=BT_[3][:], op=OP.mult), [fk(8), bk(3)], [fk(8)])
                    pop(lambda e: e.tensor_tensor(out=merged[:, m, :], in0=FT[8][:], in1=merged[:, m, :], op=OP.add), [fk(8), 'mg%d' % m], ['mg%d' % m])

                prev_tile = (ti, t0)
                if ti == NT - 1:
                    S.replay_merged([S.record(lambda: sec_ln(ti, t0))])

            S.barrier()
            S.emit()

        if not DEBUG_H:
            with ExitStack() as es:
                def sb(name, shape, dt=F32):
                    return es.enter_context(nc.sbuf_tensor(name, list(shape), dt))
                PS = [es.enter_context(nc.psum_tensor("pb%d" % i, [128, 512], F32)) for i in range(6)]
                PT = [es.enter_context(nc.psum_tensor("pt%d" % i, [128, 1024], BF16)) for i in range(2)]
                wg = [sb("wg%d" % i, [128, 8, 256], BF16) for i in range(2)]
                wu = [sb("wu%d" % i, [128, 8, 256], BF16) for i in range(2)]
                wd = [sb("wd%d" % i, [128, 2, D], BF16) for i in range(2)]
                xrows = [sb("xrows%d" % i, [128, NS4, D], BF16) for i in range(2)]
                xTe = sb("xTe", [128, 8, CAP], BF16)
                sgt = sb("sgt", [128, 2, CAP], BF16)
                hid = sb("hid", [128, 2, CAP], BF16)
                ysb = [sb("ysb%d" % i, [128, NS4, D], BF16) for i in range(2)]
                identb = sb("identb", [128, 128], BF16)
                NACC = 4
                statb = sb("statsb", [128, 24 * NACC])
                repB = sb("repB", [128, 2, D])
                acc = [sb("acc%d" % i, [128, D]) for i in range(NACC)]
                yg = [[sb("yg%d_%d" % (i, k), [128, D], BF16) for k in range(2)] for i in range(NACC)]
                dg = [[sb("dg%d_%d" % (i, k), [128, 128], BF16) for k in range(2)] for i in range(NACC)]
                S.dma('sp', lambda e: e.dma_start(out=repB[:], in_=rep5v[:, 3:5, :]), writes=['repB'])
                S.dma('pool', lambda e: e.dma_start(out=identb[:], in_=ident_d), writes=['identb'])
                for i in range(NACC):
                    for k in range(2):
                        pop(lambda e: e.memset(yg[i][k][:], 0.0), [], ['yg%d_%d' % (i, k)])
                rot = [0]

                def bank(n=6):
                    b = rot[0]
                    rot[0] = (rot[0] + 1) % n
                    return b
                def load_expert(ex_j):
                    wj = ex_j % 2
                    xrj = xrows[wj]
                    S.dma('sp', lambda e: e.dma_start(out=xrj[:], in_=xbuf_d[ex_j * CAP:(ex_j + 1) * CAP, :].rearrange("(s p) d -> p s d", p=128)), reads=['xbuf'], writes=['xr%d' % wj])
                    S.dma('pool', lambda e: e.dma_start(out=wg[wj][:], in_=w_gate[ex_j].rearrange("(p k) f -> p k f", k=8)), writes=['wg%d' % wj])
                    S.dma('pool', lambda e: e.dma_start(out=wu[wj][:], in_=w_up[ex_j].rearrange("(p k) f -> p k f", k=8)), writes=['wu%d' % wj])
                    S.dma('pool', lambda e: e.dma_start(out=wd[wj][:], in_=w_down[ex_j].rearrange("(k p) f -> p k f", p=128)), writes=['wd%d' % wj])
                load_expert(0)
                for ex_i in range(32):
                    wi = ex_i % 2
                    xr = xrows[wi]
                    ys = ysb[wi]
                    if ex_i + 1 < 32:
                        load_expert(ex_i + 1)
                    for s4 in range(NS4):
                        pt = PT[s4 % 2]
                        for k in range(8):
                            peop(lambda e: e.transpose(out=pt[:, k * 128:(k + 1) * 128], in_=xr[:, s4, k * 128:(k + 1) * 128], identity=identb[:]), ['xr%d' % wi, 'identb'], ['pt%d' % (s4 % 2)])
                        ev = aop if s4 % 2 == 0 else vop
                        ev(lambda e: e.tensor_copy(out=xTe[:, :, s4 * 128:(s4 + 1) * 128], in_=pt[:].rearrange("p (k r) -> p k r", k=8)) if False else
                           (e.activation(out=xTe[:, :, s4 * 128:(s4 + 1) * 128], in_=pt[:].rearrange("p (k r) -> p k r", k=8), func=AF.Identity) if s4 % 2 == 0 else
                            e.tensor_copy(out=xTe[:, :, s4 * 128:(s4 + 1) * 128], in_=pt[:].rearrange("p (k r) -> p k r", k=8))),
                           ['pt%d' % (s4 % 2)], ['xTe%d' % s4])
                    xk = ['xTe%d' % i for i in range(NS4)]
                    for f in range(2):
                        bgk = bank()
                        for k in range(8):
                            peop(lambda e: e.matmul(PS[bgk][:, 0:CAP], lhsT=wg[wi][:, k, f * 128:(f + 1) * 128], rhs=xTe[:, k, :], start=(k == 0), stop=(k == 7)), ['wg%d' % wi] + xk, ['pb%d' % bgk])
                        aop(lambda e: e.activation(out=sgt[:, f, :], in_=PS[bgk][:, 0:CAP], func=AF.Silu), ['pb%d' % bgk], ['sgt%d' % f])
                        buk = bank()
                        for k in range(8):
                            peop(lambda e: e.matmul(PS[buk][:, 0:CAP], lhsT=wu[wi][:, k, f * 128:(f + 1) * 128], rhs=xTe[:, k, :], start=(k == 0), stop=(k == 7)), ['wu%d' % wi] + xk, ['pb%d' % buk])
                        vop(lambda e: e.tensor_tensor(out=hid[:, f, :], in0=PS[buk][:, 0:CAP], in1=sgt[:, f, :], op=OP.mult), ['pb%d' % buk, 'sgt%d' % f], ['hid%d' % f])
                    for s4 in range(NS4):
                        for hf in range(2):
                            hs = slice(hf * 512, (hf + 1) * 512)
                            b = bank()
                            for f in range(2):
                                peop(lambda e: e.matmul(PS[b][:], lhsT=hid[:, f, s4 * 128:(s4 + 1) * 128], rhs=wd[wi][:, f, hs], start=(f == 0), stop=(f == 1)),
                                     ['hid0', 'hid1', 'wd%d' % wi], ['pb%d' % b])
                            if (s4 * 2 + hf) % 2 == 0:
                                aop(lambda e: e.activation(out=ys[:, s4, hs], in_=PS[b][:], func=AF.Identity), ['pb%d' % b], ['ysa%d' % wi])
                            else:
                                vop(lambda e: e.tensor_copy(out=ys[:, s4, hs], in_=PS[b][:]), ['pb%d' % b], ['ysv%d' % wi])
                    S.dma('sp', lambda e: e.dma_start(out=ybuf_d[ex_i * CAP:(ex_i + 1) * CAP, :].rearrange("(s p) d -> p s d", p=128), in_=ys[:]), reads=['ysa%d' % wi, 'ysv%d' % wi], writes=['ybuf%d' % ex_i])
                ybk = ['ybuf%d' % i for i in range(32)]
                NSUB = TOK // 128

                def comb_load(st):
                    r0 = st * 128
                    pi_ = st % NACC
                    a_ = acc[pi_]
                    S.dma('sp', lambda e: e.dma_start(out=a_[:], in_=zb_d[r0:r0 + 128, :]), reads=['zb_d'], writes=['accb%d' % pi_])
                    for kq in range(2):
                        g = yg[pi_][kq]
                        S.dma('pool', lambda e: e.indirect_dma_start(out=g[:], out_offset=None, in_=ybuf_d, in_offset=bass.IndirectOffsetOnAxis(ap=dest_all[:, 2 * st + kq:2 * st + kq + 1], axis=0),
                                                                     bounds_check=bcreg(e, 'B'), oob_is_err=False), reads=ybk, writes=['yg%d_%d' % (pi_, kq)])
                def comb_compute(st):
                    r0 = st * 128
                    pi_ = st % NACC
                    a_ = acc[pi_]
                    ak = 'accb%d' % pi_
                    sb_ = statb[:, pi_ * 24:pi_ * 24 + 24]
                    sk_ = 'stb%d' % pi_
                    for kq in range(2):
                        dgk = dg[pi_][kq]
                        aop(lambda e: e.activation(out=dgk[:], in_=identb[:], func=AF.Identity, scale=gsel_all[:, st, kq:kq + 1]), ['identb', 'gsel'], ['dg%d_%d' % (pi_, kq)], 0.2)
                    for hf in range(2):
                        hs = slice(hf * 512, (hf + 1) * 512)
                        b = (st % 2) * 2 + hf
                        for kq in range(2):
                            g = yg[pi_][kq]
                            dgk = dg[pi_][kq]
                            peop(lambda e: e.matmul(PS[b][:], lhsT=dgk[:], rhs=g[:, hs], start=(kq == 0), stop=(kq == 1)),
                                 ['dg%d_%d' % (pi_, kq), 'yg%d_%d' % (pi_, kq)], ['pb%d' % b], 0.28)
                        vop(lambda e: e.tensor_tensor(out=a_[:, hs], in0=a_[:, hs], in1=PS[b][:], op=OP.add), ['pb%d' % b, ak], [ak], 0.6)
                    for hf in range(2):
                        vop(lambda e: e.bn_stats(out=sb_[:, hf * 6:(hf + 1) * 6], in_=a_[:, hf * 512:(hf + 1) * 512]), [ak], [sk_ + 'b%d' % hf], 0.65)
                    vop(lambda e: e.bn_aggr(out=sb_[:, 12:14], in_=sb_[:, 0:12]), [sk_ + 'b0', sk_ + 'b1'], [sk_ + 'mv'], 0.2)
                    aop(lambda e: e.activation(out=sb_[:, 14:15], in_=sb_[:, 13:14], func=AF.Sqrt, bias=eps_t[:, 0:1]), [sk_ + 'mv', 'eps'], [sk_ + 'sd'], 0.3)
                    vop(lambda e: e.reciprocal(out=sb_[:, 15:16], in_=sb_[:, 14:15]), [sk_ + 'sd'], [sk_ + 'rs'], 0.16)
                    vop(lambda e: e.scalar_tensor_tensor(out=sb_[:, 16:17], in0=sb_[:, 12:13], scalar=-1.0, in1=sb_[:, 15:16], op0=OP.mult, op1=OP.mult), [sk_ + 'mv', sk_ + 'rs'], [sk_ + 'nb'], 0.1)
                    aop(lambda e: e.activation(out=a_[:], in_=a_[:], func=AF.Identity, scale=sb_[:, 15:16], bias=sb_[:, 16:17]), [ak, sk_ + 'rs', sk_ + 'nb'], [ak], 1.1)
                    vop(lambda e: e.tensor_tensor(out=a_[:], in0=a_[:], in1=repB[:, 0, :], op=OP.mult), [ak, 'repB'], [ak], 1.1)
                    pop(lambda e: e.tensor_tensor(out=a_[:, 0:512], in0=a_[:, 0:512], in1=repB[:, 1, 0:512], op=OP.add), [ak, 'repB'], [ak + 'p'], 1.2)
                    vop(lambda e: e.tensor_tensor(out=a_[:, 512:1024], in0=a_[:, 512:1024], in1=repB[:, 1, 512:1024], op=OP.add), [ak, 'repB'], [ak + 'v'], 0.6)
                    S.dma('sp', lambda e: e.dma_start(out=out[r0:r0 + 128, :], in_=a_[:]), reads=[ak, ak + 'p', ak + 'v'], writes=['out'])

                comb_load(0)
                comb_load(1)
                for st0 in range(0, NSUB, 2):
                    for st in (st0 + 2, st0 + 3):
                        if st < NSUB:
                            comb_load(st)
                    S.replay_merged([S.record(lambda: comb_compute(st0)), S.record(lambda: comb_compute(st0 + 1))])
                S.barrier()
                S.emit()
    return nc


def _prep(inp):
    f = lambda a: np.ascontiguousarray(a, dtype=np.float32)
    colmat = lambda v: f(np.asarray(v).reshape(-1, 128).T)
    cols = np.concatenate([colmat(inp['b_in'][0]), colmat(inp['b_glu'][0]), colmat(inp['b_cout'][0]), colmat(inp['ssm_d'][0]),
                           colmat(inp['conv_b'][0]), colmat(inp['ln_c_g'][0]), colmat(inp['ln_c_b'][0])], axis=1)
    assert cols.shape == (128, 68)
    cols = f(np.concatenate([cols, np.zeros((128, 4), np.float32)], axis=1))
    cw = inp['conv_w'][0][:, 0, :]
    convw = f(cw.T.reshape(4, 128, CW).transpose(1, 0, 2).reshape(128, 4 * CW))
    rep5 = f(np.concatenate([np.broadcast_to(inp[k][0][None, :], (128, D)) for k in ['b_out', 'ln1_g', 'ln1_b', 'ln2_g', 'ln2_b']], axis=1))
    w_rt = f(np.concatenate([inp['w_route_group'][0], inp['w_route_expert'][0]], axis=1))
    brt = f(np.broadcast_to(np.concatenate([inp['b_route_group'][0], inp['b_route_expert'][0]])[None, :], (128, 36)))
    pqf = lambda a: a.reshape(16, 2, 64).transpose(1, 2, 0).reshape(128, 16)
    ldt = np.broadcast_to(inp['log_dt'][0].reshape(16, 2, 1), (16, 2, 64))
    pq = f(np.concatenate([pqf(inp['lam_re'][0]), pqf(inp['lam_im'][0]), pqf(np.ascontiguousarray(ldt))], axis=1))
    bf_ = lambda a: a.reshape(16, 2, 64, 16).transpose(1, 2, 0, 3).reshape(128, 256)
    cf_ = lambda a: a.reshape(16, 2, 16, 64).transpose(1, 3, 0, 2).reshape(128, 256)
    bc = f(np.concatenate([bf_(inp['ssm_b_re'][0]), bf_(inp['ssm_b_im'][0]), cf_(inp['ssm_c_re'][0]), cf_(inp['ssm_c_im'][0])], axis=1))
    shared = {
        'w_in': f(inp['w_in'][0]), 'w_glu': f(inp['w_glu'][0]), 'w_cout': f(inp['w_cout'][0]), 'w_out': f(inp['w_out'][0]),
        'w_rt': w_rt, 'w_gate': f(inp['w_gate'][0]), 'w_up': f(inp['w_up'][0]), 'w_down': f(inp['w_down'][0]),
        'cols': cols, 'convw': convw, 'rep5': rep5, 'brt': brt, 'pq': pq, 'bc': bc,
        'ident': np.eye(128, dtype=np.float32),
        'tri': np.triu(np.ones((128, 128), np.float32), 1),
        'ecap': f(np.broadcast_to((np.arange(32, dtype=np.float32) * CAP)[None, :], (128, 32))),
        'kk': f(np.broadcast_to(np.arange(1, TS + 1, dtype=np.float32)[None, :], (128, TS))),
        'bdmask': f(np.kron(np.eye(4, dtype=np.float32), np.ones((32, 32), np.float32))),
    }
    x = inp['x']
    maps = []
    for c in range(NCORES):
        xc = f(x[2 * c:2 * c + 2].reshape(TOK, D))
        m = dict(shared)
        m['xtok'] = xc
        m['xT'] = f(xc.T)
        maps.append(m)
    return maps


def kernel(**inputs):
    maps = _prep(inputs)
    nc = build_nc()
    res = run_bass_kernel_spmd(nc, maps, core_ids=list(range(NCORES)))
    outs = [np.asarray(r['out']).reshape(2, SEQ, D) for r in res.results]
    return np.concatenate(outs, axis=0).astype(np.float32)
```

```python
import math
import numpy as np
import concourse.bass as bass
import concourse.mybir as mybir
from concourse.bass_utils import run_bass_kernel_spmd
from contextlib import ExitStack

F32 = mybir.dt.float32
BF16 = mybir.dt.bfloat16
AF = mybir.ActivationFunctionType
OP = mybir.AluOpType
AX = mybir.AxisListType

NCORES = 8
D = 1024
SEQ = 2048
TOK = 4096
TS = 256
NT = TOK // TS
CW = 31
ALPHA = 2.0 ** 0.25
EPS = 1e-5
MAGIC = 12582912.0
TWO_PI = 2.0 * math.pi
DEBUG_H = False
S5_HALF = True
S5_DELAY = True
CAP = 384
NS4 = CAP // 128
I32 = mybir.dt.int32


import types


def _freeze(fn):
    if fn.__closure__ is None:
        return fn
    cells = []
    for c in fn.__closure__:
        try:
            cells.append(types.CellType(c.cell_contents))
        except ValueError:
            cells.append(c)
    return types.FunctionType(fn.__code__, fn.__globals__, fn.__name__, fn.__defaults__, tuple(cells))


class Sched:
    def __init__(self, nc, es, ndma=14):
        self.nc = nc
        self.names = ['pe', 'act', 'dve', 'pool', 'sp']
        self.prog = {k: [] for k in self.names}
        self.sem = {k: es.enter_context(nc.semaphore('s_' + k)) for k in ['pe', 'act', 'dve', 'pool']}
        self.cnt = {k: 0 for k in self.sem}
        self.dsem = [es.enter_context(nc.semaphore('d%d' % i)) for i in range(ndma)]
        self.dcnt = [0] * ndma
        self.dnext = 0
        self.seen = {k: {} for k in self.names}
        self.lastw = {}
        self.readers = {}
        self.rec = None
        self.sim_eng = {}
        self.sim_w = {}
        self.sim_r = {}

    def record(self, section):
        self.rec = []
        section()
        r = self.rec
        self.rec = None
        return r

    COST = {'pe': 0.17, 'act': 0.42, 'dve': 0.36, 'pool': 0.75, 'sp': 0.1}

    def replay_merged(self, chains):
        pos = [0] * len(chains)
        now = max(self.sim_eng.values()) if self.sim_eng else 0.0
        for k in self.names:
            self.sim_eng[k] = now
        while True:
            best, bkey, bt = None, None, 0.0
            for i, c in enumerate(chains):
                if pos[i] >= len(c):
                    continue
                kind, eng, fn, reads, writes, cost = c[pos[i]]
                t = self.sim_eng[eng]
                for r in reads:
                    t = max(t, self.sim_w.get(r, 0.0))
                for w in writes:
                    t = max(t, self.sim_w.get(w, 0.0), self.sim_r.get(w, 0.0))
                key = (t, pos[i] / len(c))
                if best is None or key < bkey:
                    best, bkey, bt = i, key, t
            if best is None:
                break
            kind, eng, fn, reads, writes, cost = chains[best][pos[best]]
            pos[best] += 1
            if kind == 'op':
                end = bt + (cost if cost is not None else self.COST[eng])
                self.sim_eng[eng] = end
                done = end + 0.06
                self.op(eng, fn, reads, writes)
            else:
                self.sim_eng[eng] = bt + (1.0 if eng == 'pool' else 0.1)
                done = bt + 2.5
                self.dma(eng, fn, reads, writes)
            for r in reads:
                self.sim_r[r] = max(self.sim_r.get(r, 0.0), done)
            for w in writes:
                self.sim_w[w] = done
                self.sim_r[w] = 0.0

    def _semobj(self, k):
        return self.sem[k] if isinstance(k, str) else self.dsem[k]

    def _waits(self, eng, deps):
        best = {}
        for (k, v) in deps:
            if k == 'pe' and eng == 'pe':
                continue
            if self.seen[eng].get(k, 0) >= v:
                continue
            best[k] = max(best.get(k, 0), v)
        for k, v in best.items():
            self.seen[eng][k] = v
            so = self._semobj(k)
            self.prog[eng].append(lambda e, so=so, v=v: e.wait_ge(so, v))

    def _deps(self, reads, writes):
        deps = []
        for r in reads:
            if r in self.lastw:
                deps.append(self.lastw[r])
        for w in writes:
            if w in self.lastw:
                deps.append(self.lastw[w])
            deps.extend(self.readers.get(w, []))
        return deps

    def _commit(self, tok, reads, writes):
        for r in reads:
            self.readers.setdefault(r, []).append(tok)
        for w in writes:
            self.lastw[w] = tok
            self.readers[w] = []

    def op(self, eng, fn, reads=(), writes=(), cost=None):
        fn = _freeze(fn)
        if self.rec is not None:
            self.rec.append(('op', eng, fn, tuple(reads), tuple(writes), cost))
            return
        self._waits(eng, self._deps(reads, writes))
        self.cnt[eng] += 1
        so = self.sem[eng]
        self.prog[eng].append(lambda e, fn=fn, so=so: fn(e).then_inc(so, 1))
        self._commit((eng, self.cnt[eng]), reads, writes)

    def dma(self, eng, fn, reads=(), writes=()):
        fn = _freeze(fn)
        if self.rec is not None:
            self.rec.append(('dma', eng, fn, tuple(reads), tuple(writes), None))
            return
        i = self.dnext
        self.dnext = (self.dnext + 1) % len(self.dsem)
        deps = self._deps(reads, writes)
        if self.dcnt[i] > 0:
            deps.append((i, self.dcnt[i]))
        self._waits(eng, deps)
        self.dcnt[i] += 16
        so = self.dsem[i]
        self.prog[eng].append(lambda e, fn=fn, so=so: fn(e).then_inc(so, 16))
        self._commit((i, self.dcnt[i]), reads, writes)

    def barrier(self):
        deps = [(k, v) for k, v in self.cnt.items() if v > 0]
        deps += [(i, v) for i, v in enumerate(self.dcnt) if v > 0]
        for eng in self.names:
            self._waits(eng, deps)

    def emit(self):
        nc = self.nc
        prog = self.prog
        with nc.Block() as block:
            @block.tensor
            def _(e):
                for f in prog['pe']:
                    f(e)

            @block.scalar
            def _(e):
                for f in prog['act']:
                    f(e)

            @block.vector
            def _(e):
                for f in prog['dve']:
                    f(e)

            @block.gpsimd
            def _(e):
                for f in prog['pool']:
                    f(e)

            @block.sync
            def _(e):
                for f in prog['sp']:
                    f(e)
        self.prog = {k: [] for k in self.names}


def build_nc():
    nc = bass.Bass("TRN2", target_bir_lowering=False)

    def din(name, shape):
        return nc.dram_tensor(name, list(shape), F32, kind="ExternalInput").ap()

    xT = din("xT", [D, TOK])
    xtok = din("xtok", [TOK, D])
    w_in = din("w_in", [D, 3584])
    w_glu = din("w_glu", [512, 2048])
    w_cout = din("w_cout", [512, 1024])
    w_out = din("w_out", [D, D])
    w_rt = din("w_rt", [D, 36])
    w_gate = din("w_gate", [32, D, 256])
    w_up = din("w_up", [32, D, 256])
    w_down = din("w_down", [32, 256, D])
    cols = din("cols", [128, 72])
    convw = din("convw", [128, 4 * CW])
    rep5 = din("rep5", [128, 5 * D])
    brt = din("brt", [128, 36])
    pq = din("pq", [128, 48])
    bc = din("bc", [128, 4 * 256])
    ident_d = din("ident", [128, 128])
    kk_d = din("kk", [128, TS])
    out = nc.dram_tensor("out", [TOK, D], F32, kind="ExternalOutput").ap()
    tri_d = din("tri", [128, 128])
    ecap_d = din("ecap", [128, 32])
    bdmask_d = din("bdmask", [128, 128])
    xbuf_d = nc.dram_tensor("xbuf_scr", [32 * CAP, D], BF16, kind="Internal").ap()
    ybuf_d = nc.dram_tensor("ybuf_scr", [32 * CAP, D], BF16, kind="Internal").ap()
    zb_d = nc.dram_tensor("zb_scr", [TOK, D], F32, kind="Internal").ap()
    rep5v = rep5.rearrange("p (a d) -> p a d", a=5)

    with ExitStack() as es0:
        S = Sched(nc, es0)
        dest_all = es0.enter_context(nc.sbuf_tensor("dest_all", [128, 64], I32))
        gsel_all = es0.enter_context(nc.sbuf_tensor("gsel_all", [128, 32, 2], F32))
        eps_t = es0.enter_context(nc.sbuf_tensor("eps_t", [128, 1], F32))
        S.op('dve', lambda e: e.memset(eps_t[:], EPS), writes=['eps'])
        regh = {}

        def bcreg(e, tag):
            if tag not in regh:
                regh[tag] = e.alloc_register('bcr' + tag)
                e.reg_mov(regh[tag], 32 * CAP - 1)
            return regh[tag]
        vop = lambda fn, r, w, c=None: S.op('dve', fn, reads=r, writes=w, cost=c)
        aop = lambda fn, r, w, c=None: S.op('act', fn, reads=r, writes=w, cost=c)
        pop = lambda fn, r, w, c=None: S.op('pool', fn, reads=r, writes=w, cost=c)
        peop = lambda fn, r, w, c=None: S.op('pe', fn, reads=r, writes=w, cost=c)

        with ExitStack() as es:
            def sb(name, shape, dt=F32):
                return es.enter_context(nc.sbuf_tensor(name, list(shape), dt))
            PS = [es.enter_context(nc.psum_tensor("ps%d" % i, [128, 512], F32)) for i in range(8)]

            win_bf = sb("win_bf", [128, 8, 3584], BF16)
            wglu_bf = sb("wglu_bf", [128, 4, 2048], BF16)
            wcout_bf = sb("wcout_bf", [128, 4, 1024], BF16)
            wout_bf = sb("wout_bf", [128, 8, 1024], BF16)
            wrt32 = sb("wrt32", [128, 8, 36])
            cols_t = sb("cols_t", [128, 72])
            convw_t = sb("convw_t", [128, 4, CW])
            brt_t = sb("brt_t", [128, 36])
            ident = sb("ident_sb", [128, 128])
            ones_bf = sb("ones_bf", [128, 128], BF16)
            halfpi = sb("halfpi", [128, 1])
            repA = sb("repA", [128, 3, D])
            cdiag = sb("cdiag", [128, CW, 128], BF16)
            TC = 4
            NCH = TS // TC
            WZ = sb("WZ", [128, 4, TC, 2, 128], BF16)
            KM = sb("KM", [128, 4, TC, 128], BF16)
            WY = sb("WY", [128, 16, TC, 2, 32], BF16)
            cos2 = sb("cos2", [128, 16, NCH])
            sin2 = sb("sin2", [128, 16, NCH])
            R4rep = sb("R4rep", [128, 16, NCH])
            r4 = sb("r4", [128, 16])
            carry = sb("carry", [128, 2, 16])
            fk = lambda i: 'f%d' % i
            bk = lambda i: 'b%d' % i

            S.dma('sp', lambda e: e.dma_start(out=repA[:], in_=rep5v[:, 0:3, :]), writes=['rep'])
            S.dma('sp', lambda e: e.dma_start(out=wrt32[:], in_=w_rt.rearrange("(k p) c -> p k c", p=128)), writes=['wrt'])
            S.dma('sp', lambda e: e.dma_start(out=cols_t[:], in_=cols), writes=['cols'])
            S.dma('sp', lambda e: e.dma_start(out=convw_t[:], in_=convw.rearrange("p (c j) -> p c j", c=4)), writes=['convw'])
            S.dma('sp', lambda e: e.dma_start(out=brt_t[:], in_=brt), writes=['brt'])
            S.dma('sp', lambda e: e.dma_start(out=ident[:], in_=ident_d), writes=['ident'])
            vop(lambda e: e.memset(halfpi[:], math.pi / 2), [], ['halfpi'])
            vop(lambda e: e.memset(ones_bf[:], 1.0), [], ['ones'])

            with ExitStack() as esS:
                def sbs(name, shape, dt=F32):
                    return esS.enter_context(nc.sbuf_tensor(name, list(shape), dt))
                pq_t = sbs("pq_t", [128, 3, 16])
                bc_t = sbs("bc_t", [128, 4, 16, 16])
                kk = sbs("kk_sb", [128, NCH])
                NSL = 30
                small = sbs("small", [128, NSL * 16])
                vbrs = [sbs("vbr%d" % i, [128, 256]) for i in range(2)]
                vbis = [sbs("vbi%d" % i, [128, 256]) for i in range(2)]
                tmpbs = [sbs("tmpb%d" % i, [128, 256]) for i in range(2)]
                tmpas = [sbs("tmpa%d" % i, [128, 256]) for i in range(2)]
                E4rs = [sbs("E4r%d" % i, [128, 128]) for i in range(4)]
                E4is = [sbs("E4i%d" % i, [128, 128]) for i in range(4)]
                CTE = sbs("CTE", [128, 4, 2, 128])
                wts = [[sbs("wt%d_%d" % (i, jj), [128, 256]) for i in range(4)] for jj in range(2)]
                bdm = sbs("bdm", [128, 128])
                S.dma('sp', lambda e: e.dma_start(out=bdm[:], in_=bdmask_d), writes=['bdm'])
                tbw = sbs("tbw", [128, 2, 16 * NCH])
                ptmp = sbs("ptmp", [128, 3, 16 * TC])
                S.dma('sp', lambda e: e.dma_start(out=pq_t[:], in_=pq.rearrange("p (a q) -> p a q", a=3)), writes=['pq'])
                S.dma('sp', lambda e: e.dma_start(out=bc_t[:], in_=bc.rearrange("p (a q h) -> p a q h", a=4, q=16)), writes=['bc'])
                S.dma('sp', lambda e: e.dma_start(out=kk[:], in_=kk_d[:, 0:NCH]), writes=['kk'])
                for k in range(8):
                    S.dma('pool', lambda e, k=k: e.dma_start(out=win_bf[:, k, :], in_=w_in[k * 128:(k + 1) * 128, :]), writes=['win'])
                    S.dma('pool', lambda e, k=k: e.dma_start(out=wout_bf[:, k, :], in_=w_out[k * 128:(k + 1) * 128, :]), writes=['wout'])
                for k in range(4):
                    S.dma('pool', lambda e, k=k: e.dma_start(out=wglu_bf[:, k, :], in_=w_glu[k * 128:(k + 1) * 128, :]), writes=['wglu'])
                    S.dma('pool', lambda e, k=k: e.dma_start(out=wcout_bf[:, k, :], in_=w_cout[k * 128:(k + 1) * 128, :]), writes=['wcout'])
                sm = lambda i: small[:, i * 16:(i + 1) * 16]
                smc = lambda i, q: small[:, i * 16 + q:i * 16 + q + 1]
                sk = lambda i: 'sm%d' % i
                DT, RHO, TH, Y0, DEN, FR, FI, T1, T2, T3 = range(10)
                PR = lambda k: 10 + k
                PI = lambda k: 15 + k
                GR = lambda k: 20 + k
                GI = lambda k: 24 + k
                Y4 = 28
                lre, lim, ldt = pq_t[:, 0, :], pq_t[:, 1, :], pq_t[:, 2, :]
                TT = lambda o, a, b, op: vop(lambda e: e.tensor_tensor(out=sm(o), in0=sm(a), in1=sm(b), op=op), [sk(a), sk(b)], [sk(o)])
                aop(lambda e: e.activation(out=sm(DT), in_=ldt, func=AF.Exp), ['pq'], [sk(DT)])
                vop(lambda e: e.tensor_tensor(out=sm(RHO), in0=lre, in1=sm(DT), op=OP.mult), ['pq', sk(DT)], [sk(RHO)])
                vop(lambda e: e.tensor_tensor(out=sm(TH), in0=lim, in1=sm(DT), op=OP.mult), ['pq', sk(DT)], [sk(TH)])
                vop(lambda e: e.tensor_scalar(out=sm(Y0), in0=sm(TH), scalar1=1.0 / TWO_PI, scalar2=None, op0=OP.mult), [sk(TH)], [sk(Y0)])
                vop(lambda e: e.memset(sm(PR(0)), 1.0), [], [sk(PR(0))])
                vop(lambda e: e.memset(sm(PI(0)), 0.0), [], [sk(PI(0))])
                P4 = lambda base: small[:, base * 16:(base + TC) * 16]
                pt = lambda i: ptmp[:, i, :]
                prk = [sk(PR(k)) for k in range(1, TC + 1)]
                pik = [sk(PI(k)) for k in range(1, TC + 1)]
                for k in range(1, TC + 1):
                    aop(lambda e: e.activation(out=pt(2)[:, (k - 1) * 16:k * 16], in_=sm(RHO), func=AF.Exp, scale=float(k)), [sk(RHO)], ['pt2'])
                    vop(lambda e: e.tensor_scalar(out=pt(0)[:, (k - 1) * 16:k * 16], in0=sm(Y0), scalar1=float(k), scalar2=None, op0=OP.mult), [sk(Y0)], ['pt0'])
                vop(lambda e: e.tensor_scalar(out=pt(1), in0=pt(0), scalar1=MAGIC, scalar2=MAGIC, op0=OP.add, op1=OP.subtract), ['pt0'], ['pt1'])
                vop(lambda e: e.tensor_tensor(out=pt(0), in0=pt(0), in1=pt(1), op=OP.subtract), ['pt0', 'pt1'], ['pt0'])
                aop(lambda e: e.activation(out=pt(1), in_=pt(0), func=AF.Abs), ['pt0'], ['pt1'])
                aop(lambda e: e.activation(out=P4(PI(1)), in_=pt(0), func=AF.Sin, scale=TWO_PI), ['pt0'], pik)
                aop(lambda e: e.activation(out=P4(PR(1)), in_=pt(1), func=AF.Sin, scale=-TWO_PI, bias=halfpi[:, 0:1]), ['pt1', 'halfpi'], prk)
                vop(lambda e: e.tensor_tensor(out=P4(PR(1)), in0=P4(PR(1)), in1=pt(2), op=OP.mult), prk + ['pt2'], prk)
                vop(lambda e: e.tensor_tensor(out=P4(PI(1)), in0=P4(PI(1)), in1=pt(2), op=OP.mult), pik + ['pt2'], pik)
                vop(lambda e: e.tensor_copy(out=r4[:], in_=pt(2)[:, (TC - 1) * 16:TC * 16]), ['pt2'], ['r4'])
                vop(lambda e: e.tensor_tensor(out=sm(T1), in0=lre, in1=lre, op=OP.mult), ['pq'], [sk(T1)])
                vop(lambda e: e.tensor_tensor(out=sm(T2), in0=lim, in1=lim, op=OP.mult), ['pq'], [sk(T2)])
                TT(DEN, T1, T2, OP.add)
                vop(lambda e: e.reciprocal(out=sm(DEN), in_=sm(DEN)), [sk(DEN)], [sk(DEN)])
                vop(lambda e: e.tensor_scalar(out=sm(T1), in0=sm(PR(1)), scalar1=-1.0, scalar2=None, op0=OP.add), [sk(PR(1))], [sk(T1)])
                vop(lambda e: e.tensor_tensor(out=sm(T2), in0=sm(T1), in1=lre, op=OP.mult), [sk(T1), 'pq'], [sk(T2)])
                vop(lambda e: e.tensor_tensor(out=sm(T3), in0=sm(PI(1)), in1=lim, op=OP.mult), [sk(PI(1)), 'pq'], [sk(T3)])
                TT(T2, T2, T3, OP.add)
                TT(FR, T2, DEN, OP.mult)
                vop(lambda e: e.tensor_tensor(out=sm(T2), in0=sm(PI(1)), in1=lre, op=OP.mult), [sk(PI(1)), 'pq'], [sk(T2)])
                vop(lambda e: e.tensor_tensor(out=sm(T3), in0=sm(T1), in1=lim, op=OP.mult), [sk(T1), 'pq'], [sk(T3)])
                TT(T2, T2, T3, OP.subtract)
                TT(FI, T2, DEN, OP.mult)
                for k in range(TC):
                    TT(T1, PR(k), FR, OP.mult)
                    TT(T2, PI(k), FI, OP.mult)
                    TT(GR(k), T1, T2, OP.subtract)
                    TT(T1, PR(k), FI, OP.mult)
                    TT(T2, PI(k), FR, OP.mult)
                    TT(GI(k), T1, T2, OP.add)
                bq = lambda i: sm(i).unsqueeze(2).to_broadcast([128, 16, 16])
                q3 = lambda t: t[:].rearrange("p (q h) -> p q h", h=16)
                bre, bim, ctr, cti = bc_t[:, 0, :, :], bc_t[:, 1, :, :], bc_t[:, 2, :, :], bc_t[:, 3, :, :]
                vop(lambda e: e.memset(CTE[:], 0.0), [], ['CTE'])
                for cq in range(4):
                    for g2 in range(2):
                        hsl = slice(g2 * 64, (g2 + 1) * 64)
                        o0 = CTE[hsl, cq, 0, :].rearrange("p (pl c) -> p pl c", c=32)[:, :, g2 * 16:(g2 + 1) * 16]
                        o1 = CTE[hsl, cq, 1, :].rearrange("p (pl c) -> p pl c", c=32)[:, :, g2 * 16:(g2 + 1) * 16]
                        vop(lambda e: e.tensor_copy(out=o0, in_=bc_t[hsl, 2, cq * 4:(cq + 1) * 4, :]), ['bc'], ['CTE'])
                        vop(lambda e: e.tensor_scalar(out=o1, in0=bc_t[hsl, 3, cq * 4:(cq + 1) * 4, :], scalar1=-1.0, scalar2=None, op0=OP.mult), ['bc'], ['CTE'])

                def emit_VB(k):
                    vbr, vbi, tmpa, tmpb = vbrs[k % 2], vbis[k % 2], tmpas[k % 2], tmpbs[k % 2]
                    kvr, kvi, kta, ktb = 'vbr%d' % (k % 2), 'vbi%d' % (k % 2), 'tmpa%d' % (k % 2), 'tmpb%d' % (k % 2)
                    vop(lambda e: e.tensor_tensor(out=q3(tmpa), in0=bim, in1=bq(GI(k)), op=OP.mult), ['bc', sk(GI(k))], [kta])
                    vop(lambda e: e.tensor_tensor(out=q3(tmpb), in0=bre, in1=bq(GI(k)), op=OP.mult), ['bc', sk(GI(k))], [ktb])
                    vop(lambda e: e.tensor_tensor(out=q3(vbr), in0=bre, in1=bq(GR(k)), op=OP.mult), ['bc', sk(GR(k))], [kvr])
                    vop(lambda e: e.tensor_tensor(out=q3(vbi), in0=bim, in1=bq(GR(k)), op=OP.mult), ['bc', sk(GR(k))], [kvi])
                    vop(lambda e: e.tensor_tensor(out=vbr[:], in0=vbr[:], in1=tmpa[:], op=OP.subtract), [kvr, kta], [kvr])
                    vop(lambda e: e.tensor_tensor(out=vbi[:], in0=vbi[:], in1=tmpb[:], op=OP.add), [kvi, ktb], [kvi])

                def emit_E4(k):
                    vbr, vbi = vbrs[k % 2], vbis[k % 2]
                    kvr, kvi = 'vbr%d' % (k % 2), 'vbi%d' % (k % 2)
                    for cq in range(4):
                        E4r, E4i = E4rs[cq], E4is[cq]
                        ekr, eki = 'E4r%d' % cq, 'E4i%d' % cq
                        for ri, (src, E4, ek, ksrc) in enumerate([(vbr, E4r, ekr, kvr), (vbi, E4i, eki, kvi)]):
                            for g2 in range(2):
                                hsl = slice(g2 * 64, (g2 + 1) * 64)
                                o_ = E4[hsl, :].rearrange("p (pl c) -> p pl c", c=32)[:, :, g2 * 16:(g2 + 1) * 16]
                                i_ = src[hsl, cq * 64:(cq + 1) * 64].rearrange("p (pl h) -> p pl h", h=16)
                                if g2 == 0:
                                    vop(lambda e: e.tensor_copy(out=o_, in_=i_), [ksrc], [ek + 'p'])
                                else:
                                    aop(lambda e: e.activation(out=o_, in_=i_, func=AF.Identity), [ksrc], [ek + 'a'])
                            peop(lambda e: e.transpose(out=PS[cq][:, ri * 128:(ri + 1) * 128], in_=E4[:], identity=ident[:]), [ek + 'p', ek + 'a', 'ident'], ['ps%d' % cq])
                            aop(lambda e: e.activation(out=WZ[:, cq, TC - 1 - k, ri, :], in_=PS[cq][:, ri * 128:(ri + 1) * 128], func=AF.Identity), ['ps%d' % cq], ['WZ%d' % cq])
                        peop(lambda e: e.matmul(PS[4 + cq][:, 0:128], lhsT=E4r[:], rhs=CTE[:, cq, 0, :], start=True, stop=False), [ekr + 'p', ekr + 'a', 'CTE'], ['ps%d' % (4 + cq)])
                        peop(lambda e: e.matmul(PS[4 + cq][:, 0:128], lhsT=E4i[:], rhs=CTE[:, cq, 1, :], start=False, stop=True), [eki + 'p', eki + 'a', 'CTE'], ['ps%d' % (4 + cq)])
                        vop(lambda e: e.tensor_tensor(out=KM[:, cq, k, :], in0=PS[4 + cq][:, 0:128], in1=bdm[:], op=OP.mult), ['ps%d' % (4 + cq), 'bdm'], ['KM%d' % cq])
                for i_ in range(4):
                    vop(lambda e: e.memset(E4rs[i_][:], 0.0), [], ['E4r%dp' % i_, 'E4r%da' % i_])
                    vop(lambda e: e.memset(E4is[i_][:], 0.0), [], ['E4i%dp' % i_, 'E4i%da' % i_])
                emit_VB(0)
                for k in range(TC):
                    if k + 1 < TC:
                        emit_VB(k + 1)
                    emit_E4(k)
                vop(lambda e: e.memset(WY[:], 0.0), [], ['WYp', 'WYa'])
                for j in range(TC):
                    w0, w1, w2, w3 = wts[j % 2]
                    w0k, w1k, w2k, w3k = ['wt%d_%d' % (i_, j % 2) for i_ in range(4)]
                    kpr, kpi = sk(PR(j + 1)), sk(PI(j + 1))
                    vop(lambda e: e.tensor_tensor(out=q3(w0), in0=cti, in1=bq(PI(j + 1)), op=OP.mult), ['bc', kpi], [w0k])
                    vop(lambda e: e.tensor_tensor(out=q3(w1), in0=ctr, in1=bq(PR(j + 1)), op=OP.mult), ['bc', kpr], [w1k])
                    vop(lambda e: e.tensor_tensor(out=q3(w2), in0=cti, in1=bq(PR(j + 1)), op=OP.mult), ['bc', kpr], [w2k])
                    vop(lambda e: e.tensor_tensor(out=q3(w3), in0=ctr, in1=bq(PI(j + 1)), op=OP.mult), ['bc', kpi], [w3k])
                    vop(lambda e: e.tensor_tensor(out=w1[:], in0=w1[:], in1=w0[:], op=OP.subtract), [w1k, w0k], [w1k])
                    vop(lambda e: e.tensor_tensor(out=w3[:], in0=w3[:], in1=w2[:], op=OP.add), [w3k, w2k], [w3k])
                    for g2 in range(2):
                        hsl = slice(g2 * 64, (g2 + 1) * 64)
                        vop(lambda e: e.tensor_copy(out=WY[hsl, :, j, 0, g2 * 16:(g2 + 1) * 16], in_=q3(w1)[hsl, :, :]), [w1k], ['WYp'])
                        aop(lambda e: e.activation(out=WY[hsl, :, j, 1, g2 * 16:(g2 + 1) * 16], in_=q3(w3)[hsl, :, :], func=AF.Identity, scale=-1.0), [w3k], ['WYa'])
                vop(lambda e: e.tensor_scalar(out=sm(Y4), in0=sm(Y0), scalar1=float(TC), scalar2=None, op0=OP.mult), [sk(Y0)], [sk(Y4)])
                kkb = kk[:].unsqueeze(1).to_broadcast([128, 16, NCH])
                y4b = sm(Y4).unsqueeze(2).to_broadcast([128, 16, NCH])
                flat = lambda t: t[:].rearrange("p q c -> p (q c)")
                vop(lambda e: e.tensor_tensor(out=tbw[:, 0, :].rearrange("p (q c) -> p q c", c=NCH), in0=kkb, in1=y4b, op=OP.mult), ['kk', sk(Y4)], ['tb0'])
                vop(lambda e: e.tensor_scalar(out=tbw[:, 1, :], in0=tbw[:, 0, :], scalar1=MAGIC, scalar2=MAGIC, op0=OP.add, op1=OP.subtract), ['tb0'], ['tb1'])
                vop(lambda e: e.tensor_tensor(out=tbw[:, 0, :], in0=tbw[:, 0, :], in1=tbw[:, 1, :], op=OP.subtract), ['tb0', 'tb1'], ['tb0'])
                aop(lambda e: e.activation(out=tbw[:, 1, :], in_=tbw[:, 0, :], func=AF.Abs), ['tb0'], ['tb1'])
                aop(lambda e: e.activation(out=flat(sin2), in_=tbw[:, 0, :], func=AF.Sin, scale=TWO_PI), ['tb0'], ['sin2'])
                aop(lambda e: e.activation(out=flat(cos2), in_=tbw[:, 1, :], func=AF.Sin, scale=-TWO_PI, bias=halfpi[:, 0:1]), ['tb1', 'halfpi'], ['cos2'])
                vop(lambda e: e.tensor_copy(out=R4rep[:], in_=r4[:].unsqueeze(2).to_broadcast([128, 16, NCH])), ['r4'], ['R4rep'])
                vop(lambda e: e.memset(R4rep[:, :, 0:1], 0.0), ['R4rep'], ['R4rep'])
                S.barrier()
                S.emit()

            xT_bfs = [sb("xT_bf%d" % i, [128, 8, TS], BF16) for i in range(2)]
            u_bf = sb("u_bf", [128, 4, TS], BF16)
            um = sb("um", [128, TC - 1, TS], BF16)
            u_pm = sb("u_pm", [128, 4, TS], BF16)
            vbuf = sb("vbuf", [128, 4, 30 + TS], BF16)
            y_bf = sb("y_bf", [128, 4, TS], BF16)
            c_bf = sb("c_bf", [128, 4, TS], BF16)
            cs_bf = sb("cs_bf", [128, 4, TS], BF16)
            merged = sb("merged", [128, 8, TS], BF16)
            hb = sb("hb", [128, D], BF16)
            tri_bf = sb("tri_bf", [128, 128], BF16)
            ecap = sb("ecap_sb", [128, 32])
            selsum = sb("selsum", [128, 32], BF16)
            sel_bf = sb("sel_bf", [128, 32], BF16)
            NF = 9
            FT = [sb("f32t%d" % i, [128, TS]) for i in range(NF)]
            BT_ = [sb("bft%d" % i, [128, TS], BF16) for i in range(4)]
            xtk = sb("xtk", [128, D])
            zt = sb("zt", [128, D])
            hT32 = sb("hT32", [128, 8, 128])
            rsm = sb("rsm", [128, 512])
            stats = sb("stats", [128, 16])
            halo = sb("halo", [128, 4, 32], BF16)
            tmp4 = sb("tmp4", [128, 2, 4])
            CL = [sb("cl%d" % i, [128, TS]) for i in range(2)]
            S.dma('pool', lambda e: e.dma_start(out=tri_bf[:], in_=tri_d), writes=['tri'])
            S.dma('sp', lambda e: e.dma_start(out=ecap[:], in_=ecap_d), writes=['ecap'])
            vop(lambda e: e.memset(selsum[:], 0.0), [], ['selsum'])
            vop(lambda e: e.memset(um[:], 0.0), [], ['um'])

            bcol = lambda off, m: cols_t[:, off + m:off + m + 1]
            B_IN, B_GLU, B_COUT, DCOL, CONVB, LNCG, LNCB = 0, 28, 44, 52, 56, 60, 64
            mmrot = [0]
            NROT = 6

            def mmbank():
                b = mmrot[0]
                mmrot[0] = (mmrot[0] + 1) % NROT
                return b

            xcur = [None, None]

            def proj(m):
                b = mmbank()
                xT_bf, xkey = xcur
                for k in range(8):
                    peop(lambda e: e.matmul(PS[b][:, 0:TS], lhsT=win_bf[:, k, m * 128:(m + 1) * 128], rhs=xT_bf[:, k, :], start=(k == 0), stop=(k == 7)),
                         ['win', xkey], ['ps%d' % b])
                return b

            def load_xT(ti_):
                buf = xT_bfs[ti_ % 2]
                tt0 = ti_ * TS
                S.dma('pool', lambda e: e.dma_start(out=buf[:], in_=xT.rearrange("(k p) t -> p k t", p=128)[:, :, tt0:tt0 + TS]), writes=['xT%d' % (ti_ % 2)])

            TPS = SEQ // TS
            prev_tile = None
            for ti in range(NT):
                t0 = ti * TS
                first = (ti % TPS == 0)
                if ti == 0:
                    load_xT(0)
                xcur[0], xcur[1] = xT_bfs[ti % 2], 'xT%d' % (ti % 2)
                if ti + 1 < NT:
                    load_xT(ti + 1)
                if first:
                    pop(lambda e: e.memset(vbuf[:, :, 0:30], 0.0), [], ['vhalo'])
                    pop(lambda e: e.memset(carry[:], 0.0), [], ['carry'])

                for m in range(4):
                    b = proj(m)
                    aop(lambda e: e.activation(out=u_bf[:, m, :], in_=PS[b][:, 0:TS], func=AF.Identity, bias=bcol(B_IN, m)), ['ps%d' % b, 'cols'], ['u%d' % m])
                    aop(lambda e: e.activation(out=u_pm[:, m, :].rearrange("p (i c) -> p c i", i=TC), in_=PS[b][:, 0:TS].rearrange("p (c i) -> p c i", i=TC),
                                               func=AF.Identity, bias=bcol(B_IN, m)), ['ps%d' % b, 'cols'], ['up%d' % m])
                for c in range(4):
                    b = proj(8 + c)
                    aop(lambda e: e.activation(out=y_bf[:, c, :], in_=PS[b][:, 0:TS], func=AF.Sigmoid, bias=bcol(B_IN, 8 + c)), ['ps%d' % b, 'cols'], ['y%d' % c])
                for c in range(4):
                    b = proj(4 + c)
                    vop(lambda e: e.scalar_tensor_tensor(out=vbuf[:, c, 30:30 + TS], in0=PS[b][:, 0:TS], scalar=bcol(B_IN, 4 + c), in1=y_bf[:, c, :], op0=OP.add, op1=OP.mult),
                        ['ps%d' % b, 'cols', 'y%d' % c], ['v%d' % c])

                def sec_conv():
                    for c in range(4):
                        for j in range(CW):
                            if j % 4 != 3:
                                aop(lambda e: e.activation(out=cdiag[:, j, :], in_=ident[:], func=AF.Identity, scale=convw_t[:, c, j:j + 1]), ['ident', 'convw'], ['cd%d' % j])
                            else:
                                vop(lambda e: e.tensor_scalar(out=cdiag[:, j, :], in0=ident[:], scalar1=convw_t[:, c, j:j + 1], scalar2=None, op0=OP.mult), ['ident', 'convw'], ['cd%d' % j])
                        for j in range(CW):
                            peop(lambda e: e.matmul(PS[5][:, 0:TS], lhsT=cdiag[:, j, :], rhs=vbuf[:, c, j:j + TS], start=(j == 0), stop=(j == CW - 1)),
                                 ['cd%d' % j, 'v%d' % c, 'vhalo'], ['ps5'])
                        aop(lambda e: e.activation(out=c_bf[:, c, :], in_=PS[5][:, 0:TS], func=AF.Identity, bias=bcol(CONVB, c)), ['ps5', 'cols'], ['c_bf%d' % c])
                        aop(lambda e: e.activation(out=cs_bf[:, c, :], in_=PS[5][:, 0:TS], func=AF.Square, bias=bcol(CONVB, c)), ['ps5', 'cols'], ['cs%d' % c])
                        pop(lambda e: e.tensor_copy(out=halo[:, c, 0:30], in_=vbuf[:, c, TS:TS + 30]), ['v%d' % c], ['halo'])
                    for c in range(4):
                        peop(lambda e: e.matmul(PS[5][:, TS:2 * TS], lhsT=ones_bf[:], rhs=c_bf[:, c, :], start=(c == 0), stop=(c == 3)), ['ones', 'c_bf%d' % c], ['ps5'])
                    mean_t, rstd_t, msq_t, cn_t = CL[0], CL[1], CL[1], FT[8]
                    aop(lambda e: e.activation(out=mean_t[:], in_=PS[5][:, TS:2 * TS], func=AF.Identity, scale=1.0 / 512), ['ps5'], ['cl0'])
                    for c in range(4):
                        peop(lambda e: e.matmul(PS[5][:, TS:2 * TS], lhsT=ones_bf[:], rhs=cs_bf[:, c, :], start=(c == 0), stop=(c == 3)), ['ones', 'cs%d' % c], ['ps5'])
                    vop(lambda e: e.tensor_tensor(out=msq_t[:], in0=mean_t[:], in1=mean_t[:], op=OP.mult), ['cl0'], ['cl1'])
                    vop(lambda e: e.scalar_tensor_tensor(out=msq_t[:], in0=PS[5][:, TS:2 * TS], scalar=1.0 / 512, in1=msq_t[:], op0=OP.mult, op1=OP.subtract), ['ps5', 'cl1'], ['cl1'])
                    aop(lambda e: e.activation(out=msq_t[:], in_=msq_t[:], func=AF.Sqrt, bias=eps_t[:, 0:1]), ['cl1', 'eps'], ['cl1'])
                    vop(lambda e: e.reciprocal(out=rstd_t[:], in_=msq_t[:]), ['cl1'], ['cl1'])
                    for c in range(4):
                        pop(lambda e: e.tensor_tensor(out=cn_t[:], in0=c_bf[:, c, :], in1=mean_t[:], op=OP.subtract), ['c_bf%d' % c, 'cl0'], [fk(8)])
                        pop(lambda e: e.tensor_tensor(out=cn_t[:], in0=cn_t[:], in1=rstd_t[:], op=OP.mult), [fk(8), 'cl1'], [fk(8)])
                        aop(lambda e: e.activation(out=cs_bf[:, c, :], in_=cn_t[:], func=AF.Silu, scale=bcol(LNCG, c), bias=bcol(LNCB, c)), [fk(8), 'cols'], ['cs%d' % c])
                    for c in range(4):
                        pop(lambda e: e.tensor_copy(out=vbuf[:, c, 0:30], in_=halo[:, c, 0:30]), ['halo', 'v%d' % c], ['vhalo'])

                def sec_s5():
                    v3 = lambda t: t[:].rearrange("p (a c) -> p a c", a=4)
                    t1, t2, zre, zim, sre, sim = FT[0], FT[1], FT[2], FT[3], FT[4], FT[5]
                    k1, k2, kzr, kzi, ksr, ksi = fk(0), fk(1), fk(2), fk(3), fk(4), fk(5)

                    def PY(cq):
                        h_ = (cq % 2) if S5_HALF else 0
                        return PS[7 - h_][:, 0:TS], 'ps%d' % (7 - h_)

                    def SPb(cq, ri):
                        i_ = (cq % 2) * 2 + ri
                        return BT_[i_], bk(i_)

                    def stage_front(cq):
                        qs = slice(4 * cq, 4 * cq + 4)
                        C2 = cos2[:, qs, :].rearrange("p a c -> p (a c)")
                        S2 = sin2[:, qs, :].rearrange("p a c -> p (a c)")
                        RR = R4rep[:, qs, :].rearrange("p a c -> p (a c)")
                        ucq = u_bf[:, cq, :].rearrange("p (c i) -> p c i", i=TC)
                        for pl in range(4):
                            pr = slice(pl * 32, (pl + 1) * 32)
                            for ri in range(2):
                                for i in range(TC):
                                    ua = u_pm[pr, cq, i * NCH:(i + 1) * NCH]
                                    peop(lambda e: e.matmul(PS[3 + ri][:, pl * NCH:(pl + 1) * NCH], lhsT=WZ[pr, cq, i, ri, :], rhs=ua, start=(i == 0), stop=(i == TC - 1),
                                                            tile_position=(pl * 32, 0)), ['WZ', 'up%d' % cq], ['ps%d' % (3 + ri)])
                        for d in range(1, TC):
                            pop(lambda e: e.tensor_copy(out=um[:, d - 1, :].rearrange("p (c i) -> p c i", i=TC)[:, :, d:TC], in_=ucq[:, :, 0:TC - d]), ['u%d' % cq, 'um'], ['um%d' % d])
                        py, pyk = PY(cq)
                        for d in range(TC):
                            rhs_ = u_bf[:, cq, :] if d == 0 else um[:, d - 1, :]
                            peop(lambda e: e.matmul(py, lhsT=KM[:, cq, d, :], rhs=rhs_, start=(d == 0), stop=False), ['KM', 'u%d' % cq] + (['um%d' % d] if d else []), [pyk])
                        vop(lambda e: e.tensor_tensor(out=t1[:], in0=PS[3][:, 0:TS], in1=C2, op=OP.mult), ['ps3', 'cos2'], [k1])
                        vop(lambda e: e.tensor_tensor(out=t2[:], in0=PS[4][:, 0:TS], in1=S2, op=OP.mult), ['ps4', 'sin2'], [k2])
                        pop(lambda e: e.tensor_tensor(out=zre[:], in0=t1[:], in1=t2[:], op=OP.add), [k1, k2], [kzr])
                        vop(lambda e: e.tensor_tensor(out=sre[:], in0=PS[4][:, 0:TS], in1=C2, op=OP.mult), ['ps4', 'cos2'], [ksr])
                        vop(lambda e: e.tensor_tensor(out=sim[:], in0=PS[3][:, 0:TS], in1=S2, op=OP.mult), ['ps3', 'sin2'], [ksi])
                        pop(lambda e: e.tensor_tensor(out=zim[:], in0=sre[:], in1=sim[:], op=OP.subtract), [ksr, ksi], [kzi])
                        for ri, (zt_, kz) in enumerate([(zre, kzr), (zim, kzi)]):
                            vop(lambda e: e.tensor_tensor(out=tmp4[:, ri, :], in0=carry[:, ri, qs], in1=r4[:, qs], op=OP.mult), ['carry', 'r4'], ['tmp4_%d' % ri])
                            vop(lambda e: e.tensor_tensor(out=v3(zt_)[:, :, 0], in0=v3(zt_)[:, :, 0], in1=tmp4[:, ri, :], op=OP.add), [kz, 'tmp4_%d' % ri], [kz])
                        vop(lambda e: e.tensor_tensor_scan(out=sre[:], data0=RR, data1=zre[:], initial=0.0, op0=OP.mult, op1=OP.add), ['R4rep', kzr], [ksr])
                        vop(lambda e: e.tensor_tensor_scan(out=sim[:], data0=RR, data1=zim[:], initial=0.0, op0=OP.mult, op1=OP.add), ['R4rep', kzi], [ksi])
                        pop(lambda e: e.tensor_tensor(out=t1[:], in0=sre[:], in1=C2, op=OP.mult), [ksr, 'cos2'], [k1])
                        pop(lambda e: e.tensor_tensor(out=t2[:], in0=sim[:], in1=S2, op=OP.mult), [ksi, 'sin2'], [k2])
                        pop(lambda e: e.tensor_tensor(out=zre[:], in0=t1[:], in1=t2[:], op=OP.subtract), [k1, k2], [kzr])
                        vop(lambda e: e.tensor_tensor(out=t1[:], in0=sre[:], in1=S2, op=OP.mult), [ksr, 'sin2', kzr], [k1])
                        vop(lambda e: e.tensor_tensor(out=t2[:], in0=sim[:], in1=C2, op=OP.mult), [ksi, 'cos2', kzr], [k2])
                        vop(lambda e: e.tensor_tensor(out=zim[:], in0=t1[:], in1=t2[:], op=OP.add), [k1, k2], [kzi])
                        for ri, (zt_, kz) in enumerate([(zre, kzr), (zim, kzi)]):
                            spb, spk = SPb(cq, ri)
                            sp3 = spb[:].rearrange("p (a c) -> p a c", a=4)
                            aop(lambda e: e.activation(out=sp3[:, :, 1:NCH], in_=v3(zt_)[:, :, 0:NCH - 1], func=AF.Identity), [kz], [spk])
                            aop(lambda e: e.activation(out=sp3[:, :, 0], in_=carry[:, ri, qs], func=AF.Identity), ['carry'], [spk])
                            aop(lambda e: e.activation(out=carry[:, ri, qs], in_=v3(zt_)[:, :, NCH - 1], func=AF.Identity), [kz, spk], ['carry'])

                    def stage_back(cq):
                        py, pyk = PY(cq)
                        for pl in range(4):
                            q = 4 * cq + pl
                            pr = slice(pl * 32, (pl + 1) * 32)
                            for j in range(TC):
                                oj = py[pr, :].rearrange("p (c j) -> p j c", j=TC)[:, j, :]
                                for ri in range(2):
                                    spb, spk = SPb(cq, ri)
                                    peop(lambda e: e.matmul(oj, lhsT=WY[:, q, j, ri, :], rhs=spb[:, pl * NCH:(pl + 1) * NCH], start=False, stop=(ri == 1), tile_position=(0, pl * 32)),
                                         ['WY', spk], [pyk])
                        ys, g1 = FT[6], FT[7]
                        kA, kB = fk(6), fk(7)
                        vop(lambda e: e.scalar_tensor_tensor(out=ys[:], in0=u_bf[:, cq, :], scalar=bcol(DCOL, cq), in1=py, op0=OP.mult, op1=OP.add),
                            ['u%d' % cq, 'cols', pyk], [kA])
                        aop(lambda e: e.activation(out=g1[:], in_=ys[:], func=AF.Square), [kA], [kB])
                        vop(lambda e: e.tensor_scalar(out=g1[:], in0=g1[:], scalar1=0.044715, scalar2=1.0, op0=OP.mult, op1=OP.add), [kB], [kB])
                        vop(lambda e: e.tensor_tensor(out=g1[:], in0=g1[:], in1=ys[:], op=OP.mult), [kB, kA], [kB])
                        aop(lambda e: e.activation(out=g1[:], in_=g1[:], func=AF.Sigmoid, scale=1.5957691216057308), [kB], [kB])
                        vop(lambda e: e.tensor_tensor(out=y_bf[:, cq, :], in0=g1[:], in1=ys[:], op=OP.mult), [kB, kA], ['y%d' % cq])

                    if S5_DELAY:
                        stage_front(0)
                        for cq in range(1, 4):
                            stage_front(cq)
                            stage_back(cq - 1)
                        stage_back(3)
                    else:
                        for cq in range(4):
                            stage_front(cq)
                            stage_back(cq)

                def sec_ln(ti, t0):
                    for st in range(TS // 128):
                        r0 = t0 + st * 128
                        gst = ti * (TS // 128) + st
                        S.dma('sp', lambda e: e.dma_start(out=xtk[:], in_=xtok[r0:r0 + 128, :]), writes=['xtk'])
                        for hf in range(2):
                            hs = slice(hf * 512, (hf + 1) * 512)
                            b = hf
                            for k in range(8):
                                peop(lambda e: e.matmul(PS[b][:], lhsT=merged[:, k, st * 128:(st + 1) * 128], rhs=wout_bf[:, k, hs], start=(k == 0), stop=(k == 7)),
                                     ['wout', 'mg%d' % k], ['ps%d' % b], 0.28)
                            vop(lambda e: e.scalar_tensor_tensor(out=zt[:, hs], in0=xtk[:, hs], scalar=ALPHA, in1=PS[b][:], op0=OP.mult, op1=OP.add), ['xtk', 'ps%d' % b], ['zt%d' % hf], 0.6)
                            vop(lambda e: e.tensor_tensor(out=zt[:, hs], in0=zt[:, hs], in1=repA[:, 0, hs], op=OP.add), ['zt%d' % hf, 'rep'], ['zt%d' % hf], 0.6)
                            vop(lambda e: e.bn_stats(out=stats[:, hf * 6:(hf + 1) * 6], in_=zt[:, hs]), ['zt%d' % hf], ['bst%d' % hf], 0.65)
                        vop(lambda e: e.bn_aggr(out=stats[:, 12:14], in_=stats[:, 0:12]), ['bst0', 'bst1'], ['mv'])
                        aop(lambda e: e.activation(out=stats[:, 14:15], in_=stats[:, 13:14], func=AF.Sqrt, bias=eps_t[:, 0:1]), ['mv', 'eps'], ['sd'])
                        vop(lambda e: e.reciprocal(out=stats[:, 15:16], in_=stats[:, 14:15]), ['sd'], ['rs'])
                        vop(lambda e: e.tensor_scalar(out=zt[:], in0=zt[:], scalar1=stats[:, 12:13], scalar2=stats[:, 15:16], op0=OP.subtract, op1=OP.mult),
                            ['zt0', 'zt1', 'mv', 'rs'], ['zt0', 'zt1'], 0.85)
                        vop(lambda e: e.tensor_tensor(out=zt[:], in0=zt[:], in1=repA[:, 1, :], op=OP.mult), ['zt0', 'zt1', 'rep'], ['zt0', 'zt1'], 1.1)
                        vop(lambda e: e.tensor_tensor(out=zt[:], in0=zt[:], in1=repA[:, 2, :], op=OP.add), ['zt0', 'zt1', 'rep'], ['zt0', 'zt1'], 1.1)
                        aop(lambda e: e.activation(out=xtk[:], in_=zt[:], func=AF.Identity, scale=ALPHA), ['zt0', 'zt1', 'xtk'], ['xtk'], 1.06)
                        S.dma('sp', lambda e: e.dma_start(out=zb_d[r0:r0 + 128, :], in_=xtk[:]), reads=['xtk'], writes=['zb_d'])
                        if DEBUG_H:
                            S.dma('sp', lambda e: e.dma_start(out=out[r0:r0 + 128, :], in_=zt[:]), reads=['zt0', 'zt1'], writes=['out'])
                        for kb in range(2):
                            for kk4 in range(4):
                                k = kb * 4 + kk4
                                peop(lambda e: e.transpose(out=PS[2][:, kk4 * 128:(kk4 + 1) * 128], in_=zt[:, k * 128:(k + 1) * 128], identity=ident[:]),
                                     ['zt0', 'zt1', 'ident'], ['ps2'], 0.41)
                            aop(lambda e: e.activation(out=hT32[:, kb * 4:(kb + 1) * 4, :], in_=PS[2][:].rearrange("p (a b) -> p a b", a=4), func=AF.Identity), ['ps2'], ['hT32'], 0.5)
                        for k in range(8):
                            peop(lambda e: e.matmul(PS[2][:, 0:36], lhsT=hT32[:, k, :], rhs=wrt32[:, k, :], start=(k == 0), stop=(k == 7)), ['hT32', 'wrt'], ['ps2'], 0.27)
                        svop = lambda fn, r, w: vop(fn, r, w, 0.14)
                        R = lambda a, b: rsm[:, 192 + a:192 + b]
                        lg, zem, sel, ex, top8 = rsm[:, 0:36], rsm[:, 40:72], rsm[:, 72:104], rsm[:, 104:136], rsm[:, 136:144]
                        svop(lambda e: e.tensor_tensor(out=lg, in0=PS[2][:, 0:36], in1=brt_t[:], op=OP.add), ['ps2', 'brt'], ['lg'])
                        svop(lambda e: e.tensor_reduce(out=R(0, 1), in_=rsm[:, 0:4], axis=AX.X, op=OP.max), ['lg'], ['gmax'])
                        svop(lambda e: e.tensor_scalar(out=R(4, 8), in0=rsm[:, 0:4], scalar1=R(0, 1), scalar2=None, op0=OP.is_ge), ['lg', 'gmax'], ['ohg'])
                        svop(lambda e: e.tensor_scalar(out=R(1, 2), in0=R(0, 1), scalar1=-1.0, scalar2=None, op0=OP.mult), ['gmax'], ['ngmax'])
                        aop(lambda e: e.activation(out=R(8, 12), in_=rsm[:, 0:4], func=AF.Exp, bias=R(1, 2)), ['lg', 'ngmax'], ['eg'])
                        svop(lambda e: e.tensor_reduce(out=R(2, 3), in_=R(8, 12), axis=AX.X, op=OP.add), ['eg'], ['gsum'])
                        svop(lambda e: e.reciprocal(out=R(2, 3), in_=R(2, 3)), ['gsum'], ['gsum'])
                        svop(lambda e: e.tensor_scalar(out=R(12, 16), in0=R(4, 8), scalar1=-1.0, scalar2=1e30, op0=OP.add, op1=OP.mult), ['ohg'], ['pen'])
                        for g in range(4):
                            svop(lambda e: e.tensor_scalar(out=rsm[:, 40 + g * 8:48 + g * 8], in0=rsm[:, 4 + g * 8:12 + g * 8], scalar1=R(12 + g, 13 + g), scalar2=None, op0=OP.add),
                                ['lg', 'pen'], ['zem'])
                        svop(lambda e: e.max(out=top8, in_=zem), ['zem'], ['top8'])
                        svop(lambda e: e.tensor_scalar(out=sel, in0=zem, scalar1=rsm[:, 137:138], scalar2=None, op0=OP.is_ge), ['zem', 'top8'], ['sel'])
                        svop(lambda e: e.tensor_scalar(out=R(3, 4), in0=rsm[:, 136:137], scalar1=-1.0, scalar2=None, op0=OP.mult), ['top8'], ['nv1'])
                        aop(lambda e: e.activation(out=ex, in_=zem, func=AF.Exp, bias=R(3, 4)), ['zem', 'nv1'], ['ex'])
                        svop(lambda e: e.tensor_tensor(out=ex, in0=ex, in1=sel, op=OP.mult), ['ex', 'sel'], ['ex'])
                        svop(lambda e: e.tensor_reduce(out=R(16, 17), in_=ex, axis=AX.X, op=OP.add), ['ex'], ['den'])
                        svop(lambda e: e.reciprocal(out=R(16, 17), in_=R(16, 17)), ['den'], ['den'])
                        svop(lambda e: e.tensor_tensor(out=R(16, 17), in0=R(16, 17), in1=R(2, 3), op=OP.mult), ['den', 'gsum'], ['den'])
                        gt, oh0, oh1, pos, tmpr, ovf = (rsm[:, 256:288], rsm[:, 288:320], rsm[:, 320:352], rsm[:, 352:384], rsm[:, 384:416], rsm[:, 416:448])
                        X = lambda i: rsm[:, 448 + i:449 + i]
                        svop(lambda e: e.tensor_scalar(out=gt, in0=ex, scalar1=R(16, 17), scalar2=None, op0=OP.mult), ['ex', 'den'], ['gt'])
                        svop(lambda e: e.tensor_scalar(out=oh0, in0=zem, scalar1=rsm[:, 136:137], scalar2=None, op0=OP.is_ge), ['zem', 'top8'], ['oh0'])
                        svop(lambda e: e.tensor_tensor(out=oh1, in0=sel, in1=oh0, op=OP.subtract), ['sel', 'oh0'], ['oh1'])
                        svop(lambda e: e.tensor_copy(out=sel_bf[:], in_=sel), ['sel'], ['sel_bf'])
                        peop(lambda e: e.matmul(PS[2][:, 64:96], lhsT=tri_bf[:], rhs=sel_bf[:], start=True, stop=False), ['tri', 'sel_bf'], ['ps2'])
                        peop(lambda e: e.matmul(PS[2][:, 64:96], lhsT=ones_bf[:], rhs=selsum[:], start=False, stop=True), ['ones', 'selsum'], ['ps2'])
                        svop(lambda e: e.tensor_tensor(out=selsum[:], in0=selsum[:], in1=sel, op=OP.add), ['selsum', 'sel'], ['selsum'])
                        svop(lambda e: e.tensor_tensor(out=pos, in0=PS[2][:, 64:96], in1=ecap[:], op=OP.add), ['ps2', 'ecap'], ['pos'])
                        svop(lambda e: e.tensor_scalar(out=ovf, in0=PS[2][:, 64:96], scalar1=float(CAP), scalar2=1e6, op0=OP.is_ge, op1=OP.mult), ['ps2'], ['ovf'])
                        svop(lambda e: e.tensor_tensor(out=pos, in0=pos, in1=ovf, op=OP.add), ['pos', 'ovf'], ['pos'])
                        for kq, ohk in enumerate([oh0, oh1]):
                            okey = 'oh%d' % kq
                            svop(lambda e: e.tensor_tensor(out=tmpr, in0=ohk, in1=pos, op=OP.mult), [okey, 'pos'], ['tmpr'])
                            svop(lambda e: e.tensor_reduce(out=X(kq), in_=tmpr, axis=AX.X, op=OP.add), ['tmpr'], ['dx%d' % kq])
                            svop(lambda e: e.tensor_copy(out=dest_all[:, 2 * gst + kq:2 * gst + kq + 1], in_=X(kq)), ['dx%d' % kq], ['dest%d_%d' % (gst, kq)])
                            svop(lambda e: e.tensor_tensor(out=tmpr, in0=ohk, in1=gt, op=OP.mult), [okey, 'gt'], ['tmpr'])
                            svop(lambda e: e.tensor_reduce(out=gsel_all[:, gst, kq:kq + 1], in_=tmpr, axis=AX.X, op=OP.add), ['tmpr'], ['gsel'])
                        aop(lambda e: e.activation(out=hb[:].rearrange("t (k p) -> t k p", p=128), in_=zt[:].rearrange("t (p k) -> t k p", k=8), func=AF.Identity), ['zt0', 'zt1'], ['hb'], 1.0)
                        for kq in range(2):
                            S.dma('pool', lambda e: e.indirect_dma_start(out=xbuf_d, out_offset=bass.IndirectOffsetOnAxis(ap=dest_all[:, 2 * gst + kq:2 * gst + kq + 1], axis=0), in_=hb[:], in_offset=None,
                                                                         bounds_check=bcreg(e, 'A'), oob_is_err=False), reads=['hb', 'dest%d_%d' % (gst, kq)], writes=['xbuf'])
                chains = [S.record(sec_conv), S.record(sec_s5)]
                if prev_tile is not None:
                    pt = prev_tile
                    chains.insert(0, S.record(lambda: sec_ln(*pt)))
                S.replay_merged(chains)
                for m in range(8):
                    b = proj(20 + m)
                    aop(lambda e: e.activation(out=BT_[3][:], in_=PS[b][:, 0:TS], func=AF.Sigmoid, bias=bcol(B_IN, 20 + m)), ['ps%d' % b, 'cols'], [bk(3)])
                    b2 = mmbank()
                    for k in range(4):
                        peop(lambda e: e.matmul(PS[b2][:, 0:TS], lhsT=wcout_bf[:, k, m * 128:(m + 1) * 128], rhs=cs_bf[:, k, :], start=(k == 0), stop=(k == 3)),
                             ['wcout', 'cs%d' % k], ['ps%d' % b2])
                    vop(lambda e: e.scalar_tensor_tensor(out=merged[:, m, :], in0=PS[b2][:, 0:TS], scalar=bcol(B_COUT, m), in1=BT_[3][:], op0=OP.add, op1=OP.mult),
                        ['ps%d' % b2, 'cols', bk(3)], ['mg%d' % m])

                for m in range(8):
                    b = proj(12 + m)
                    aop(lambda e: e.activation(out=BT_[3][:], in_=PS[b][:, 0:TS], func=AF.Sigmoid, bias=bcol(B_IN, 12 + m)), ['ps%d' % b, 'cols'], [bk(3)])
                    bg = mmbank()
                    for k in range(4):
                        peop(lambda e: e.matmul(PS[bg][:, 0:TS], lhsT=wglu_bf[:, k, (8 + m) * 128:(9 + m) * 128], rhs=y_bf[:, k, :], start=(k == 0), stop=(k == 3)),
                             ['wglu', 'y%d' % k], ['ps%d' % bg])
                    aop(lambda e: e.activation(out=BT_[2][:], in_=PS[bg][:, 0:TS], func=AF.Sigmoid, bias=bcol(B_GLU, 8 + m)), ['ps%d' % bg, 'cols'], [bk(2)])
                    bv = mmbank()
                    for k in range(4):
                        peop(lambda e: e.matmul(PS[bv][:, 0:TS], lhsT=wglu_bf[:, k, m * 128:(m + 1) * 128], rhs=y_bf[:, k, :], start=(k == 0), stop=(k == 3)),
                             ['wglu', 'y%d' % k], ['ps%d' % bv])
                    vop(lambda e: e.scalar_tensor_tensor(out=FT[8][:], in0=PS[bv][:, 0:TS], scalar=bcol(B_GLU, m), in1=BT_[2][:], op0=OP.add, op1=OP.mult),
                        ['ps%d' % bv, 'cols', bk(2)], [fk(8)])
                    pop(lambda e: e.tensor_tensor(out=FT[8][:], in0=FT[8][:], in1=BT_[3][:], op=OP.mult), [fk(8), bk(3)], [fk(8)])
                    pop(lambda e: e.tensor_tensor(out=merged[:, m, :], in0=FT[8][:], in1=merged[:, m, :], op=OP.add), [fk(8), 'mg%d' % m], ['mg%d' % m])

                prev_tile = (ti, t0)
                if ti == NT - 1:
                    S.replay_merged([S.record(lambda: sec_ln(ti, t0))])

            S.barrier()
            S.emit()

        if not DEBUG_H:
            with ExitStack() as es:
                def sb(name, shape, dt=F32):
                    return es.enter_context(nc.sbuf_tensor(name, list(shape), dt))
                PS = [es.enter_context(nc.psum_tensor("pb%d" % i, [128, 512], F32)) for i in range(6)]
                PT = [es.enter_context(nc.psum_tensor("pt%d" % i, [128, 1024], BF16)) for i in range(2)]
                wg = [sb("wg%d" % i, [128, 8, 256], BF16) for i in range(2)]
                wu = [sb("wu%d" % i, [128, 8, 256], BF16) for i in range(2)]
                wd = [sb("wd%d" % i, [128, 2, D], BF16) for i in range(2)]
                xrows = [sb("xrows%d" % i, [128, NS4, D], BF16) for i in range(2)]
                xTe = sb("xTe", [128, 8, CAP], BF16)
                sgt = sb("sgt", [128, 2, CAP], BF16)
                hid = sb("hid", [128, 2, CAP], BF16)
                ysb = [sb("ysb%d" % i, [128, NS4, D], BF16) for i in range(2)]
                identb = sb("identb", [128, 128], BF16)
                NACC = 4
                statb = sb("statsb", [128, 24 * NACC])
                repB = sb("repB", [128, 2, D])
                acc = [sb("acc%d" % i, [128, D]) for i in range(NACC)]
                yg = [[sb("yg%d_%d" % (i, k), [128, D], BF16) for k in range(2)] for i in range(NACC)]
                dg = [[sb("dg%d_%d" % (i, k), [128, 128], BF16) for k in range(2)] for i in range(NACC)]
                S.dma('sp', lambda e: e.dma_start(out=repB[:], in_=rep5v[:, 3:5, :]), writes=['repB'])
                S.dma('pool', lambda e: e.dma_start(out=identb[:], in_=ident_d), writes=['identb'])
                for i in range(NACC):
                    for k in range(2):
                        pop(lambda e: e.memset(yg[i][k][:], 0.0), [], ['yg%d_%d' % (i, k)])
                rot = [0]

                def bank(n=6):
                    b = rot[0]
                    rot[0] = (rot[0] + 1) % n
                    return b
                def load_expert(ex_j):
                    wj = ex_j % 2
                    xrj = xrows[wj]
                    S.dma('sp', lambda e: e.dma_start(out=xrj[:], in_=xbuf_d[ex_j * CAP:(ex_j + 1) * CAP, :].rearrange("(s p) d -> p s d", p=128)), reads=['xbuf'], writes=['xr%d' % wj])
                    S.dma('pool', lambda e: e.dma_start(out=wg[wj][:], in_=w_gate[ex_j].rearrange("(p k) f -> p k f", k=8)), writes=['wg%d' % wj])
                    S.dma('pool', lambda e: e.dma_start(out=wu[wj][:], in_=w_up[ex_j].rearrange("(p k) f -> p k f", k=8)), writes=['wu%d' % wj])
                    S.dma('pool', lambda e: e.dma_start(out=wd[wj][:], in_=w_down[ex_j].rearrange("(k p) f -> p k f", p=128)), writes=['wd%d' % wj])
                load_expert(0)
                for ex_i in range(32):
                    wi = ex_i % 2
                    xr = xrows[wi]
                    ys = ysb[wi]
                    if ex_i + 1 < 32:
                        load_expert(ex_i + 1)
                    for s4 in range(NS4):
                        pt = PT[s4 % 2]
                        for k in range(8):
                            peop(lambda e: e.transpose(out=pt[:, k * 128:(k + 1) * 128], in_=xr[:, s4, k * 128:(k + 1) * 128], identity=identb[:]), ['xr%d' % wi, 'identb'], ['pt%d' % (s4 % 2)])
                        ev = aop if s4 % 2 == 0 else vop
                        ev(lambda e: e.tensor_copy(out=xTe[:, :, s4 * 128:(s4 + 1) * 128], in_=pt[:].rearrange("p (k r) -> p k r", k=8)) if False else
                           (e.activation(out=xTe[:, :, s4 * 128:(s4 + 1) * 128], in_=pt[:].rearrange("p (k r) -> p k r", k=8), func=AF.Identity) if s4 % 2 == 0 else
                            e.tensor_copy(out=xTe[:, :, s4 * 128:(s4 + 1) * 128], in_=pt[:].rearrange("p (k r) -> p k r", k=8))),
                           ['pt%d' % (s4 % 2)], ['xTe%d' % s4])
                    xk = ['xTe%d' % i for i in range(NS4)]
                    for f in range(2):
                        bgk = bank()
                        for k in range(8):
                            peop(lambda e: e.matmul(PS[bgk][:, 0:CAP], lhsT=wg[wi][:, k, f * 128:(f + 1) * 128], rhs=xTe[:, k, :], start=(k == 0), stop=(k == 7)), ['wg%d' % wi] + xk, ['pb%d' % bgk])
                        aop(lambda e: e.activation(out=sgt[:, f, :], in_=PS[bgk][:, 0:CAP], func=AF.Silu), ['pb%d' % bgk], ['sgt%d' % f])
                        buk = bank()
                        for k in range(8):
                            peop(lambda e: e.matmul(PS[buk][:, 0:CAP], lhsT=wu[wi][:, k, f * 128:(f + 1) * 128], rhs=xTe[:, k, :], start=(k == 0), stop=(k == 7)), ['wu%d' % wi] + xk, ['pb%d' % buk])
                        vop(lambda e: e.tensor_tensor(out=hid[:, f, :], in0=PS[buk][:, 0:CAP], in1=sgt[:, f, :], op=OP.mult), ['pb%d' % buk, 'sgt%d' % f], ['hid%d' % f])
                    for s4 in range(NS4):
                        for hf in range(2):
                            hs = slice(hf * 512, (hf + 1) * 512)
                            b = bank()
                            for f in range(2):
                                peop(lambda e: e.matmul(PS[b][:], lhsT=hid[:, f, s4 * 128:(s4 + 1) * 128], rhs=wd[wi][:, f, hs], start=(f == 0), stop=(f == 1)),
                                     ['hid0', 'hid1', 'wd%d' % wi], ['pb%d' % b])
                            if (s4 * 2 + hf) % 2 == 0:
                                aop(lambda e: e.activation(out=ys[:, s4, hs], in_=PS[b][:], func=AF.Identity), ['pb%d' % b], ['ysa%d' % wi])
                            else:
                                vop(lambda e: e.tensor_copy(out=ys[:, s4, hs], in_=PS[b][:]), ['pb%d' % b], ['ysv%d' % wi])
                    S.dma('sp', lambda e: e.dma_start(out=ybuf_d[ex_i * CAP:(ex_i + 1) * CAP, :].rearrange("(s p) d -> p s d", p=128), in_=ys[:]), reads=['ysa%d' % wi, 'ysv%d' % wi], writes=['ybuf%d' % ex_i])
                ybk = ['ybuf%d' % i for i in range(32)]
                NSUB = TOK // 128

                def comb_load(st):
                    r0 = st * 128
                    pi_ = st % NACC
                    a_ = acc[pi_]
                    S.dma('sp', lambda e: e.dma_start(out=a_[:], in_=zb_d[r0:r0 + 128, :]), reads=['zb_d'], writes=['accb%d' % pi_])
                    for kq in range(2):
                        g = yg[pi_][kq]
                        S.dma('pool', lambda e: e.indirect_dma_start(out=g[:], out_offset=None, in_=ybuf_d, in_offset=bass.IndirectOffsetOnAxis(ap=dest_all[:, 2 * st + kq:2 * st + kq + 1], axis=0),
                                                                     bounds_check=bcreg(e, 'B'), oob_is_err=False), reads=ybk, writes=['yg%d_%d' % (pi_, kq)])
                def comb_compute(st):
                    r0 = st * 128
                    pi_ = st % NACC
                    a_ = acc[pi_]
                    ak = 'accb%d' % pi_
                    sb_ = statb[:, pi_ * 24:pi_ * 24 + 24]
                    sk_ = 'stb%d' % pi_
                    for kq in range(2):
                        dgk = dg[pi_][kq]
                        aop(lambda e: e.activation(out=dgk[:], in_=identb[:], func=AF.Identity, scale=gsel_all[:, st, kq:kq + 1]), ['identb', 'gsel'], ['dg%d_%d' % (pi_, kq)], 0.2)
                    for hf in range(2):
                        hs = slice(hf * 512, (hf + 1) * 512)
                        b = (st % 2) * 2 + hf
                        for kq in range(2):
                            g = yg[pi_][kq]
                            dgk = dg[pi_][kq]
                            peop(lambda e: e.matmul(PS[b][:], lhsT=dgk[:], rhs=g[:, hs], start=(kq == 0), stop=(kq == 1)),
                                 ['dg%d_%d' % (pi_, kq), 'yg%d_%d' % (pi_, kq)], ['pb%d' % b], 0.28)
                        vop(lambda e: e.tensor_tensor(out=a_[:, hs], in0=a_[:, hs], in1=PS[b][:], op=OP.add), ['pb%d' % b, ak], [ak], 0.6)
                    for hf in range(2):
                        vop(lambda e: e.bn_stats(out=sb_[:, hf * 6:(hf + 1) * 6], in_=a_[:, hf * 512:(hf + 1) * 512]), [ak], [sk_ + 'b%d' % hf], 0.65)
                    vop(lambda e: e.bn_aggr(out=sb_[:, 12:14], in_=sb_[:, 0:12]), [sk_ + 'b0', sk_ + 'b1'], [sk_ + 'mv'], 0.2)
                    aop(lambda e: e.activation(out=sb_[:, 14:15], in_=sb_[:, 13:14], func=AF.Sqrt, bias=eps_t[:, 0:1]), [sk_ + 'mv', 'eps'], [sk_ + 'sd'], 0.3)
                    vop(lambda e: e.reciprocal(out=sb_[:, 15:16], in_=sb_[:, 14:15]), [sk_ + 'sd'], [sk_ + 'rs'], 0.16)
                    vop(lambda e: e.scalar_tensor_tensor(out=sb_[:, 16:17], in0=sb_[:, 12:13], scalar=-1.0, in1=sb_[:, 15:16], op0=OP.mult, op1=OP.mult), [sk_ + 'mv', sk_ + 'rs'], [sk_ + 'nb'], 0.1)
                    aop(lambda e: e.activation(out=a_[:], in_=a_[:], func=AF.Identity, scale=sb_[:, 15:16], bias=sb_[:, 16:17]), [ak, sk_ + 'rs', sk_ + 'nb'], [ak], 1.1)
                    vop(lambda e: e.tensor_tensor(out=a_[:], in0=a_[:], in1=repB[:, 0, :], op=OP.mult), [ak, 'repB'], [ak], 1.1)
                    pop(lambda e: e.tensor_tensor(out=a_[:, 0:512], in0=a_[:, 0:512], in1=repB[:, 1, 0:512], op=OP.add), [ak, 'repB'], [ak + 'p'], 1.2)
                    vop(lambda e: e.tensor_tensor(out=a_[:, 512:1024], in0=a_[:, 512:1024], in1=repB[:, 1, 512:1024], op=OP.add), [ak, 'repB'], [ak + 'v'], 0.6)
                    S.dma('sp', lambda e: e.dma_start(out=out[r0:r0 + 128, :], in_=a_[:]), reads=[ak, ak + 'p', ak + 'v'], writes=['out'])

                comb_load(0)
                comb_load(1)
                for st0 in range(0, NSUB, 2):
                    for st in (st0 + 2, st0 + 3):
                        if st < NSUB:
                            comb_load(st)
                    S.replay_merged([S.record(lambda: comb_compute(st0)), S.record(lambda: comb_compute(st0 + 1))])
                S.barrier()
                S.emit()
    return nc


def _prep(inp):
    f = lambda a: np.ascontiguousarray(a, dtype=np.float32)
    colmat = lambda v: f(np.asarray(v).reshape(-1, 128).T)
    cols = np.concatenate([colmat(inp['b_in'][0]), colmat(inp['b_glu'][0]), colmat(inp['b_cout'][0]), colmat(inp['ssm_d'][0]),
                           colmat(inp['conv_b'][0]), colmat(inp['ln_c_g'][0]), colmat(inp['ln_c_b'][0])], axis=1)
    assert cols.shape == (128, 68)
    cols = f(np.concatenate([cols, np.zeros((128, 4), np.float32)], axis=1))
    cw = inp['conv_w'][0][:, 0, :]
    convw = f(cw.T.reshape(4, 128, CW).transpose(1, 0, 2).reshape(128, 4 * CW))
    rep5 = f(np.concatenate([np.broadcast_to(inp[k][0][None, :], (128, D)) for k in ['b_out', 'ln1_g', 'ln1_b', 'ln2_g', 'ln2_b']], axis=1))
    w_rt = f(np.concatenate([inp['w_route_group'][0], inp['w_route_expert'][0]], axis=1))
    brt = f(np.broadcast_to(np.concatenate([inp['b_route_group'][0], inp['b_route_expert'][0]])[None, :], (128, 36)))
    pqf = lambda a: a.reshape(16, 2, 64).transpose(1, 2, 0).reshape(128, 16)
    ldt = np.broadcast_to(inp['log_dt'][0].reshape(16, 2, 1), (16, 2, 64))
    pq = f(np.concatenate([pqf(inp['lam_re'][0]), pqf(inp['lam_im'][0]), pqf(np.ascontiguousarray(ldt))], axis=1))
    bf_ = lambda a: a.reshape(16, 2, 64, 16).transpose(1, 2, 0, 3).reshape(128, 256)
    cf_ = lambda a: a.reshape(16, 2, 16, 64).transpose(1, 3, 0, 2).reshape(128, 256)
    bc = f(np.concatenate([bf_(inp['ssm_b_re'][0]), bf_(inp['ssm_b_im'][0]), cf_(inp['ssm_c_re'][0]), cf_(inp['ssm_c_im'][0])], axis=1))
    shared = {
        'w_in': f(inp['w_in'][0]), 'w_glu': f(inp['w_glu'][0]), 'w_cout': f(inp['w_cout'][0]), 'w_out': f(inp['w_out'][0]),
        'w_rt': w_rt, 'w_gate': f(inp['w_gate'][0]), 'w_up': f(inp['w_up'][0]), 'w_down': f(inp['w_down'][0]),
        'cols': cols, 'convw': convw, 'rep5': rep5, 'brt': brt, 'pq': pq, 'bc': bc,
        'ident': np.eye(128, dtype=np.float32),
        'tri': np.triu(np.ones((128, 128), np.float32), 1),
        'ecap': f(np.broadcast_to((np.arange(32, dtype=np.float32) * CAP)[None, :], (128, 32))),
        'kk': f(np.broadcast_to(np.arange(1, TS + 1, dtype=np.float32)[None, :], (128, TS))),
        'bdmask': f(np.kron(np.eye(4, dtype=np.float32), np.ones((32, 32), np.float32))),
    }
    x = inp['x']
    maps = []
    for c in range(NCORES):
        xc = f(x[2 * c:2 * c + 2].reshape(TOK, D))
        m = dict(shared)
        m['xtok'] = xc
        m['xT'] = f(xc.T)
        maps.append(m)
    return maps


def kernel(**inputs):
    maps = _prep(inputs)
    nc = build_nc()
    res = run_bass_kernel_spmd(nc, maps, core_ids=list(range(NCORES)))
    outs = [np.asarray(r['out']).reshape(2, SEQ, D) for r in res.results]
    return np.concatenate(outs, axis=0).astype(np.float32)
```
